# Optimizing a Trainium2 kernel written in Bass

```python
import math
import jax, jax.numpy as jnp
from jax import lax
import numpy as np

D_MODEL = 1024
BATCH = 32
SEQ = 2048
DEPTH = 4

GRID_W = 64
CTX_LEN = 256
HEAD_DIM = 64
N_MIXERS = 4
GROUP_W = D_MODEL // N_MIXERS
HEADS = GROUP_W // HEAD_DIM
KV_HEADS_A = HEADS // 2
KV_HEADS_D = HEADS // 2
DIFF_DIM = HEAD_DIM // 2
WINDOW = 128
BLOCK_Q = 128
NA_ROWS = 8
NA_COLS = 16
ROPE_BASE = 10000.0
N_EXPERTS = 16
N_EXPERT_GROUPS = 4
EXPERTS_PER_GROUP = N_EXPERTS // N_EXPERT_GROUPS
TOP_K = 2
D_EXPERT = D_MODEL // 2
MOE_BLOCK = 256
ADA_CHUNKS = 6
EPS = 1e-6
NEG = -1e30
F32 = jnp.float32
COL_SIZES = (HEADS * HEAD_DIM, KV_HEADS_A * HEAD_DIM, KV_HEADS_A * HEAD_DIM,
             HEADS * HEAD_DIM, HEADS * HEAD_DIM, HEADS * HEAD_DIM,
             HEADS * 2 * DIFF_DIM, HEADS * 2 * DIFF_DIM, HEADS * HEAD_DIM,
             HEADS * HEAD_DIM, KV_HEADS_D * HEAD_DIM, KV_HEADS_D * HEAD_DIM)
D_IN = sum(COL_SIZES)

kernel_name = "hybrid_headgroup_diffusion_moe_trunk"


def rms(x, eps=EPS):
    x32 = x.astype(F32)
    return (x32 * lax.rsqrt(jnp.mean(x32 * x32, axis=-1, keepdims=True) + eps)).astype(x.dtype)


def rms_norm(x, g):
    return rms(x) * g.astype(x.dtype)


def modulate(h, shift, scale):
    return h * (1.0 + scale) + shift


def to_heads(t, n):
    B, T = t.shape[:2]
    return t.reshape(B, T, n, -1).transpose(0, 2, 1, 3)


def from_heads(o):
    B, H, T, d = o.shape
    return o.transpose(0, 2, 1, 3).reshape(B, T, H * d)


def to_diff_heads(t):
    B, T = t.shape[:2]
    return t.reshape(B, T, HEADS, 2, DIFF_DIM).transpose(0, 2, 3, 1, 4)


def split_cols(u):
    points, acc = [], 0
    for size in COL_SIZES[:-1]:
        acc += size
        points.append(acc)
    return jnp.split(u, points, axis=-1)


def rope_1d(x, pos):
    half = x.shape[-1] // 2
    inv = ROPE_BASE ** (-jnp.arange(half, dtype=F32) / half)
    ang = pos.astype(F32)[:, None] * inv
    cos, sin = jnp.cos(ang), jnp.sin(ang)
    x32 = x.astype(F32)
    x1, x2 = x32[..., :half], x32[..., half:]
    return jnp.concatenate([x1 * cos - x2 * sin, x1 * sin + x2 * cos], axis=-1).astype(x.dtype)


def rope_2d(x, row, col):
    h = x.shape[-1] // 2
    return jnp.concatenate([rope_1d(x[..., :h], row), rope_1d(x[..., h:], col)], axis=-1)


def attend_gqa(q, k, v, scale):
    s = jnp.einsum('bhgqd,bhkd->bhgqk', q, k).astype(F32) * scale
    p = jax.nn.softmax(s, axis=-1).astype(v.dtype)
    return jnp.einsum('bhgqk,bhkd->bhgqd', p, v)


def mixer_window(q, k, v, qc, kc, vc, g, sink, row, col, need_ctx):
    q = rope_2d(rms_norm(to_heads(q, HEADS), g[0]), row, col)
    k = rope_2d(rms_norm(to_heads(k, KV_HEADS_A), g[1]), row, col)
    v = to_heads(v, KV_HEADS_A)
    qc = rms_norm(to_heads(qc, HEADS), g[0])
    kc = rms_norm(to_heads(kc, KV_HEADS_A), g[1])
    vc = to_heads(vc, KV_HEADS_A)
    B, H, S, d = q.shape
    G = H // KV_HEADS_A
    NB, BQ, L = S // BLOCK_Q, BLOCK_Q, kc.shape[2]
    scale = d ** -0.5
    pad = ((0, 0), (0, 0), (BQ, BQ), (0, 0))
    kp, vp = jnp.pad(k, pad), jnp.pad(v, pad)

    def band(t):
        return jnp.concatenate([t[:, :, o * BQ:o * BQ + S].reshape(B, KV_HEADS_A, NB, BQ, d) for o in range(3)], axis=3)

    kb, vb = band(kp), band(vp)
    qb = q.reshape(B, KV_HEADS_A, G, NB, BQ, d)
    qpos = jnp.arange(S).reshape(NB, BQ, 1)
    kpos = (jnp.arange(NB)[:, None, None] - 1) * BQ + jnp.arange(3 * BQ)[None, None, :]
    valid = (jnp.abs(kpos - qpos) <= WINDOW) & (kpos >= 0) & (kpos < S)
    s_loc = jnp.where(valid, jnp.einsum('bhgnqd,bhnkd->bhgnqk', qb, kb).astype(F32) * scale, NEG)
    s_ctx = jnp.einsum('bhgnqd,bhkd->bhgnqk', qb, kc).astype(F32) * scale
    sink_b = sink.astype(F32).reshape(1, KV_HEADS_A, G, 1, 1, 1)
    s_sink = jnp.broadcast_to(sink_b, s_ctx.shape[:-1] + (1,))
    p = jax.nn.softmax(jnp.concatenate([s_loc, s_ctx, s_sink], axis=-1), axis=-1).astype(v.dtype)
    o = (jnp.einsum('bhgnqk,bhnkd->bhgnqd', p[..., :3 * BQ], vb)
         + jnp.einsum('bhgnqk,bhkd->bhgnqd', p[..., 3 * BQ:3 * BQ + L], vc))
    o = o.reshape(B, H, S, d)
    oc = None
    if need_ctx:
        qg = qc.reshape(B, KV_HEADS_A, G, L, d)
        s = jnp.einsum('bhgqd,bhkd->bhgqk', qg, kc).astype(F32) * scale
        s_sink_c = jnp.broadcast_to(sink_b[..., 0], s.shape[:-1] + (1,))
        pc = jax.nn.softmax(jnp.concatenate([s, s_sink_c], axis=-1), axis=-1).astype(vc.dtype)
        oc = from_heads(jnp.einsum('bhgqk,bhkd->bhgqd', pc[..., :L], vc).reshape(B, H, L, d))
    return from_heads(o), oc


def mixer_neighbourhood(q, k, v, qc, kc, vc, g, rpb, need_ctx):
    q = rms_norm(to_heads(q, HEADS), g[0])
    k = rms_norm(to_heads(k, HEADS), g[1])
    v = to_heads(v, HEADS)
    qc = rms_norm(to_heads(qc, HEADS), g[0])
    kc = rms_norm(to_heads(kc, HEADS), g[1])
    vc = to_heads(vc, HEADS)
    B, H, S, d = q.shape
    W = GRID_W
    R = S // W
    NR, NC = min(NA_ROWS, R), NA_COLS
    KN = NR * W
    scale = d ** -0.5
    qg, kg, vg = (t.reshape(B, H, R, W, d) for t in (q, k, v))
    q_col = jnp.arange(W)
    c_start = jnp.clip(q_col - NC // 2, 0, W - NC)
    k_col = jnp.tile(jnp.arange(W), NR)
    k_row = jnp.repeat(jnp.arange(NR), W)
    col_ok = (k_col[None, :] >= c_start[:, None]) & (k_col[None, :] < c_start[:, None] + NC)
    dc_idx = jnp.clip(k_col[None, :] - q_col[:, None] + NA_COLS - 1, 0, 2 * NA_COLS - 2)

    def row_block(r):
        r_start = jnp.clip(r - NR // 2, 0, R - NR)
        kr = lax.dynamic_slice_in_dim(kg, r_start, NR, axis=2).reshape(B, H, KN, d)
        vr = lax.dynamic_slice_in_dim(vg, r_start, NR, axis=2).reshape(B, H, KN, d)
        qr = lax.dynamic_index_in_dim(qg, r, axis=2, keepdims=False)
        dr_idx = r_start + k_row - r + NA_ROWS - 1
        bias = rpb[:, dr_idx[None, :], dc_idx].astype(F32)
        s_loc = jnp.einsum('bhqd,bhkd->bhqk', qr, kr).astype(F32) * scale + bias
        s_loc = jnp.where(col_ok, s_loc, NEG)
        s_ctx = jnp.einsum('bhqd,bhkd->bhqk', qr, kc).astype(F32) * scale
        p = jax.nn.softmax(jnp.concatenate([s_loc, s_ctx], axis=-1), axis=-1).astype(v.dtype)
        return jnp.einsum('bhqk,bhkd->bhqd', p[..., :KN], vr) + jnp.einsum('bhqk,bhkd->bhqd', p[..., KN:], vc)

    o = lax.map(row_block, jnp.arange(R))
    o = o.transpose(1, 2, 0, 3, 4).reshape(B, H, S, d)
    oc = from_heads(attend_gqa(qc[:, :, None], kc, vc, scale)[:, :, 0]) if need_ctx else None
    return from_heads(o), oc


def diff_attend(q, k, v, lam, scale):
    s = jnp.einsum('bhiqd,bhikd->bhiqk', q, k).astype(F32) * scale
    p = jax.nn.softmax(s, axis=-1)
    a = (p[:, :, 0] - lam * p[:, :, 1]).astype(v.dtype)
    return jnp.einsum('bhqk,bhkd->bhqd', a, v)


def mixer_diff(q, k, v, qc, kc, vc, g, lam_p, lam_init, row, col, need_ctx):
    q = rope_2d(rms_norm(to_diff_heads(q), g[0]), row, col)
    k = rope_2d(rms_norm(to_diff_heads(k), g[1]), row, col)
    v = to_heads(v, HEADS)
    qc = rms_norm(to_diff_heads(qc), g[0])
    kc = rms_norm(to_diff_heads(kc), g[1])
    vc = to_heads(vc, HEADS)
    B, H, _, S, dh = q.shape
    NB = S // BLOCK_Q
    scale = dh ** -0.5
    lp = lam_p.astype(F32)
    lam = jnp.exp(jnp.sum(lp[0] * lp[1])) - jnp.exp(jnp.sum(lp[2] * lp[3])) + lam_init
    k_all = jnp.concatenate([k, kc], axis=3)
    v_all = jnp.concatenate([v, vc], axis=2)
    qb = q.reshape(B, H, 2, NB, BLOCK_Q, dh).transpose(3, 0, 1, 2, 4, 5)
    o = lax.map(lambda blk: diff_attend(blk, k_all, v_all, lam, scale), qb)
    o = o.transpose(1, 2, 0, 3, 4).reshape(B, H, S, -1)

    def post(t):
        return from_heads(rms(t) * (1.0 - lam_init))

    oc = post(diff_attend(qc, kc, vc, lam, scale)) if need_ctx else None
    return post(o), oc


def mixer_dense(q, k, v, qc, kc, vc, g, row, col, need_ctx):
    q = rope_2d(rms_norm(to_heads(q, HEADS), g[0]), row, col)
    k = rope_2d(rms_norm(to_heads(k, KV_HEADS_D), g[1]), row, col)
    v = to_heads(v, KV_HEADS_D)
    qc = rms_norm(to_heads(qc, HEADS), g[0])
    kc = rms_norm(to_heads(kc, KV_HEADS_D), g[1])
    vc = to_heads(vc, KV_HEADS_D)
    B, H, S, d = q.shape
    G = H // KV_HEADS_D
    NB, L = S // BLOCK_Q, kc.shape[2]
    scale = d ** -0.5
    k_all = jnp.concatenate([k, kc], axis=2)
    v_all = jnp.concatenate([v, vc], axis=2)
    qb = q.reshape(B, KV_HEADS_D, G, NB, BLOCK_Q, d).transpose(3, 0, 1, 2, 4, 5)
    o = lax.map(lambda blk: attend_gqa(blk, k_all, v_all, scale), qb)
    o = o.transpose(1, 2, 3, 0, 4, 5).reshape(B, H, S, d)
    oc = from_heads(attend_gqa(qc.reshape(B, KV_HEADS_D, G, L, d), kc, vc, scale).reshape(B, H, L, d)) if need_ctx else None
    return from_heads(o), oc


def token_mix(u, uc, layer, g_win, g_na, g_diff, g_gqa, sink, rpb, lam_p, gain, row, col, need_ctx):
    pl, pc = split_cols(u), split_cols(uc)
    lam_init = 0.8 - 0.6 * math.exp(-0.3 * layer)
    y_win, c_win = mixer_window(*pl[0:3], *pc[0:3], g_win, sink, row, col, need_ctx)
    y_na, c_na = mixer_neighbourhood(*pl[3:6], *pc[3:6], g_na, rpb, need_ctx)
    y_diff, c_diff = mixer_diff(*pl[6:9], *pc[6:9], g_diff, lam_p, lam_init, row, col, need_ctx)
    y_gqa, c_gqa = mixer_dense(*pl[9:12], *pc[9:12], g_gqa, row, col, need_ctx)

    def merge(a, b, dd, e):
        return jnp.concatenate([rms(a), rms(b), dd, rms(e)], axis=-1) * gain.astype(a.dtype)

    y = merge(y_win, y_na, y_diff, y_gqa)
    yc = merge(c_win, c_na, c_diff, c_gqa) if need_ctx else None
    return y, yc


def route(h, router_w, router_b):
    N = h.shape[0]
    score = jax.nn.sigmoid((h @ router_w).astype(F32))
    sel = score + router_b.astype(F32)
    sel_g = sel.reshape(N, N_EXPERT_GROUPS, EXPERTS_PER_GROUP)
    g_score = lax.top_k(sel_g, TOP_K)[0].sum(-1)
    g_idx = jnp.argmax(g_score, axis=-1).astype(jnp.int32)
    idx = jnp.broadcast_to(g_idx[:, None, None], (N, 1, EXPERTS_PER_GROUP))
    in_group = jnp.take_along_axis(sel_g, idx, axis=1)[:, 0]
    _, local = lax.top_k(in_group, TOP_K)
    expert = (g_idx[:, None] * EXPERTS_PER_GROUP + local).astype(jnp.int32)
    w = jnp.take_along_axis(score, expert, axis=-1)
    return expert, w / jnp.sum(w, axis=-1, keepdims=True)


def moe_ffn(h, router_w, router_b, w_gate, w_up, w_down):
    shp = h.shape
    hf = h.reshape(-1, shp[-1])
    N = hf.shape[0]
    expert, w = route(hf, router_w, router_b)
    A = N * TOP_K
    e_flat = expert.reshape(A)
    t_flat = jnp.repeat(jnp.arange(N, dtype=jnp.int32), TOP_K)
    w_flat = w.reshape(A)
    order = jnp.argsort(e_flat)
    e_s, t_s, w_s = e_flat[order], t_flat[order], w_flat[order]
    counts = jnp.bincount(e_flat, length=N_EXPERTS)
    padded = (counts + MOE_BLOCK - 1) // MOE_BLOCK * MOE_BLOCK
    p_end = jnp.cumsum(padded)
    p_start = p_end - padded
    c_start = jnp.cumsum(counts) - counts
    dest = p_start[e_s] + jnp.arange(A, dtype=jnp.int32) - c_start[e_s]
    n_blocks = -(-A // MOE_BLOCK) + N_EXPERTS
    P = n_blocks * MOE_BLOCK
    buf_t = jnp.zeros((P,), jnp.int32).at[dest].set(t_s)
    buf_w = jnp.zeros((P,), F32).at[dest].set(w_s)
    blk_e = jnp.minimum(jnp.searchsorted(p_end, jnp.arange(n_blocks, dtype=jnp.int32) * MOE_BLOCK, side='right'),
                        N_EXPERTS - 1)
    xb = hf[buf_t].reshape(n_blocks, MOE_BLOCK, shp[-1])

    def expert_block(args):
        xe, e = args
        return (jax.nn.silu(xe @ w_gate[e]) * (xe @ w_up[e])) @ w_down[e]

    yb = lax.map(expert_block, (xb, blk_e)).reshape(P, shp[-1])
    y = jnp.zeros_like(hf).at[buf_t].add(yb * buf_w[:, None].astype(yb.dtype))
    return y.reshape(shp)


def setup_inputs(seed: int = 0) -> dict:
    key = jax.random.key(seed)
    ks = jax.random.split(key, 23)
    D = D_MODEL

    def nrm(k, shape, s):
        return jax.random.normal(k, shape, jnp.float32) * s

    def gain(k, shape):
        return 1.0 + 0.02 * jax.random.normal(k, shape, jnp.float32)

    return {
        "x": nrm(ks[0], (BATCH, SEQ, D), 1.0),
        "c": nrm(ks[1], (BATCH, D), 1.0),
        "ctx": nrm(ks[2], (BATCH, CTX_LEN, D), 1.0),
        "c_ctx": nrm(ks[3], (D,), 1.0),
        "ada_w": nrm(ks[4], (DEPTH, D, ADA_CHUNKS * D), 0.25 * D ** -0.5),
        "ada_b": nrm(ks[5], (DEPTH, ADA_CHUNKS * D), 0.02),
        "norm_mix_g": gain(ks[6], (DEPTH, D)),
        "norm_ffn_g": gain(ks[7], (DEPTH, D)),
        "w_in": nrm(ks[8], (DEPTH, D, D_IN), D ** -0.5),
        "qk_g_win": gain(ks[9], (DEPTH, 2, HEAD_DIM)),
        "qk_g_na": gain(ks[10], (DEPTH, 2, HEAD_DIM)),
        "qk_g_diff": gain(ks[11], (DEPTH, 2, DIFF_DIM)),
        "qk_g_gqa": gain(ks[12], (DEPTH, 2, HEAD_DIM)),
        "sink_win": nrm(ks[13], (DEPTH, HEADS), 0.5),
        "rpb_na": nrm(ks[14], (DEPTH, HEADS, 2 * NA_ROWS - 1, 2 * NA_COLS - 1), 0.1),
        "lambda_diff": nrm(ks[15], (DEPTH, 4, DIFF_DIM), 0.1),
        "out_gain": gain(ks[16], (DEPTH, D)),
        "w_out": nrm(ks[17], (DEPTH, D, D), D ** -0.5),
        "router_w": nrm(ks[18], (D, N_EXPERTS), D ** -0.5),
        "router_b": nrm(ks[19], (N_EXPERTS,), 0.01),
        "w_gate": nrm(ks[20], (DEPTH, N_EXPERTS, D, D_EXPERT), D ** -0.5),
        "w_up": nrm(ks[21], (DEPTH, N_EXPERTS, D, D_EXPERT), D ** -0.5),
        "w_down": nrm(ks[22], (DEPTH, N_EXPERTS, D_EXPERT, D), D_EXPERT ** -0.5),
    }


def reference(x, c, ctx, c_ctx, ada_w, ada_b, norm_mix_g, norm_ffn_g, w_in, qk_g_win, qk_g_na, qk_g_diff,
              qk_g_gqa, sink_win, rpb_na, lambda_diff, out_gain, w_out, router_w, router_b, w_gate, w_up, w_down):
    S = x.shape[1]
    pos = jnp.arange(S, dtype=jnp.int32)
    row, col = pos // GRID_W, pos % GRID_W
    s_lat = jax.nn.silu(c)
    s_ctx = jax.nn.silu(c_ctx)
    for layer in range(DEPTH):
        need_ctx = layer < DEPTH - 1
        m_lat = jnp.split((s_lat @ ada_w[layer] + ada_b[layer])[:, None, :], ADA_CHUNKS, axis=-1)
        m_ctx = jnp.split(s_ctx @ ada_w[layer] + ada_b[layer], ADA_CHUNKS, axis=-1)
        h = modulate(rms_norm(x, norm_mix_g[layer]), m_lat[0], m_lat[1])
        hc = modulate(rms_norm(ctx, norm_mix_g[layer]), m_ctx[0], m_ctx[1])
        y, yc = token_mix(h @ w_in[layer], hc @ w_in[layer], layer, qk_g_win[layer], qk_g_na[layer],
                          qk_g_diff[layer], qk_g_gqa[layer], sink_win[layer], rpb_na[layer], lambda_diff[layer],
                          out_gain[layer], row, col, need_ctx)
        x = x + m_lat[2] * (y @ w_out[layer])
        h2 = modulate(rms_norm(x, norm_ffn_g[layer]), m_lat[3], m_lat[4])
        x = x + m_lat[5] * moe_ffn(h2, router_w, router_b, w_gate[layer], w_up[layer], w_down[layer])
        if need_ctx:
            ctx = ctx + m_ctx[2] * (yc @ w_out[layer])
            hc2 = modulate(rms_norm(ctx, norm_ffn_g[layer]), m_ctx[3], m_ctx[4])
            ctx = ctx + m_ctx[5] * moe_ffn(hc2, router_w, router_b, w_gate[layer], w_up[layer], w_down[layer])
    return x
```

```python
import math
import os
from contextlib import ExitStack
import numpy as np
import concourse.bass as bass
import concourse.mybir as mybir
from concourse.bass_utils import run_bass_kernel_spmd

F32 = mybir.dt.float32
BF16 = mybir.dt.bfloat16
AF = mybir.ActivationFunctionType
ALU = mybir.AluOpType
AX = mybir.AxisListType

D = 1024
S = 2048
L = 256
T = S + L
NTT = T // 128
DEPTH_FULL = 4
EPS = 1e-6
NE = 16
DE = 512
N_CORES = 8
NDS = 40
MIX = [("A", 0, 256, 128, 128, 2, 64), ("B", 512, 256, 256, 256, 4, 64),
       ("C", 1280, 256, 256, 256, 4, 32), ("D", 2048, 256, 128, 128, 2, 64)]


class StopBuild(Exception):
    pass


class TK:
    def __init__(self, nc, es):
        self.nc = nc
        self.eng = {"pe": nc.tensor, "act": nc.scalar, "dve": nc.vector, "pool": nc.gpsimd, "sp": nc.sync}
        self.sem = {k: es.enter_context(nc.semaphore("s_" + k)) for k in self.eng}
        self.cnt = {k: 0 for k in self.eng}
        self.seen = {k: {} for k in self.eng}
        self.dsem = [es.enter_context(nc.semaphore("d%d" % i)) for i in range(NDS)]
        self.dval = [0] * NDS
        self.dnext = 0
        self.bw = {}
        self.br = {}
        self.pe_open = False
        self.nops = 0
        self.limit = int(os.environ.get("TK_LIMIT", "0"))

    def _tick(self):
        self.nops += 1
        if self.limit and self.nops > self.limit:
            raise StopBuild()

    def _semh(self, sk):
        return self.sem[sk] if isinstance(sk, str) else self.dsem[sk[1]]

    def _need(self, e, reads, writes):
        need = {}

        def add(ev, raw):
            if ev is None:
                return
            sk, v = ev
            if sk == e and (not raw or e == "pe"):
                return
            if need.get(sk, 0) < v:
                need[sk] = v

        for k in reads:
            add(self.bw.get(k), True)
        for k in writes:
            add(self.bw.get(k), False)
            r = self.br.get(k)
            if r:
                for sk, v in r.items():
                    add((sk, v), False)
        return need

    def _emit_waits(self, e, need):
        for sk, v in need.items():
            if self.seen[e].get(sk, 0) >= v:
                continue
            self.eng[e].wait_ge(self._semh(sk), v)
            self.seen[e][sk] = v

    def _record(self, ev, reads, writes):
        sk, v = ev
        for k in reads:
            d = self.br.setdefault(k, {})
            if d.get(sk, 0) < v:
                d[sk] = v
        for k in writes:
            self.bw[k] = ev
            self.br[k] = {}

    def op(self, e, fn, reads=(), writes=(), inc=True):
        inc = True
        self._tick()
        need = self._need(e, reads, writes)
        self._emit_waits(e, need)
        ins = fn(self.eng[e])
        if inc:
            self.cnt[e] += 1
            ins.then_inc(self.sem[e], 1)
            ev = (e, self.cnt[e])
            if e == "pe":
                self.pe_open = False
        else:
            ev = (e, self.cnt[e] + 1)
            if e == "pe":
                self.pe_open = True
        self._record(ev, reads, writes)

    def dma(self, q, fn, reads=(), writes=()):
        self._tick()
        need = self._need(q, reads, writes)
        i = self.dnext
        self.dnext = (i + 1) % NDS
        if self.dval[i] > 0:
            sk = ("d", i)
            if need.get(sk, 0) < self.dval[i]:
                need[sk] = self.dval[i]
        self._emit_waits(q, need)
        ins = fn(self.eng[q])
        self.dval[i] += 16
        ins.then_inc(self.dsem[i], 16)
        self._record((("d", i), self.dval[i]), reads, writes)

    def barrier(self):
        assert not self.pe_open
        for e in self.eng:
            need = {f: self.cnt[f] for f in self.eng if f != e and self.cnt[f] > 0}
            for i in range(NDS):
                if self.dval[i] > 0:
                    need[("d", i)] = self.dval[i]
            self._emit_waits(e, need)
        self.bw = {}
        self.br = {}


def host_consts():
    pos = np.arange(S)
    row, col = pos // 64, pos % 64

    def tabs(hd):
        e = hd // 4
        inv = 10000.0 ** (-np.arange(e, dtype=np.float32) / e)
        ar = row[:, None].astype(np.float32) * inv
        ac = col[:, None].astype(np.float32) * inv
        cos = np.concatenate([np.cos(ar), np.cos(ar), np.cos(ac), np.cos(ac)], 1)
        sins = np.concatenate([-np.sin(ar), np.sin(ar), -np.sin(ac), np.sin(ac)], 1)
        return cos.astype(np.float32), sins.astype(np.float32)

    c64, s64 = tabs(64)
    c32, s32 = tabs(32)
    rope = np.concatenate([c64, s64, c32, s32], 1)
    b = np.arange(128)[:, None]
    a = np.arange(128)[None, :]
    wmask = np.stack([(a <= b), (b <= a)], 1).astype(np.float32)
    return {"ident": np.eye(128, dtype=np.float32), "rope": np.ascontiguousarray(rope),
            "wmask": np.ascontiguousarray(wmask.reshape(128, 256))}


def na_variant(i):
    return {0: 0, 1: 1, 14: 3, 15: 4}.get(i, 2)


def na_ts(i):
    return min(max(i - 2, 0), 11)


def host_na_bias(rpb):
    dl = rpb.shape[0]
    out = np.empty((dl, 4, 5, 128, 640), np.float32)
    kl = np.arange(128) // 64
    kc = np.arange(128) % 64
    ql = np.arange(128) // 64
    qc = np.arange(128) % 64
    for vi, i in enumerate([0, 1, 5, 14, 15]):
        ts = na_ts(i)
        for j in range(5):
            krow = 2 * (ts + j) + kl[:, None]
            qrow = 2 * i + ql[None, :]
            rs = np.clip(qrow - 4, 0, 24)
            rowok = (krow >= rs) & (krow < rs + 8)
            cs = np.clip(qc[None, :] - 8, 0, 48)
            colok = (kc[:, None] >= cs) & (kc[:, None] < cs + 16)
            dr = np.clip(krow - qrow + 7, 0, 14)
            dc = np.clip(kc[:, None] - qc[None, :] + 15, 0, 30)
            ok = rowok & colok
            g = rpb[:, :, dr, dc]
            out[:, :, vi, :, j * 128:(j + 1) * 128] = np.where(ok[None, None], g, np.float32(-30000.0))
    return out


def build(NB, DEPTH, dbg=None):
    nc = bass.Bass("TRN2", target_bir_lowering=False)

    def dram(name, shape, kind="ExternalInput", dtype=F32):
        return nc.dram_tensor(name, list(shape), dtype, kind=kind).ap()

    x_d = dram("x", [NB, S, D])
    c_d = dram("c", [NB + 1, D])
    ctx_d = dram("ctx", [NB, L, D])
    ada_w = dram("ada_w", [DEPTH, D, 6 * D])
    ada_b = dram("ada_b", [DEPTH, 6 * D])
    ng1 = dram("norm_mix_g", [DEPTH, D])
    ng2 = dram("norm_ffn_g", [DEPTH, D])
    w_in = dram("w_in", [DEPTH, D, 2560])
    qkg = {"A": dram("qk_g_win", [DEPTH, 2, 64]), "B": dram("qk_g_na", [DEPTH, 2, 64]),
           "C": dram("qk_g_diff", [DEPTH, 2, 32]), "D": dram("qk_g_gqa", [DEPTH, 2, 64])}
    sink_d = dram("sink_win", [DEPTH, 4])
    nab_d = dram("na_bias", [DEPTH, 4, 5, 128, 640])
    lam_d = dram("lambda_diff", [DEPTH, 4, 32])
    og_d = dram("out_gain", [DEPTH, D])
    w_out = dram("w_out", [DEPTH, D, D])
    rw_d = dram("router_w", [D, NE])
    rb_d = dram("router_b", [1, NE])
    wg_d = dram("w_gate", [DEPTH, NE, D, DE])
    wu_d = dram("w_up", [DEPTH, NE, D, DE])
    wd_d = dram("w_down", [DEPTH, NE, DE, D])
    ident_d = dram("ident", [128, 128])
    rope_d = dram("rope", [S, 192])
    wmask_d = dram("wmask", [128, 256])
    out_d = dram("out", [NB, S, D], kind="ExternalOutput")
    dbg_d = dram("dbg", [128, 8192], kind="ExternalOutput") if dbg else None

    es = ExitStack()
    with es:
        nc_ctx = es.enter_context(nc.allow_non_contiguous_dma(reason="small strided parameter loads"))
        tk = TK(nc, es)
        build_info = {}

        def sb(name, shape, dtype=F32):
            return es.enter_context(nc.sbuf_tensor(name, list(shape), dtype))

        xT = sb("xT", [128, 8, T])
        hT = sb("hT", [128, 8, T], BF16)
        arena = sb("arena", [128, 32768], BF16)
        ident_f = sb("ident_fs", [128, 128])
        ident_b = sb("ident_bs", [128, 128], BF16)
        ones_b = sb("ones_b", [128, 128], BF16)
        selb = sb("selb", [16, NE, 128], BF16)
        rw_hi = sb("rw_hi", [128, 8, NE], BF16)
        rw_lo = sb("rw_lo", [128, 8, NE], BF16)
        wmask = sb("wmask_sb", [128, 256], BF16)
        mT = sb("mT", [128, DEPTH, 48, NB + 1])
        sT = sb("sT", [128, 8, NB + 1])
        TMP = [sb("tmp%d" % i, [128, 512]) for i in range(5)]
        RB = sb("rbuf", [128, 512])
        XS = arena[:, 0:2048].bitcast(F32)
        PB = [sb("pb%d" % i, [128, 512], BF16) for i in range(3)]
        ropet = sb("ropet", [128, 192])
        small = sb("small", [128, 512])
        TQ = sb("tq", [128, 256], BF16)
        TK_ = sb("tkp", [128, 512], BF16)
        otok = sb("otok", [128, 4, 256])
        ytok = sb("ytok", [128, 256], BF16)
        nabm = sb("nabm", [128, 640], BF16)
        stat = sb("stat", [128, 64])
        PS = [es.enter_context(nc.psum_tensor("ps%d" % i, [128, 512], F32)) for i in range(8)]

        pass

        def psk(i):
            return ("ps", i)

        tmp_i = [0]

        def tmp():
            i = tmp_i[0]
            tmp_i[0] = (i + 1) % len(TMP)
            return TMP[i], ("tmp", i)

        pb_i = [0]

        def pbuf():
            i = pb_i[0]
            pb_i[0] = (i + 1) % len(PB)
            return PB[i], ("pb", i)

        A1 = small[:, 0:16].rearrange("p (j r) -> p j r", r=2)
        A2 = small[:, 16:32].rearrange("p (j r) -> p j r", r=2)
        g1T = small[:, 32:40]
        g2T = small[:, 40:48]
        ogT = small[:, 48:56]
        abT = small[:, 56:104]
        rb_bc = small[:, 104:120]
        sinkx = small[:, 120:124]
        lamt = small[:, 124:128]
        gq = small[:, 128:192]
        gk = small[:, 192:256]
        rwT = small[:, 256:384].rearrange("p (j e) -> p j e", e=NE)
        lamraw = small[:, 384:512]

        tk.dma("sp", lambda q: q.dma_start(out=ident_f[:], in_=ident_d[:, :]), writes=["ident_f"])
        tk.op("dve", lambda v: v.tensor_copy(out=ident_b[:], in_=ident_f[:]), reads=["ident_f"], writes=["ident_b"])
        tk.op("dve", lambda v: v.memset(ones_b[:], 1.0), writes=["ones_b"])
        tk.dma("sp", lambda q: q.dma_start(out=TMP[0][:, 0:256], in_=wmask_d[:, :]), writes=[("tmp", 0)])
        tk.op("dve", lambda v: v.tensor_copy(out=wmask[:], in_=TMP[0][:, 0:256]), reads=[("tmp", 0)], writes=["wmask"])
        tk.dma("sp", lambda q: q.dma_start(out=rb_bc, in_=rb_d[0:1, :].partition_broadcast(128)), writes=["small"])
        tk.dma("sp", lambda q: q.dma_start(out=rwT, in_=rw_d.rearrange("(j p) e -> p j e", p=128)), writes=["small"])
        tk.op("dve", lambda v: v.tensor_copy(out=rw_hi[:], in_=rwT), reads=["small"], writes=["rw"])
        tk.op("dve", lambda v: v.tensor_tensor(out=rw_lo[:], in0=rwT, in1=rw_hi[:], op=ALU.subtract), reads=["small", "rw"], writes=["rw2"])
        tk.op("dve", lambda v: v.tensor_copy(out=selb[:], in_=ident_b[0:16, 0:16].unsqueeze(2).broadcast_to([16, NE, 128])),
              reads=["ident_b"], writes=["selb"])
        tk.op("dve", lambda v: v.memset(TK_[:], 0.0), writes=["tkp"])
        tk.op("dve", lambda v: v.memset(arena[:], 0.0), writes=["arena"])

        for r in range(NB + 1):
            tk.dma("sp", lambda q, r=r: q.dma_start(out=sT[:, :, r], in_=c_d[r].rearrange("(j p) -> p j", p=128)),
                   writes=["sT"])
        tk.op("act", lambda a: a.activation(out=sT[:], in_=sT[:], func=AF.Silu), reads=["sT"], writes=["sT"])
        for l in range(DEPTH):
            for j6 in range(6):
                tk.dma("sp", lambda q, l=l, j6=j6: q.dma_start(
                    out=abT[:, j6 * 8:(j6 + 1) * 8],
                    in_=ada_b[l, j6 * 1024:(j6 + 1) * 1024].rearrange("(j p) -> p j", p=128)), writes=["abT"])
            psm = PS[0][:, 0:48 * (NB + 1)].rearrange("p (j r) -> p j r", r=NB + 1)
            for j in range(48):
                wt, wk = tmp()
                wt2, wk2 = tmp()
                tk.dma("sp", lambda q, l=l, j=j, wt=wt: q.dma_start(
                    out=wt[:].rearrange("p (k c) -> p k c", c=128),
                    in_=ada_w[l, 0:512, j * 128:(j + 1) * 128].rearrange("(k p) c -> p k c", p=128)), writes=[wk])
                tk.dma("sp", lambda q, l=l, j=j, wt2=wt2: q.dma_start(
                    out=wt2[:].rearrange("p (k c) -> p k c", c=128),
                    in_=ada_w[l, 512:1024, j * 128:(j + 1) * 128].rearrange("(k p) c -> p k c", p=128)), writes=[wk2])
                for kc in range(8):
                    src, sk_ = (wt, wk) if kc < 4 else (wt2, wk2)
                    tk.op("pe", lambda t, j=j, kc=kc, src=src: t.matmul(
                        psm[:, j, :], src[:, (kc % 4) * 128:(kc % 4 + 1) * 128], sT[:, kc, :],
                        start=(kc == 0), stop=(kc == 7)),
                        reads=[sk_, "sT"], writes=[psk(0)], inc=(kc == 7))
            tk.op("dve", lambda v, l=l: v.tensor_tensor(
                out=mT[:, l], in0=psm, in1=abT.unsqueeze(2).broadcast_to([128, 48, NB + 1]), op=ALU.add),
                reads=[psk(0), "abT"], writes=["mT"])

        def load_tokens_T(src_ap, tok0):
            tk.dma("sp", lambda q: q.dma_start(out=XS[:], in_=src_ap), writes=["xs"])
            for g in range(2):
                bank = 6 + g
                for jj in range(4):
                    j = g * 4 + jj
                    tk.op("pe", lambda t, j=j, jj=jj, bank=bank: t.transpose(
                        out=PS[bank][:, jj * 128:(jj + 1) * 128], in_=XS[:, j * 128:(j + 1) * 128],
                        identity=ident_f[:]), reads=["xs", "ident_f"], writes=[psk(bank)], inc=(jj == 3))
                eng = "act" if g == 0 else "dve"
                if eng == "act":
                    tk.op("act", lambda a, g=g, bank=bank: a.copy(
                        out=xT[:, g * 4:(g + 1) * 4, tok0:tok0 + 128],
                        in_=PS[bank][:].rearrange("p (j t) -> p j t", t=128)),
                        reads=[psk(bank)], writes=[("xT", tok0 // 128)])
                else:
                    tk.op("dve", lambda v, g=g, bank=bank: v.tensor_copy(
                        out=xT[:, g * 4:(g + 1) * 4, tok0:tok0 + 128],
                        in_=PS[bank][:].rearrange("p (j t) -> p j t", t=128)),
                        reads=[psk(bank)], writes=[("xT", tok0 // 128)])

        def xkeys(t0, n):
            return [("xT", i) for i in range(t0 // 128, (t0 + n) // 128)]

        def hkeys(t0, n):
            return [("hT", i) for i in range(t0 // 128, (t0 + n) // 128)]

        def norm_to_h(t0, n, Aap, Bap, router_bank=None):
            xk = xkeys(t0, n)
            hk = hkeys(t0, n)
            for j in range(8):
                sq, sqk = pbuf()
                tk.op("act", lambda a, j=j, sq=sq: a.activation(out=sq[:, :n], in_=xT[:, j, t0:t0 + n], func=AF.Square),
                      reads=xk, writes=[sqk])
                tk.op("pe", lambda t, j=j, sq=sq: t.matmul(PS[5][:, :n], ones_b[:], sq[:, :n], start=(j == 0), stop=(j == 7)),
                      reads=[sqk, "ones_b"], writes=[psk(5)], inc=(j == 7))
            rb, rbk = RB, "rbuf"
            tk.op("act", lambda a: a.activation(out=rb[:, :n], in_=PS[5][:, :n], func=AF.Sqrt, bias=EPS, scale=1.0 / D),
                  reads=[psk(5)], writes=[rbk])
            tk.op("dve", lambda v: v.reciprocal(out=rb[:, :n], in_=rb[:, :n]), reads=[rbk], writes=[rbk])
            for j in range(8):
                t1, t1k = tmp()
                tk.op("dve", lambda v, j=j, t1=t1: v.tensor_tensor(out=t1[:, :n], in0=xT[:, j, t0:t0 + n], in1=rb[:, :n],
                                                                    op=ALU.mult), reads=xk + [rbk], writes=[t1k])
                if router_bank is None:
                    tk.op("act", lambda a, j=j, t1=t1: a.activation(
                        out=hT[:, j, t0:t0 + n], in_=t1[:, :n], func=AF.Identity, scale=Aap[:, j:j + 1], bias=Bap[:, j:j + 1]),
                        reads=[t1k, "small", "mT"], writes=hk)
                else:
                    tk.op("act", lambda a, j=j, t1=t1: a.activation(
                        out=t1[:, :n], in_=t1[:, :n], func=AF.Identity, scale=Aap[:, j:j + 1], bias=Bap[:, j:j + 1]),
                        reads=[t1k, "small", "mT"], writes=[t1k])
                    tk.op("pool", lambda g, j=j, t1=t1: g.tensor_copy(out=hT[:, j, t0:t0 + n], in_=t1[:, :n]),
                          reads=[t1k], writes=hk)
                    lo, lok = pbuf()
                    tk.op("dve", lambda v, j=j, t1=t1, lo=lo: v.tensor_tensor(out=lo[:, :n], in0=t1[:, :n], in1=hT[:, j, t0:t0 + n],
                                                                              op=ALU.subtract), reads=[t1k] + hk, writes=[lok])
                    tk.op("pe", lambda t, j=j: t.matmul(PS[router_bank][0:16, :n], rw_hi[:, j, :], hT[:, j, t0:t0 + n],
                                                        start=(j == 0), stop=False),
                          reads=hk + ["rw"], writes=[psk(router_bank)], inc=False)
                    tk.op("pe", lambda t, j=j: t.matmul(PS[router_bank][0:16, :n], rw_lo[:, j, :], hT[:, j, t0:t0 + n],
                                                        start=False, stop=False),
                          reads=hk + ["rw2"], writes=[psk(router_bank)], inc=False)
                    tk.op("pe", lambda t, j=j, lo=lo: t.matmul(PS[router_bank][0:16, :n], rw_hi[:, j, :], lo[:, :n],
                                                               start=False, stop=(j == 7)),
                          reads=[lok, "rw"], writes=[psk(router_bank)], inc=(j == 7))

        def dump(ap, width, col0, keys):
            if dbg_d is None:
                return
            tk.dma("pool", lambda q: q.dma_start(out=dbg_d[0:ap.shape[0], col0:col0 + width], in_=ap), reads=keys)

        QT = arena[:, 0:4608].rearrange("p (c t) -> p c t", c=2)
        KT = arena[:, 4608:13824].rearrange("p (c t) -> p c t", c=4)
        VA = arena[:, 13824:18576].rearrange("p (t h d) -> p t h d", t=NTT, h=4)
        YT = arena[:, 18576:23184].rearrange("p (c t) -> p c t", c=2)
        WI = arena[:, 23184:29328].rearrange("p (j c) -> p j c", j=8)
        WO = arena[:, 29328:31376].rearrange("p (k c) -> p k c", k=2)
        EW = [arena[:, i * 12288:(i + 1) * 12288] for i in range(2)]
        HM = arena[:, 24576:26624].rearrange("p (f t) -> p f t", f=4)
        WT = arena[0:16, 26624:31232].bitcast(F32) if False else None

        wrt_hi = arena[0:16, 26624:26624 + T]
        wrt_lo = arena[0:16, 26624 + T:26624 + 2 * T]

        def qk_post(ps_ap, nh, hd, gain_ap, rope_tile, out_views, tt, is_lat):
            width = nh * hd
            sq, sqk = tmp()
            tk.op("act", lambda a: a.activation(out=sq[:, :width], in_=ps_ap, func=AF.Square),
                  reads=[psk(ps_bank[0])], writes=[sqk])
            ss = stat[:, 0:nh]
            tk.op("dve", lambda v: v.tensor_reduce(out=ss, in_=sq[:, :width].rearrange("p (h d) -> p h d", h=nh),
                                                   axis=AX.X, op=ALU.add), reads=[sqk], writes=["stat"])
            tk.op("act", lambda a: a.activation(out=ss, in_=ss, func=AF.Sqrt, bias=EPS, scale=1.0 / hd),
                  reads=["stat"], writes=["stat"])
            tk.op("dve", lambda v: v.reciprocal(out=ss, in_=ss), reads=["stat"], writes=["stat"])
            qn, qnk = tmp()
            tk.op("dve", lambda v: v.tensor_tensor(
                out=qn[:, :width].rearrange("p (h d) -> p h d", h=nh), in0=ps_ap.rearrange("p (h d) -> p h d", h=nh),
                in1=ss.unsqueeze(2).broadcast_to([128, nh, hd]), op=ALU.mult),
                reads=[psk(ps_bank[0]), "stat"], writes=[qnk])
            qg, qgk = tmp()
            tk.op("pool", lambda g: g.tensor_tensor(
                out=qg[:, :width].rearrange("p (h d) -> p h d", h=nh), in0=qn[:, :width].rearrange("p (h d) -> p h d", h=nh),
                in1=gain_ap.unsqueeze(1).broadcast_to([128, nh, hd]), op=ALU.mult),
                reads=[qnk, "small"], writes=[qgk])
            if not is_lat:
                for (oap, sel) in out_views:
                    src = qg[:, :width] if sel is None else sel(qg[:, :width])
                    tk.op("dve", lambda v, oap=oap, src=src: v.tensor_copy(out=oap, in_=src), reads=[qgk],
                          writes=[out_key[0]])
                return
            cos_ap, sin_ap = rope_tile
            e4 = hd // 4
            t1, t1k = tmp()
            tk.op("pool", lambda g: g.tensor_tensor(
                out=t1[:, :width].rearrange("p (h d) -> p h d", h=nh), in0=qg[:, :width].rearrange("p (h d) -> p h d", h=nh),
                in1=cos_ap.unsqueeze(1).broadcast_to([128, nh, hd]), op=ALU.mult), reads=[qgk, "ropet"], writes=[t1k])
            t2, t2k = tmp()
            qv = qg[:, :width].rearrange("p (h b s e) -> p h b s e", h=nh, b=2, s=2)
            tv = t2[:, :width].rearrange("p (h b s e) -> p h b s e", h=nh, b=2, s=2)
            sv = sin_ap.rearrange("p (b s e) -> p b s e", b=2, s=2)
            for s_ in range(2):
                tk.op("dve", lambda v, s_=s_: v.tensor_tensor(
                    out=tv[:, :, :, s_, :], in0=qv[:, :, :, 1 - s_, :],
                    in1=sv[:, :, s_, :].unsqueeze(1).broadcast_to([128, nh, 2, e4]), op=ALU.mult),
                    reads=[qgk, "ropet"], writes=[t2k])
            for (oap, sel) in out_views:
                a_ = t1[:, :width] if sel is None else sel(t1[:, :width])
                b_ = t2[:, :width] if sel is None else sel(t2[:, :width])
                tk.op("dve", lambda v, oap=oap, a_=a_, b_=b_: v.tensor_tensor(out=oap, in0=a_, in1=b_, op=ALU.add),
                      reads=[t1k, t2k], writes=[out_key[0]])

        ps_bank = [0]
        out_key = ["tq"]

        if dbg == "p0":
            tk.barrier()
            dump(mT[:, 0].rearrange("p j r -> p (j r)"), 48 * (NB + 1), 0, [])
            tk.barrier()
            return nc
        for b in range(NB):
          try:
            for tt in range(16):
                load_tokens_T(x_d[b, tt * 128:(tt + 1) * 128, :], tt * 128)
            for tt in range(2):
                load_tokens_T(ctx_d[b, tt * 128:(tt + 1) * 128, :], S + tt * 128)
            tk.barrier()
            if dbg == "ld":
                dump(xT[:, 0, :], 2304, 0, [])
                dump(xT[:, 7, :], 2304, 2304, [])
                tk.barrier()
                return nc

            for l in range(DEPTH):
                pass
                need_ctx = l < DEPTH - 1
                lam_init = 0.8 - 0.6 * math.exp(-0.3 * l)
                mrow = lambda k, r: mT[:, l, k * 8:(k + 1) * 8, r]
                tk.dma("sp", lambda q: q.dma_start(out=g1T, in_=ng1[l].rearrange("(j p) -> p j", p=128)), writes=["small"])
                tk.dma("sp", lambda q: q.dma_start(out=g2T, in_=ng2[l].rearrange("(j p) -> p j", p=128)), writes=["small"])
                tk.dma("sp", lambda q: q.dma_start(out=ogT, in_=og_d[l].rearrange("(j p) -> p j", p=128)), writes=["small"])
                tk.dma("sp", lambda q: q.dma_start(out=sinkx, in_=sink_d[l:l + 1, :].partition_broadcast(128)), writes=["small"])
                tk.dma("sp", lambda q: q.dma_start(
                    out=lamraw, in_=lam_d[l:l + 1].rearrange("o a d -> o (a d)").partition_broadcast(128)), writes=["small"])
                for r in range(2):
                    rr = b if r == 0 else NB
                    tk.op("dve", lambda v, r=r, rr=rr: v.scalar_tensor_tensor(
                        out=A1[:, :, r], in0=mrow(1, rr), scalar=1.0, in1=g1T, op0=ALU.add, op1=ALU.mult),
                        reads=["small", "mT"], writes=["small"])
                    tk.op("dve", lambda v, r=r, rr=rr: v.scalar_tensor_tensor(
                        out=A2[:, :, r], in0=mrow(4, rr), scalar=1.0, in1=g2T, op0=ALU.add, op1=ALU.mult),
                        reads=["small", "mT"], writes=["small"])
                tk.op("act", lambda a: a.activation(out=sinkx, in_=sinkx, func=AF.Exp), reads=["small"], writes=["small"])
                lr = lamraw.rearrange("p (a d) -> p a d", a=4)
                lw = stat[:, 32:36]
                tk.op("dve", lambda v: v.tensor_tensor(out=lamraw[:, 0:32], in0=lr[:, 0, :], in1=lr[:, 1, :], op=ALU.mult),
                      reads=["small"], writes=["small"])
                tk.op("dve", lambda v: v.tensor_tensor(out=lamraw[:, 64:96], in0=lr[:, 2, :], in1=lr[:, 3, :], op=ALU.mult),
                      reads=["small"], writes=["small"])
                tk.op("dve", lambda v: v.tensor_reduce(out=lw[:, 0:2], in_=lamraw.rearrange("p (a d) -> p a d", a=2)[:, :, 0:32],
                                                       axis=AX.X, op=ALU.add), reads=["small"], writes=["stat2"])
                tk.op("act", lambda a: a.activation(out=lw[:, 0:2], in_=lw[:, 0:2], func=AF.Exp), reads=["stat2"], writes=["stat2"])
                tk.op("dve", lambda v: v.tensor_tensor(out=lw[:, 2:3], in0=lw[:, 0:1], in1=lw[:, 1:2], op=ALU.subtract),
                      reads=["stat2"], writes=["stat2"])
                tk.op("act", lambda a: a.activation(out=lamt[:, 0:1], in_=lw[:, 2:3], func=AF.Identity, scale=-1.0, bias=-lam_init),
                      reads=["stat2"], writes=["small"])

                if dbg == "sm":
                    print("nops at sm", tk.nops)
                    tk.barrier()
                    dump(small[:, :], 512, 0, [])
                    tk.barrier()
                    return nc
                for cch in range(4):
                    norm_to_h(cch * 512, 512, A1[:, :, 0], mrow(0, b))
                norm_to_h(S, L, A1[:, :, 1], mrow(0, NB))
                if dbg == "h":
                    tk.barrier()
                    dump(hT[:, 0, 0:2304], 2304, 0, hkeys(0, T))
                    dump(hT[:, 7, 0:2304], 2304, 2304, hkeys(0, T))
                    tk.barrier()
                    return nc

                for mi, (mname, col0, nq, nk, nv, nkv, hd) in enumerate(MIX):
                    ncols = nq + nk + nv
                    nh_q = nq // hd
                    nh_k = nk // hd
                    scale = hd ** -0.5
                    tk.dma("pool", lambda q: q.dma_start(
                        out=WI[:, :, 0:ncols], in_=w_in[l, :, col0:col0 + ncols].rearrange("(j p) c -> p j c", p=128)),
                        writes=["WI"])
                    tk.dma("pool", lambda q: q.dma_start(
                        out=WO[:, :, :], in_=w_out[l, mi * 256:(mi + 1) * 256, :].rearrange("(k p) c -> p k c", p=128)),
                        writes=["WO"])
                    gd = qkg[mname]
                    tk.dma("sp", lambda q: q.dma_start(out=gq[:, 0:hd], in_=gd[l, 0:1, :].partition_broadcast(128)), writes=["small"])
                    tk.dma("sp", lambda q: q.dma_start(out=gk[:, 0:hd], in_=gd[l, 1:2, :].partition_broadcast(128)), writes=["small"])
                    tk.op("dve", lambda v: v.tensor_scalar(out=gq[:, 0:hd], in0=gq[:, 0:hd], scalar1=scale, scalar2=None,
                                                           op0=ALU.mult), reads=["small"], writes=["small"])
                    tk.op("dve", lambda v: v.memset(VA[:, :, :, 64:65], 1.0), writes=["VA"])

                    if mname == "C":
                        tk.op("dve", lambda v: v.memset(TK_[:], 0.0), writes=["tkp"])
                    for tt in range(NTT):
                        is_lat = tt < 16 and mname != "B"
                        if not is_lat and False:
                            pass
                        banks = [0, 1] if tt % 2 == 0 else [2, 3]
                        pieces = [(0, min(512, ncols), banks[0])]
                        if ncols > 512:
                            pieces.append((512, ncols, banks[1]))
                        for (c0, c1, bank) in pieces:
                            for j in range(8):
                                tk.op("pe", lambda t, j=j, c0=c0, c1=c1, bank=bank: t.matmul(
                                    PS[bank][:, 0:c1 - c0], hT[:, j, tt * 128:(tt + 1) * 128], WI[:, j, c0:c1],
                                    start=(j == 0), stop=(j == 7)),
                                    reads=[("hT", tt), "WI"], writes=[psk(bank)], inc=(j == 7))
                        if tt < 16:
                            tk.dma("sp", lambda q: q.dma_start(out=ropet[:], in_=rope_d[tt * 128:(tt + 1) * 128, :]),
                                   writes=["ropet"])
                        rt = (ropet[:, 0:64], ropet[:, 64:128]) if hd == 64 else (ropet[:, 128:160], ropet[:, 160:192])
                        ps_bank[0] = banks[0]
                        out_key[0] = "tq"
                        if mname in ("A", "D"):
                            qviews = [(TQ[:].rearrange("p (c s d) -> p s c d", c=2, s=2),
                                       (lambda ap: ap.rearrange("p (s c d) -> p s c d", s=2, c=2)))]
                        else:
                            qviews = [(TQ[:, :], None)]
                        qk_post(PS[banks[0]][:, 0:256], nh_q if hd == 64 else 8, hd, gq[:, 0:hd], rt, qviews, tt, is_lat)
                        for cc in range(2):
                            tk.op("pe", lambda t, cc=cc: t.transpose(
                                out=PS[4][:].bitcast(BF16)[:, cc * 128:(cc + 1) * 128], in_=TQ[:, cc * 128:(cc + 1) * 128],
                                identity=ident_b[:]), reads=["tq", "ident_b"], writes=[psk(4)], inc=(cc == 1))
                        tk.op("act", lambda a: a.copy(out=QT[:, :, tt * 128:(tt + 1) * 128],
                                                      in_=PS[4][:].bitcast(BF16)[:, 0:256].rearrange("p (c t) -> p c t", c=2)),
                              reads=[psk(4)], writes=[("QT", tt)])
                        out_key[0] = "tkp"
                        if mname == "C":
                            tkv = TK_[:].rearrange("p (hp i hh i2 d) -> p hp i hh i2 d", hp=2, i=2, hh=2, i2=2)
                            views = []
                            for i_ in range(2):
                                views.append((tkv[:, :, i_, :, i_, :],
                                              (lambda ap, i_=i_: ap.rearrange("p (hp hh i d) -> p hp hh i d", hp=2, hh=2, i=2)[:, :, :, i_, :])))
                            qk_post(PS[banks[0]][:, 256:512], 8, 32, gk[:, 0:32], rt, views, tt, is_lat)
                            nkc = 4
                        else:
                            qk_post(PS[banks[0]][:, 256:256 + nk], nh_k, hd, gk[:, 0:hd], rt, [(TK_[:, 0:nk], None)], tt, is_lat)
                            nkc = nk // 128
                        for cc in range(nkc):
                            tk.op("pe", lambda t, cc=cc: t.transpose(
                                out=PS[4][:].bitcast(BF16)[:, 512 + cc * 128:512 + (cc + 1) * 128],
                                in_=TK_[:, cc * 128:(cc + 1) * 128], identity=ident_b[:]),
                                reads=["tkp", "ident_b"], writes=[psk(4)], inc=(cc == nkc - 1))
                        tk.op("act", lambda a, nkc=nkc: a.copy(
                            out=KT[:, 0:nkc, tt * 128:(tt + 1) * 128],
                            in_=PS[4][:].bitcast(BF16)[:, 512:512 + nkc * 128].rearrange("p (c t) -> p c t", c=nkc)),
                            reads=[psk(4)], writes=[("KT", tt)])
                        if nk == 128:
                            vsrc = PS[banks[0]][:, 384:512]
                            vb = banks[0]
                        else:
                            vsrc = PS[banks[1]][:, 0:256]
                            vb = banks[1]
                        tk.op("act", lambda a, vsrc=vsrc: a.copy(out=VA[:, tt, 0:nkv, 0:64],
                                                                in_=vsrc.rearrange("p (h d) -> p h d", h=nkv)),
                              reads=[psk(vb)], writes=[("VA", tt)])
                    if dbg == "qkv" and mname == dbg_mixer[0]:
                        tk.barrier()
                        dump(QT[:, 0, :], 2304, 0, [])
                        dump(KT[:, 0, :], 2304, 2304, [])
                        dump(VA[:, 3, :, :].rearrange("p h d -> p (h d)"), 264, 4608, [])
                        dump(KT[:, 3, :], 2304, 4900, [])
                        tk.barrier()
                        return nc

                    ranges = [(r0 * 512, 512) for r0 in range(4)] + ([(S, L)] if need_ctx else [])
                    for (q0, n) in ranges:
                        nblk = n // 128
                        is_ctxq = q0 >= S
                        units = []
                        if mname in ("A", "D"):
                            for h in range(4):
                                units.append((h, 0, h % 2, (h // 2) * 64, 0, (h // 2) * 64, h // 2))
                        elif mname == "B":
                            for h in range(4):
                                units.append((h, 0, h // 2, (h % 2) * 64, h // 2, (h % 2) * 64, h))
                        else:
                            for h in range(4):
                                for i_ in range(2):
                                    units.append((h, i_, h // 2, (h % 2) * 64, (h // 2) * 2 + i_, (h % 2) * 64, h))
                        for (h, br, qc, qp, kc_, kp, vh) in units:
                            subs = []
                            if is_ctxq or mname in ("C", "D"):
                                kts = [16, 17] if is_ctxq else list(range(18))
                                subs.append((0, n, [(kt, None, None) for kt in kts]))
                            elif mname == "A":
                                for bl in range(nblk):
                                    i = q0 // 128 + bl
                                    lst = []
                                    if i - 1 >= 0:
                                        lst.append((i - 1, wmask[:, 0:128], "wmask"))
                                    lst.append((i, None, None))
                                    if i + 1 < 16:
                                        lst.append((i + 1, wmask[:, 128:256], "wmask"))
                                    lst += [(16, None, None), (17, None, None)]
                                    subs.append((bl * 128, 128, lst))
                            else:
                                for bl in range(nblk):
                                    i = q0 // 128 + bl
                                    ts = na_ts(i)
                                    vi = na_variant(i)
                                    stg, stgk = tmp()
                                    stg2, stg2k = tmp()
                                    tk.dma("sp", lambda q, vi=vi, stg=stg: q.dma_start(out=stg[:, 0:512], in_=nab_d[l, h, vi, :, 0:512]),
                                           writes=[stgk])
                                    tk.dma("sp", lambda q, vi=vi, stg2=stg2: q.dma_start(out=stg2[:, 0:128], in_=nab_d[l, h, vi, :, 512:640]),
                                           writes=[stg2k])
                                    tk.op("act", lambda a, stg=stg: a.activation(out=nabm[:, 0:512], in_=stg[:, 0:512], func=AF.Exp),
                                          reads=[stgk], writes=["nabm"])
                                    tk.op("act", lambda a, stg2=stg2: a.activation(out=nabm[:, 512:640], in_=stg2[:, 0:128], func=AF.Exp),
                                          reads=[stg2k], writes=["nabm"])
                                    lst = [(ts + j, nabm[:, j * 128:(j + 1) * 128], "nabm") for j in range(5)]
                                    lst += [(16, None, None), (17, None, None)]
                                    subs.append((bl * 128, 128, lst))
                            ob = 6
                            for (qa, qn, lst) in subs:
                                for ki, (kt, mask_ap, mkey) in enumerate(lst):
                                    sbank = 4 + (ki % 2)
                                    tk.op("pe", lambda t, kt=kt, sbank=sbank, qa=qa, qn=qn: t.matmul(
                                        PS[sbank][:, 0:qn], KT[kp:kp + 64, kc_, kt * 128:(kt + 1) * 128],
                                        QT[qp:qp + 64, qc, q0 + qa:q0 + qa + qn], start=True, stop=True),
                                        reads=[("KT", kt)] + [("QT", (q0 + qa) // 128 + z) for z in range(qn // 128)],
                                        writes=[psk(sbank)])
                                    pbt, pbk = pbuf()
                                    tk.op("act", lambda a, sbank=sbank, pbt=pbt, qn=qn: a.activation(
                                        out=pbt[:, 0:qn], in_=PS[sbank][:, 0:qn], func=AF.Exp), reads=[psk(sbank)], writes=[pbk])
                                    if mask_ap is not None:
                                        tk.op("dve", lambda v, pbt=pbt, qn=qn, mask_ap=mask_ap: v.tensor_tensor(
                                            out=pbt[:, 0:qn], in0=pbt[:, 0:qn], in1=mask_ap, op=ALU.mult),
                                            reads=[pbk, mkey], writes=[pbk])
                                    tk.op("pe", lambda t, kt=kt, pbt=pbt, qa=qa, qn=qn, ki=ki, nl=len(lst): t.matmul(
                                        PS[ob][0:65, qa:qa + qn], VA[:, kt, vh, 0:65], pbt[:, 0:qn],
                                        start=(ki == 0), stop=(ki == nl - 1)),
                                        reads=[("VA", kt), "VA", pbk], writes=[psk(ob)], inc=(ki == len(lst) - 1))
                            ot, otk = tmp()
                            tk.op("dve", lambda v, ot=ot: v.tensor_copy(out=ot[0:65, 0:n], in_=PS[ob][0:65, 0:n]),
                                  reads=[psk(ob)], writes=[otk])
                            for bl in range(nblk):
                                tk.op("pe", lambda t, bl=bl, ot=ot: t.transpose(
                                    out=PS[7][:, bl * 128:bl * 128 + 65], in_=ot[0:65, bl * 128:(bl + 1) * 128],
                                    identity=ident_f[0:65, 0:65]), reads=[otk, "ident_f"], writes=[psk(7)], inc=(bl == nblk - 1))
                            p7 = PS[7][:].rearrange("p (b d) -> p b d", d=128)
                            rd = stat[:, 8:8 + nblk]
                            if mname == "A":
                                tk.op("dve", lambda v: v.tensor_scalar(out=rd, in0=p7[:, 0:nblk, 64], scalar1=sinkx[:, h:h + 1],
                                                                       scalar2=None, op0=ALU.add),
                                      reads=[psk(7), "small"], writes=["stat3"])
                                tk.op("dve", lambda v: v.reciprocal(out=rd, in_=rd), reads=["stat3"], writes=["stat3"])
                            else:
                                tk.op("dve", lambda v: v.reciprocal(out=rd, in_=p7[:, 0:nblk, 64]), reads=[psk(7)], writes=["stat3"])
                            if br == 1:
                                tk.op("dve", lambda v: v.tensor_scalar(out=rd, in0=rd, scalar1=lamt[:, 0:1], scalar2=None, op0=ALU.mult),
                                      reads=["stat3", "small"], writes=["stat3"])
                            for bl in range(nblk):
                                if br == 0:
                                    tk.op("dve", lambda v, bl=bl: v.tensor_scalar(
                                        out=otok[:, bl, h * 64:(h + 1) * 64], in0=p7[:, bl, 0:64], scalar1=rd[:, bl:bl + 1],
                                        scalar2=None, op0=ALU.mult), reads=[psk(7), "stat3"], writes=[("otok", bl)])
                                else:
                                    tk.op("dve", lambda v, bl=bl: v.scalar_tensor_tensor(
                                        out=otok[:, bl, h * 64:(h + 1) * 64], in0=p7[:, bl, 0:64], scalar=rd[:, bl:bl + 1],
                                        in1=otok[:, bl, h * 64:(h + 1) * 64], op0=ALU.mult, op1=ALU.add),
                                        reads=[psk(7), "stat3", ("otok", bl)], writes=[("otok", bl)])
                        for bl in range(nblk):
                            tok = q0 + bl * 128
                            ng = 1 if mname != "C" else 4
                            gsz = 256 // ng
                            sq, sqk = tmp()
                            tk.op("act", lambda a, bl=bl, sq=sq: a.activation(out=sq[:, 0:256], in_=otok[:, bl, :], func=AF.Square),
                                  reads=[("otok", bl)], writes=[sqk])
                            ssg = stat[:, 16:16 + ng]
                            tk.op("dve", lambda v, sq=sq: v.tensor_reduce(out=ssg, in_=sq[:, 0:256].rearrange("p (g d) -> p g d", g=ng),
                                                                          axis=AX.X, op=ALU.add), reads=[sqk], writes=["stat4"])
                            tk.op("act", lambda a: a.activation(out=ssg, in_=ssg, func=AF.Sqrt, bias=EPS, scale=1.0 / gsz),
                                  reads=["stat4"], writes=["stat4"])
                            tk.op("dve", lambda v: v.reciprocal(out=ssg, in_=ssg), reads=["stat4"], writes=["stat4"])
                            if mname == "C":
                                tk.op("dve", lambda v: v.tensor_scalar(out=ssg, in0=ssg, scalar1=(1.0 - lam_init), scalar2=None,
                                                                       op0=ALU.mult), reads=["stat4"], writes=["stat4"])
                            tk.op("dve", lambda v, bl=bl: v.tensor_tensor(
                                out=ytok[:].rearrange("p (g d) -> p g d", g=ng), in0=otok[:, bl, :].rearrange("p (g d) -> p g d", g=ng),
                                in1=ssg.unsqueeze(2).broadcast_to([128, ng, gsz]), op=ALU.mult),
                                reads=[("otok", bl), "stat4"], writes=["ytok"])
                            for cc in range(2):
                                tk.op("pe", lambda t, cc=cc: t.transpose(
                                    out=PS[7][:].bitcast(BF16)[:, 512 + cc * 128:512 + (cc + 1) * 128],
                                    in_=ytok[:, cc * 128:(cc + 1) * 128], identity=ident_b[:]),
                                    reads=["ytok", "ident_b"], writes=[psk(7)], inc=(cc == 1))
                            for cc in range(2):
                                tk.op("act", lambda a, cc=cc, tok=tok: a.activation(
                                    out=YT[:, cc, tok:tok + 128], in_=PS[7][:].bitcast(BF16)[:, 512 + cc * 128:512 + (cc + 1) * 128],
                                    func=AF.Identity, scale=ogT[:, mi * 2 + cc:mi * 2 + cc + 1]),
                                    reads=[psk(7), "small"], writes=[("YT", tok // 128)])
                    if dbg == "y" and mname == dbg_mixer[0]:
                        tk.barrier()
                        dump(YT[:, 0, :], 2304, 0, [])
                        dump(YT[:, 1, :], 2304, 2304, [])
                        tk.barrier()
                        return nc

                    for (q0, n) in ranges:
                        rr = NB if q0 >= S else b
                        for jo in range(8):
                            bank = jo % 4
                            for kc2 in range(2):
                                tk.op("pe", lambda t, jo=jo, kc2=kc2, bank=bank: t.matmul(
                                    PS[bank][:, 0:n], WO[:, kc2, jo * 128:(jo + 1) * 128], YT[:, kc2, q0:q0 + n],
                                    start=(kc2 == 0), stop=(kc2 == 1)),
                                    reads=["WO"] + [("YT", q0 // 128 + z) for z in range(n // 128)], writes=[psk(bank)],
                                    inc=(kc2 == 1))
                            tk.op("dve", lambda v, jo=jo, bank=bank, rr=rr: v.scalar_tensor_tensor(
                                out=xT[:, jo, q0:q0 + n], in0=PS[bank][:, 0:n], scalar=mT[:, l, 16 + jo, rr:rr + 1],
                                in1=xT[:, jo, q0:q0 + n], op0=ALU.mult, op1=ALU.add),
                                reads=[psk(bank), "mT"] + xkeys(q0, n), writes=xkeys(q0, n))
                    tk.barrier()
                if dbg == "xattn":
                    dump(xT[:, 0, :], 2304, 0, [])
                    dump(xT[:, 5, :], 2304, 2304, [])
                    tk.barrier()
                    return nc

                ranges = [(r0 * 512, 512) for r0 in range(4)] + ([(S, L)] if need_ctx else [])
                for (q0, n) in ranges:
                    r = 1 if q0 >= S else 0
                    rr = NB if q0 >= S else b
                    norm_to_h(q0, n, A2[:, :, r], mrow(3, rr), router_bank=6)
                    lg, lgk = RB, "rbuf"
                    tk.op("dve", lambda v, lg=lg: v.tensor_copy(out=lg[0:16, 0:n], in_=PS[6][0:16, 0:n]), reads=[psk(6)], writes=[lgk])
                    for bl in range(n // 128):
                        tk.op("pe", lambda t, bl=bl, lg=lg: t.transpose(out=PS[7][:, 0:16], in_=lg[0:16, bl * 128:(bl + 1) * 128],
                                                                       identity=ident_f[0:16, 0:16]),
                              reads=[lgk, "ident_f"], writes=[psk(7)])
                        sc = stat[:, 0:16]
                        sel = stat[:, 16:32]
                        w8 = stat[:, 32:40]
                        tk.op("act", lambda a: a.activation(out=sc, in_=PS[7][:, 0:16], func=AF.Sigmoid), reads=[psk(7)], writes=["stat"])
                        tk.op("dve", lambda v: v.tensor_tensor(out=sel, in0=sc, in1=rb_bc, op=ALU.add), reads=["stat", "small"], writes=["stat"])
                        s4 = sel.rearrange("p (g a c) -> p g a c", g=4, a=2)
                        pq = stat[:, 40:48].rearrange("p (g a) -> p g a", g=4)
                        rs_ = stat[:, 48:56].rearrange("p (g a) -> p g a", g=4)
                        tk.op("dve", lambda v: v.tensor_tensor(out=pq, in0=s4[:, :, :, 0], in1=s4[:, :, :, 1], op=ALU.max), reads=["stat"], writes=["stat"])
                        tk.op("dve", lambda v: v.tensor_tensor(out=rs_, in0=s4[:, :, :, 0], in1=s4[:, :, :, 1], op=ALU.min), reads=["stat"], writes=["stat"])
                        m1 = stat[:, 56:60]
                        m2_ = stat[:, 60:64]
                        tk.op("dve", lambda v: v.tensor_tensor(out=m1, in0=pq[:, :, 0], in1=pq[:, :, 1], op=ALU.max), reads=["stat"], writes=["stat"])
                        tk.op("dve", lambda v: v.tensor_tensor(out=m2_, in0=pq[:, :, 0], in1=pq[:, :, 1], op=ALU.min), reads=["stat"], writes=["stat"])
                        tk.op("dve", lambda v: v.tensor_tensor(out=pq[:, :, 0], in0=rs_[:, :, 0], in1=rs_[:, :, 1], op=ALU.max), reads=["stat"], writes=["stat"])
                        tk.op("dve", lambda v: v.tensor_tensor(out=m2_, in0=m2_, in1=pq[:, :, 0], op=ALU.max), reads=["stat"], writes=["stat"])
                        tk.op("dve", lambda v: v.tensor_tensor(out=m1, in0=m1, in1=m2_, op=ALU.add), reads=["stat"], writes=["stat"])
                        gmx = w8[:, 0:1]
                        tk.op("dve", lambda v: v.tensor_reduce(out=gmx, in_=m1, axis=AX.X, op=ALU.max), reads=["stat"], writes=["stat"])
                        tk.op("dve", lambda v: v.tensor_scalar(out=m2_, in0=m1, scalar1=gmx, scalar2=None, op0=ALU.is_ge), reads=["stat"], writes=["stat"])
                        tk.op("act", lambda a: a.activation(out=m1, in_=m2_, func=AF.Identity, scale=100.0, bias=-100.0),
                              reads=["stat"], writes=["stat"])
                        sel3 = sel.rearrange("p (g c) -> p g c", g=4)
                        tk.op("dve", lambda v: v.tensor_tensor(out=sel3, in0=sel3, in1=m2_.unsqueeze(2).broadcast_to([128, 4, 4]), op=ALU.mult),
                              reads=["stat"], writes=["stat"])
                        tk.op("dve", lambda v: v.tensor_tensor(out=sel3, in0=sel3, in1=m1.unsqueeze(2).broadcast_to([128, 4, 4]), op=ALU.add),
                              reads=["stat"], writes=["stat"])
                        tk.op("dve", lambda v: v.max(out=w8, in_=sel), reads=["stat"], writes=["stat"])
                        tk.op("dve", lambda v: v.tensor_scalar(out=sel, in0=sel, scalar1=w8[:, 1:2], scalar2=None, op0=ALU.is_ge),
                              reads=["stat"], writes=["stat"])
                        tk.op("dve", lambda v: v.tensor_tensor(out=sc, in0=sc, in1=sel, op=ALU.mult), reads=["stat"], writes=["stat"])
                        tk.op("dve", lambda v: v.tensor_reduce(out=gmx, in_=sc, axis=AX.X, op=ALU.add), reads=["stat"], writes=["stat"])
                        tk.op("dve", lambda v: v.reciprocal(out=gmx, in_=gmx), reads=["stat"], writes=["stat"])
                        wtok, wtokk = tmp()
                        tk.op("dve", lambda v, wtok=wtok: v.tensor_scalar(out=wtok[:, 0:16], in0=sc, scalar1=gmx, scalar2=None, op0=ALU.mult),
                              reads=["stat"], writes=[wtokk])
                        tk.op("pe", lambda t, wtok=wtok: t.transpose(out=PS[7][0:16, 128:256], in_=wtok[:, 0:16], identity=ident_f[:]),
                              reads=[wtokk, "ident_f"], writes=[psk(7)])
                        tk.op("act", lambda a, bl=bl: a.copy(out=wrt_hi[:, q0 + bl * 128:q0 + (bl + 1) * 128], in_=PS[7][0:16, 128:256]),
                              reads=[psk(7)], writes=[("wrt", (q0 // 128) + bl)])
                        tk.op("dve", lambda v, bl=bl: v.tensor_tensor(
                            out=wrt_lo[:, q0 + bl * 128:q0 + (bl + 1) * 128], in0=PS[7][0:16, 128:256],
                            in1=wrt_hi[:, q0 + bl * 128:q0 + (bl + 1) * 128], op=ALU.subtract),
                            reads=[psk(7), ("wrt", (q0 // 128) + bl)], writes=[("wrtl", (q0 // 128) + bl)])
                if dbg == "route":
                    tk.barrier()
                    dump(wrt_hi[:, :], 2304, 0, [])
                    dump(hT[:, 0, 0:2304], 2304, 2304, [])
                    tk.barrier()
                    return nc
                tk.barrier()

                for e in range(NE):
                    ew = EW[e % 2]
                    ewk = ("EW", e % 2)
                    WG = ew[:, 0:4096].rearrange("p (j f) -> p j f", j=8)
                    WU = ew[:, 4096:8192].rearrange("p (j f) -> p j f", j=8)
                    WD = ew[:, 8192:12288].rearrange("p (k c) -> p k c", k=4)
                    tk.dma("pool", lambda q, WG=WG: q.dma_start(out=WG, in_=wg_d[l, e].rearrange("(j p) f -> p j f", p=128)), writes=[ewk])
                    tk.dma("pool", lambda q, WU=WU: q.dma_start(out=WU, in_=wu_d[l, e].rearrange("(j p) f -> p j f", p=128)), writes=[ewk])
                    tk.dma("pool", lambda q, WD=WD: q.dma_start(out=WD, in_=wd_d[l, e].rearrange("(k p) c -> p k c", p=128)), writes=[ewk])
                    for (q0, n) in ranges:
                        rr = NB if q0 >= S else b
                        wk = [("wrt", q0 // 128 + z) for z in range(n // 128)]
                        hk = hkeys(q0, n)
                        wk = wk + [("wrtl", q0 // 128 + z) for z in range(n // 128)]
                        tk.op("pe", lambda t: t.matmul(PS[0][:, 0:n], selb[0:16, e, :], wrt_hi[:, q0:q0 + n],
                                                       start=True, stop=False), reads=wk + ["selb"], writes=[psk(0)], inc=False)
                        tk.op("pe", lambda t: t.matmul(PS[0][:, 0:n], selb[0:16, e, :], wrt_lo[:, q0:q0 + n],
                                                       start=False, stop=True), reads=wk + ["selb"], writes=[psk(0)])
                        for fc in range(4):
                            gb = 1 + (fc % 2)
                            ub = 3 + (fc % 2)
                            for j in range(8):
                                tk.op("pe", lambda t, j=j, fc=fc, gb=gb: t.matmul(
                                    PS[gb][:, 0:n], WG[:, j, fc * 128:(fc + 1) * 128], hT[:, j, q0:q0 + n], start=(j == 0), stop=(j == 7)),
                                    reads=[ewk] + hk, writes=[psk(gb)], inc=(j == 7))
                            for j in range(8):
                                tk.op("pe", lambda t, j=j, fc=fc, ub=ub: t.matmul(
                                    PS[ub][:, 0:n], WU[:, j, fc * 128:(fc + 1) * 128], hT[:, j, q0:q0 + n], start=(j == 0), stop=(j == 7)),
                                    reads=[ewk] + hk, writes=[psk(ub)], inc=(j == 7))
                            sg, sgk = tmp()
                            tk.op("act", lambda a, sg=sg, gb=gb: a.activation(out=sg[:, 0:n], in_=PS[gb][:, 0:n], func=AF.Silu),
                                  reads=[psk(gb)], writes=[sgk])
                            tk.op("dve", lambda v, sg=sg, ub=ub: v.tensor_tensor(out=sg[:, 0:n], in0=sg[:, 0:n], in1=PS[ub][:, 0:n], op=ALU.mult),
                                  reads=[sgk, psk(ub)], writes=[sgk])
                            tk.op("dve", lambda v, sg=sg, fc=fc: v.tensor_tensor(out=HM[:, fc, 0:n], in0=sg[:, 0:n], in1=PS[0][:, 0:n], op=ALU.mult),
                                  reads=[sgk, psk(0)], writes=[("HM", fc)])
                        for jo in range(8):
                            yb = 5 + (jo % 3)
                            for fc in range(4):
                                tk.op("pe", lambda t, jo=jo, fc=fc, yb=yb: t.matmul(
                                    PS[yb][:, 0:n], WD[:, fc, jo * 128:(jo + 1) * 128], HM[:, fc, 0:n], start=(fc == 0), stop=(fc == 3)),
                                    reads=[ewk, ("HM", fc)], writes=[psk(yb)], inc=(fc == 3))
                            tk.op("dve", lambda v, jo=jo, yb=yb, rr=rr: v.scalar_tensor_tensor(
                                out=xT[:, jo, q0:q0 + n], in0=PS[yb][:, 0:n], scalar=mT[:, l, 40 + jo, rr:rr + 1],
                                in1=xT[:, jo, q0:q0 + n], op0=ALU.mult, op1=ALU.add),
                                reads=[psk(yb), "mT"] + xkeys(q0, n), writes=xkeys(q0, n))
                tk.barrier()

            for tt in range(16):
                for g in range(2):
                    bank = 6 + g
                    for jj in range(4):
                        j = g * 4 + jj
                        tk.op("pe", lambda t, j=j, jj=jj, bank=bank: t.transpose(
                            out=PS[bank][:, jj * 128:(jj + 1) * 128], in_=xT[:, j, tt * 128:(tt + 1) * 128], identity=ident_f[:]),
                            reads=[("xT", tt), "ident_f"], writes=[psk(bank)], inc=(jj == 3))
                    if g == 0:
                        tk.op("act", lambda a, bank=bank: a.copy(out=XS[:, 0:512], in_=PS[bank][:]), reads=[psk(bank)], writes=["xs"])
                    else:
                        tk.op("dve", lambda v, bank=bank: v.tensor_copy(out=XS[:, 512:1024], in_=PS[bank][:]), reads=[psk(bank)], writes=["xs"])
                tk.dma("sp", lambda q: q.dma_start(out=out_d[b, tt * 128:(tt + 1) * 128, :], in_=XS[:]), reads=["xs"], writes=["out"])
            tk.barrier()
          except StopBuild:
            print("STOPPED at", tk.nops)
            tk.pe_open = False
            tk.limit = 0
            tk.barrier()
            if dbg_d is not None:
                dump(small[:, :], 512, 0, [])
                tk.barrier()
            return nc
        tk.barrier()
    return nc


dbg_mixer = ["D"]

_W_NAMES = ["ada_w", "ada_b", "norm_mix_g", "norm_ffn_g", "w_in", "qk_g_win", "qk_g_na", "qk_g_diff", "qk_g_gqa",
            "sink_win", "lambda_diff", "out_gain", "w_out", "w_gate", "w_up", "w_down"]


def make_in_maps(inputs, n_cores, NB, DEPTH):
    f = lambda a: np.ascontiguousarray(np.asarray(a, dtype=np.float32))
    shared = {k: f(inputs[k])[:DEPTH] for k in _W_NAMES}
    shared["router_w"] = f(inputs["router_w"])
    shared["router_b"] = f(inputs["router_b"]).reshape(1, NE)
    shared["na_bias"] = host_na_bias(f(inputs["rpb_na"])[:DEPTH])
    shared.update(host_consts())
    x = f(inputs["x"])
    c = f(inputs["c"])
    ctx = f(inputs["ctx"])
    cctx = f(inputs["c_ctx"]).reshape(1, D)
    maps = []
    for i in range(n_cores):
        m = dict(shared)
        m["x"] = x[i * NB:(i + 1) * NB]
        m["ctx"] = ctx[i * NB:(i + 1) * NB]
        m["c"] = np.ascontiguousarray(np.concatenate([c[i * NB:(i + 1) * NB], cctx], 0))
        maps.append(m)
    return maps


def kernel(**inputs):
    NB = inputs["x"].shape[0] // N_CORES
    nc = build(NB, DEPTH_FULL)
    maps = make_in_maps(inputs, N_CORES, NB, DEPTH_FULL)
    res = run_bass_kernel_spmd(nc, maps, core_ids=list(range(N_CORES)))
    return np.concatenate([r["out"] for r in res.results], axis=0).astype(np.float32)
```

```python
import math
import os
from contextlib import ExitStack
import numpy as np
import concourse.bass as bass
import concourse.mybir as mybir
from concourse.bass_utils import run_bass_kernel_spmd

F32 = mybir.dt.float32
BF16 = mybir.dt.bfloat16
AF = mybir.ActivationFunctionType
ALU = mybir.AluOpType
AX = mybir.AxisListType

D = 1024
S = 2048
L = 256
T = S + L
NTT = T // 128
DEPTH_FULL = 4
EPS = 1e-6
NE = 16
DE = 512
N_CORES = 8
NDS = 40
MIX = [("A", 0, 256, 128, 128, 2, 64), ("B", 512, 256, 256, 256, 4, 64),
       ("C", 1280, 256, 256, 256, 4, 32), ("D", 2048, 256, 128, 128, 2, 64)]


class StopBuild(Exception):
    pass


class TK:
    def __init__(self, nc, es):
        self.nc = nc
        self.eng = {"pe": nc.tensor, "act": nc.scalar, "dve": nc.vector, "pool": nc.gpsimd, "sp": nc.sync}
        self.sem = {k: es.enter_context(nc.semaphore("s_" + k)) for k in self.eng}
        self.cnt = {k: 0 for k in self.eng}
        self.seen = {k: {} for k in self.eng}
        self.dsem = [es.enter_context(nc.semaphore("d%d" % i)) for i in range(NDS)]
        self.dval = [0] * NDS
        self.dnext = 0
        self.bw = {}
        self.br = {}
        self.pe_open = False
        self.marks = []
        self.nops = 0
        self.limit = int(os.environ.get("TK_LIMIT", "0"))

    def _tick(self):
        self.nops += 1
        if self.limit and self.nops > self.limit:
            raise StopBuild()

    def _semh(self, sk):
        return self.sem[sk] if isinstance(sk, str) else self.dsem[sk[1]]

    def _need(self, e, reads, writes):
        need = {}

        def add(ev, raw):
            if ev is None:
                return
            sk, v = ev
            if sk == e and (not raw or e == "pe"):
                return
            if need.get(sk, 0) < v:
                need[sk] = v

        for k in reads:
            add(self.bw.get(k), True)
        for k in writes:
            add(self.bw.get(k), False)
            r = self.br.get(k)
            if r:
                for sk, v in r.items():
                    add((sk, v), False)
        return need

    def _emit_waits(self, e, need):
        for sk, v in need.items():
            if self.seen[e].get(sk, 0) >= v:
                continue
            self.eng[e].wait_ge(self._semh(sk), v)
            self.seen[e][sk] = v

    def _record(self, ev, reads, writes):
        sk, v = ev
        for k in reads:
            d = self.br.setdefault(k, {})
            if d.get(sk, 0) < v:
                d[sk] = v
        for k in writes:
            self.bw[k] = ev
            self.br[k] = {}

    def op(self, e, fn, reads=(), writes=(), inc=True):
        inc = True
        self._tick()
        need = self._need(e, reads, writes)
        self._emit_waits(e, need)
        ins = fn(self.eng[e])
        if inc:
            self.cnt[e] += 1
            ins.then_inc(self.sem[e], 1)
            ev = (e, self.cnt[e])
            if e == "pe":
                self.pe_open = False
        else:
            ev = (e, self.cnt[e] + 1)
            if e == "pe":
                self.pe_open = True
        self._record(ev, reads, writes)

    def dma(self, q, fn, reads=(), writes=()):
        self._tick()
        need = self._need(q, reads, writes)
        i = self.dnext
        self.dnext = (i + 1) % NDS
        if self.dval[i] > 0:
            sk = ("d", i)
            if need.get(sk, 0) < self.dval[i]:
                need[sk] = self.dval[i]
        self._emit_waits(q, need)
        ins = fn(self.eng[q])
        self.dval[i] += 16
        ins.then_inc(self.dsem[i], 16)
        self._record((("d", i), self.dval[i]), reads, writes)

    def mark(self, name):
        self.marks.append((name, dict(self.cnt)))

    def barrier(self):
        assert not self.pe_open
        for e in self.eng:
            need = {f: self.cnt[f] for f in self.eng if f != e and self.cnt[f] > 0}
            for i in range(NDS):
                if self.dval[i] > 0:
                    need[("d", i)] = self.dval[i]
            self._emit_waits(e, need)
        self.bw = {}
        self.br = {}


def host_consts():
    pos = np.arange(S)
    row, col = pos // 64, pos % 64

    def tabs(hd):
        e = hd // 4
        inv = 10000.0 ** (-np.arange(e, dtype=np.float32) / e)
        ar = row[:, None].astype(np.float32) * inv
        ac = col[:, None].astype(np.float32) * inv
        cos = np.concatenate([np.cos(ar), np.cos(ar), np.cos(ac), np.cos(ac)], 1)
        sins = np.concatenate([-np.sin(ar), np.sin(ar), -np.sin(ac), np.sin(ac)], 1)
        return cos.astype(np.float32), sins.astype(np.float32)

    c64, s64 = tabs(64)
    c32, s32 = tabs(32)
    rope = np.concatenate([c64, s64, c32, s32], 1)
    b = np.arange(128)[:, None]
    a = np.arange(128)[None, :]
    wmask = np.stack([(a <= b), (b <= a)], 1).astype(np.float32)
    return {"ident": np.eye(128, dtype=np.float32), "rope": np.ascontiguousarray(rope),
            "wmask": np.ascontiguousarray(wmask.reshape(128, 256))}


def na_variant(i):
    return {0: 0, 1: 1, 14: 3, 15: 4}.get(i, 2)


def na_ts(i):
    return min(max(i - 2, 0), 11)


def host_na_bias(rpb):
    dl = rpb.shape[0]
    out = np.empty((dl, 4, 5, 128, 640), np.float32)
    kl = np.arange(128) // 64
    kc = np.arange(128) % 64
    ql = np.arange(128) // 64
    qc = np.arange(128) % 64
    for vi, i in enumerate([0, 1, 5, 14, 15]):
        ts = na_ts(i)
        for j in range(5):
            krow = 2 * (ts + j) + kl[:, None]
            qrow = 2 * i + ql[None, :]
            rs = np.clip(qrow - 4, 0, 24)
            rowok = (krow >= rs) & (krow < rs + 8)
            cs = np.clip(qc[None, :] - 8, 0, 48)
            colok = (kc[:, None] >= cs) & (kc[:, None] < cs + 16)
            dr = np.clip(krow - qrow + 7, 0, 14)
            dc = np.clip(kc[:, None] - qc[None, :] + 15, 0, 30)
            ok = rowok & colok
            g = rpb[:, :, dr, dc]
            out[:, :, vi, :, j * 128:(j + 1) * 128] = np.where(ok[None, None], g, np.float32(-30000.0))
    return out


def build(NB, DEPTH, dbg=None):
    nc = bass.Bass("TRN2", target_bir_lowering=False)

    def dram(name, shape, kind="ExternalInput", dtype=F32):
        return nc.dram_tensor(name, list(shape), dtype, kind=kind).ap()

    x_d = dram("x", [NB, S, D])
    c_d = dram("c", [NB + 1, D])
    ctx_d = dram("ctx", [NB, L, D])
    ada_w = dram("ada_w", [DEPTH, D, 6 * D])
    ada_b = dram("ada_b", [DEPTH, 6 * D])
    ng1 = dram("norm_mix_g", [DEPTH, D])
    ng2 = dram("norm_ffn_g", [DEPTH, D])
    w_in = dram("w_in", [DEPTH, D, 2560])
    qkg = {"A": dram("qk_g_win", [DEPTH, 2, 64]), "B": dram("qk_g_na", [DEPTH, 2, 64]),
           "C": dram("qk_g_diff", [DEPTH, 2, 32]), "D": dram("qk_g_gqa", [DEPTH, 2, 64])}
    sink_d = dram("sink_win", [DEPTH, 4])
    nab_d = dram("na_bias", [DEPTH, 4, 5, 128, 640])
    lam_d = dram("lambda_diff", [DEPTH, 4, 32])
    og_d = dram("out_gain", [DEPTH, D])
    w_out = dram("w_out", [DEPTH, D, D])
    rw_d = dram("router_w", [D, NE])
    rb_d = dram("router_b", [1, NE])
    wg_d = dram("w_gate", [DEPTH, NE, D, DE])
    wu_d = dram("w_up", [DEPTH, NE, D, DE])
    wd_d = dram("w_down", [DEPTH, NE, DE, D])
    ident_d = dram("ident", [128, 128])
    rope_d = dram("rope", [S, 192])
    wmask_d = dram("wmask", [128, 256])
    out_d = dram("out", [NB, S, D], kind="ExternalOutput")
    dbg_d = dram("dbg", [128, 8192], kind="ExternalOutput") if dbg else None

    es = ExitStack()
    with es:
        nc_ctx = es.enter_context(nc.allow_non_contiguous_dma(reason="small strided parameter loads"))
        tk = TK(nc, es)
        build_info = {}

        def sb(name, shape, dtype=F32):
            return es.enter_context(nc.sbuf_tensor(name, list(shape), dtype))

        xT = sb("xT", [128, 8, T])
        hT = sb("hT", [128, 8, T], BF16)
        arena = sb("arena", [128, 32768], BF16)
        ident_f = sb("ident_fs", [128, 128])
        ident_b = sb("ident_bs", [128, 128], BF16)
        ones_b = sb("ones_b", [128, 128], BF16)
        selb = sb("selb", [16, NE, 128], BF16)
        rw_hi = sb("rw_hi", [128, 8, NE], BF16)
        rw_lo = sb("rw_lo", [128, 8, NE], BF16)
        wmask = sb("wmask_sb", [128, 256], BF16)
        mT = sb("mT", [128, DEPTH, 48, NB + 1])
        sT = sb("sT", [128, 8, NB + 1])
        TMP = [sb("tmp%d" % i, [128, 512]) for i in range(5)]
        RB = sb("rbuf", [128, 512])
        XS = arena[:, 0:2048].bitcast(F32)
        PB = [sb("pb%d" % i, [128, 512], BF16) for i in range(3)]
        ropet = sb("ropet", [128, 192])
        small = sb("small", [128, 512])
        TQ = sb("tq", [128, 256], BF16)
        TK_ = sb("tkp", [128, 512], BF16)
        otok = sb("otok", [128, 4, 256])
        ytok = sb("ytok", [128, 256], BF16)
        nabm = sb("nabm", [128, 640], BF16)
        stat = sb("stat", [128, 64])
        PS = [es.enter_context(nc.psum_tensor("ps%d" % i, [128, 512], F32)) for i in range(8)]

        pass

        def psk(i):
            return ("ps", i)

        tmp_i = [0]

        def tmp():
            i = tmp_i[0]
            tmp_i[0] = (i + 1) % len(TMP)
            return TMP[i], ("tmp", i)

        pb_i = [0]

        def pbuf():
            i = pb_i[0]
            pb_i[0] = (i + 1) % len(PB)
            return PB[i], ("pb", i)

        A1 = small[:, 0:16].rearrange("p (j r) -> p j r", r=2)
        A2 = small[:, 16:32].rearrange("p (j r) -> p j r", r=2)
        g1T = small[:, 32:40]
        g2T = small[:, 40:48]
        ogT = small[:, 48:56]
        abT = small[:, 56:104]
        rb_bc = small[:, 104:120]
        sinkx = small[:, 120:124]
        lamt = small[:, 124:128]
        gq = small[:, 128:192]
        gk = small[:, 192:256]
        rwT = small[:, 256:384].rearrange("p (j e) -> p j e", e=NE)
        lamraw = small[:, 384:512]

        tk.dma("sp", lambda q: q.dma_start(out=ident_f[:], in_=ident_d[:, :]), writes=["ident_f"])
        tk.op("dve", lambda v: v.tensor_copy(out=ident_b[:], in_=ident_f[:]), reads=["ident_f"], writes=["ident_b"])
        tk.op("dve", lambda v: v.memset(ones_b[:], 1.0), writes=["ones_b"])
        tk.dma("sp", lambda q: q.dma_start(out=TMP[0][:, 0:256], in_=wmask_d[:, :]), writes=[("tmp", 0)])
        tk.op("dve", lambda v: v.tensor_copy(out=wmask[:], in_=TMP[0][:, 0:256]), reads=[("tmp", 0)], writes=["wmask"])
        tk.dma("sp", lambda q: q.dma_start(out=rb_bc, in_=rb_d[0:1, :].partition_broadcast(128)), writes=["small"])
        tk.dma("sp", lambda q: q.dma_start(out=rwT, in_=rw_d.rearrange("(j p) e -> p j e", p=128)), writes=["small"])
        tk.op("dve", lambda v: v.tensor_copy(out=rw_hi[:], in_=rwT), reads=["small"], writes=["rw"])
        tk.op("dve", lambda v: v.tensor_tensor(out=rw_lo[:], in0=rwT, in1=rw_hi[:], op=ALU.subtract), reads=["small", "rw"], writes=["rw2"])
        tk.op("dve", lambda v: v.tensor_copy(out=selb[:], in_=ident_b[0:16, 0:16].unsqueeze(2).broadcast_to([16, NE, 128])),
              reads=["ident_b"], writes=["selb"])
        tk.op("dve", lambda v: v.memset(TK_[:], 0.0), writes=["tkp0"])
        tk.op("dve", lambda v: v.memset(arena[:], 0.0), writes=["arena"])

        for r in range(NB + 1):
            tk.dma("sp", lambda q, r=r: q.dma_start(out=sT[:, :, r], in_=c_d[r].rearrange("(j p) -> p j", p=128)),
                   writes=["sT"])
        tk.op("act", lambda a: a.activation(out=sT[:], in_=sT[:], func=AF.Silu), reads=["sT"], writes=["sT"])
        for l in range(DEPTH):
            for j6 in range(6):
                tk.dma("sp", lambda q, l=l, j6=j6: q.dma_start(
                    out=abT[:, j6 * 8:(j6 + 1) * 8],
                    in_=ada_b[l, j6 * 1024:(j6 + 1) * 1024].rearrange("(j p) -> p j", p=128)), writes=["abT"])
            psm = PS[0][:, 0:48 * (NB + 1)].rearrange("p (j r) -> p j r", r=NB + 1)
            for j in range(48):
                wt, wk = tmp()
                wt2, wk2 = tmp()
                tk.dma("sp", lambda q, l=l, j=j, wt=wt: q.dma_start(
                    out=wt[:].rearrange("p (k c) -> p k c", c=128),
                    in_=ada_w[l, 0:512, j * 128:(j + 1) * 128].rearrange("(k p) c -> p k c", p=128)), writes=[wk])
                tk.dma("sp", lambda q, l=l, j=j, wt2=wt2: q.dma_start(
                    out=wt2[:].rearrange("p (k c) -> p k c", c=128),
                    in_=ada_w[l, 512:1024, j * 128:(j + 1) * 128].rearrange("(k p) c -> p k c", p=128)), writes=[wk2])
                for kc in range(8):
                    src, sk_ = (wt, wk) if kc < 4 else (wt2, wk2)
                    tk.op("pe", lambda t, j=j, kc=kc, src=src: t.matmul(
                        psm[:, j, :], src[:, (kc % 4) * 128:(kc % 4 + 1) * 128], sT[:, kc, :],
                        start=(kc == 0), stop=(kc == 7)),
                        reads=[sk_, "sT"], writes=[psk(0)], inc=(kc == 7))
            tk.op("dve", lambda v, l=l: v.tensor_tensor(
                out=mT[:, l], in0=psm, in1=abT.unsqueeze(2).broadcast_to([128, 48, NB + 1]), op=ALU.add),
                reads=[psk(0), "abT"], writes=["mT"])

        def load_tokens_T(src_ap, tok0):
            tk.dma("sp", lambda q: q.dma_start(out=XS[:], in_=src_ap), writes=["xs"])
            for g in range(2):
                bank = 6 + g
                for jj in range(4):
                    j = g * 4 + jj
                    tk.op("pe", lambda t, j=j, jj=jj, bank=bank: t.transpose(
                        out=PS[bank][:, jj * 128:(jj + 1) * 128], in_=XS[:, j * 128:(j + 1) * 128],
                        identity=ident_f[:]), reads=["xs", "ident_f"], writes=[psk(bank)], inc=(jj == 3))
                eng = "act" if g == 0 else "dve"
                if eng == "act":
                    tk.op("act", lambda a, g=g, bank=bank: a.copy(
                        out=xT[:, g * 4:(g + 1) * 4, tok0:tok0 + 128],
                        in_=PS[bank][:].rearrange("p (j t) -> p j t", t=128)),
                        reads=[psk(bank)], writes=[("xT", tok0 // 128)])
                else:
                    tk.op("dve", lambda v, g=g, bank=bank: v.tensor_copy(
                        out=xT[:, g * 4:(g + 1) * 4, tok0:tok0 + 128],
                        in_=PS[bank][:].rearrange("p (j t) -> p j t", t=128)),
                        reads=[psk(bank)], writes=[("xT", tok0 // 128)])

        def xkeys(t0, n):
            return [("xT", i) for i in range(t0 // 128, (t0 + n) // 128)]

        def hkeys(t0, n):
            return [("hT", i) for i in range(t0 // 128, (t0 + n) // 128)]

        def norm_to_h(t0, n, Aap, Bap, router_bank=None):
            xk = xkeys(t0, n)
            hk = hkeys(t0, n)
            for j in range(8):
                sq, sqk = pbuf()
                tk.op("act", lambda a, j=j, sq=sq: a.activation(out=sq[:, :n], in_=xT[:, j, t0:t0 + n], func=AF.Square),
                      reads=xk, writes=[sqk])
                tk.op("pe", lambda t, j=j, sq=sq: t.matmul(PS[5][:, :n], ones_b[:], sq[:, :n], start=(j == 0), stop=(j == 7)),
                      reads=[sqk, "ones_b"], writes=[psk(5)], inc=(j == 7))
            rb, rbk = RB, "rbuf"
            tk.op("act", lambda a: a.activation(out=rb[:, :n], in_=PS[5][:, :n], func=AF.Sqrt, bias=EPS, scale=1.0 / D),
                  reads=[psk(5)], writes=[rbk])
            tk.op("dve", lambda v: v.reciprocal(out=rb[:, :n], in_=rb[:, :n]), reads=[rbk], writes=[rbk])
            for j in range(8):
                t1, t1k = tmp()
                tk.op("dve", lambda v, j=j, t1=t1: v.tensor_tensor(out=t1[:, :n], in0=xT[:, j, t0:t0 + n], in1=rb[:, :n],
                                                                    op=ALU.mult), reads=xk + [rbk], writes=[t1k])
                if router_bank is None:
                    tk.op("act", lambda a, j=j, t1=t1: a.activation(
                        out=hT[:, j, t0:t0 + n], in_=t1[:, :n], func=AF.Identity, scale=Aap[:, j:j + 1], bias=Bap[:, j:j + 1]),
                        reads=[t1k, "small", "mT"], writes=hk)
                else:
                    tk.op("act", lambda a, j=j, t1=t1: a.activation(
                        out=t1[:, :n], in_=t1[:, :n], func=AF.Identity, scale=Aap[:, j:j + 1], bias=Bap[:, j:j + 1]),
                        reads=[t1k, "small", "mT"], writes=[t1k])
                    tk.op("pool", lambda g, j=j, t1=t1: g.tensor_copy(out=hT[:, j, t0:t0 + n], in_=t1[:, :n]),
                          reads=[t1k], writes=hk)
                    lo, lok = pbuf()
                    tk.op("dve", lambda v, j=j, t1=t1, lo=lo: v.tensor_tensor(out=lo[:, :n], in0=t1[:, :n], in1=hT[:, j, t0:t0 + n],
                                                                              op=ALU.subtract), reads=[t1k] + hk, writes=[lok])
                    tk.op("pe", lambda t, j=j: t.matmul(PS[router_bank][0:16, :n], rw_hi[:, j, :], hT[:, j, t0:t0 + n],
                                                        start=(j == 0), stop=False),
                          reads=hk + ["rw"], writes=[psk(router_bank)], inc=False)
                    tk.op("pe", lambda t, j=j: t.matmul(PS[router_bank][0:16, :n], rw_lo[:, j, :], hT[:, j, t0:t0 + n],
                                                        start=False, stop=False),
                          reads=hk + ["rw2"], writes=[psk(router_bank)], inc=False)
                    tk.op("pe", lambda t, j=j, lo=lo: t.matmul(PS[router_bank][0:16, :n], rw_hi[:, j, :], lo[:, :n],
                                                               start=False, stop=(j == 7)),
                          reads=[lok, "rw"], writes=[psk(router_bank)], inc=(j == 7))

        def dump(ap, width, col0, keys):
            if dbg_d is None:
                return
            tk.dma("pool", lambda q: q.dma_start(out=dbg_d[0:ap.shape[0], col0:col0 + width], in_=ap), reads=keys)

        QT = arena[:, 0:4608].rearrange("p (c t) -> p c t", c=2)
        KT = arena[:, 4608:13824].rearrange("p (c t) -> p c t", c=4)
        VA = arena[:, 13824:18576].rearrange("p (t h d) -> p t h d", t=NTT, h=4)
        YT = arena[:, 18576:23184].rearrange("p (c t) -> p c t", c=2)
        WI = arena[:, 23184:29328].rearrange("p (j c) -> p j c", j=8)
        WO = arena[:, 29328:31376].rearrange("p (k c) -> p k c", k=2)
        EW = [arena[:, i * 12288:(i + 1) * 12288] for i in range(2)]
        HM = arena[:, 24576:26624].rearrange("p (f t) -> p f t", f=4)
        WT = arena[0:16, 26624:31232].bitcast(F32) if False else None

        wrt_hi = arena[0:16, 26624:26624 + T]
        wrt_lo = arena[0:16, 26624 + T:26624 + 2 * T]

        def qk_post(ps_ap, nh, hd, gain_ap, rope_tile, out_views, tt, is_lat):
            width = nh * hd
            sq, sqk = tmp()
            tk.op("act", lambda a: a.activation(out=sq[:, :width], in_=ps_ap, func=AF.Square),
                  reads=[psk(ps_bank[0])], writes=[sqk])
            slot = stat_slot[0]
            sc0 = [0, 40, 48, 56][slot]
            stk = "statq%d" % slot
            ss = stat[:, sc0:sc0 + nh]
            tk.op("dve", lambda v: v.tensor_reduce(out=ss, in_=sq[:, :width].rearrange("p (h d) -> p h d", h=nh),
                                                   axis=AX.X, op=ALU.add), reads=[sqk], writes=[stk])
            tk.op("act", lambda a: a.activation(out=ss, in_=ss, func=AF.Sqrt, bias=EPS, scale=1.0 / hd),
                  reads=[stk], writes=[stk])
            tk.op("dve", lambda v: v.reciprocal(out=ss, in_=ss), reads=[stk], writes=[stk])
            qn, qnk = tmp()
            tk.op("dve", lambda v: v.tensor_tensor(
                out=qn[:, :width].rearrange("p (h d) -> p h d", h=nh), in0=ps_ap.rearrange("p (h d) -> p h d", h=nh),
                in1=ss.unsqueeze(2).broadcast_to([128, nh, hd]), op=ALU.mult),
                reads=[psk(ps_bank[0]), stk], writes=[qnk])
            qg, qgk = tmp()
            tk.op("pool", lambda g: g.tensor_tensor(
                out=qg[:, :width].rearrange("p (h d) -> p h d", h=nh), in0=qn[:, :width].rearrange("p (h d) -> p h d", h=nh),
                in1=gain_ap.unsqueeze(1).broadcast_to([128, nh, hd]), op=ALU.mult),
                reads=[qnk, "small"], writes=[qgk])
            if not is_lat:
                for (oap, sel) in out_views:
                    src = qg[:, :width] if sel is None else sel(qg[:, :width])
                    tk.op("dve", lambda v, oap=oap, src=src: v.tensor_copy(out=oap, in_=src), reads=[qgk],
                          writes=[out_key[0]])
                return
            cos_ap, sin_ap = rope_tile
            e4 = hd // 4
            t1, t1k = tmp()
            tk.op("pool", lambda g: g.tensor_tensor(
                out=t1[:, :width].rearrange("p (h d) -> p h d", h=nh), in0=qg[:, :width].rearrange("p (h d) -> p h d", h=nh),
                in1=cos_ap.unsqueeze(1).broadcast_to([128, nh, hd]), op=ALU.mult), reads=[qgk, "ropet"], writes=[t1k])
            t2, t2k = tmp()
            qv = qg[:, :width].rearrange("p (h b s e) -> p h b s e", h=nh, b=2, s=2)
            tv = t2[:, :width].rearrange("p (h b s e) -> p h b s e", h=nh, b=2, s=2)
            sv = sin_ap.rearrange("p (b s e) -> p b s e", b=2, s=2)
            for s_ in range(2):
                tk.op("dve", lambda v, s_=s_: v.tensor_tensor(
                    out=tv[:, :, :, s_, :], in0=qv[:, :, :, 1 - s_, :],
                    in1=sv[:, :, s_, :].unsqueeze(1).broadcast_to([128, nh, 2, e4]), op=ALU.mult),
                    reads=[qgk, "ropet"], writes=[t2k])
            for (oap, sel) in out_views:
                a_ = t1[:, :width] if sel is None else sel(t1[:, :width])
                b_ = t2[:, :width] if sel is None else sel(t2[:, :width])
                tk.op("dve", lambda v, oap=oap, a_=a_, b_=b_: v.tensor_tensor(out=oap, in0=a_, in1=b_, op=ALU.add),
                      reads=[t1k, t2k], writes=[out_key[0]])

        ps_bank = [0]
        out_key = ["tq"]
        stat_slot = [0]
        unit_ctr = [0]
        nabm_state = [None]
        TQb = [TQ[:, :], arena[:, 31376:31632]]
        TKb = [TK_[:, :], arena[:, 31632:32144]]

        if dbg == "p0":
            tk.barrier()
            dump(mT[:, 0].rearrange("p j r -> p (j r)"), 48 * (NB + 1), 0, [])
            tk.barrier()
            return nc
        for b in range(NB):
          try:
            for tt in range(16):
                load_tokens_T(x_d[b, tt * 128:(tt + 1) * 128, :], tt * 128)
            for tt in range(2):
                load_tokens_T(ctx_d[b, tt * 128:(tt + 1) * 128, :], S + tt * 128)
            tk.barrier()
            if dbg == "ld":
                dump(xT[:, 0, :], 2304, 0, [])
                dump(xT[:, 7, :], 2304, 2304, [])
                tk.barrier()
                return nc

            for l in range(DEPTH):
                pass
                need_ctx = l < DEPTH - 1
                lam_init = 0.8 - 0.6 * math.exp(-0.3 * l)
                mrow = lambda k, r: mT[:, l, k * 8:(k + 1) * 8, r]
                tk.dma("sp", lambda q: q.dma_start(out=g1T, in_=ng1[l].rearrange("(j p) -> p j", p=128)), writes=["small"])
                tk.dma("sp", lambda q: q.dma_start(out=g2T, in_=ng2[l].rearrange("(j p) -> p j", p=128)), writes=["small"])
                tk.dma("sp", lambda q: q.dma_start(out=ogT, in_=og_d[l].rearrange("(j p) -> p j", p=128)), writes=["small"])
                tk.dma("sp", lambda q: q.dma_start(out=sinkx, in_=sink_d[l:l + 1, :].partition_broadcast(128)), writes=["small"])
                tk.dma("sp", lambda q: q.dma_start(
                    out=lamraw, in_=lam_d[l:l + 1].rearrange("o a d -> o (a d)").partition_broadcast(128)), writes=["small"])
                for r in range(2):
                    rr = b if r == 0 else NB
                    tk.op("dve", lambda v, r=r, rr=rr: v.scalar_tensor_tensor(
                        out=A1[:, :, r], in0=mrow(1, rr), scalar=1.0, in1=g1T, op0=ALU.add, op1=ALU.mult),
                        reads=["small", "mT"], writes=["small"])
                    tk.op("dve", lambda v, r=r, rr=rr: v.scalar_tensor_tensor(
                        out=A2[:, :, r], in0=mrow(4, rr), scalar=1.0, in1=g2T, op0=ALU.add, op1=ALU.mult),
                        reads=["small", "mT"], writes=["small"])
                tk.op("act", lambda a: a.activation(out=sinkx, in_=sinkx, func=AF.Exp), reads=["small"], writes=["small"])
                lr = lamraw.rearrange("p (a d) -> p a d", a=4)
                lw = stat[:, 32:36]
                tk.op("dve", lambda v: v.tensor_tensor(out=lamraw[:, 0:32], in0=lr[:, 0, :], in1=lr[:, 1, :], op=ALU.mult),
                      reads=["small"], writes=["small"])
                tk.op("dve", lambda v: v.tensor_tensor(out=lamraw[:, 64:96], in0=lr[:, 2, :], in1=lr[:, 3, :], op=ALU.mult),
                      reads=["small"], writes=["small"])
                tk.op("dve", lambda v: v.tensor_reduce(out=lw[:, 0:2], in_=lamraw.rearrange("p (a d) -> p a d", a=2)[:, :, 0:32],
                                                       axis=AX.X, op=ALU.add), reads=["small"], writes=["stat2"])
                tk.op("act", lambda a: a.activation(out=lw[:, 0:2], in_=lw[:, 0:2], func=AF.Exp), reads=["stat2"], writes=["stat2"])
                tk.op("dve", lambda v: v.tensor_tensor(out=lw[:, 2:3], in0=lw[:, 0:1], in1=lw[:, 1:2], op=ALU.subtract),
                      reads=["stat2"], writes=["stat2"])
                tk.op("act", lambda a: a.activation(out=lamt[:, 0:1], in_=lw[:, 2:3], func=AF.Identity, scale=-1.0, bias=-lam_init),
                      reads=["stat2"], writes=["small"])

                if dbg == "sm":
                    print("nops at sm", tk.nops)
                    tk.barrier()
                    dump(small[:, :], 512, 0, [])
                    tk.barrier()
                    return nc
                tk.mark("b%d l%d norm1" % (b, l))
                for cch in range(4):
                    norm_to_h(cch * 512, 512, A1[:, :, 0], mrow(0, b))
                norm_to_h(S, L, A1[:, :, 1], mrow(0, NB))
                if dbg == "h":
                    tk.barrier()
                    dump(hT[:, 0, 0:2304], 2304, 0, hkeys(0, T))
                    dump(hT[:, 7, 0:2304], 2304, 2304, hkeys(0, T))
                    tk.barrier()
                    return nc

                for mi, (mname, col0, nq, nk, nv, nkv, hd) in enumerate(MIX):
                    ncols = nq + nk + nv
                    nh_q = nq // hd
                    nh_k = nk // hd
                    scale = hd ** -0.5
                    tk.dma("pool", lambda q: q.dma_start(
                        out=WI[:, :, 0:ncols], in_=w_in[l, :, col0:col0 + ncols].rearrange("(j p) c -> p j c", p=128)),
                        writes=["WI"])
                    tk.dma("pool", lambda q: q.dma_start(
                        out=WO[:, :, :], in_=w_out[l, mi * 256:(mi + 1) * 256, :].rearrange("(k p) c -> p k c", p=128)),
                        writes=["WO"])
                    gd = qkg[mname]
                    tk.dma("sp", lambda q: q.dma_start(out=gq[:, 0:hd], in_=gd[l, 0:1, :].partition_broadcast(128)), writes=["small"])
                    tk.dma("sp", lambda q: q.dma_start(out=gk[:, 0:hd], in_=gd[l, 1:2, :].partition_broadcast(128)), writes=["small"])
                    tk.op("dve", lambda v: v.tensor_scalar(out=gq[:, 0:hd], in0=gq[:, 0:hd], scalar1=scale, scalar2=None,
                                                           op0=ALU.mult), reads=["small"], writes=["small"])
                    tk.op("dve", lambda v: v.memset(VA[:, :, :, 64:65], 1.0), writes=["VA"])

                    tk.mark("b%d l%d %s proj" % (b, l, mname))
                    if mname == "C":
                        tk.op("dve", lambda v: v.memset(TKb[0], 0.0), writes=["tkp0"])
                        tk.op("dve", lambda v: v.memset(TKb[1], 0.0), writes=["tkp1"])
                    for tt in range(NTT):
                        is_lat = tt < 16 and mname != "B"
                        if not is_lat and False:
                            pass
                        banks = [0, 1] if tt % 2 == 0 else [2, 3]
                        pieces = [(0, min(512, ncols), banks[0])]
                        if ncols > 512:
                            pieces.append((512, ncols, banks[1]))
                        for (c0, c1, bank) in pieces:
                            for j in range(8):
                                tk.op("pe", lambda t, j=j, c0=c0, c1=c1, bank=bank: t.matmul(
                                    PS[bank][:, 0:c1 - c0], hT[:, j, tt * 128:(tt + 1) * 128], WI[:, j, c0:c1],
                                    start=(j == 0), stop=(j == 7)),
                                    reads=[("hT", tt), "WI"], writes=[psk(bank)], inc=(j == 7))
                        if tt < 16:
                            tk.dma("sp", lambda q: q.dma_start(out=ropet[:], in_=rope_d[tt * 128:(tt + 1) * 128, :]),
                                   writes=["ropet"])
                        rt = (ropet[:, 0:64], ropet[:, 64:128]) if hd == 64 else (ropet[:, 128:160], ropet[:, 160:192])
                        par = tt % 2
                        TQc = TQb[par]
                        TKc = TKb[par]
                        ps_bank[0] = banks[0]
                        out_key[0] = "tq%d" % par
                        stat_slot[0] = par * 2
                        if mname in ("A", "D"):
                            qviews = [(TQc.rearrange("p (c s d) -> p s c d", c=2, s=2),
                                       (lambda ap: ap.rearrange("p (s c d) -> p s c d", s=2, c=2)))]
                        else:
                            qviews = [(TQc, None)]
                        qk_post(PS[banks[0]][:, 0:256], nh_q if hd == 64 else 8, hd, gq[:, 0:hd], rt, qviews, tt, is_lat)
                        for cc in range(2):
                            tk.op("pe", lambda t, cc=cc: t.transpose(
                                out=PS[4][:].bitcast(BF16)[:, cc * 128:(cc + 1) * 128], in_=TQc[:, cc * 128:(cc + 1) * 128],
                                identity=ident_b[:]), reads=["tq%d" % par, "ident_b"], writes=[psk(4)], inc=(cc == 1))
                        tk.op("act", lambda a: a.copy(out=QT[:, :, tt * 128:(tt + 1) * 128],
                                                      in_=PS[4][:].bitcast(BF16)[:, 0:256].rearrange("p (c t) -> p c t", c=2)),
                              reads=[psk(4)], writes=[("QT", tt)])
                        out_key[0] = "tkp%d" % par
                        stat_slot[0] = par * 2 + 1
                        if mname == "C":
                            tkv = TKc.rearrange("p (hp i hh i2 d) -> p hp i hh i2 d", hp=2, i=2, hh=2, i2=2)
                            views = []
                            for i_ in range(2):
                                views.append((tkv[:, :, i_, :, i_, :],
                                              (lambda ap, i_=i_: ap.rearrange("p (hp hh i d) -> p hp hh i d", hp=2, hh=2, i=2)[:, :, :, i_, :])))
                            qk_post(PS[banks[0]][:, 256:512], 8, 32, gk[:, 0:32], rt, views, tt, is_lat)
                            nkc = 4
                        else:
                            qk_post(PS[banks[0]][:, 256:256 + nk], nh_k, hd, gk[:, 0:hd], rt, [(TKc[:, 0:nk], None)], tt, is_lat)
                            nkc = nk // 128
                        for cc in range(nkc):
                            tk.op("pe", lambda t, cc=cc: t.transpose(
                                out=PS[4][:].bitcast(BF16)[:, 512 + cc * 128:512 + (cc + 1) * 128],
                                in_=TKc[:, cc * 128:(cc + 1) * 128], identity=ident_b[:]),
                                reads=["tkp%d" % par, "ident_b"], writes=[psk(4)], inc=(cc == nkc - 1))
                        tk.op("act", lambda a, nkc=nkc: a.copy(
                            out=KT[:, 0:nkc, tt * 128:(tt + 1) * 128],
                            in_=PS[4][:].bitcast(BF16)[:, 512:512 + nkc * 128].rearrange("p (c t) -> p c t", c=nkc)),
                            reads=[psk(4)], writes=[("KT", tt)])
                        if nk == 128:
                            vsrc = PS[banks[0]][:, 384:512]
                            vb = banks[0]
                        else:
                            vsrc = PS[banks[1]][:, 0:256]
                            vb = banks[1]
                        tk.op("act", lambda a, vsrc=vsrc: a.copy(out=VA[:, tt, 0:nkv, 0:64],
                                                                in_=vsrc.rearrange("p (h d) -> p h d", h=nkv)),
                              reads=[psk(vb)], writes=[("VA", tt)])
                    if dbg == "qkv" and mname == dbg_mixer[0]:
                        tk.barrier()
                        dump(QT[:, 0, :], 2304, 0, [])
                        dump(KT[:, 0, :], 2304, 2304, [])
                        dump(VA[:, 3, :, :].rearrange("p h d -> p (h d)"), 264, 4608, [])
                        dump(KT[:, 3, :], 2304, 4900, [])
                        tk.barrier()
                        return nc

                    tk.mark("b%d l%d %s attn" % (b, l, mname))
                    ranges = [(r0 * 512, 512) for r0 in range(4)] + ([(S, L)] if need_ctx else [])
                    pending = []

                    def flush():
                        for f_ in pending:
                            f_()
                        del pending[:]

                    for (q0, n) in ranges:
                        nblk = n // 128
                        is_ctxq = q0 >= S
                        units = []
                        if mname in ("A", "D"):
                            for h in range(4):
                                units.append((h, 0, h % 2, (h // 2) * 64, 0, (h // 2) * 64, h // 2))
                        elif mname == "B":
                            for h in range(4):
                                units.append((h, 0, h // 2, (h % 2) * 64, h // 2, (h % 2) * 64, h))
                        else:
                            for h in range(4):
                                for i_ in range(2):
                                    units.append((h, i_, h // 2, (h % 2) * 64, (h // 2) * 2 + i_, (h % 2) * 64, h))
                        for (h, br, qc, qp, kc_, kp, vh) in units:
                            ob = 6 if unit_ctr[0] % 2 == 0 else 3
                            unit_ctr[0] += 1
                            steps = []
                            if is_ctxq or mname in ("C", "D"):
                                kts = [16, 17] if is_ctxq else list(range(18))
                                for ki, kt in enumerate(kts):
                                    steps.append((0, n, kt, None, None, ki == 0, ki == len(kts) - 1, None))
                            elif mname == "A":
                                for bl in range(nblk):
                                    i = q0 // 128 + bl
                                    lst = []
                                    if i - 1 >= 0:
                                        lst.append((i - 1, wmask[:, 0:128], "wmask"))
                                    lst.append((i, None, None))
                                    if i + 1 < 16:
                                        lst.append((i + 1, wmask[:, 128:256], "wmask"))
                                    lst += [(16, None, None), (17, None, None)]
                                    for ki, (kt, ma, mk_) in enumerate(lst):
                                        steps.append((bl * 128, 128, kt, ma, mk_, ki == 0, ki == len(lst) - 1, None))
                            else:
                                for bl in range(nblk):
                                    i = q0 // 128 + bl
                                    ts = na_ts(i)
                                    vi = na_variant(i)

                                    def load_mask(vi=vi, h=h):
                                        if nabm_state[0] == (b, l, h, vi):
                                            return
                                        nabm_state[0] = (b, l, h, vi)
                                        stg, stgk = tmp()
                                        stg2, stg2k = tmp()
                                        tk.dma("sp", lambda q: q.dma_start(out=stg[:, 0:512], in_=nab_d[l, h, vi, :, 0:512]), writes=[stgk])
                                        tk.dma("sp", lambda q: q.dma_start(out=stg2[:, 0:128], in_=nab_d[l, h, vi, :, 512:640]), writes=[stg2k])
                                        tk.op("act", lambda a: a.activation(out=nabm[:, 0:512], in_=stg[:, 0:512], func=AF.Exp),
                                              reads=[stgk], writes=["nabm"])
                                        tk.op("act", lambda a: a.activation(out=nabm[:, 512:640], in_=stg2[:, 0:128], func=AF.Exp),
                                              reads=[stg2k], writes=["nabm"])
                                    lst = [(ts + j, nabm[:, j * 128:(j + 1) * 128], "nabm") for j in range(5)]
                                    lst += [(16, None, None), (17, None, None)]
                                    for ki, (kt, ma, mk_) in enumerate(lst):
                                        steps.append((bl * 128, 128, kt, ma, mk_, ki == 0, ki == len(lst) - 1, load_mask if ki == 0 else None))

                            def emit_qk(si, st):
                                (qa, qn, kt, mask_ap, mkey, first, last, pre) = st
                                if pre is not None:
                                    pre()
                                sbank = 4 + (si % 2)
                                tk.op("pe", lambda t: t.matmul(
                                    PS[sbank][:, 0:qn], KT[kp:kp + 64, kc_, kt * 128:(kt + 1) * 128],
                                    QT[qp:qp + 64, qc, q0 + qa:q0 + qa + qn], start=True, stop=True),
                                    reads=[("KT", kt)] + [("QT", (q0 + qa) // 128 + z) for z in range(qn // 128)], writes=[psk(sbank)])
                                pbt, pbk = pbuf()
                                tk.op("act", lambda a: a.activation(out=pbt[:, 0:qn], in_=PS[sbank][:, 0:qn], func=AF.Exp),
                                      reads=[psk(sbank)], writes=[pbk])
                                if mask_ap is not None:
                                    tk.op("dve", lambda v: v.tensor_tensor(out=pbt[:, 0:qn], in0=pbt[:, 0:qn], in1=mask_ap, op=ALU.mult),
                                          reads=[pbk, mkey], writes=[pbk])
                                return pbt, pbk

                            def emit_pv(st, pbt, pbk):
                                (qa, qn, kt, mask_ap, mkey, first, last, pre) = st
                                tk.op("pe", lambda t: t.matmul(PS[ob][0:65, qa:qa + qn], VA[:, kt, vh, 0:65], pbt[:, 0:qn], start=first, stop=last),
                                      reads=[("VA", kt), "VA", pbk], writes=[psk(ob)])

                            prev = None
                            for si, st in enumerate(steps):
                                cur = emit_qk(si, st)
                                if prev is not None:
                                    emit_pv(*prev)
                                prev = (st,) + cur
                                if si == min(2, len(steps) - 1):
                                    flush()
                            emit_pv(*prev)

                            def post(h=h, br=br, ob=ob, nblk=nblk, n=n):
                                ot, otk = tmp()
                                tk.op("dve", lambda v: v.tensor_copy(out=ot[0:65, 0:n], in_=PS[ob][0:65, 0:n]), reads=[psk(ob)], writes=[otk])
                                for bl in range(nblk):
                                    tk.op("pe", lambda t, bl=bl: t.transpose(
                                        out=PS[7][:, bl * 128:bl * 128 + 65], in_=ot[0:65, bl * 128:(bl + 1) * 128],
                                        identity=ident_f[0:65, 0:65]), reads=[otk, "ident_f"], writes=[psk(7)])
                                p7 = PS[7][:].rearrange("p (b d) -> p b d", d=128)
                                rd = stat[:, 8:8 + nblk]
                                if mname == "A":
                                    tk.op("dve", lambda v: v.tensor_scalar(out=rd, in0=p7[:, 0:nblk, 64], scalar1=sinkx[:, h:h + 1],
                                                                           scalar2=None, op0=ALU.add), reads=[psk(7), "small"], writes=["stat3"])
                                    tk.op("dve", lambda v: v.reciprocal(out=rd, in_=rd), reads=["stat3"], writes=["stat3"])
                                else:
                                    tk.op("dve", lambda v: v.reciprocal(out=rd, in_=p7[:, 0:nblk, 64]), reads=[psk(7)], writes=["stat3"])
                                if br == 1:
                                    tk.op("dve", lambda v: v.tensor_scalar(out=rd, in0=rd, scalar1=lamt[:, 0:1], scalar2=None, op0=ALU.mult),
                                          reads=["stat3", "small"], writes=["stat3"])
                                for bl in range(nblk):
                                    if br == 0:
                                        tk.op("dve", lambda v, bl=bl: v.tensor_scalar(
                                            out=otok[:, bl, h * 64:(h + 1) * 64], in0=p7[:, bl, 0:64], scalar1=rd[:, bl:bl + 1],
                                            scalar2=None, op0=ALU.mult), reads=[psk(7), "stat3"], writes=[("otok", bl)])
                                    else:
                                        tk.op("dve", lambda v, bl=bl: v.scalar_tensor_tensor(
                                            out=otok[:, bl, h * 64:(h + 1) * 64], in0=p7[:, bl, 0:64], scalar=rd[:, bl:bl + 1],
                                            in1=otok[:, bl, h * 64:(h + 1) * 64], op0=ALU.mult, op1=ALU.add),
                                            reads=[psk(7), "stat3", ("otok", bl)], writes=[("otok", bl)])
                            pending.append(post)

                        def merge(q0=q0, nblk=nblk):
                            for bl in range(nblk):
                                tok = q0 + bl * 128
                                ng = 1 if mname != "C" else 4
                                gsz = 256 // ng
                                sq, sqk = tmp()
                                tk.op("act", lambda a: a.activation(out=sq[:, 0:256], in_=otok[:, bl, :], func=AF.Square),
                                      reads=[("otok", bl)], writes=[sqk])
                                ssg = stat[:, 16:16 + ng]
                                tk.op("dve", lambda v: v.tensor_reduce(out=ssg, in_=sq[:, 0:256].rearrange("p (g d) -> p g d", g=ng),
                                                                       axis=AX.X, op=ALU.add), reads=[sqk], writes=["stat4"])
                                tk.op("act", lambda a: a.activation(out=ssg, in_=ssg, func=AF.Sqrt, bias=EPS, scale=1.0 / gsz),
                                      reads=["stat4"], writes=["stat4"])
                                tk.op("dve", lambda v: v.reciprocal(out=ssg, in_=ssg), reads=["stat4"], writes=["stat4"])
                                if mname == "C":
                                    tk.op("dve", lambda v: v.tensor_scalar(out=ssg, in0=ssg, scalar1=(1.0 - lam_init), scalar2=None,
                                                                           op0=ALU.mult), reads=["stat4"], writes=["stat4"])
                                tk.op("dve", lambda v: v.tensor_tensor(
                                    out=ytok[:].rearrange("p (g d) -> p g d", g=ng), in0=otok[:, bl, :].rearrange("p (g d) -> p g d", g=ng),
                                    in1=ssg.unsqueeze(2).broadcast_to([128, ng, gsz]), op=ALU.mult),
                                    reads=[("otok", bl), "stat4"], writes=["ytok"])
                                for cc in range(2):
                                    tk.op("pe", lambda t, cc=cc: t.transpose(
                                        out=PS[2][:].bitcast(BF16)[:, cc * 128:(cc + 1) * 128],
                                        in_=ytok[:, cc * 128:(cc + 1) * 128], identity=ident_b[:]),
                                        reads=["ytok", "ident_b"], writes=[psk(2)])
                                for cc in range(2):
                                    tk.op("act", lambda a, cc=cc: a.activation(
                                        out=YT[:, cc, tok:tok + 128], in_=PS[2][:].bitcast(BF16)[:, cc * 128:(cc + 1) * 128],
                                        func=AF.Identity, scale=ogT[:, mi * 2 + cc:mi * 2 + cc + 1]),
                                        reads=[psk(2), "small"], writes=[("YT", tok // 128)])
                        pending.append(merge)
                    flush()
                    if dbg == "y" and mname == dbg_mixer[0]:
                        tk.barrier()
                        dump(YT[:, 0, :], 2304, 0, [])
                        dump(YT[:, 1, :], 2304, 2304, [])
                        tk.barrier()
                        return nc

                    tk.mark("b%d l%d %s wout" % (b, l, mname))
                    for (q0, n) in ranges:
                        rr = NB if q0 >= S else b
                        for jo in range(8):
                            bank = jo % 4
                            for kc2 in range(2):
                                tk.op("pe", lambda t, jo=jo, kc2=kc2, bank=bank: t.matmul(
                                    PS[bank][:, 0:n], WO[:, kc2, jo * 128:(jo + 1) * 128], YT[:, kc2, q0:q0 + n],
                                    start=(kc2 == 0), stop=(kc2 == 1)),
                                    reads=["WO"] + [("YT", q0 // 128 + z) for z in range(n // 128)], writes=[psk(bank)],
                                    inc=(kc2 == 1))
                            tk.op("dve", lambda v, jo=jo, bank=bank, rr=rr: v.scalar_tensor_tensor(
                                out=xT[:, jo, q0:q0 + n], in0=PS[bank][:, 0:n], scalar=mT[:, l, 16 + jo, rr:rr + 1],
                                in1=xT[:, jo, q0:q0 + n], op0=ALU.mult, op1=ALU.add),
                                reads=[psk(bank), "mT"] + xkeys(q0, n), writes=xkeys(q0, n))
                    tk.barrier()
                if dbg == "xattn":
                    dump(xT[:, 0, :], 2304, 0, [])
                    dump(xT[:, 5, :], 2304, 2304, [])
                    tk.barrier()
                    return nc

                if dbg == "xattn2":
                    dump(xT[:, 0, :], 2304, 0, [])
                    dump(xT[:, 5, :], 2304, 2304, [])
                    tk.barrier()
                    return nc
                tk.mark("b%d l%d norm2+route" % (b, l))
                ranges = [(r0 * 512, 512) for r0 in range(4)] + ([(S, L)] if need_ctx else [])
                for (q0, n) in ranges:
                    r = 1 if q0 >= S else 0
                    rr = NB if q0 >= S else b
                    norm_to_h(q0, n, A2[:, :, r], mrow(3, rr), router_bank=6)
                    lg, lgk = RB, "rbuf"
                    tk.op("dve", lambda v, lg=lg: v.tensor_copy(out=lg[0:16, 0:n], in_=PS[6][0:16, 0:n]), reads=[psk(6)], writes=[lgk])
                    for bl in range(n // 128):
                        tk.op("pe", lambda t, bl=bl, lg=lg: t.transpose(out=PS[7][:, 0:16], in_=lg[0:16, bl * 128:(bl + 1) * 128],
                                                                       identity=ident_f[0:16, 0:16]),
                              reads=[lgk, "ident_f"], writes=[psk(7)])
                        sc = stat[:, 0:16]
                        sel = stat[:, 16:32]
                        w8 = stat[:, 32:40]
                        tk.op("act", lambda a: a.activation(out=sc, in_=PS[7][:, 0:16], func=AF.Sigmoid), reads=[psk(7)], writes=["stat"])
                        tk.op("dve", lambda v: v.tensor_tensor(out=sel, in0=sc, in1=rb_bc, op=ALU.add), reads=["stat", "small"], writes=["stat"])
                        s4 = sel.rearrange("p (g a c) -> p g a c", g=4, a=2)
                        pq = stat[:, 40:48].rearrange("p (g a) -> p g a", g=4)
                        rs_ = stat[:, 48:56].rearrange("p (g a) -> p g a", g=4)
                        tk.op("dve", lambda v: v.tensor_tensor(out=pq, in0=s4[:, :, :, 0], in1=s4[:, :, :, 1], op=ALU.max), reads=["stat"], writes=["stat"])
                        tk.op("dve", lambda v: v.tensor_tensor(out=rs_, in0=s4[:, :, :, 0], in1=s4[:, :, :, 1], op=ALU.min), reads=["stat"], writes=["stat"])
                        m1 = stat[:, 56:60]
                        m2_ = stat[:, 60:64]
                        tk.op("dve", lambda v: v.tensor_tensor(out=m1, in0=pq[:, :, 0], in1=pq[:, :, 1], op=ALU.max), reads=["stat"], writes=["stat"])
                        tk.op("dve", lambda v: v.tensor_tensor(out=m2_, in0=pq[:, :, 0], in1=pq[:, :, 1], op=ALU.min), reads=["stat"], writes=["stat"])
                        tk.op("dve", lambda v: v.tensor_tensor(out=pq[:, :, 0], in0=rs_[:, :, 0], in1=rs_[:, :, 1], op=ALU.max), reads=["stat"], writes=["stat"])
                        tk.op("dve", lambda v: v.tensor_tensor(out=m2_, in0=m2_, in1=pq[:, :, 0], op=ALU.max), reads=["stat"], writes=["stat"])
                        tk.op("dve", lambda v: v.tensor_tensor(out=m1, in0=m1, in1=m2_, op=ALU.add), reads=["stat"], writes=["stat"])
                        gmx = w8[:, 0:1]
                        tk.op("dve", lambda v: v.tensor_reduce(out=gmx, in_=m1, axis=AX.X, op=ALU.max), reads=["stat"], writes=["stat"])
                        tk.op("dve", lambda v: v.tensor_scalar(out=m2_, in0=m1, scalar1=gmx, scalar2=None, op0=ALU.is_ge), reads=["stat"], writes=["stat"])
                        tk.op("act", lambda a: a.activation(out=m1, in_=m2_, func=AF.Identity, scale=100.0, bias=-100.0),
                              reads=["stat"], writes=["stat"])
                        sel3 = sel.rearrange("p (g c) -> p g c", g=4)
                        tk.op("dve", lambda v: v.tensor_tensor(out=sel3, in0=sel3, in1=m2_.unsqueeze(2).broadcast_to([128, 4, 4]), op=ALU.mult),
                              reads=["stat"], writes=["stat"])
                        tk.op("dve", lambda v: v.tensor_tensor(out=sel3, in0=sel3, in1=m1.unsqueeze(2).broadcast_to([128, 4, 4]), op=ALU.add),
                              reads=["stat"], writes=["stat"])
                        tk.op("dve", lambda v: v.max(out=w8, in_=sel), reads=["stat"], writes=["stat"])
                        tk.op("dve", lambda v: v.tensor_scalar(out=sel, in0=sel, scalar1=w8[:, 1:2], scalar2=None, op0=ALU.is_ge),
                              reads=["stat"], writes=["stat"])
                        tk.op("dve", lambda v: v.tensor_tensor(out=sc, in0=sc, in1=sel, op=ALU.mult), reads=["stat"], writes=["stat"])
                        tk.op("dve", lambda v: v.tensor_reduce(out=gmx, in_=sc, axis=AX.X, op=ALU.add), reads=["stat"], writes=["stat"])
                        tk.op("dve", lambda v: v.reciprocal(out=gmx, in_=gmx), reads=["stat"], writes=["stat"])
                        wtok, wtokk = tmp()
                        tk.op("dve", lambda v, wtok=wtok: v.tensor_scalar(out=wtok[:, 0:16], in0=sc, scalar1=gmx, scalar2=None, op0=ALU.mult),
                              reads=["stat"], writes=[wtokk])
                        tk.op("pe", lambda t, wtok=wtok: t.transpose(out=PS[7][0:16, 128:256], in_=wtok[:, 0:16], identity=ident_f[:]),
                              reads=[wtokk, "ident_f"], writes=[psk(7)])
                        tk.op("act", lambda a, bl=bl: a.copy(out=wrt_hi[:, q0 + bl * 128:q0 + (bl + 1) * 128], in_=PS[7][0:16, 128:256]),
                              reads=[psk(7)], writes=[("wrt", (q0 // 128) + bl)])
                        tk.op("dve", lambda v, bl=bl: v.tensor_tensor(
                            out=wrt_lo[:, q0 + bl * 128:q0 + (bl + 1) * 128], in0=PS[7][0:16, 128:256],
                            in1=wrt_hi[:, q0 + bl * 128:q0 + (bl + 1) * 128], op=ALU.subtract),
                            reads=[psk(7), ("wrt", (q0 // 128) + bl)], writes=[("wrtl", (q0 // 128) + bl)])
                if dbg == "route":
                    tk.barrier()
                    dump(wrt_hi[:, :], 2304, 0, [])
                    dump(hT[:, 0, 0:2304], 2304, 2304, [])
                    tk.barrier()
                    return nc
                tk.barrier()

                tk.mark("b%d l%d experts" % (b, l))
                items = [(e, q0, n) for e in range(NE) for (q0, n) in ranges]

                def ew_views(e):
                    ew = EW[e % 2]
                    return (ew[:, 0:4096].rearrange("p (j f) -> p j f", j=8), ew[:, 4096:8192].rearrange("p (j f) -> p j f", j=8),
                            ew[:, 8192:12288].rearrange("p (k c) -> p k c", k=4), ("EW", e % 2))

                def hm_buf(idx, fc):
                    if idx % 2 == 0:
                        return HM[:, fc, :]
                    return [PB[0], PB[1], PB[2], TK_][fc]

                def load_expert(e):
                    WG, WU, WD, ewk = ew_views(e)
                    tk.dma("pool", lambda q: q.dma_start(out=WG, in_=wg_d[l, e].rearrange("(j p) f -> p j f", p=128)), writes=[ewk])
                    tk.dma("pool", lambda q: q.dma_start(out=WU, in_=wu_d[l, e].rearrange("(j p) f -> p j f", p=128)), writes=[ewk])
                    tk.dma("pool", lambda q: q.dma_start(out=WD, in_=wd_d[l, e].rearrange("(k p) c -> p k c", p=128)), writes=[ewk])

                def emit_bc(idx):
                    e, q0, n = items[idx]
                    bcb = 0 if idx % 2 == 0 else 7
                    wk = [("wrt", q0 // 128 + z) for z in range(n // 128)] + [("wrtl", q0 // 128 + z) for z in range(n // 128)]
                    tk.op("pe", lambda t: t.matmul(PS[bcb][:, 0:n], selb[0:16, e, :], wrt_hi[:, q0:q0 + n], start=True, stop=False),
                          reads=wk + ["selb"], writes=[psk(bcb)])
                    tk.op("pe", lambda t: t.matmul(PS[bcb][:, 0:n], selb[0:16, e, :], wrt_lo[:, q0:q0 + n], start=False, stop=True),
                          reads=wk + ["selb"], writes=[psk(bcb)])

                def emit_gu(idx, fc):
                    e, q0, n = items[idx]
                    bcb = 0 if idx % 2 == 0 else 7
                    WG, WU, WD, ewk = ew_views(e)
                    hk = hkeys(q0, n)
                    gb = 1 + (fc % 2)
                    ub = 3 + (fc % 2)
                    for j in range(8):
                        tk.op("pe", lambda t, j=j: t.matmul(PS[gb][:, 0:n], WG[:, j, fc * 128:(fc + 1) * 128], hT[:, j, q0:q0 + n],
                                                             start=(j == 0), stop=(j == 7)), reads=[ewk] + hk, writes=[psk(gb)])
                    for j in range(8):
                        tk.op("pe", lambda t, j=j: t.matmul(PS[ub][:, 0:n], WU[:, j, fc * 128:(fc + 1) * 128], hT[:, j, q0:q0 + n],
                                                             start=(j == 0), stop=(j == 7)), reads=[ewk] + hk, writes=[psk(ub)])
                    sg, sgk = tmp()
                    tk.op("act", lambda a: a.activation(out=sg[:, 0:n], in_=PS[gb][:, 0:n], func=AF.Silu), reads=[psk(gb)], writes=[sgk])
                    tk.op("dve", lambda v: v.tensor_tensor(out=sg[:, 0:n], in0=sg[:, 0:n], in1=PS[ub][:, 0:n], op=ALU.mult),
                          reads=[sgk, psk(ub)], writes=[sgk])
                    tk.op("dve", lambda v: v.tensor_tensor(out=hm_buf(idx, fc)[:, 0:n], in0=sg[:, 0:n], in1=PS[bcb][:, 0:n], op=ALU.mult),
                          reads=[sgk, psk(bcb)], writes=[("HM", idx % 2, fc)])

                def emit_down(idx):
                    e, q0, n = items[idx]
                    rr = NB if q0 >= S else b
                    WG, WU, WD, ewk = ew_views(e)
                    for jo in range(8):
                        yb = 5 + (jo % 2)
                        for fc in range(4):
                            tk.op("pe", lambda t, fc=fc: t.matmul(PS[yb][:, 0:n], WD[:, fc, jo * 128:(jo + 1) * 128], hm_buf(idx, fc)[:, 0:n],
                                                                   start=(fc == 0), stop=(fc == 3)), reads=[ewk, ("HM", idx % 2, fc)], writes=[psk(yb)])
                        tk.op("dve", lambda v: v.scalar_tensor_tensor(
                            out=xT[:, jo, q0:q0 + n], in0=PS[yb][:, 0:n], scalar=mT[:, l, 40 + jo, rr:rr + 1],
                            in1=xT[:, jo, q0:q0 + n], op0=ALU.mult, op1=ALU.add),
                            reads=[psk(yb), "mT"] + xkeys(q0, n), writes=xkeys(q0, n))

                load_expert(0)
                emit_bc(0)
                emit_gu(0, 0)
                for idx in range(len(items)):
                    e, q0, n = items[idx]
                    if (q0, n) == ranges[0] and e + 1 < NE:
                        load_expert(e + 1)
                    for fc in range(1, 4):
                        emit_gu(idx, fc)
                    if idx + 1 < len(items):
                        emit_bc(idx + 1)
                        emit_gu(idx + 1, 0)
                    emit_down(idx)
                tk.barrier()
                if dbg == "x2":
                    dump(xT[:, 0, :], 2304, 0, [])
                    dump(xT[:, 5, :], 2304, 2304, [])
                    tk.barrier()
                    return nc

            tk.mark("b%d store" % b)
            for tt in range(16):
                for g in range(2):
                    bank = 6 + g
                    for jj in range(4):
                        j = g * 4 + jj
                        tk.op("pe", lambda t, j=j, jj=jj, bank=bank: t.transpose(
                            out=PS[bank][:, jj * 128:(jj + 1) * 128], in_=xT[:, j, tt * 128:(tt + 1) * 128], identity=ident_f[:]),
                            reads=[("xT", tt), "ident_f"], writes=[psk(bank)], inc=(jj == 3))
                    if g == 0:
                        tk.op("act", lambda a, bank=bank: a.copy(out=XS[:, 0:512], in_=PS[bank][:]), reads=[psk(bank)], writes=["xs"])
                    else:
                        tk.op("dve", lambda v, bank=bank: v.tensor_copy(out=XS[:, 512:1024], in_=PS[bank][:]), reads=[psk(bank)], writes=["xs"])
                tk.dma("sp", lambda q: q.dma_start(out=out_d[b, tt * 128:(tt + 1) * 128, :], in_=XS[:]), reads=["xs"], writes=["out"])
            tk.barrier()
          except StopBuild:
            print("STOPPED at", tk.nops)
            tk.pe_open = False
            tk.limit = 0
            tk.barrier()
            if dbg_d is not None:
                dump(small[:, :], 512, 0, [])
                tk.barrier()
            return nc
        tk.barrier()
        tk.mark("end")
        LAST_MARKS[:] = tk.marks
    return nc


LAST_MARKS = []
dbg_mixer = ["D"]

_W_NAMES = ["ada_w", "ada_b", "norm_mix_g", "norm_ffn_g", "w_in", "qk_g_win", "qk_g_na", "qk_g_diff", "qk_g_gqa",
            "sink_win", "lambda_diff", "out_gain", "w_out", "w_gate", "w_up", "w_down"]


def make_in_maps(inputs, n_cores, NB, DEPTH):
    f = lambda a: np.ascontiguousarray(np.asarray(a, dtype=np.float32))
    shared = {k: f(inputs[k])[:DEPTH] for k in _W_NAMES}
    shared["router_w"] = f(inputs["router_w"])
    shared["router_b"] = f(inputs["router_b"]).reshape(1, NE)
    shared["na_bias"] = host_na_bias(f(inputs["rpb_na"])[:DEPTH])
    shared.update(host_consts())
    x = f(inputs["x"])
    c = f(inputs["c"])
    ctx = f(inputs["ctx"])
    cctx = f(inputs["c_ctx"]).reshape(1, D)
    maps = []
    for i in range(n_cores):
        m = dict(shared)
        m["x"] = x[i * NB:(i + 1) * NB]
        m["ctx"] = ctx[i * NB:(i + 1) * NB]
        m["c"] = np.ascontiguousarray(np.concatenate([c[i * NB:(i + 1) * NB], cctx], 0))
        maps.append(m)
    return maps


def kernel(**inputs):
    NB = inputs["x"].shape[0] // N_CORES
    nc = build(NB, DEPTH_FULL)
    maps = make_in_maps(inputs, N_CORES, NB, DEPTH_FULL)
    res = run_bass_kernel_spmd(nc, maps, core_ids=list(range(N_CORES)))
    return np.concatenate([r["out"] for r in res.results], axis=0).astype(np.float32)
```

```python
import math
import os
from contextlib import ExitStack
import numpy as np
import concourse.bass as bass
import concourse.mybir as mybir
from concourse.bass_utils import run_bass_kernel_spmd

F32 = mybir.dt.float32
BF16 = mybir.dt.bfloat16
AF = mybir.ActivationFunctionType
ALU = mybir.AluOpType
AX = mybir.AxisListType

D = 1024
S = 2048
L = 256
T = S + L
NTT = T // 128
DEPTH_FULL = 4
EPS = 1e-6
NE = 16
DE = 512
N_CORES = 8
NDS = 40
MIX = [("A", 0, 256, 128, 128, 2, 64), ("B", 512, 256, 256, 256, 4, 64),
       ("C", 1280, 256, 256, 256, 4, 32), ("D", 2048, 256, 128, 128, 2, 64)]


class StopBuild(Exception):
    pass


class TK:
    def __init__(self, nc, es):
        self.nc = nc
        self.eng = {"pe": nc.tensor, "act": nc.scalar, "dve": nc.vector, "pool": nc.gpsimd, "sp": nc.sync}
        self.sem = {k: es.enter_context(nc.semaphore("s_" + k)) for k in self.eng}
        self.cnt = {k: 0 for k in self.eng}
        self.seen = {k: {} for k in self.eng}
        self.dsem = [es.enter_context(nc.semaphore("d%d" % i)) for i in range(NDS)]
        self.dval = [0] * NDS
        self.dnext = 0
        self.bw = {}
        self.br = {}
        self.pe_open = False
        self.marks = []
        self.nops = 0
        self.limit = int(os.environ.get("TK_LIMIT", "0"))

    def _tick(self):
        self.nops += 1
        if self.limit and self.nops > self.limit:
            raise StopBuild()

    def _semh(self, sk):
        return self.sem[sk] if isinstance(sk, str) else self.dsem[sk[1]]

    def _need(self, e, reads, writes):
        need = {}

        def add(ev, raw):
            if ev is None:
                return
            sk, v = ev
            if sk == e and (not raw or e == "pe"):
                return
            if need.get(sk, 0) < v:
                need[sk] = v

        for k in reads:
            add(self.bw.get(k), True)
        for k in writes:
            add(self.bw.get(k), False)
            r = self.br.get(k)
            if r:
                for sk, v in r.items():
                    add((sk, v), False)
        return need

    def _emit_waits(self, e, need):
        for sk, v in need.items():
            if self.seen[e].get(sk, 0) >= v:
                continue
            self.eng[e].wait_ge(self._semh(sk), v)
            self.seen[e][sk] = v

    def _record(self, ev, reads, writes):
        sk, v = ev
        for k in reads:
            d = self.br.setdefault(k, {})
            if d.get(sk, 0) < v:
                d[sk] = v
        for k in writes:
            self.bw[k] = ev
            self.br[k] = {}

    def op(self, e, fn, reads=(), writes=(), inc=True):
        inc = True
        self._tick()
        need = self._need(e, reads, writes)
        self._emit_waits(e, need)
        ins = fn(self.eng[e])
        if inc:
            self.cnt[e] += 1
            ins.then_inc(self.sem[e], 1)
            ev = (e, self.cnt[e])
            if e == "pe":
                self.pe_open = False
        else:
            ev = (e, self.cnt[e] + 1)
            if e == "pe":
                self.pe_open = True
        self._record(ev, reads, writes)

    def dma(self, q, fn, reads=(), writes=()):
        self._tick()
        need = self._need(q, reads, writes)
        i = self.dnext
        self.dnext = (i + 1) % NDS
        if self.dval[i] > 0:
            sk = ("d", i)
            if need.get(sk, 0) < self.dval[i]:
                need[sk] = self.dval[i]
        self._emit_waits(q, need)
        ins = fn(self.eng[q])
        self.dval[i] += 16
        ins.then_inc(self.dsem[i], 16)
        self._record((("d", i), self.dval[i]), reads, writes)

    def mark(self, name):
        self.marks.append((name, dict(self.cnt)))

    def barrier(self):
        assert not self.pe_open
        for e in self.eng:
            need = {f: self.cnt[f] for f in self.eng if f != e and self.cnt[f] > 0}
            for i in range(NDS):
                if self.dval[i] > 0:
                    need[("d", i)] = self.dval[i]
            self._emit_waits(e, need)
        self.bw = {}
        self.br = {}


def host_consts():
    pos = np.arange(S)
    row, col = pos // 64, pos % 64

    def tabs(hd):
        e = hd // 4
        inv = 10000.0 ** (-np.arange(e, dtype=np.float32) / e)
        ar = row[:, None].astype(np.float32) * inv
        ac = col[:, None].astype(np.float32) * inv
        cos = np.concatenate([np.cos(ar), np.cos(ar), np.cos(ac), np.cos(ac)], 1)
        sins = np.concatenate([-np.sin(ar), np.sin(ar), -np.sin(ac), np.sin(ac)], 1)
        return cos.astype(np.float32), sins.astype(np.float32)

    c64, s64 = tabs(64)
    c32, s32 = tabs(32)
    rope = np.concatenate([c64, s64, c32, s32], 1)
    b = np.arange(128)[:, None]
    a = np.arange(128)[None, :]
    wmask = np.stack([(a <= b), (b <= a)], 1).astype(np.float32)
    return {"ident": np.eye(128, dtype=np.float32), "rope": np.ascontiguousarray(rope),
            "wmask": np.ascontiguousarray(wmask.reshape(128, 256))}


def na_variant(i):
    return {0: 0, 1: 1, 14: 3, 15: 4}.get(i, 2)


def na_ts(i):
    return min(max(i - 2, 0), 11)


def host_na_bias(rpb):
    dl = rpb.shape[0]
    out = np.empty((dl, 4, 5, 128, 640), np.float32)
    kl = np.arange(128) // 64
    kc = np.arange(128) % 64
    ql = np.arange(128) // 64
    qc = np.arange(128) % 64
    for vi, i in enumerate([0, 1, 5, 14, 15]):
        ts = na_ts(i)
        for j in range(5):
            krow = 2 * (ts + j) + kl[:, None]
            qrow = 2 * i + ql[None, :]
            rs = np.clip(qrow - 4, 0, 24)
            rowok = (krow >= rs) & (krow < rs + 8)
            cs = np.clip(qc[None, :] - 8, 0, 48)
            colok = (kc[:, None] >= cs) & (kc[:, None] < cs + 16)
            dr = np.clip(krow - qrow + 7, 0, 14)
            dc = np.clip(kc[:, None] - qc[None, :] + 15, 0, 30)
            ok = rowok & colok
            g = rpb[:, :, dr, dc]
            out[:, :, vi, :, j * 128:(j + 1) * 128] = np.where(ok[None, None], g, np.float32(-30000.0))
    return out


def build(NB, DEPTH, dbg=None):
    nc = bass.Bass("TRN2", target_bir_lowering=False)

    def dram(name, shape, kind="ExternalInput", dtype=F32):
        return nc.dram_tensor(name, list(shape), dtype, kind=kind).ap()

    x_d = dram("x", [NB, S, D])
    c_d = dram("c", [NB + 1, D])
    ctx_d = dram("ctx", [NB, L, D])
    ada_w = dram("ada_w", [DEPTH, D, 6 * D])
    ada_b = dram("ada_b", [DEPTH, 6 * D])
    ng1 = dram("norm_mix_g", [DEPTH, D])
    ng2 = dram("norm_ffn_g", [DEPTH, D])
    w_in = dram("w_in", [DEPTH, D, 2560])
    qkg = {"A": dram("qk_g_win", [DEPTH, 2, 64]), "B": dram("qk_g_na", [DEPTH, 2, 64]),
           "C": dram("qk_g_diff", [DEPTH, 2, 32]), "D": dram("qk_g_gqa", [DEPTH, 2, 64])}
    sink_d = dram("sink_win", [DEPTH, 4])
    nab_d = dram("na_bias", [DEPTH, 4, 5, 128, 640])
    lam_d = dram("lambda_diff", [DEPTH, 4, 32])
    og_d = dram("out_gain", [DEPTH, D])
    w_out = dram("w_out", [DEPTH, D, D])
    rw_d = dram("router_w", [D, NE])
    rb_d = dram("router_b", [1, NE])
    wg_d = dram("w_gate", [DEPTH, NE, D, DE])
    wu_d = dram("w_up", [DEPTH, NE, D, DE])
    wd_d = dram("w_down", [DEPTH, NE, DE, D])
    ident_d = dram("ident", [128, 128])
    rope_d = dram("rope", [S, 192])
    wmask_d = dram("wmask", [128, 256])
    out_d = dram("out", [NB, S, D], kind="ExternalOutput")
    dbg_d = dram("dbg", [128, 8192], kind="ExternalOutput") if dbg else None

    es = ExitStack()
    with es:
        nc_ctx = es.enter_context(nc.allow_non_contiguous_dma(reason="small strided parameter loads"))
        tk = TK(nc, es)
        build_info = {}

        def sb(name, shape, dtype=F32):
            return es.enter_context(nc.sbuf_tensor(name, list(shape), dtype))

        xT = sb("xT", [128, 8, T])
        hT = sb("hT", [128, 8, T], BF16)
        arena = sb("arena", [128, 32768], BF16)
        ident_f = sb("ident_fs", [128, 128])
        ident_b = sb("ident_bs", [128, 128], BF16)
        ones_b = sb("ones_b", [128, 128], BF16)
        selb = sb("selb", [16, NE, 128], BF16)
        rw_hi = sb("rw_hi", [128, 8, NE], BF16)
        rw_lo = sb("rw_lo", [128, 8, NE], BF16)
        wmask = sb("wmask_sb", [128, 256], BF16)
        mT = sb("mT", [128, DEPTH, 48, NB + 1])
        sT = sb("sT", [128, 8, NB + 1])
        TMP = [sb("tmp%d" % i, [128, 512]) for i in range(5)]
        RB = sb("rbuf", [128, 512])
        XS = arena[:, 0:2048].bitcast(F32)
        PB = [sb("pb%d" % i, [128, 512], BF16) for i in range(3)]
        ropet = sb("ropet", [128, 192])
        small = sb("small", [128, 512])
        TQ = sb("tq", [128, 256], BF16)
        TK_ = sb("tkp", [128, 512], BF16)
        otok = sb("otok", [128, 4, 256])
        ytok = sb("ytok", [128, 256], BF16)
        nabm = sb("nabm", [128, 640], BF16)
        stat = sb("stat", [128, 64])
        PS = [es.enter_context(nc.psum_tensor("ps%d" % i, [128, 512], F32)) for i in range(8)]

        pass

        def psk(i):
            return ("ps", i)

        tmp_i = [0]

        def tmp():
            i = tmp_i[0]
            tmp_i[0] = (i + 1) % len(TMP)
            return TMP[i], ("tmp", i)

        pb_i = [0]

        def pbuf():
            i = pb_i[0]
            pb_i[0] = (i + 1) % len(PB)
            return PB[i], ("pb", i)

        A1 = small[:, 0:16].rearrange("p (j r) -> p j r", r=2)
        A2 = small[:, 16:32].rearrange("p (j r) -> p j r", r=2)
        g1T = small[:, 32:40]
        g2T = small[:, 40:48]
        ogT = small[:, 48:56]
        abT = small[:, 56:104]
        rb_bc = small[:, 104:120]
        sinkx = small[:, 120:124]
        lamt = small[:, 124:128]
        gq = small[:, 128:192]
        gk = small[:, 192:256]
        rwT = small[:, 256:384].rearrange("p (j e) -> p j e", e=NE)
        lamraw = small[:, 384:512]

        tk.dma("sp", lambda q: q.dma_start(out=ident_f[:], in_=ident_d[:, :]), writes=["ident_f"])
        tk.op("dve", lambda v: v.tensor_copy(out=ident_b[:], in_=ident_f[:]), reads=["ident_f"], writes=["ident_b"])
        tk.op("dve", lambda v: v.memset(ones_b[:], 1.0), writes=["ones_b"])
        tk.dma("sp", lambda q: q.dma_start(out=TMP[0][:, 0:256], in_=wmask_d[:, :]), writes=[("tmp", 0)])
        tk.op("dve", lambda v: v.tensor_copy(out=wmask[:], in_=TMP[0][:, 0:256]), reads=[("tmp", 0)], writes=["wmask"])
        tk.dma("sp", lambda q: q.dma_start(out=rb_bc, in_=rb_d[0:1, :].partition_broadcast(128)), writes=["small"])
        tk.dma("sp", lambda q: q.dma_start(out=rwT, in_=rw_d.rearrange("(j p) e -> p j e", p=128)), writes=["small"])
        tk.op("dve", lambda v: v.tensor_copy(out=rw_hi[:], in_=rwT), reads=["small"], writes=["rw"])
        tk.op("dve", lambda v: v.tensor_tensor(out=rw_lo[:], in0=rwT, in1=rw_hi[:], op=ALU.subtract), reads=["small", "rw"], writes=["rw2"])
        tk.op("dve", lambda v: v.tensor_copy(out=selb[:], in_=ident_b[0:16, 0:16].unsqueeze(2).broadcast_to([16, NE, 128])),
              reads=["ident_b"], writes=["selb"])
        tk.op("dve", lambda v: v.memset(TK_[:], 0.0), writes=["tkp0"])
        tk.op("dve", lambda v: v.memset(arena[:], 0.0), writes=["arena"])

        for r in range(NB + 1):
            tk.dma("sp", lambda q, r=r: q.dma_start(out=sT[:, :, r], in_=c_d[r].rearrange("(j p) -> p j", p=128)),
                   writes=["sT"])
        tk.op("act", lambda a: a.activation(out=sT[:], in_=sT[:], func=AF.Silu), reads=["sT"], writes=["sT"])
        for l in range(DEPTH):
            for j6 in range(6):
                tk.dma("sp", lambda q, l=l, j6=j6: q.dma_start(
                    out=abT[:, j6 * 8:(j6 + 1) * 8],
                    in_=ada_b[l, j6 * 1024:(j6 + 1) * 1024].rearrange("(j p) -> p j", p=128)), writes=["abT"])
            psm = PS[0][:, 0:48 * (NB + 1)].rearrange("p (j r) -> p j r", r=NB + 1)
            for j in range(48):
                wt, wk = tmp()
                wt2, wk2 = tmp()
                tk.dma("sp", lambda q, l=l, j=j, wt=wt: q.dma_start(
                    out=wt[:].rearrange("p (k c) -> p k c", c=128),
                    in_=ada_w[l, 0:512, j * 128:(j + 1) * 128].rearrange("(k p) c -> p k c", p=128)), writes=[wk])
                tk.dma("sp", lambda q, l=l, j=j, wt2=wt2: q.dma_start(
                    out=wt2[:].rearrange("p (k c) -> p k c", c=128),
                    in_=ada_w[l, 512:1024, j * 128:(j + 1) * 128].rearrange("(k p) c -> p k c", p=128)), writes=[wk2])
                for kc in range(8):
                    src, sk_ = (wt, wk) if kc < 4 else (wt2, wk2)
                    tk.op("pe", lambda t, j=j, kc=kc, src=src: t.matmul(
                        psm[:, j, :], src[:, (kc % 4) * 128:(kc % 4 + 1) * 128], sT[:, kc, :],
                        start=(kc == 0), stop=(kc == 7)),
                        reads=[sk_, "sT"], writes=[psk(0)], inc=(kc == 7))
            tk.op("dve", lambda v, l=l: v.tensor_tensor(
                out=mT[:, l], in0=psm, in1=abT.unsqueeze(2).broadcast_to([128, 48, NB + 1]), op=ALU.add),
                reads=[psk(0), "abT"], writes=["mT"])

        def load_tokens_T(src_ap, tok0):
            tk.dma("sp", lambda q: q.dma_start(out=XS[:], in_=src_ap), writes=["xs"])
            for g in range(2):
                bank = 6 + g
                for jj in range(4):
                    j = g * 4 + jj
                    tk.op("pe", lambda t, j=j, jj=jj, bank=bank: t.transpose(
                        out=PS[bank][:, jj * 128:(jj + 1) * 128], in_=XS[:, j * 128:(j + 1) * 128],
                        identity=ident_f[:]), reads=["xs", "ident_f"], writes=[psk(bank)], inc=(jj == 3))
                eng = "act" if g == 0 else "dve"
                if eng == "act":
                    tk.op("act", lambda a, g=g, bank=bank: a.copy(
                        out=xT[:, g * 4:(g + 1) * 4, tok0:tok0 + 128],
                        in_=PS[bank][:].rearrange("p (j t) -> p j t", t=128)),
                        reads=[psk(bank)], writes=[("xT", tok0 // 128)])
                else:
                    tk.op("dve", lambda v, g=g, bank=bank: v.tensor_copy(
                        out=xT[:, g * 4:(g + 1) * 4, tok0:tok0 + 128],
                        in_=PS[bank][:].rearrange("p (j t) -> p j t", t=128)),
                        reads=[psk(bank)], writes=[("xT", tok0 // 128)])

        def xkeys(t0, n):
            return [("xT", i) for i in range(t0 // 128, (t0 + n) // 128)]

        def hkeys(t0, n):
            return [("hT", i) for i in range(t0 // 128, (t0 + n) // 128)]

        def norm_to_h(t0, n, Aap, Bap, router_bank=None):
            xk = xkeys(t0, n)
            hk = hkeys(t0, n)
            for j in range(8):
                sq, sqk = pbuf()
                tk.op("act", lambda a, j=j, sq=sq: a.activation(out=sq[:, :n], in_=xT[:, j, t0:t0 + n], func=AF.Square),
                      reads=xk, writes=[sqk])
                tk.op("pe", lambda t, j=j, sq=sq: t.matmul(PS[5][:, :n], ones_b[:], sq[:, :n], start=(j == 0), stop=(j == 7)),
                      reads=[sqk, "ones_b"], writes=[psk(5)], inc=(j == 7))
            rb, rbk = RB, "rbuf"
            tk.op("act", lambda a: a.activation(out=rb[:, :n], in_=PS[5][:, :n], func=AF.Sqrt, bias=EPS, scale=1.0 / D),
                  reads=[psk(5)], writes=[rbk])
            tk.op("dve", lambda v: v.reciprocal(out=rb[:, :n], in_=rb[:, :n]), reads=[rbk], writes=[rbk])
            for j in range(8):
                t1, t1k = tmp()
                tk.op("dve", lambda v, j=j, t1=t1: v.tensor_tensor(out=t1[:, :n], in0=xT[:, j, t0:t0 + n], in1=rb[:, :n],
                                                                    op=ALU.mult), reads=xk + [rbk], writes=[t1k])
                if router_bank is None:
                    tk.op("act", lambda a, j=j, t1=t1: a.activation(
                        out=hT[:, j, t0:t0 + n], in_=t1[:, :n], func=AF.Identity, scale=Aap[:, j:j + 1], bias=Bap[:, j:j + 1]),
                        reads=[t1k, "small", "mT"], writes=hk)
                else:
                    tk.op("act", lambda a, j=j, t1=t1: a.activation(
                        out=t1[:, :n], in_=t1[:, :n], func=AF.Identity, scale=Aap[:, j:j + 1], bias=Bap[:, j:j + 1]),
                        reads=[t1k, "small", "mT"], writes=[t1k])
                    tk.op("pool", lambda g, j=j, t1=t1: g.tensor_copy(out=hT[:, j, t0:t0 + n], in_=t1[:, :n]),
                          reads=[t1k], writes=hk)
                    lo, lok = pbuf()
                    tk.op("dve", lambda v, j=j, t1=t1, lo=lo: v.tensor_tensor(out=lo[:, :n], in0=t1[:, :n], in1=hT[:, j, t0:t0 + n],
                                                                              op=ALU.subtract), reads=[t1k] + hk, writes=[lok])
                    tk.op("pe", lambda t, j=j: t.matmul(PS[router_bank][0:16, :n], rw_hi[:, j, :], hT[:, j, t0:t0 + n],
                                                        start=(j == 0), stop=False),
                          reads=hk + ["rw"], writes=[psk(router_bank)], inc=False)
                    tk.op("pe", lambda t, j=j: t.matmul(PS[router_bank][0:16, :n], rw_lo[:, j, :], hT[:, j, t0:t0 + n],
                                                        start=False, stop=False),
                          reads=hk + ["rw2"], writes=[psk(router_bank)], inc=False)
                    tk.op("pe", lambda t, j=j, lo=lo: t.matmul(PS[router_bank][0:16, :n], rw_hi[:, j, :], lo[:, :n],
                                                               start=False, stop=(j == 7)),
                          reads=[lok, "rw"], writes=[psk(router_bank)], inc=(j == 7))

        def dump(ap, width, col0, keys):
            if dbg_d is None:
                return
            tk.dma("pool", lambda q: q.dma_start(out=dbg_d[0:ap.shape[0], col0:col0 + width], in_=ap), reads=keys)

        QT = arena[:, 0:4608].rearrange("p (c t) -> p c t", c=2)
        KT = arena[:, 4608:13824].rearrange("p (c t) -> p c t", c=4)
        VA = arena[:, 13824:18576].rearrange("p (t h d) -> p t h d", t=NTT, h=4)
        YT = arena[:, 18576:23184].rearrange("p (c t) -> p c t", c=2)
        WI = arena[:, 23184:29328].rearrange("p (j c) -> p j c", j=8)
        WO = arena[:, 29328:31376].rearrange("p (k c) -> p k c", k=2)
        EW = [arena[:, i * 12288:(i + 1) * 12288] for i in range(2)]
        HM = arena[:, 24576:26624].rearrange("p (f t) -> p f t", f=4)
        WT = arena[0:16, 26624:31232].bitcast(F32) if False else None

        wrt_hi = arena[0:16, 26624:26624 + T]
        wrt_lo = arena[0:16, 26624 + T:26624 + 2 * T]

        def qk_post(ps_ap, nh, hd, gain_ap, rope_tile, out_views, is_lat, bank, okey, slot, cs):
            width = nh * hd
            Wb, Wk = TMP[2 * cs], ("tmp", 2 * cs)
            Yb, Yk = TMP[2 * cs + 1], ("tmp", 2 * cs + 1)
            sq = Wb[:, 0:width]
            X = Wb[:, 256:256 + width]
            Y = Yb[:, 0:width]
            tk.op("act", lambda a: a.activation(out=sq, in_=ps_ap, func=AF.Square), reads=[psk(bank)], writes=[Wk])
            yield
            sc0 = [0, 40, 48, 56][slot]
            stk = "statq%d" % slot
            ss = stat[:, sc0:sc0 + nh]
            tk.op("dve", lambda v: v.tensor_reduce(out=ss, in_=sq.rearrange("p (h d) -> p h d", h=nh),
                                                   axis=AX.X, op=ALU.add), reads=[Wk], writes=[stk])
            yield
            tk.op("act", lambda a: a.activation(out=ss, in_=ss, func=AF.Sqrt, bias=EPS, scale=1.0 / hd),
                  reads=[stk], writes=[stk])
            yield
            tk.op("dve", lambda v: v.reciprocal(out=ss, in_=ss), reads=[stk], writes=[stk])
            yield
            tk.op("dve", lambda v: v.tensor_tensor(
                out=X.rearrange("p (h d) -> p h d", h=nh), in0=ps_ap.rearrange("p (h d) -> p h d", h=nh),
                in1=ss.unsqueeze(2).broadcast_to([128, nh, hd]), op=ALU.mult),
                reads=[psk(bank), stk], writes=[Wk])
            yield
            tk.op("pool", lambda g: g.tensor_tensor(
                out=X.rearrange("p (h d) -> p h d", h=nh), in0=X.rearrange("p (h d) -> p h d", h=nh),
                in1=gain_ap.unsqueeze(1).broadcast_to([128, nh, hd]), op=ALU.mult),
                reads=[Wk, "small"], writes=[Wk])
            yield
            if not is_lat:
                for (oap, sel) in out_views:
                    src = X if sel is None else sel(X)
                    tk.op("dve", lambda v, oap=oap, src=src: v.tensor_copy(out=oap, in_=src), reads=[Wk], writes=[okey])
                    yield
                return
            cos_ap, sin_ap = rope_tile
            e4 = hd // 4
            tk.op("pool", lambda g: g.tensor_tensor(
                out=Y.rearrange("p (h d) -> p h d", h=nh), in0=X.rearrange("p (h d) -> p h d", h=nh),
                in1=cos_ap.unsqueeze(1).broadcast_to([128, nh, hd]), op=ALU.mult), reads=[Wk, "ropet"], writes=[Yk])
            yield
            qv = X.rearrange("p (h b s e) -> p h b s e", h=nh, b=2, s=2)
            tv = sq.rearrange("p (h b s e) -> p h b s e", h=nh, b=2, s=2)
            sv = sin_ap.rearrange("p (b s e) -> p b s e", b=2, s=2)
            for s_ in range(2):
                tk.op("dve", lambda v, s_=s_: v.tensor_tensor(
                    out=tv[:, :, :, s_, :], in0=qv[:, :, :, 1 - s_, :],
                    in1=sv[:, :, s_, :].unsqueeze(1).broadcast_to([128, nh, 2, e4]), op=ALU.mult),
                    reads=[Wk, "ropet"], writes=[Wk])
                yield
            for (oap, sel) in out_views:
                a_ = Y if sel is None else sel(Y)
                b_ = sq if sel is None else sel(sq)
                tk.op("dve", lambda v, oap=oap, a_=a_, b_=b_: v.tensor_tensor(out=oap, in0=a_, in1=b_, op=ALU.add),
                      reads=[Wk, Yk], writes=[okey])
                yield

        def run_zipped(gens):
            gens = list(gens)
            while gens:
                for g_ in list(gens):
                    try:
                        next(g_)
                    except StopIteration:
                        gens.remove(g_)

        ps_bank = [0]
        out_key = ["tq"]
        stat_slot = [0]
        unit_ctr = [0]
        nabm_state = [None]
        TQb = [TQ[:, :], arena[:, 31376:31632]]
        TKb = [TK_[:, :], arena[:, 31632:32144]]

        if dbg == "p0":
            tk.barrier()
            dump(mT[:, 0].rearrange("p j r -> p (j r)"), 48 * (NB + 1), 0, [])
            tk.barrier()
            return nc
        for b in range(NB):
          try:
            for tt in range(16):
                load_tokens_T(x_d[b, tt * 128:(tt + 1) * 128, :], tt * 128)
            for tt in range(2):
                load_tokens_T(ctx_d[b, tt * 128:(tt + 1) * 128, :], S + tt * 128)
            tk.barrier()
            if dbg == "ld":
                dump(xT[:, 0, :], 2304, 0, [])
                dump(xT[:, 7, :], 2304, 2304, [])
                tk.barrier()
                return nc

            for l in range(DEPTH):
                pass
                need_ctx = l < DEPTH - 1
                lam_init = 0.8 - 0.6 * math.exp(-0.3 * l)
                mrow = lambda k, r: mT[:, l, k * 8:(k + 1) * 8, r]
                tk.dma("sp", lambda q: q.dma_start(out=g1T, in_=ng1[l].rearrange("(j p) -> p j", p=128)), writes=["small"])
                tk.dma("sp", lambda q: q.dma_start(out=g2T, in_=ng2[l].rearrange("(j p) -> p j", p=128)), writes=["small"])
                tk.dma("sp", lambda q: q.dma_start(out=ogT, in_=og_d[l].rearrange("(j p) -> p j", p=128)), writes=["small"])
                tk.dma("sp", lambda q: q.dma_start(out=sinkx, in_=sink_d[l:l + 1, :].partition_broadcast(128)), writes=["small"])
                tk.dma("sp", lambda q: q.dma_start(
                    out=lamraw, in_=lam_d[l:l + 1].rearrange("o a d -> o (a d)").partition_broadcast(128)), writes=["small"])
                for r in range(2):
                    rr = b if r == 0 else NB
                    tk.op("dve", lambda v, r=r, rr=rr: v.scalar_tensor_tensor(
                        out=A1[:, :, r], in0=mrow(1, rr), scalar=1.0, in1=g1T, op0=ALU.add, op1=ALU.mult),
                        reads=["small", "mT"], writes=["small"])
                    tk.op("dve", lambda v, r=r, rr=rr: v.scalar_tensor_tensor(
                        out=A2[:, :, r], in0=mrow(4, rr), scalar=1.0, in1=g2T, op0=ALU.add, op1=ALU.mult),
                        reads=["small", "mT"], writes=["small"])
                tk.op("act", lambda a: a.activation(out=sinkx, in_=sinkx, func=AF.Exp), reads=["small"], writes=["small"])
                lr = lamraw.rearrange("p (a d) -> p a d", a=4)
                lw = stat[:, 32:36]
                tk.op("dve", lambda v: v.tensor_tensor(out=lamraw[:, 0:32], in0=lr[:, 0, :], in1=lr[:, 1, :], op=ALU.mult),
                      reads=["small"], writes=["small"])
                tk.op("dve", lambda v: v.tensor_tensor(out=lamraw[:, 64:96], in0=lr[:, 2, :], in1=lr[:, 3, :], op=ALU.mult),
                      reads=["small"], writes=["small"])
                tk.op("dve", lambda v: v.tensor_reduce(out=lw[:, 0:2], in_=lamraw.rearrange("p (a d) -> p a d", a=2)[:, :, 0:32],
                                                       axis=AX.X, op=ALU.add), reads=["small"], writes=["stat2"])
                tk.op("act", lambda a: a.activation(out=lw[:, 0:2], in_=lw[:, 0:2], func=AF.Exp), reads=["stat2"], writes=["stat2"])
                tk.op("dve", lambda v: v.tensor_tensor(out=lw[:, 2:3], in0=lw[:, 0:1], in1=lw[:, 1:2], op=ALU.subtract),
                      reads=["stat2"], writes=["stat2"])
                tk.op("act", lambda a: a.activation(out=lamt[:, 0:1], in_=lw[:, 2:3], func=AF.Identity, scale=-1.0, bias=-lam_init),
                      reads=["stat2"], writes=["small"])

                if dbg == "sm":
                    print("nops at sm", tk.nops)
                    tk.barrier()
                    dump(small[:, :], 512, 0, [])
                    tk.barrier()
                    return nc
                tk.mark("b%d l%d norm1" % (b, l))
                for cch in range(4):
                    norm_to_h(cch * 512, 512, A1[:, :, 0], mrow(0, b))
                norm_to_h(S, L, A1[:, :, 1], mrow(0, NB))
                if dbg == "h":
                    tk.barrier()
                    dump(hT[:, 0, 0:2304], 2304, 0, hkeys(0, T))
                    dump(hT[:, 7, 0:2304], 2304, 2304, hkeys(0, T))
                    tk.barrier()
                    return nc

                for mi, (mname, col0, nq, nk, nv, nkv, hd) in enumerate(MIX):
                    ncols = nq + nk + nv
                    nh_q = nq // hd
                    nh_k = nk // hd
                    scale = hd ** -0.5
                    tk.dma("pool", lambda q: q.dma_start(
                        out=WI[:, :, 0:ncols], in_=w_in[l, :, col0:col0 + ncols].rearrange("(j p) c -> p j c", p=128)),
                        writes=["WI"])
                    tk.dma("pool", lambda q: q.dma_start(
                        out=WO[:, :, :], in_=w_out[l, mi * 256:(mi + 1) * 256, :].rearrange("(k p) c -> p k c", p=128)),
                        writes=["WO"])
                    gd = qkg[mname]
                    tk.dma("sp", lambda q: q.dma_start(out=gq[:, 0:hd], in_=gd[l, 0:1, :].partition_broadcast(128)), writes=["small"])
                    tk.dma("sp", lambda q: q.dma_start(out=gk[:, 0:hd], in_=gd[l, 1:2, :].partition_broadcast(128)), writes=["small"])
                    tk.op("dve", lambda v: v.tensor_scalar(out=gq[:, 0:hd], in0=gq[:, 0:hd], scalar1=scale, scalar2=None,
                                                           op0=ALU.mult), reads=["small"], writes=["small"])
                    tk.op("dve", lambda v: v.memset(VA[:, :, :, 64:65], 1.0), writes=["VA"])

                    tk.mark("b%d l%d %s proj" % (b, l, mname))
                    if mname == "C":
                        tk.op("dve", lambda v: v.memset(TKb[0], 0.0), writes=["tkp0"])
                        tk.op("dve", lambda v: v.memset(TKb[1], 0.0), writes=["tkp1"])
                    nkc = 4 if mname == "C" else nk // 128

                    def tile_banks(tt):
                        return [0, 1] if tt % 2 == 0 else [2, 3]

                    def emit_mm(tt):
                        banks = tile_banks(tt)
                        pieces = [(0, min(512, ncols), banks[0])]
                        if ncols > 512:
                            pieces.append((512, ncols, banks[1]))
                        for (c0, c1, bank) in pieces:
                            for j in range(8):
                                tk.op("pe", lambda t, j=j: t.matmul(
                                    PS[bank][:, 0:c1 - c0], hT[:, j, tt * 128:(tt + 1) * 128], WI[:, j, c0:c1],
                                    start=(j == 0), stop=(j == 7)), reads=[("hT", tt), "WI"], writes=[psk(bank)])

                    def emit_chains(tt):
                        is_lat = tt < 16 and mname != "B"
                        banks = tile_banks(tt)
                        par = tt % 2
                        TQc = TQb[par]
                        TKc = TKb[par]
                        if tt < 16 and mname != "B":
                            tk.dma("sp", lambda q: q.dma_start(out=ropet[:], in_=rope_d[tt * 128:(tt + 1) * 128, :]), writes=["ropet"])
                        rt = (ropet[:, 0:64], ropet[:, 64:128]) if hd == 64 else (ropet[:, 128:160], ropet[:, 160:192])
                        if mname in ("A", "D"):
                            qviews = [(TQc.rearrange("p (c s d) -> p s c d", c=2, s=2),
                                       (lambda ap: ap.rearrange("p (s c d) -> p s c d", s=2, c=2)))]
                        else:
                            qviews = [(TQc, None)]
                        gq_ = qk_post(PS[banks[0]][:, 0:256], nh_q if hd == 64 else 8, hd, gq[:, 0:hd], rt, qviews, is_lat,
                                      banks[0], "tq%d" % par, par * 2, 0)
                        if mname == "C":
                            tkv = TKc.rearrange("p (hp i hh i2 d) -> p hp i hh i2 d", hp=2, i=2, hh=2, i2=2)
                            views = []
                            for i_ in range(2):
                                views.append((tkv[:, :, i_, :, i_, :],
                                              (lambda ap, i_=i_: ap.rearrange("p (hp hh i d) -> p hp hh i d", hp=2, hh=2, i=2)[:, :, :, i_, :])))
                            gk_ = qk_post(PS[banks[0]][:, 256:512], 8, 32, gk[:, 0:32], rt, views, is_lat, banks[0], "tkp%d" % par, par * 2 + 1, 1)
                        else:
                            gk_ = qk_post(PS[banks[0]][:, 256:256 + nk], nh_k, hd, gk[:, 0:hd], rt, [(TKc[:, 0:nk], None)], is_lat,
                                          banks[0], "tkp%d" % par, par * 2 + 1, 1)
                        run_zipped([gq_, gk_])
                        if nk == 128:
                            vsrc = PS[banks[0]][:, 384:512]
                            vb = banks[0]
                        else:
                            vsrc = PS[banks[1]][:, 0:256]
                            vb = banks[1]
                        tk.op("act", lambda a: a.copy(out=VA[:, tt, 0:nkv, 0:64], in_=vsrc.rearrange("p (h d) -> p h d", h=nkv)),
                              reads=[psk(vb)], writes=[("VA", tt)])

                    def emit_tr(tt):
                        par = tt % 2
                        TQc = TQb[par]
                        TKc = TKb[par]
                        for cc in range(2):
                            tk.op("pe", lambda t, cc=cc: t.transpose(
                                out=PS[4][:].bitcast(BF16)[:, cc * 128:(cc + 1) * 128], in_=TQc[:, cc * 128:(cc + 1) * 128],
                                identity=ident_b[:]), reads=["tq%d" % par, "ident_b"], writes=[psk(4)])
                        tk.op("act", lambda a: a.copy(out=QT[:, :, tt * 128:(tt + 1) * 128],
                                                      in_=PS[4][:].bitcast(BF16)[:, 0:256].rearrange("p (c t) -> p c t", c=2)),
                              reads=[psk(4)], writes=[("QT", tt)])
                        for cc in range(nkc):
                            tk.op("pe", lambda t, cc=cc: t.transpose(
                                out=PS[5][:].bitcast(BF16)[:, cc * 128:(cc + 1) * 128],
                                in_=TKc[:, cc * 128:(cc + 1) * 128], identity=ident_b[:]),
                                reads=["tkp%d" % par, "ident_b"], writes=[psk(5)])
                        tk.op("act", lambda a: a.copy(
                            out=KT[:, 0:nkc, tt * 128:(tt + 1) * 128],
                            in_=PS[5][:].bitcast(BF16)[:, 0:nkc * 128].rearrange("p (c t) -> p c t", c=nkc)),
                            reads=[psk(5)], writes=[("KT", tt)])

                    emit_mm(0)
                    for tt in range(NTT):
                        emit_chains(tt)
                        if tt + 1 < NTT:
                            emit_mm(tt + 1)
                        emit_tr(tt)
                    if dbg == "qkv" and mname == dbg_mixer[0]:
                        tk.barrier()
                        dump(QT[:, 0, :], 2304, 0, [])
                        dump(KT[:, 0, :], 2304, 2304, [])
                        dump(VA[:, 3, :, :].rearrange("p h d -> p (h d)"), 264, 4608, [])
                        dump(KT[:, 3, :], 2304, 4900, [])
                        tk.barrier()
                        return nc

                    tk.mark("b%d l%d %s attn" % (b, l, mname))
                    ranges = [(r0 * 512, 512) for r0 in range(4)] + ([(S, L)] if need_ctx else [])
                    pending = []

                    def flush():
                        for f_ in pending:
                            f_()
                        del pending[:]

                    for (q0, n) in ranges:
                        nblk = n // 128
                        is_ctxq = q0 >= S
                        units = []
                        if mname in ("A", "D"):
                            for h in range(4):
                                units.append((h, 0, h % 2, (h // 2) * 64, 0, (h // 2) * 64, h // 2))
                        elif mname == "B":
                            for h in range(4):
                                units.append((h, 0, h // 2, (h % 2) * 64, h // 2, (h % 2) * 64, h))
                        else:
                            for h in range(4):
                                for i_ in range(2):
                                    units.append((h, i_, h // 2, (h % 2) * 64, (h // 2) * 2 + i_, (h % 2) * 64, h))
                        for (h, br, qc, qp, kc_, kp, vh) in units:
                            ob = 6 if unit_ctr[0] % 2 == 0 else 3
                            unit_ctr[0] += 1
                            steps = []
                            if is_ctxq or mname in ("C", "D"):
                                kts = [16, 17] if is_ctxq else list(range(18))
                                for ki, kt in enumerate(kts):
                                    steps.append((0, n, kt, None, None, ki == 0, ki == len(kts) - 1, None))
                            elif mname == "A":
                                for bl in range(nblk):
                                    i = q0 // 128 + bl
                                    lst = []
                                    if i - 1 >= 0:
                                        lst.append((i - 1, wmask[:, 0:128], "wmask"))
                                    lst.append((i, None, None))
                                    if i + 1 < 16:
                                        lst.append((i + 1, wmask[:, 128:256], "wmask"))
                                    lst += [(16, None, None), (17, None, None)]
                                    for ki, (kt, ma, mk_) in enumerate(lst):
                                        steps.append((bl * 128, 128, kt, ma, mk_, ki == 0, ki == len(lst) - 1, None))
                            else:
                                for bl in range(nblk):
                                    i = q0 // 128 + bl
                                    ts = na_ts(i)
                                    vi = na_variant(i)

                                    def load_mask(vi=vi, h=h):
                                        if nabm_state[0] == (b, l, h, vi):
                                            return
                                        nabm_state[0] = (b, l, h, vi)
                                        stg, stgk = tmp()
                                        stg2, stg2k = tmp()
                                        tk.dma("sp", lambda q: q.dma_start(out=stg[:, 0:512], in_=nab_d[l, h, vi, :, 0:512]), writes=[stgk])
                                        tk.dma("sp", lambda q: q.dma_start(out=stg2[:, 0:128], in_=nab_d[l, h, vi, :, 512:640]), writes=[stg2k])
                                        tk.op("act", lambda a: a.activation(out=nabm[:, 0:512], in_=stg[:, 0:512], func=AF.Exp),
                                              reads=[stgk], writes=["nabm"])
                                        tk.op("act", lambda a: a.activation(out=nabm[:, 512:640], in_=stg2[:, 0:128], func=AF.Exp),
                                              reads=[stg2k], writes=["nabm"])
                                    lst = [(ts + j, nabm[:, j * 128:(j + 1) * 128], "nabm") for j in range(5)]
                                    lst += [(16, None, None), (17, None, None)]
                                    for ki, (kt, ma, mk_) in enumerate(lst):
                                        steps.append((bl * 128, 128, kt, ma, mk_, ki == 0, ki == len(lst) - 1, load_mask if ki == 0 else None))

                            def emit_qk(si, st):
                                (qa, qn, kt, mask_ap, mkey, first, last, pre) = st
                                if pre is not None:
                                    pre()
                                sbank = 4 + (si % 2)
                                tk.op("pe", lambda t: t.matmul(
                                    PS[sbank][:, 0:qn], KT[kp:kp + 64, kc_, kt * 128:(kt + 1) * 128],
                                    QT[qp:qp + 64, qc, q0 + qa:q0 + qa + qn], start=True, stop=True),
                                    reads=[("KT", kt)] + [("QT", (q0 + qa) // 128 + z) for z in range(qn // 128)], writes=[psk(sbank)])
                                pbt, pbk = pbuf()
                                tk.op("act", lambda a: a.activation(out=pbt[:, 0:qn], in_=PS[sbank][:, 0:qn], func=AF.Exp),
                                      reads=[psk(sbank)], writes=[pbk])
                                if mask_ap is not None:
                                    tk.op("dve", lambda v: v.tensor_tensor(out=pbt[:, 0:qn], in0=pbt[:, 0:qn], in1=mask_ap, op=ALU.mult),
                                          reads=[pbk, mkey], writes=[pbk])
                                return pbt, pbk

                            def emit_pv(st, pbt, pbk):
                                (qa, qn, kt, mask_ap, mkey, first, last, pre) = st
                                tk.op("pe", lambda t: t.matmul(PS[ob][0:65, qa:qa + qn], VA[:, kt, vh, 0:65], pbt[:, 0:qn], start=first, stop=last),
                                      reads=[("VA", kt), "VA", pbk], writes=[psk(ob)])

                            prev = None
                            for si, st in enumerate(steps):
                                cur = emit_qk(si, st)
                                if prev is not None:
                                    emit_pv(*prev)
                                prev = (st,) + cur
                                if si == min(2, len(steps) - 1):
                                    flush()
                            emit_pv(*prev)

                            def post(h=h, br=br, ob=ob, nblk=nblk, n=n):
                                ot, otk = tmp()
                                tk.op("dve", lambda v: v.tensor_copy(out=ot[0:65, 0:n], in_=PS[ob][0:65, 0:n]), reads=[psk(ob)], writes=[otk])
                                for bl in range(nblk):
                                    tk.op("pe", lambda t, bl=bl: t.transpose(
                                        out=PS[7][:, bl * 128:bl * 128 + 65], in_=ot[0:65, bl * 128:(bl + 1) * 128],
                                        identity=ident_f[0:65, 0:65]), reads=[otk, "ident_f"], writes=[psk(7)])
                                p7 = PS[7][:].rearrange("p (b d) -> p b d", d=128)
                                rd = stat[:, 8:8 + nblk]
                                if mname == "A":
                                    tk.op("dve", lambda v: v.tensor_scalar(out=rd, in0=p7[:, 0:nblk, 64], scalar1=sinkx[:, h:h + 1],
                                                                           scalar2=None, op0=ALU.add), reads=[psk(7), "small"], writes=["stat3"])
                                    tk.op("dve", lambda v: v.reciprocal(out=rd, in_=rd), reads=["stat3"], writes=["stat3"])
                                else:
                                    tk.op("dve", lambda v: v.reciprocal(out=rd, in_=p7[:, 0:nblk, 64]), reads=[psk(7)], writes=["stat3"])
                                if br == 1:
                                    tk.op("dve", lambda v: v.tensor_scalar(out=rd, in0=rd, scalar1=lamt[:, 0:1], scalar2=None, op0=ALU.mult),
                                          reads=["stat3", "small"], writes=["stat3"])
                                for bl in range(nblk):
                                    if br == 0:
                                        tk.op("dve", lambda v, bl=bl: v.tensor_scalar(
                                            out=otok[:, bl, h * 64:(h + 1) * 64], in0=p7[:, bl, 0:64], scalar1=rd[:, bl:bl + 1],
                                            scalar2=None, op0=ALU.mult), reads=[psk(7), "stat3"], writes=[("otok", bl)])
                                    else:
                                        tk.op("dve", lambda v, bl=bl: v.scalar_tensor_tensor(
                                            out=otok[:, bl, h * 64:(h + 1) * 64], in0=p7[:, bl, 0:64], scalar=rd[:, bl:bl + 1],
                                            in1=otok[:, bl, h * 64:(h + 1) * 64], op0=ALU.mult, op1=ALU.add),
                                            reads=[psk(7), "stat3", ("otok", bl)], writes=[("otok", bl)])
                            pending.append(post)

                        def merge(q0=q0, nblk=nblk):
                            for bl in range(nblk):
                                tok = q0 + bl * 128
                                ng = 1 if mname != "C" else 4
                                gsz = 256 // ng
                                sq, sqk = tmp()
                                tk.op("act", lambda a: a.activation(out=sq[:, 0:256], in_=otok[:, bl, :], func=AF.Square),
                                      reads=[("otok", bl)], writes=[sqk])
                                ssg = stat[:, 16:16 + ng]
                                tk.op("dve", lambda v: v.tensor_reduce(out=ssg, in_=sq[:, 0:256].rearrange("p (g d) -> p g d", g=ng),
                                                                       axis=AX.X, op=ALU.add), reads=[sqk], writes=["stat4"])
                                tk.op("act", lambda a: a.activation(out=ssg, in_=ssg, func=AF.Sqrt, bias=EPS, scale=1.0 / gsz),
                                      reads=["stat4"], writes=["stat4"])
                                tk.op("dve", lambda v: v.reciprocal(out=ssg, in_=ssg), reads=["stat4"], writes=["stat4"])
                                if mname == "C":
                                    tk.op("dve", lambda v: v.tensor_scalar(out=ssg, in0=ssg, scalar1=(1.0 - lam_init), scalar2=None,
                                                                           op0=ALU.mult), reads=["stat4"], writes=["stat4"])
                                tk.op("dve", lambda v: v.tensor_tensor(
                                    out=ytok[:].rearrange("p (g d) -> p g d", g=ng), in0=otok[:, bl, :].rearrange("p (g d) -> p g d", g=ng),
                                    in1=ssg.unsqueeze(2).broadcast_to([128, ng, gsz]), op=ALU.mult),
                                    reads=[("otok", bl), "stat4"], writes=["ytok"])
                                for cc in range(2):
                                    tk.op("pe", lambda t, cc=cc: t.transpose(
                                        out=PS[2][:].bitcast(BF16)[:, cc * 128:(cc + 1) * 128],
                                        in_=ytok[:, cc * 128:(cc + 1) * 128], identity=ident_b[:]),
                                        reads=["ytok", "ident_b"], writes=[psk(2)])
                                for cc in range(2):
                                    tk.op("act", lambda a, cc=cc: a.activation(
                                        out=YT[:, cc, tok:tok + 128], in_=PS[2][:].bitcast(BF16)[:, cc * 128:(cc + 1) * 128],
                                        func=AF.Identity, scale=ogT[:, mi * 2 + cc:mi * 2 + cc + 1]),
                                        reads=[psk(2), "small"], writes=[("YT", tok // 128)])
                        pending.append(merge)
                    flush()
                    if dbg == "y" and mname == dbg_mixer[0]:
                        tk.barrier()
                        dump(YT[:, 0, :], 2304, 0, [])
                        dump(YT[:, 1, :], 2304, 2304, [])
                        tk.barrier()
                        return nc

                    tk.mark("b%d l%d %s wout" % (b, l, mname))
                    for (q0, n) in ranges:
                        rr = NB if q0 >= S else b
                        for jo in range(8):
                            bank = jo % 4
                            for kc2 in range(2):
                                tk.op("pe", lambda t, jo=jo, kc2=kc2, bank=bank: t.matmul(
                                    PS[bank][:, 0:n], WO[:, kc2, jo * 128:(jo + 1) * 128], YT[:, kc2, q0:q0 + n],
                                    start=(kc2 == 0), stop=(kc2 == 1)),
                                    reads=["WO"] + [("YT", q0 // 128 + z) for z in range(n // 128)], writes=[psk(bank)],
                                    inc=(kc2 == 1))
                            tk.op("dve", lambda v, jo=jo, bank=bank, rr=rr: v.scalar_tensor_tensor(
                                out=xT[:, jo, q0:q0 + n], in0=PS[bank][:, 0:n], scalar=mT[:, l, 16 + jo, rr:rr + 1],
                                in1=xT[:, jo, q0:q0 + n], op0=ALU.mult, op1=ALU.add),
                                reads=[psk(bank), "mT"] + xkeys(q0, n), writes=xkeys(q0, n))
                    tk.barrier()
                if dbg == "xattn":
                    dump(xT[:, 0, :], 2304, 0, [])
                    dump(xT[:, 5, :], 2304, 2304, [])
                    tk.barrier()
                    return nc

                if dbg == "xattn2":
                    dump(xT[:, 0, :], 2304, 0, [])
                    dump(xT[:, 5, :], 2304, 2304, [])
                    tk.barrier()
                    return nc
                tk.mark("b%d l%d norm2+route" % (b, l))
                ranges = [(r0 * 512, 512) for r0 in range(4)] + ([(S, L)] if need_ctx else [])
                for (q0, n) in ranges:
                    r = 1 if q0 >= S else 0
                    rr = NB if q0 >= S else b
                    norm_to_h(q0, n, A2[:, :, r], mrow(3, rr), router_bank=6)
                    lg, lgk = RB, "rbuf"
                    tk.op("dve", lambda v, lg=lg: v.tensor_copy(out=lg[0:16, 0:n], in_=PS[6][0:16, 0:n]), reads=[psk(6)], writes=[lgk])
                    for bl in range(n // 128):
                        tk.op("pe", lambda t, bl=bl, lg=lg: t.transpose(out=PS[7][:, 0:16], in_=lg[0:16, bl * 128:(bl + 1) * 128],
                                                                       identity=ident_f[0:16, 0:16]),
                              reads=[lgk, "ident_f"], writes=[psk(7)])
                        sc = stat[:, 0:16]
                        sel = stat[:, 16:32]
                        w8 = stat[:, 32:40]
                        tk.op("act", lambda a: a.activation(out=sc, in_=PS[7][:, 0:16], func=AF.Sigmoid), reads=[psk(7)], writes=["stat"])
                        tk.op("dve", lambda v: v.tensor_tensor(out=sel, in0=sc, in1=rb_bc, op=ALU.add), reads=["stat", "small"], writes=["stat"])
                        s4 = sel.rearrange("p (g a c) -> p g a c", g=4, a=2)
                        pq = stat[:, 40:48].rearrange("p (g a) -> p g a", g=4)
                        rs_ = stat[:, 48:56].rearrange("p (g a) -> p g a", g=4)
                        tk.op("dve", lambda v: v.tensor_tensor(out=pq, in0=s4[:, :, :, 0], in1=s4[:, :, :, 1], op=ALU.max), reads=["stat"], writes=["stat"])
                        tk.op("dve", lambda v: v.tensor_tensor(out=rs_, in0=s4[:, :, :, 0], in1=s4[:, :, :, 1], op=ALU.min), reads=["stat"], writes=["stat"])
                        m1 = stat[:, 56:60]
                        m2_ = stat[:, 60:64]
                        tk.op("dve", lambda v: v.tensor_tensor(out=m1, in0=pq[:, :, 0], in1=pq[:, :, 1], op=ALU.max), reads=["stat"], writes=["stat"])
                        tk.op("dve", lambda v: v.tensor_tensor(out=m2_, in0=pq[:, :, 0], in1=pq[:, :, 1], op=ALU.min), reads=["stat"], writes=["stat"])
                        tk.op("dve", lambda v: v.tensor_tensor(out=pq[:, :, 0], in0=rs_[:, :, 0], in1=rs_[:, :, 1], op=ALU.max), reads=["stat"], writes=["stat"])
                        tk.op("dve", lambda v: v.tensor_tensor(out=m2_, in0=m2_, in1=pq[:, :, 0], op=ALU.max), reads=["stat"], writes=["stat"])
                        tk.op("dve", lambda v: v.tensor_tensor(out=m1, in0=m1, in1=m2_, op=ALU.add), reads=["stat"], writes=["stat"])
                        gmx = w8[:, 0:1]
                        tk.op("dve", lambda v: v.tensor_reduce(out=gmx, in_=m1, axis=AX.X, op=ALU.max), reads=["stat"], writes=["stat"])
                        tk.op("dve", lambda v: v.tensor_scalar(out=m2_, in0=m1, scalar1=gmx, scalar2=None, op0=ALU.is_ge), reads=["stat"], writes=["stat"])
                        tk.op("act", lambda a: a.activation(out=m1, in_=m2_, func=AF.Identity, scale=100.0, bias=-100.0),
                              reads=["stat"], writes=["stat"])
                        sel3 = sel.rearrange("p (g c) -> p g c", g=4)
                        tk.op("dve", lambda v: v.tensor_tensor(out=sel3, in0=sel3, in1=m2_.unsqueeze(2).broadcast_to([128, 4, 4]), op=ALU.mult),
                              reads=["stat"], writes=["stat"])
                        tk.op("dve", lambda v: v.tensor_tensor(out=sel3, in0=sel3, in1=m1.unsqueeze(2).broadcast_to([128, 4, 4]), op=ALU.add),
                              reads=["stat"], writes=["stat"])
                        tk.op("dve", lambda v: v.max(out=w8, in_=sel), reads=["stat"], writes=["stat"])
                        tk.op("dve", lambda v: v.tensor_scalar(out=sel, in0=sel, scalar1=w8[:, 1:2], scalar2=None, op0=ALU.is_ge),
                              reads=["stat"], writes=["stat"])
                        tk.op("dve", lambda v: v.tensor_tensor(out=sc, in0=sc, in1=sel, op=ALU.mult), reads=["stat"], writes=["stat"])
                        tk.op("dve", lambda v: v.tensor_reduce(out=gmx, in_=sc, axis=AX.X, op=ALU.add), reads=["stat"], writes=["stat"])
                        tk.op("dve", lambda v: v.reciprocal(out=gmx, in_=gmx), reads=["stat"], writes=["stat"])
                        wtok, wtokk = tmp()
                        tk.op("dve", lambda v, wtok=wtok: v.tensor_scalar(out=wtok[:, 0:16], in0=sc, scalar1=gmx, scalar2=None, op0=ALU.mult),
                              reads=["stat"], writes=[wtokk])
                        tk.op("pe", lambda t, wtok=wtok: t.transpose(out=PS[7][0:16, 128:256], in_=wtok[:, 0:16], identity=ident_f[:]),
                              reads=[wtokk, "ident_f"], writes=[psk(7)])
                        tk.op("act", lambda a, bl=bl: a.copy(out=wrt_hi[:, q0 + bl * 128:q0 + (bl + 1) * 128], in_=PS[7][0:16, 128:256]),
                              reads=[psk(7)], writes=[("wrt", (q0 // 128) + bl)])
                        tk.op("dve", lambda v, bl=bl: v.tensor_tensor(
                            out=wrt_lo[:, q0 + bl * 128:q0 + (bl + 1) * 128], in0=PS[7][0:16, 128:256],
                            in1=wrt_hi[:, q0 + bl * 128:q0 + (bl + 1) * 128], op=ALU.subtract),
                            reads=[psk(7), ("wrt", (q0 // 128) + bl)], writes=[("wrtl", (q0 // 128) + bl)])
                if dbg == "route":
                    tk.barrier()
                    dump(wrt_hi[:, :], 2304, 0, [])
                    dump(hT[:, 0, 0:2304], 2304, 2304, [])
                    tk.barrier()
                    return nc
                tk.barrier()

                tk.mark("b%d l%d experts" % (b, l))
                items = [(e, q0, n) for e in range(NE) for (q0, n) in ranges]

                def ew_views(e):
                    ew = EW[e % 2]
                    return (ew[:, 0:4096].rearrange("p (j f) -> p j f", j=8), ew[:, 4096:8192].rearrange("p (j f) -> p j f", j=8),
                            ew[:, 8192:12288].rearrange("p (k c) -> p k c", k=4), ("EW", e % 2))

                def hm_buf(idx, fc):
                    if idx % 2 == 0:
                        return HM[:, fc, :]
                    return [PB[0], PB[1], PB[2], TK_][fc]

                def load_expert(e):
                    WG, WU, WD, ewk = ew_views(e)
                    tk.dma("pool", lambda q: q.dma_start(out=WG, in_=wg_d[l, e].rearrange("(j p) f -> p j f", p=128)), writes=[ewk])
                    tk.dma("pool", lambda q: q.dma_start(out=WU, in_=wu_d[l, e].rearrange("(j p) f -> p j f", p=128)), writes=[ewk])
                    tk.dma("pool", lambda q: q.dma_start(out=WD, in_=wd_d[l, e].rearrange("(k p) c -> p k c", p=128)), writes=[ewk])

                def emit_bc(idx):
                    e, q0, n = items[idx]
                    bcb = 0 if idx % 2 == 0 else 7
                    wk = [("wrt", q0 // 128 + z) for z in range(n // 128)] + [("wrtl", q0 // 128 + z) for z in range(n // 128)]
                    tk.op("pe", lambda t: t.matmul(PS[bcb][:, 0:n], selb[0:16, e, :], wrt_hi[:, q0:q0 + n], start=True, stop=False),
                          reads=wk + ["selb"], writes=[psk(bcb)])
                    tk.op("pe", lambda t: t.matmul(PS[bcb][:, 0:n], selb[0:16, e, :], wrt_lo[:, q0:q0 + n], start=False, stop=True),
                          reads=wk + ["selb"], writes=[psk(bcb)])

                def emit_gu(idx, fc):
                    e, q0, n = items[idx]
                    bcb = 0 if idx % 2 == 0 else 7
                    WG, WU, WD, ewk = ew_views(e)
                    hk = hkeys(q0, n)
                    gb = 1 + (fc % 2)
                    ub = 3 + (fc % 2)
                    for j in range(8):
                        tk.op("pe", lambda t, j=j: t.matmul(PS[gb][:, 0:n], WG[:, j, fc * 128:(fc + 1) * 128], hT[:, j, q0:q0 + n],
                                                             start=(j == 0), stop=(j == 7)), reads=[ewk] + hk, writes=[psk(gb)])
                    for j in range(8):
                        tk.op("pe", lambda t, j=j: t.matmul(PS[ub][:, 0:n], WU[:, j, fc * 128:(fc + 1) * 128], hT[:, j, q0:q0 + n],
                                                             start=(j == 0), stop=(j == 7)), reads=[ewk] + hk, writes=[psk(ub)])
                    sg, sgk = tmp()
                    tk.op("act", lambda a: a.activation(out=sg[:, 0:n], in_=PS[gb][:, 0:n], func=AF.Silu), reads=[psk(gb)], writes=[sgk])
                    tk.op("dve", lambda v: v.tensor_tensor(out=sg[:, 0:n], in0=sg[:, 0:n], in1=PS[ub][:, 0:n], op=ALU.mult),
                          reads=[sgk, psk(ub)], writes=[sgk])
                    tk.op("dve", lambda v: v.tensor_tensor(out=hm_buf(idx, fc)[:, 0:n], in0=sg[:, 0:n], in1=PS[bcb][:, 0:n], op=ALU.mult),
                          reads=[sgk, psk(bcb)], writes=[("HM", idx % 2, fc)])

                def emit_down(idx):
                    e, q0, n = items[idx]
                    rr = NB if q0 >= S else b
                    WG, WU, WD, ewk = ew_views(e)
                    for jo in range(8):
                        yb = 5 + (jo % 2)
                        for fc in range(4):
                            tk.op("pe", lambda t, fc=fc: t.matmul(PS[yb][:, 0:n], WD[:, fc, jo * 128:(jo + 1) * 128], hm_buf(idx, fc)[:, 0:n],
                                                                   start=(fc == 0), stop=(fc == 3)), reads=[ewk, ("HM", idx % 2, fc)], writes=[psk(yb)])
                        tk.op("dve", lambda v: v.scalar_tensor_tensor(
                            out=xT[:, jo, q0:q0 + n], in0=PS[yb][:, 0:n], scalar=mT[:, l, 40 + jo, rr:rr + 1],
                            in1=xT[:, jo, q0:q0 + n], op0=ALU.mult, op1=ALU.add),
                            reads=[psk(yb), "mT"] + xkeys(q0, n), writes=xkeys(q0, n))

                load_expert(0)
                emit_bc(0)
                emit_gu(0, 0)
                for idx in range(len(items)):
                    e, q0, n = items[idx]
                    if (q0, n) == ranges[0] and e + 1 < NE:
                        load_expert(e + 1)
                    for fc in range(1, 4):
                        emit_gu(idx, fc)
                    if idx + 1 < len(items):
                        emit_bc(idx + 1)
                        emit_gu(idx + 1, 0)
                    emit_down(idx)
                tk.barrier()
                if dbg == "x2":
                    dump(xT[:, 0, :], 2304, 0, [])
                    dump(xT[:, 5, :], 2304, 2304, [])
                    tk.barrier()
                    return nc

            tk.mark("b%d store" % b)
            for tt in range(16):
                for g in range(2):
                    bank = 6 + g
                    for jj in range(4):
                        j = g * 4 + jj
                        tk.op("pe", lambda t, j=j, jj=jj, bank=bank: t.transpose(
                            out=PS[bank][:, jj * 128:(jj + 1) * 128], in_=xT[:, j, tt * 128:(tt + 1) * 128], identity=ident_f[:]),
                            reads=[("xT", tt), "ident_f"], writes=[psk(bank)], inc=(jj == 3))
                    if g == 0:
                        tk.op("act", lambda a, bank=bank: a.copy(out=XS[:, 0:512], in_=PS[bank][:]), reads=[psk(bank)], writes=["xs"])
                    else:
                        tk.op("dve", lambda v, bank=bank: v.tensor_copy(out=XS[:, 512:1024], in_=PS[bank][:]), reads=[psk(bank)], writes=["xs"])
                tk.dma("sp", lambda q: q.dma_start(out=out_d[b, tt * 128:(tt + 1) * 128, :], in_=XS[:]), reads=["xs"], writes=["out"])
            tk.barrier()
          except StopBuild:
            print("STOPPED at", tk.nops)
            tk.pe_open = False
            tk.limit = 0
            tk.barrier()
            if dbg_d is not None:
                dump(small[:, :], 512, 0, [])
                tk.barrier()
            return nc
        tk.barrier()
        tk.mark("end")
        LAST_MARKS[:] = tk.marks
    return nc


LAST_MARKS = []
dbg_mixer = ["D"]

_W_NAMES = ["ada_w", "ada_b", "norm_mix_g", "norm_ffn_g", "w_in", "qk_g_win", "qk_g_na", "qk_g_diff", "qk_g_gqa",
            "sink_win", "lambda_diff", "out_gain", "w_out", "w_gate", "w_up", "w_down"]


def make_in_maps(inputs, n_cores, NB, DEPTH):
    f = lambda a: np.ascontiguousarray(np.asarray(a, dtype=np.float32))
    shared = {k: f(inputs[k])[:DEPTH] for k in _W_NAMES}
    shared["router_w"] = f(inputs["router_w"])
    shared["router_b"] = f(inputs["router_b"]).reshape(1, NE)
    shared["na_bias"] = host_na_bias(f(inputs["rpb_na"])[:DEPTH])
    shared.update(host_consts())
    x = f(inputs["x"])
    c = f(inputs["c"])
    ctx = f(inputs["ctx"])
    cctx = f(inputs["c_ctx"]).reshape(1, D)
    maps = []
    for i in range(n_cores):
        m = dict(shared)
        m["x"] = x[i * NB:(i + 1) * NB]
        m["ctx"] = ctx[i * NB:(i + 1) * NB]
        m["c"] = np.ascontiguousarray(np.concatenate([c[i * NB:(i + 1) * NB], cctx], 0))
        maps.append(m)
    return maps


def kernel(**inputs):
    NB = inputs["x"].shape[0] // N_CORES
    nc = build(NB, DEPTH_FULL)
    maps = make_in_maps(inputs, N_CORES, NB, DEPTH_FULL)
    res = run_bass_kernel_spmd(nc, maps, core_ids=list(range(N_CORES)))
    return np.concatenate([r["out"] for r in res.results], axis=0).astype(np.float32)
```

```python
import math
import os
from contextlib import ExitStack
import numpy as np
import concourse.bass as bass
import concourse.mybir as mybir
from concourse.bass_utils import run_bass_kernel_spmd

F32 = mybir.dt.float32
BF16 = mybir.dt.bfloat16
AF = mybir.ActivationFunctionType
ALU = mybir.AluOpType
AX = mybir.AxisListType

D = 1024
S = 2048
L = 256
T = S + L
NTT = T // 128
DEPTH_FULL = 4
EPS = 1e-6
NE = 16
DE = 512
N_CORES = 8
NDS = 40
MIX = [("A", 0, 256, 128, 128, 2, 64), ("B", 512, 256, 256, 256, 4, 64),
       ("C", 1280, 256, 256, 256, 4, 32), ("D", 2048, 256, 128, 128, 2, 64)]


class StopBuild(Exception):
    pass


class TK:
    def __init__(self, nc, es):
        self.nc = nc
        self.eng = {"pe": nc.tensor, "act": nc.scalar, "dve": nc.vector, "pool": nc.gpsimd, "sp": nc.sync}
        self.sem = {k: es.enter_context(nc.semaphore("s_" + k)) for k in self.eng}
        self.cnt = {k: 0 for k in self.eng}
        self.seen = {k: {} for k in self.eng}
        self.dsem = [es.enter_context(nc.semaphore("d%d" % i)) for i in range(NDS)]
        self.dval = [0] * NDS
        self.dnext = 0
        self.bw = {}
        self.br = {}
        self.pe_open = False
        self.marks = []
        self.nops = 0
        self.limit = int(os.environ.get("TK_LIMIT", "0"))

    def _tick(self):
        self.nops += 1
        if self.limit and self.nops > self.limit:
            raise StopBuild()

    def _semh(self, sk):
        return self.sem[sk] if isinstance(sk, str) else self.dsem[sk[1]]

    def _need(self, e, reads, writes):
        need = {}

        def add(ev, raw):
            if ev is None:
                return
            sk, v = ev
            if sk == e and (not raw or e == "pe"):
                return
            if need.get(sk, 0) < v:
                need[sk] = v

        for k in reads:
            add(self.bw.get(k), True)
        for k in writes:
            add(self.bw.get(k), False)
            r = self.br.get(k)
            if r:
                for sk, v in r.items():
                    add((sk, v), False)
        return need

    def _emit_waits(self, e, need):
        for sk, v in need.items():
            if self.seen[e].get(sk, 0) >= v:
                continue
            self.eng[e].wait_ge(self._semh(sk), v)
            self.seen[e][sk] = v

    def _record(self, ev, reads, writes):
        sk, v = ev
        for k in reads:
            d = self.br.setdefault(k, {})
            if d.get(sk, 0) < v:
                d[sk] = v
        for k in writes:
            self.bw[k] = ev
            self.br[k] = {}

    def op(self, e, fn, reads=(), writes=(), inc=True):
        inc = True
        self._tick()
        need = self._need(e, reads, writes)
        self._emit_waits(e, need)
        ins = fn(self.eng[e])
        if inc:
            self.cnt[e] += 1
            ins.then_inc(self.sem[e], 1)
            ev = (e, self.cnt[e])
            if e == "pe":
                self.pe_open = False
        else:
            ev = (e, self.cnt[e] + 1)
            if e == "pe":
                self.pe_open = True
        self._record(ev, reads, writes)

    def dma(self, q, fn, reads=(), writes=()):
        self._tick()
        need = self._need(q, reads, writes)
        i = self.dnext
        self.dnext = (i + 1) % NDS
        if self.dval[i] > 0:
            sk = ("d", i)
            if need.get(sk, 0) < self.dval[i]:
                need[sk] = self.dval[i]
        self._emit_waits(q, need)
        ins = fn(self.eng[q])
        self.dval[i] += 16
        ins.then_inc(self.dsem[i], 16)
        self._record((("d", i), self.dval[i]), reads, writes)

    def mark(self, name):
        self.marks.append((name, dict(self.cnt)))

    def barrier(self):
        assert not self.pe_open
        for e in self.eng:
            need = {f: self.cnt[f] for f in self.eng if f != e and self.cnt[f] > 0}
            for i in range(NDS):
                if self.dval[i] > 0:
                    need[("d", i)] = self.dval[i]
            self._emit_waits(e, need)
        self.bw = {}
        self.br = {}


def host_consts():
    pos = np.arange(S)
    row, col = pos // 64, pos % 64

    def tabs(hd):
        e = hd // 4
        inv = 10000.0 ** (-np.arange(e, dtype=np.float32) / e)
        ar = row[:, None].astype(np.float32) * inv
        ac = col[:, None].astype(np.float32) * inv
        cos = np.concatenate([np.cos(ar), np.cos(ar), np.cos(ac), np.cos(ac)], 1)
        sins = np.concatenate([-np.sin(ar), np.sin(ar), -np.sin(ac), np.sin(ac)], 1)
        return cos.astype(np.float32), sins.astype(np.float32)

    c64, s64 = tabs(64)
    c32, s32 = tabs(32)
    rope = np.concatenate([c64, s64, c32, s32], 1)
    b = np.arange(128)[:, None]
    a = np.arange(128)[None, :]
    wmask = np.stack([(a <= b), (b <= a)], 1).astype(np.float32)
    return {"ident": np.eye(128, dtype=np.float32), "rope": np.ascontiguousarray(rope),
            "wmask": np.ascontiguousarray(wmask.reshape(128, 256))}


def na_variant(i):
    return {0: 0, 1: 1, 14: 3, 15: 4}.get(i, 2)


def na_ts(i):
    return min(max(i - 2, 0), 11)


def host_na_bias(rpb):
    dl = rpb.shape[0]
    out = np.empty((dl, 4, 5, 128, 640), np.float32)
    kl = np.arange(128) // 64
    kc = np.arange(128) % 64
    ql = np.arange(128) // 64
    qc = np.arange(128) % 64
    for vi, i in enumerate([0, 1, 5, 14, 15]):
        ts = na_ts(i)
        for j in range(5):
            krow = 2 * (ts + j) + kl[:, None]
            qrow = 2 * i + ql[None, :]
            rs = np.clip(qrow - 4, 0, 24)
            rowok = (krow >= rs) & (krow < rs + 8)
            cs = np.clip(qc[None, :] - 8, 0, 48)
            colok = (kc[:, None] >= cs) & (kc[:, None] < cs + 16)
            dr = np.clip(krow - qrow + 7, 0, 14)
            dc = np.clip(kc[:, None] - qc[None, :] + 15, 0, 30)
            ok = rowok & colok
            g = rpb[:, :, dr, dc]
            out[:, :, vi, :, j * 128:(j + 1) * 128] = np.where(ok[None, None], g, np.float32(-30000.0))
    return out


def build(NB, DEPTH, dbg=None):
    nc = bass.Bass("TRN2", target_bir_lowering=False)

    def dram(name, shape, kind="ExternalInput", dtype=F32):
        return nc.dram_tensor(name, list(shape), dtype, kind=kind).ap()

    x_d = dram("x", [NB, S, D])
    c_d = dram("c", [NB + 1, D])
    ctx_d = dram("ctx", [NB, L, D])
    ada_w = dram("ada_w", [DEPTH, D, 6 * D])
    ada_b = dram("ada_b", [DEPTH, 6 * D])
    ng1 = dram("norm_mix_g", [DEPTH, D])
    ng2 = dram("norm_ffn_g", [DEPTH, D])
    w_in = dram("w_in", [DEPTH, D, 2560])
    qkg = {"A": dram("qk_g_win", [DEPTH, 2, 64]), "B": dram("qk_g_na", [DEPTH, 2, 64]),
           "C": dram("qk_g_diff", [DEPTH, 2, 32]), "D": dram("qk_g_gqa", [DEPTH, 2, 64])}
    sink_d = dram("sink_win", [DEPTH, 4])
    nab_d = dram("na_bias", [DEPTH, 4, 5, 128, 640])
    lam_d = dram("lambda_diff", [DEPTH, 4, 32])
    og_d = dram("out_gain", [DEPTH, D])
    w_out = dram("w_out", [DEPTH, D, D])
    rw_d = dram("router_w", [D, NE])
    rb_d = dram("router_b", [1, NE])
    wg_d = dram("w_gate", [DEPTH, NE, D, DE])
    wu_d = dram("w_up", [DEPTH, NE, D, DE])
    wd_d = dram("w_down", [DEPTH, NE, DE, D])
    ident_d = dram("ident", [128, 128])
    rope_d = dram("rope", [S, 192])
    wmask_d = dram("wmask", [128, 256])
    out_d = dram("out", [NB, S, D], kind="ExternalOutput")
    dbg_d = dram("dbg", [128, 8192], kind="ExternalOutput") if dbg else None

    es = ExitStack()
    with es:
        nc_ctx = es.enter_context(nc.allow_non_contiguous_dma(reason="small strided parameter loads"))
        tk = TK(nc, es)
        build_info = {}

        def sb(name, shape, dtype=F32):
            return es.enter_context(nc.sbuf_tensor(name, list(shape), dtype))

        xT = sb("xT", [128, 8, T])
        hT = sb("hT", [128, 8, T], BF16)
        arena = sb("arena", [128, 32768], BF16)
        ident_f = sb("ident_fs", [128, 128])
        ident_b = sb("ident_bs", [128, 128], BF16)
        ones_b = sb("ones_b", [128, 128], BF16)
        selb = sb("selb", [16, NE, 128], BF16)
        rw_hi = sb("rw_hi", [128, 8, NE], BF16)
        rw_lo = sb("rw_lo", [128, 8, NE], BF16)
        wmask = sb("wmask_sb", [128, 256], BF16)
        mT = sb("mT", [128, DEPTH, 48, NB + 1])
        sT = sb("sT", [128, 8, NB + 1])
        TMP = [sb("tmp%d" % i, [128, 512]) for i in range(5)]
        RB = sb("rbuf", [128, 512])
        XS = arena[:, 0:2048].bitcast(F32)
        PB = [sb("pb%d" % i, [128, 512], BF16) for i in range(3)]
        ropet = sb("ropet", [128, 192])
        small = sb("small", [128, 512])
        TQ = sb("tq", [128, 256], BF16)
        TK_ = sb("tkp", [128, 512], BF16)
        otok = sb("otok", [128, 4, 256])
        ytok = sb("ytok", [128, 256], BF16)
        nabm = sb("nabm", [128, 640], BF16)
        stat = sb("stat", [128, 64])
        PS = [es.enter_context(nc.psum_tensor("ps%d" % i, [128, 512], F32)) for i in range(8)]

        pass

        def psk(i):
            return ("ps", i)

        tmp_i = [0]

        def tmp():
            i = tmp_i[0]
            tmp_i[0] = (i + 1) % len(TMP)
            return TMP[i], ("tmp", i)

        pb_i = [0]

        def pbuf():
            i = pb_i[0]
            pb_i[0] = (i + 1) % len(PB)
            return PB[i], ("pb", i)

        A1 = small[:, 0:16].rearrange("p (j r) -> p j r", r=2)
        A2 = small[:, 16:32].rearrange("p (j r) -> p j r", r=2)
        g1T = small[:, 32:40]
        g2T = small[:, 40:48]
        ogT = small[:, 48:56]
        abT = small[:, 56:104]
        rb_bc = small[:, 104:120]
        sinkx = small[:, 120:124]
        lamt = small[:, 124:128]
        gq = small[:, 128:192]
        gk = small[:, 192:256]
        rwT = small[:, 256:384].rearrange("p (j e) -> p j e", e=NE)
        lamraw = small[:, 384:512]

        tk.dma("sp", lambda q: q.dma_start(out=ident_f[:], in_=ident_d[:, :]), writes=["ident_f"])
        tk.op("dve", lambda v: v.tensor_copy(out=ident_b[:], in_=ident_f[:]), reads=["ident_f"], writes=["ident_b"])
        tk.op("dve", lambda v: v.memset(ones_b[:], 1.0), writes=["ones_b"])
        tk.dma("sp", lambda q: q.dma_start(out=TMP[0][:, 0:256], in_=wmask_d[:, :]), writes=[("tmp", 0)])
        tk.op("dve", lambda v: v.tensor_copy(out=wmask[:], in_=TMP[0][:, 0:256]), reads=[("tmp", 0)], writes=["wmask"])
        tk.dma("sp", lambda q: q.dma_start(out=rb_bc, in_=rb_d[0:1, :].partition_broadcast(128)), writes=["small"])
        tk.dma("sp", lambda q: q.dma_start(out=rwT, in_=rw_d.rearrange("(j p) e -> p j e", p=128)), writes=["small"])
        tk.op("dve", lambda v: v.tensor_copy(out=rw_hi[:], in_=rwT), reads=["small"], writes=["rw"])
        tk.op("dve", lambda v: v.tensor_tensor(out=rw_lo[:], in0=rwT, in1=rw_hi[:], op=ALU.subtract), reads=["small", "rw"], writes=["rw2"])
        tk.op("dve", lambda v: v.tensor_copy(out=selb[:], in_=ident_b[0:16, 0:16].unsqueeze(2).broadcast_to([16, NE, 128])),
              reads=["ident_b"], writes=["selb"])
        tk.op("dve", lambda v: v.memset(TK_[:], 0.0), writes=["tkp0"])
        tk.op("dve", lambda v: v.memset(arena[:], 0.0), writes=["arena"])

        for r in range(NB + 1):
            tk.dma("sp", lambda q, r=r: q.dma_start(out=sT[:, :, r], in_=c_d[r].rearrange("(j p) -> p j", p=128)),
                   writes=["sT"])
        tk.op("act", lambda a: a.activation(out=sT[:], in_=sT[:], func=AF.Silu), reads=["sT"], writes=["sT"])
        for l in range(DEPTH):
            for j6 in range(6):
                tk.dma("sp", lambda q, l=l, j6=j6: q.dma_start(
                    out=abT[:, j6 * 8:(j6 + 1) * 8],
                    in_=ada_b[l, j6 * 1024:(j6 + 1) * 1024].rearrange("(j p) -> p j", p=128)), writes=["abT"])
            psm = PS[0][:, 0:48 * (NB + 1)].rearrange("p (j r) -> p j r", r=NB + 1)
            for j in range(48):
                wt, wk = tmp()
                wt2, wk2 = tmp()
                tk.dma("sp", lambda q, l=l, j=j, wt=wt: q.dma_start(
                    out=wt[:].rearrange("p (k c) -> p k c", c=128),
                    in_=ada_w[l, 0:512, j * 128:(j + 1) * 128].rearrange("(k p) c -> p k c", p=128)), writes=[wk])
                tk.dma("sp", lambda q, l=l, j=j, wt2=wt2: q.dma_start(
                    out=wt2[:].rearrange("p (k c) -> p k c", c=128),
                    in_=ada_w[l, 512:1024, j * 128:(j + 1) * 128].rearrange("(k p) c -> p k c", p=128)), writes=[wk2])
                for kc in range(8):
                    src, sk_ = (wt, wk) if kc < 4 else (wt2, wk2)
                    tk.op("pe", lambda t, j=j, kc=kc, src=src: t.matmul(
                        psm[:, j, :], src[:, (kc % 4) * 128:(kc % 4 + 1) * 128], sT[:, kc, :],
                        start=(kc == 0), stop=(kc == 7)),
                        reads=[sk_, "sT"], writes=[psk(0)], inc=(kc == 7))
            tk.op("dve", lambda v, l=l: v.tensor_tensor(
                out=mT[:, l], in0=psm, in1=abT.unsqueeze(2).broadcast_to([128, 48, NB + 1]), op=ALU.add),
                reads=[psk(0), "abT"], writes=["mT"])

        def load_tokens_T(src_ap, tok0):
            tk.dma("sp", lambda q: q.dma_start(out=XS[:], in_=src_ap), writes=["xs"])
            for g in range(2):
                bank = 6 + g
                for jj in range(4):
                    j = g * 4 + jj
                    tk.op("pe", lambda t, j=j, jj=jj, bank=bank: t.transpose(
                        out=PS[bank][:, jj * 128:(jj + 1) * 128], in_=XS[:, j * 128:(j + 1) * 128],
                        identity=ident_f[:]), reads=["xs", "ident_f"], writes=[psk(bank)], inc=(jj == 3))
                eng = "act" if g == 0 else "dve"
                if eng == "act":
                    tk.op("act", lambda a, g=g, bank=bank: a.copy(
                        out=xT[:, g * 4:(g + 1) * 4, tok0:tok0 + 128],
                        in_=PS[bank][:].rearrange("p (j t) -> p j t", t=128)),
                        reads=[psk(bank)], writes=[("xT", tok0 // 128)])
                else:
                    tk.op("dve", lambda v, g=g, bank=bank: v.tensor_copy(
                        out=xT[:, g * 4:(g + 1) * 4, tok0:tok0 + 128],
                        in_=PS[bank][:].rearrange("p (j t) -> p j t", t=128)),
                        reads=[psk(bank)], writes=[("xT", tok0 // 128)])

        def xkeys(t0, n):
            return [("xT", i) for i in range(t0 // 128, (t0 + n) // 128)]

        def hkeys(t0, n):
            return [("hT", i) for i in range(t0 // 128, (t0 + n) // 128)]

        def norm_to_h(t0, n, Aap, Bap, router_bank=None):
            xk = xkeys(t0, n)
            hk = hkeys(t0, n)
            for j in range(8):
                sq, sqk = pbuf()
                tk.op("act", lambda a, j=j, sq=sq: a.activation(out=sq[:, :n], in_=xT[:, j, t0:t0 + n], func=AF.Square),
                      reads=xk, writes=[sqk])
                tk.op("pe", lambda t, j=j, sq=sq: t.matmul(PS[5][:, :n], ones_b[:], sq[:, :n], start=(j == 0), stop=(j == 7)),
                      reads=[sqk, "ones_b"], writes=[psk(5)], inc=(j == 7))
            rb, rbk = RB, "rbuf"
            tk.op("act", lambda a: a.activation(out=rb[:, :n], in_=PS[5][:, :n], func=AF.Sqrt, bias=EPS, scale=1.0 / D),
                  reads=[psk(5)], writes=[rbk])
            tk.op("dve", lambda v: v.reciprocal(out=rb[:, :n], in_=rb[:, :n]), reads=[rbk], writes=[rbk])
            for j in range(8):
                t1, t1k = tmp()
                tk.op("dve", lambda v, j=j, t1=t1: v.tensor_tensor(out=t1[:, :n], in0=xT[:, j, t0:t0 + n], in1=rb[:, :n],
                                                                    op=ALU.mult), reads=xk + [rbk], writes=[t1k])
                if router_bank is None:
                    tk.op("act", lambda a, j=j, t1=t1: a.activation(
                        out=hT[:, j, t0:t0 + n], in_=t1[:, :n], func=AF.Identity, scale=Aap[:, j:j + 1], bias=Bap[:, j:j + 1]),
                        reads=[t1k, "small", "mT"], writes=hk)
                else:
                    tk.op("act", lambda a, j=j, t1=t1: a.activation(
                        out=t1[:, :n], in_=t1[:, :n], func=AF.Identity, scale=Aap[:, j:j + 1], bias=Bap[:, j:j + 1]),
                        reads=[t1k, "small", "mT"], writes=[t1k])
                    tk.op("pool", lambda g, j=j, t1=t1: g.tensor_copy(out=hT[:, j, t0:t0 + n], in_=t1[:, :n]),
                          reads=[t1k], writes=hk)
                    lo, lok = pbuf()
                    tk.op("dve", lambda v, j=j, t1=t1, lo=lo: v.tensor_tensor(out=lo[:, :n], in0=t1[:, :n], in1=hT[:, j, t0:t0 + n],
                                                                              op=ALU.subtract), reads=[t1k] + hk, writes=[lok])
                    tk.op("pe", lambda t, j=j: t.matmul(PS[router_bank][0:16, :n], rw_hi[:, j, :], hT[:, j, t0:t0 + n],
                                                        start=(j == 0), stop=False),
                          reads=hk + ["rw"], writes=[psk(router_bank)], inc=False)
                    tk.op("pe", lambda t, j=j: t.matmul(PS[router_bank][0:16, :n], rw_lo[:, j, :], hT[:, j, t0:t0 + n],
                                                        start=False, stop=False),
                          reads=hk + ["rw2"], writes=[psk(router_bank)], inc=False)
                    tk.op("pe", lambda t, j=j, lo=lo: t.matmul(PS[router_bank][0:16, :n], rw_hi[:, j, :], lo[:, :n],
                                                               start=False, stop=(j == 7)),
                          reads=[lok, "rw"], writes=[psk(router_bank)], inc=(j == 7))

        def dump(ap, width, col0, keys):
            if dbg_d is None:
                return
            tk.dma("pool", lambda q: q.dma_start(out=dbg_d[0:ap.shape[0], col0:col0 + width], in_=ap), reads=keys)

        QT = arena[:, 0:4608].rearrange("p (c t) -> p c t", c=2)
        KT = arena[:, 4608:13824].rearrange("p (c t) -> p c t", c=4)
        VA = arena[:, 13824:18576].rearrange("p (t h d) -> p t h d", t=NTT, h=4)
        YT = arena[:, 18576:23184].rearrange("p (c t) -> p c t", c=2)
        WI = arena[:, 23184:29328].rearrange("p (j c) -> p j c", j=8)
        WO = arena[:, 29328:31376].rearrange("p (k c) -> p k c", k=2)
        EW = [arena[:, i * 12288:(i + 1) * 12288] for i in range(2)]
        HM = arena[:, 24576:26624].rearrange("p (f t) -> p f t", f=4)
        WT = arena[0:16, 26624:31232].bitcast(F32) if False else None

        wrt_hi = arena[0:16, 26624:26624 + T]
        wrt_lo = arena[0:16, 26624 + T:26624 + 2 * T]

        def qk_post(ps_ap, nh, hd, gain_ap, rope_tile, out_views, is_lat, bank, okey, slot, cs):
            width = nh * hd
            Wb, Wk = TMP[2 * cs], ("tmp", 2 * cs)
            Yb, Yk = TMP[2 * cs + 1], ("tmp", 2 * cs + 1)
            sq = Wb[:, 0:width]
            X = Wb[:, 256:256 + width]
            Y = Yb[:, 0:width]
            tk.op("act", lambda a: a.activation(out=sq, in_=ps_ap, func=AF.Square), reads=[psk(bank)], writes=[Wk])
            yield
            sc0 = [0, 40, 48, 56][slot]
            stk = "statq%d" % slot
            ss = stat[:, sc0:sc0 + nh]
            tk.op("dve", lambda v: v.tensor_reduce(out=ss, in_=sq.rearrange("p (h d) -> p h d", h=nh),
                                                   axis=AX.X, op=ALU.add), reads=[Wk], writes=[stk])
            yield
            tk.op("act", lambda a: a.activation(out=ss, in_=ss, func=AF.Sqrt, bias=EPS, scale=1.0 / hd),
                  reads=[stk], writes=[stk])
            yield
            tk.op("dve", lambda v: v.reciprocal(out=ss, in_=ss), reads=[stk], writes=[stk])
            yield
            tk.op("dve", lambda v: v.tensor_tensor(
                out=X.rearrange("p (h d) -> p h d", h=nh), in0=ps_ap.rearrange("p (h d) -> p h d", h=nh),
                in1=ss.unsqueeze(2).broadcast_to([128, nh, hd]), op=ALU.mult),
                reads=[psk(bank), stk], writes=[Wk])
            yield
            tk.op("pool", lambda g: g.tensor_tensor(
                out=X.rearrange("p (h d) -> p h d", h=nh), in0=X.rearrange("p (h d) -> p h d", h=nh),
                in1=gain_ap.unsqueeze(1).broadcast_to([128, nh, hd]), op=ALU.mult),
                reads=[Wk, "small"], writes=[Wk])
            yield
            if not is_lat:
                for (oap, sel) in out_views:
                    src = X if sel is None else sel(X)
                    tk.op("dve", lambda v, oap=oap, src=src: v.tensor_copy(out=oap, in_=src), reads=[Wk], writes=[okey])
                    yield
                return
            cos_ap, sin_ap = rope_tile
            e4 = hd // 4
            tk.op("pool", lambda g: g.tensor_tensor(
                out=Y.rearrange("p (h d) -> p h d", h=nh), in0=X.rearrange("p (h d) -> p h d", h=nh),
                in1=cos_ap.unsqueeze(1).broadcast_to([128, nh, hd]), op=ALU.mult), reads=[Wk, "ropet"], writes=[Yk])
            yield
            qv = X.rearrange("p (h b s e) -> p h b s e", h=nh, b=2, s=2)
            tv = sq.rearrange("p (h b s e) -> p h b s e", h=nh, b=2, s=2)
            sv = sin_ap.rearrange("p (b s e) -> p b s e", b=2, s=2)
            for s_ in range(2):
                tk.op("dve", lambda v, s_=s_: v.tensor_tensor(
                    out=tv[:, :, :, s_, :], in0=qv[:, :, :, 1 - s_, :],
                    in1=sv[:, :, s_, :].unsqueeze(1).broadcast_to([128, nh, 2, e4]), op=ALU.mult),
                    reads=[Wk, "ropet"], writes=[Wk])
                yield
            for (oap, sel) in out_views:
                a_ = Y if sel is None else sel(Y)
                b_ = sq if sel is None else sel(sq)
                tk.op("dve", lambda v, oap=oap, a_=a_, b_=b_: v.tensor_tensor(out=oap, in0=a_, in1=b_, op=ALU.add),
                      reads=[Wk, Yk], writes=[okey])
                yield

        def run_zipped(gens):
            gens = list(gens)
            while gens:
                for g_ in list(gens):
                    try:
                        next(g_)
                    except StopIteration:
                        gens.remove(g_)

        ps_bank = [0]
        out_key = ["tq"]
        stat_slot = [0]
        unit_ctr = [0]
        nabm_state = [None]
        TQb = [TQ[:, :], arena[:, 31376:31632]]
        TKb = [TK_[:, :], arena[:, 31632:32144]]

        if dbg == "p0":
            tk.barrier()
            dump(mT[:, 0].rearrange("p j r -> p (j r)"), 48 * (NB + 1), 0, [])
            tk.barrier()
            return nc
        for b in range(NB):
          try:
            for tt in range(16):
                load_tokens_T(x_d[b, tt * 128:(tt + 1) * 128, :], tt * 128)
            for tt in range(2):
                load_tokens_T(ctx_d[b, tt * 128:(tt + 1) * 128, :], S + tt * 128)
            tk.barrier()
            if dbg == "ld":
                dump(xT[:, 0, :], 2304, 0, [])
                dump(xT[:, 7, :], 2304, 2304, [])
                tk.barrier()
                return nc

            for l in range(DEPTH):
                pass
                need_ctx = l < DEPTH - 1
                lam_init = 0.8 - 0.6 * math.exp(-0.3 * l)
                mrow = lambda k, r: mT[:, l, k * 8:(k + 1) * 8, r]
                tk.dma("sp", lambda q: q.dma_start(out=g1T, in_=ng1[l].rearrange("(j p) -> p j", p=128)), writes=["small"])
                tk.dma("sp", lambda q: q.dma_start(out=g2T, in_=ng2[l].rearrange("(j p) -> p j", p=128)), writes=["small"])
                tk.dma("sp", lambda q: q.dma_start(out=ogT, in_=og_d[l].rearrange("(j p) -> p j", p=128)), writes=["small"])
                tk.dma("sp", lambda q: q.dma_start(out=sinkx, in_=sink_d[l:l + 1, :].partition_broadcast(128)), writes=["small"])
                tk.dma("sp", lambda q: q.dma_start(
                    out=lamraw, in_=lam_d[l:l + 1].rearrange("o a d -> o (a d)").partition_broadcast(128)), writes=["small"])
                for r in range(2):
                    rr = b if r == 0 else NB
                    tk.op("dve", lambda v, r=r, rr=rr: v.scalar_tensor_tensor(
                        out=A1[:, :, r], in0=mrow(1, rr), scalar=1.0, in1=g1T, op0=ALU.add, op1=ALU.mult),
                        reads=["small", "mT"], writes=["small"])
                    tk.op("dve", lambda v, r=r, rr=rr: v.scalar_tensor_tensor(
                        out=A2[:, :, r], in0=mrow(4, rr), scalar=1.0, in1=g2T, op0=ALU.add, op1=ALU.mult),
                        reads=["small", "mT"], writes=["small"])
                tk.op("act", lambda a: a.activation(out=sinkx, in_=sinkx, func=AF.Exp), reads=["small"], writes=["small"])
                lr = lamraw.rearrange("p (a d) -> p a d", a=4)
                lw = stat[:, 32:36]
                tk.op("dve", lambda v: v.tensor_tensor(out=lamraw[:, 0:32], in0=lr[:, 0, :], in1=lr[:, 1, :], op=ALU.mult),
                      reads=["small"], writes=["small"])
                tk.op("dve", lambda v: v.tensor_tensor(out=lamraw[:, 64:96], in0=lr[:, 2, :], in1=lr[:, 3, :], op=ALU.mult),
                      reads=["small"], writes=["small"])
                tk.op("dve", lambda v: v.tensor_reduce(out=lw[:, 0:2], in_=lamraw.rearrange("p (a d) -> p a d", a=2)[:, :, 0:32],
                                                       axis=AX.X, op=ALU.add), reads=["small"], writes=["stat2"])
                tk.op("act", lambda a: a.activation(out=lw[:, 0:2], in_=lw[:, 0:2], func=AF.Exp), reads=["stat2"], writes=["stat2"])
                tk.op("dve", lambda v: v.tensor_tensor(out=lw[:, 2:3], in0=lw[:, 0:1], in1=lw[:, 1:2], op=ALU.subtract),
                      reads=["stat2"], writes=["stat2"])
                tk.op("act", lambda a: a.activation(out=lamt[:, 0:1], in_=lw[:, 2:3], func=AF.Identity, scale=-1.0, bias=-lam_init),
                      reads=["stat2"], writes=["small"])

                if dbg == "sm":
                    print("nops at sm", tk.nops)
                    tk.barrier()
                    dump(small[:, :], 512, 0, [])
                    tk.barrier()
                    return nc
                tk.mark("b%d l%d norm1" % (b, l))
                for cch in range(4):
                    norm_to_h(cch * 512, 512, A1[:, :, 0], mrow(0, b))
                norm_to_h(S, L, A1[:, :, 1], mrow(0, NB))
                if dbg == "h":
                    tk.barrier()
                    dump(hT[:, 0, 0:2304], 2304, 0, hkeys(0, T))
                    dump(hT[:, 7, 0:2304], 2304, 2304, hkeys(0, T))
                    tk.barrier()
                    return nc

                for mi in (0, 1, 3, 2):
                    (mname, col0, nq, nk, nv, nkv, hd) = MIX[mi]
                    ncols = nq + nk + nv
                    nh_q = nq // hd
                    nh_k = nk // hd
                    scale = hd ** -0.5
                    tk.dma("pool", lambda q: q.dma_start(
                        out=WI[:, :, 0:ncols], in_=w_in[l, :, col0:col0 + ncols].rearrange("(j p) c -> p j c", p=128)),
                        writes=["WI"])
                    tk.dma("pool", lambda q: q.dma_start(
                        out=WO[:, :, :], in_=w_out[l, mi * 256:(mi + 1) * 256, :].rearrange("(k p) c -> p k c", p=128)),
                        writes=["WO"])
                    gd = qkg[mname]
                    tk.dma("sp", lambda q: q.dma_start(out=gq[:, 0:hd], in_=gd[l, 0:1, :].partition_broadcast(128)), writes=["small"])
                    tk.dma("sp", lambda q: q.dma_start(out=gk[:, 0:hd], in_=gd[l, 1:2, :].partition_broadcast(128)), writes=["small"])
                    tk.op("dve", lambda v: v.tensor_scalar(out=gq[:, 0:hd], in0=gq[:, 0:hd], scalar1=scale, scalar2=None,
                                                           op0=ALU.mult), reads=["small"], writes=["small"])
                    tk.op("dve", lambda v: v.memset(VA[:, :, :, 64:65], 1.0), writes=["VA"])

                    tk.mark("b%d l%d %s proj" % (b, l, mname))
                    tk.op("dve", lambda v: v.memset(TKb[0], 0.0), writes=["tkp0"])
                    tk.op("dve", lambda v: v.memset(TKb[1], 0.0), writes=["tkp1"])
                    nkc = 4 if mname == "C" else nkv
                    if mname == "C":
                        tk.op("dve", lambda v: v.memset(PB[0][:], 0.0), writes=[("pb", 0)])
                        tk.op("dve", lambda v: v.memset(PB[1][:], 0.0), writes=[("pb", 1)])

                    def tile_banks(tt):
                        return [0, 1] if tt % 2 == 0 else [2, 3]

                    def emit_mm(tt):
                        banks = tile_banks(tt)
                        pieces = [(0, min(512, ncols), banks[0])]
                        if ncols > 512:
                            pieces.append((512, ncols, banks[1]))
                        for (c0, c1, bank) in pieces:
                            for j in range(8):
                                tk.op("pe", lambda t, j=j: t.matmul(
                                    PS[bank][:, 0:c1 - c0], hT[:, j, tt * 128:(tt + 1) * 128], WI[:, j, c0:c1],
                                    start=(j == 0), stop=(j == 7)), reads=[("hT", tt), "WI"], writes=[psk(bank)])

                    def emit_chains(tt):
                        is_lat = tt < 16 and mname != "B"
                        banks = tile_banks(tt)
                        par = tt % 2
                        TQc = TQb[par]
                        TKc = TKb[par]
                        if tt < 16 and mname != "B":
                            tk.dma("sp", lambda q: q.dma_start(out=ropet[:], in_=rope_d[tt * 128:(tt + 1) * 128, :]), writes=["ropet"])
                        rt = (ropet[:, 0:64], ropet[:, 64:128]) if hd == 64 else (ropet[:, 128:160], ropet[:, 160:192])
                        if mname in ("A", "D"):
                            qviews = [(TQc.rearrange("p (c s d) -> p s c d", c=2, s=2),
                                       (lambda ap: ap.rearrange("p (s c d) -> p s c d", s=2, c=2)))]
                        elif mname == "C":
                            tqv = PB[par][:, 0:512].rearrange("p (c s d) -> p c s d", c=4, s=2)
                            qviews = [(tqv[:, h_, h_ % 2, :], (lambda ap, h_=h_: ap[:, h_ * 64:(h_ + 1) * 64])) for h_ in range(4)]
                        else:
                            qviews = [(TQc, None)]
                        gq_ = qk_post(PS[banks[0]][:, 0:256], nh_q if hd == 64 else 8, hd, gq[:, 0:hd], rt, qviews, is_lat,
                                      banks[0], ("pb", par) if mname == "C" else "tq%d" % par, par * 2, 0)
                        if mname == "C":
                            tkv = TKc.rearrange("p (hp i hh i2 d) -> p hp i hh i2 d", hp=2, i=2, hh=2, i2=2)
                            views = []
                            for i_ in range(2):
                                views.append((tkv[:, :, i_, :, i_, :],
                                              (lambda ap, i_=i_: ap.rearrange("p (hp hh i d) -> p hp hh i d", hp=2, hh=2, i=2)[:, :, :, i_, :])))
                            gk_ = qk_post(PS[banks[0]][:, 256:512], 8, 32, gk[:, 0:32], rt, views, is_lat, banks[0], "tkp%d" % par, par * 2 + 1, 1)
                        else:
                            tkv = TKc[:, 0:nkv * 128].rearrange("p (c s d) -> p c s d", c=nkv, s=2)
                            views = [(tkv[:, kvh, kvh % 2, :], (lambda ap, kvh=kvh: ap[:, kvh * 64:(kvh + 1) * 64])) for kvh in range(nkv)]
                            gk_ = qk_post(PS[banks[0]][:, 256:256 + nk], nh_k, hd, gk[:, 0:hd], rt, views, is_lat,
                                          banks[0], "tkp%d" % par, par * 2 + 1, 1)
                        run_zipped([gq_, gk_])
                        if nk == 128:
                            vsrc = PS[banks[0]][:, 384:512]
                            vb = banks[0]
                        else:
                            vsrc = PS[banks[1]][:, 0:256]
                            vb = banks[1]
                        tk.op("act", lambda a: a.copy(out=VA[:, tt, 0:nkv, 0:64], in_=vsrc.rearrange("p (h d) -> p h d", h=nkv)),
                              reads=[psk(vb)], writes=[("VA", tt)])

                    def emit_tr(tt):
                        par = tt % 2
                        TQc = TQb[par]
                        TKc = TKb[par]
                        if mname == "C":
                            for cc in range(4):
                                tk.op("pe", lambda t, cc=cc: t.transpose(
                                    out=PS[4][:].bitcast(BF16)[:, cc * 128:(cc + 1) * 128], in_=PB[par][:, cc * 128:(cc + 1) * 128],
                                    identity=ident_b[:]), reads=[("pb", par), "ident_b"], writes=[psk(4)])
                            tk.op("act", lambda a: a.copy(out=hT[:, 0:4, tt * 128:(tt + 1) * 128],
                                                          in_=PS[4][:].bitcast(BF16)[:, 0:512].rearrange("p (c t) -> p c t", c=4)),
                                  reads=[psk(4)], writes=[("QT", tt), ("hT", tt)])
                        else:
                            for cc in range(2):
                                tk.op("pe", lambda t, cc=cc: t.transpose(
                                    out=PS[4][:].bitcast(BF16)[:, cc * 128:(cc + 1) * 128], in_=TQc[:, cc * 128:(cc + 1) * 128],
                                    identity=ident_b[:]), reads=["tq%d" % par, "ident_b"], writes=[psk(4)])
                            tk.op("act", lambda a: a.copy(out=QT[:, :, tt * 128:(tt + 1) * 128],
                                                          in_=PS[4][:].bitcast(BF16)[:, 0:256].rearrange("p (c t) -> p c t", c=2)),
                                  reads=[psk(4)], writes=[("QT", tt)])
                        for cc in range(nkc):
                            tk.op("pe", lambda t, cc=cc: t.transpose(
                                out=PS[5][:].bitcast(BF16)[:, cc * 128:(cc + 1) * 128],
                                in_=TKc[:, cc * 128:(cc + 1) * 128], identity=ident_b[:]),
                                reads=["tkp%d" % par, "ident_b"], writes=[psk(5)])
                        tk.op("act", lambda a: a.copy(
                            out=KT[:, 0:nkc, tt * 128:(tt + 1) * 128],
                            in_=PS[5][:].bitcast(BF16)[:, 0:nkc * 128].rearrange("p (c t) -> p c t", c=nkc)),
                            reads=[psk(5)], writes=[("KT", tt)])

                    emit_mm(0)
                    for tt in range(NTT):
                        emit_chains(tt)
                        if tt + 1 < NTT:
                            emit_mm(tt + 1)
                        emit_tr(tt)
                    if dbg == "qkv" and mname == dbg_mixer[0]:
                        tk.barrier()
                        dump(QT[:, 0, :], 2304, 0, [])
                        dump(KT[:, 0, :], 2304, 2304, [])
                        dump(VA[:, 3, :, :].rearrange("p h d -> p (h d)"), 264, 4608, [])
                        dump(KT[:, 3, :], 2304, 4900, [])
                        tk.barrier()
                        return nc

                    tk.mark("b%d l%d %s attn" % (b, l, mname))
                    ranges = [(r0 * 512, 512) for r0 in range(4)] + ([(S, L)] if need_ctx else [])
                    pending = []

                    def flush():
                        for f_ in pending:
                            f_()
                        del pending[:]

                    for (q0, n) in ranges:
                        nblk = n // 128
                        is_ctxq = q0 >= S
                        units = []
                        if mname in ("A", "D"):
                            for h in range(4):
                                units.append((h, 0, h % 2, (h // 2) * 64, h // 2, (h // 2) * 64, h // 2))
                        elif mname == "B":
                            for h in range(4):
                                units.append((h, 0, h // 2, (h % 2) * 64, h, (h % 2) * 64, h))
                        else:
                            for h in range(4):
                                for i_ in range(2):
                                    units.append((h, i_, h // 2, (h % 2) * 64, (h // 2) * 2 + i_, (h % 2) * 64, h))
                        for (h, br, qc, qp, kc_, kp, vh) in units:
                            ob = 6 if unit_ctr[0] % 2 == 0 else 3
                            unit_ctr[0] += 1
                            steps = []
                            if is_ctxq or mname in ("C", "D"):
                                kts = [16, 17] if is_ctxq else list(range(18))
                                for ki, kt in enumerate(kts):
                                    steps.append((0, n, kt, None, None, ki == 0, ki == len(kts) - 1, None))
                            elif mname == "A":
                                for bl in range(nblk):
                                    i = q0 // 128 + bl
                                    lst = []
                                    if i - 1 >= 0:
                                        lst.append((i - 1, wmask[:, 0:128], "wmask"))
                                    lst.append((i, None, None))
                                    if i + 1 < 16:
                                        lst.append((i + 1, wmask[:, 128:256], "wmask"))
                                    lst += [(16, None, None), (17, None, None)]
                                    for ki, (kt, ma, mk_) in enumerate(lst):
                                        steps.append((bl * 128, 128, kt, ma, mk_, ki == 0, ki == len(lst) - 1, None))
                            else:
                                for bl in range(nblk):
                                    i = q0 // 128 + bl
                                    ts = na_ts(i)
                                    vi = na_variant(i)

                                    def load_mask(vi=vi, h=h):
                                        if nabm_state[0] == (b, l, h, vi):
                                            return
                                        nabm_state[0] = (b, l, h, vi)
                                        stg, stgk = tmp()
                                        stg2, stg2k = tmp()
                                        tk.dma("sp", lambda q: q.dma_start(out=stg[:, 0:512], in_=nab_d[l, h, vi, :, 0:512]), writes=[stgk])
                                        tk.dma("sp", lambda q: q.dma_start(out=stg2[:, 0:128], in_=nab_d[l, h, vi, :, 512:640]), writes=[stg2k])
                                        tk.op("act", lambda a: a.activation(out=nabm[:, 0:512], in_=stg[:, 0:512], func=AF.Exp),
                                              reads=[stgk], writes=["nabm"])
                                        tk.op("act", lambda a: a.activation(out=nabm[:, 512:640], in_=stg2[:, 0:128], func=AF.Exp),
                                              reads=[stg2k], writes=["nabm"])
                                    lst = [(ts + j, nabm[:, j * 128:(j + 1) * 128], "nabm") for j in range(5)]
                                    lst += [(16, None, None), (17, None, None)]
                                    for ki, (kt, ma, mk_) in enumerate(lst):
                                        steps.append((bl * 128, 128, kt, ma, mk_, ki == 0, ki == len(lst) - 1, load_mask if ki == 0 else None))

                            def emit_qk(si, st):
                                (qa, qn, kt, mask_ap, mkey, first, last, pre) = st
                                if pre is not None:
                                    pre()
                                sbank = 4 + (si % 2)
                                if mname == "C":
                                    k_ap = KT[:, kc_, kt * 128:(kt + 1) * 128]
                                    q_ap = hT[:, h, q0 + qa:q0 + qa + qn]
                                else:
                                    k_ap = KT[:, kc_, kt * 128:(kt + 1) * 128]
                                    q_ap = QT[:, qc, q0 + qa:q0 + qa + qn]
                                tk.op("pe", lambda t: t.matmul(PS[sbank][:, 0:qn], k_ap, q_ap, start=True, stop=True),
                                    reads=[("KT", kt)] + [("QT", (q0 + qa) // 128 + z) for z in range(qn // 128)], writes=[psk(sbank)])
                                pbt, pbk = pbuf()
                                tk.op("act", lambda a: a.activation(out=pbt[:, 0:qn], in_=PS[sbank][:, 0:qn], func=AF.Exp),
                                      reads=[psk(sbank)], writes=[pbk])
                                if mask_ap is not None:
                                    tk.op("dve", lambda v: v.tensor_tensor(out=pbt[:, 0:qn], in0=pbt[:, 0:qn], in1=mask_ap, op=ALU.mult),
                                          reads=[pbk, mkey], writes=[pbk])
                                return pbt, pbk

                            def emit_pv(st, pbt, pbk):
                                (qa, qn, kt, mask_ap, mkey, first, last, pre) = st
                                tk.op("pe", lambda t: t.matmul(PS[ob][0:65, qa:qa + qn], VA[:, kt, vh, 0:65], pbt[:, 0:qn], start=first, stop=last),
                                      reads=[("VA", kt), "VA", pbk], writes=[psk(ob)])

                            prev = None
                            for si, st in enumerate(steps):
                                cur = emit_qk(si, st)
                                if prev is not None:
                                    emit_pv(*prev)
                                prev = (st,) + cur
                                if si == min(2, len(steps) - 1):
                                    flush()
                            emit_pv(*prev)

                            def post(h=h, br=br, ob=ob, nblk=nblk, n=n):
                                ot, otk = tmp()
                                tk.op("dve", lambda v: v.tensor_copy(out=ot[0:65, 0:n], in_=PS[ob][0:65, 0:n]), reads=[psk(ob)], writes=[otk])
                                for bl in range(nblk):
                                    tk.op("pe", lambda t, bl=bl: t.transpose(
                                        out=PS[7][:, bl * 128:bl * 128 + 65], in_=ot[0:65, bl * 128:(bl + 1) * 128],
                                        identity=ident_f[0:65, 0:65]), reads=[otk, "ident_f"], writes=[psk(7)])
                                p7 = PS[7][:].rearrange("p (b d) -> p b d", d=128)
                                rd = stat[:, 8:8 + nblk]
                                if mname == "A":
                                    tk.op("dve", lambda v: v.tensor_scalar(out=rd, in0=p7[:, 0:nblk, 64], scalar1=sinkx[:, h:h + 1],
                                                                           scalar2=None, op0=ALU.add), reads=[psk(7), "small"], writes=["stat3"])
                                    tk.op("dve", lambda v: v.reciprocal(out=rd, in_=rd), reads=["stat3"], writes=["stat3"])
                                else:
                                    tk.op("dve", lambda v: v.reciprocal(out=rd, in_=p7[:, 0:nblk, 64]), reads=[psk(7)], writes=["stat3"])
                                if br == 1:
                                    tk.op("dve", lambda v: v.tensor_scalar(out=rd, in0=rd, scalar1=lamt[:, 0:1], scalar2=None, op0=ALU.mult),
                                          reads=["stat3", "small"], writes=["stat3"])
                                for bl in range(nblk):
                                    if br == 0:
                                        tk.op("dve", lambda v, bl=bl: v.tensor_scalar(
                                            out=otok[:, bl, h * 64:(h + 1) * 64], in0=p7[:, bl, 0:64], scalar1=rd[:, bl:bl + 1],
                                            scalar2=None, op0=ALU.mult), reads=[psk(7), "stat3"], writes=[("otok", bl)])
                                    else:
                                        tk.op("dve", lambda v, bl=bl: v.scalar_tensor_tensor(
                                            out=otok[:, bl, h * 64:(h + 1) * 64], in0=p7[:, bl, 0:64], scalar=rd[:, bl:bl + 1],
                                            in1=otok[:, bl, h * 64:(h + 1) * 64], op0=ALU.mult, op1=ALU.add),
                                            reads=[psk(7), "stat3", ("otok", bl)], writes=[("otok", bl)])
                            pending.append(post)

                        def merge(q0=q0, nblk=nblk):
                            for bl in range(nblk):
                                tok = q0 + bl * 128
                                ng = 1 if mname != "C" else 4
                                gsz = 256 // ng
                                sq, sqk = tmp()
                                tk.op("act", lambda a: a.activation(out=sq[:, 0:256], in_=otok[:, bl, :], func=AF.Square),
                                      reads=[("otok", bl)], writes=[sqk])
                                ssg = stat[:, 16:16 + ng]
                                tk.op("dve", lambda v: v.tensor_reduce(out=ssg, in_=sq[:, 0:256].rearrange("p (g d) -> p g d", g=ng),
                                                                       axis=AX.X, op=ALU.add), reads=[sqk], writes=["stat4"])
                                tk.op("act", lambda a: a.activation(out=ssg, in_=ssg, func=AF.Sqrt, bias=EPS, scale=1.0 / gsz),
                                      reads=["stat4"], writes=["stat4"])
                                tk.op("dve", lambda v: v.reciprocal(out=ssg, in_=ssg), reads=["stat4"], writes=["stat4"])
                                if mname == "C":
                                    tk.op("dve", lambda v: v.tensor_scalar(out=ssg, in0=ssg, scalar1=(1.0 - lam_init), scalar2=None,
                                                                           op0=ALU.mult), reads=["stat4"], writes=["stat4"])
                                tk.op("dve", lambda v: v.tensor_tensor(
                                    out=ytok[:].rearrange("p (g d) -> p g d", g=ng), in0=otok[:, bl, :].rearrange("p (g d) -> p g d", g=ng),
                                    in1=ssg.unsqueeze(2).broadcast_to([128, ng, gsz]), op=ALU.mult),
                                    reads=[("otok", bl), "stat4"], writes=["ytok"])
                                for cc in range(2):
                                    tk.op("pe", lambda t, cc=cc: t.transpose(
                                        out=PS[2][:].bitcast(BF16)[:, cc * 128:(cc + 1) * 128],
                                        in_=ytok[:, cc * 128:(cc + 1) * 128], identity=ident_b[:]),
                                        reads=["ytok", "ident_b"], writes=[psk(2)])
                                for cc in range(2):
                                    tk.op("act", lambda a, cc=cc: a.activation(
                                        out=YT[:, cc, tok:tok + 128], in_=PS[2][:].bitcast(BF16)[:, cc * 128:(cc + 1) * 128],
                                        func=AF.Identity, scale=ogT[:, mi * 2 + cc:mi * 2 + cc + 1]),
                                        reads=[psk(2), "small"], writes=[("YT", tok // 128)])
                        pending.append(merge)
                    flush()
                    if dbg == "y" and mname == dbg_mixer[0]:
                        tk.barrier()
                        dump(YT[:, 0, :], 2304, 0, [])
                        dump(YT[:, 1, :], 2304, 2304, [])
                        tk.barrier()
                        return nc

                    tk.mark("b%d l%d %s wout" % (b, l, mname))
                    for (q0, n) in ranges:
                        rr = NB if q0 >= S else b
                        for jo in range(8):
                            bank = jo % 4
                            for kc2 in range(2):
                                tk.op("pe", lambda t, jo=jo, kc2=kc2, bank=bank: t.matmul(
                                    PS[bank][:, 0:n], WO[:, kc2, jo * 128:(jo + 1) * 128], YT[:, kc2, q0:q0 + n],
                                    start=(kc2 == 0), stop=(kc2 == 1)),
                                    reads=["WO"] + [("YT", q0 // 128 + z) for z in range(n // 128)], writes=[psk(bank)],
                                    inc=(kc2 == 1))
                            tk.op("dve", lambda v, jo=jo, bank=bank, rr=rr: v.scalar_tensor_tensor(
                                out=xT[:, jo, q0:q0 + n], in0=PS[bank][:, 0:n], scalar=mT[:, l, 16 + jo, rr:rr + 1],
                                in1=xT[:, jo, q0:q0 + n], op0=ALU.mult, op1=ALU.add),
                                reads=[psk(bank), "mT"] + xkeys(q0, n), writes=xkeys(q0, n))
                    tk.barrier()
                if dbg == "xattn":
                    dump(xT[:, 0, :], 2304, 0, [])
                    dump(xT[:, 5, :], 2304, 2304, [])
                    tk.barrier()
                    return nc

                if dbg == "xattn2":
                    dump(xT[:, 0, :], 2304, 0, [])
                    dump(xT[:, 5, :], 2304, 2304, [])
                    tk.barrier()
                    return nc
                tk.mark("b%d l%d norm2+route" % (b, l))
                ranges = [(r0 * 512, 512) for r0 in range(4)] + ([(S, L)] if need_ctx else [])
                for (q0, n) in ranges:
                    r = 1 if q0 >= S else 0
                    rr = NB if q0 >= S else b
                    norm_to_h(q0, n, A2[:, :, r], mrow(3, rr), router_bank=6)
                    lg, lgk = RB, "rbuf"
                    tk.op("dve", lambda v, lg=lg: v.tensor_copy(out=lg[0:16, 0:n], in_=PS[6][0:16, 0:n]), reads=[psk(6)], writes=[lgk])
                    for bl in range(n // 128):
                        tk.op("pe", lambda t, bl=bl, lg=lg: t.transpose(out=PS[7][:, 0:16], in_=lg[0:16, bl * 128:(bl + 1) * 128],
                                                                       identity=ident_f[0:16, 0:16]),
                              reads=[lgk, "ident_f"], writes=[psk(7)])
                        sc = stat[:, 0:16]
                        sel = stat[:, 16:32]
                        w8 = stat[:, 32:40]
                        tk.op("act", lambda a: a.activation(out=sc, in_=PS[7][:, 0:16], func=AF.Sigmoid), reads=[psk(7)], writes=["stat"])
                        tk.op("dve", lambda v: v.tensor_tensor(out=sel, in0=sc, in1=rb_bc, op=ALU.add), reads=["stat", "small"], writes=["stat"])
                        s4 = sel.rearrange("p (g a c) -> p g a c", g=4, a=2)
                        pq = stat[:, 40:48].rearrange("p (g a) -> p g a", g=4)
                        rs_ = stat[:, 48:56].rearrange("p (g a) -> p g a", g=4)
                        tk.op("dve", lambda v: v.tensor_tensor(out=pq, in0=s4[:, :, :, 0], in1=s4[:, :, :, 1], op=ALU.max), reads=["stat"], writes=["stat"])
                        tk.op("dve", lambda v: v.tensor_tensor(out=rs_, in0=s4[:, :, :, 0], in1=s4[:, :, :, 1], op=ALU.min), reads=["stat"], writes=["stat"])
                        m1 = stat[:, 56:60]
                        m2_ = stat[:, 60:64]
                        tk.op("dve", lambda v: v.tensor_tensor(out=m1, in0=pq[:, :, 0], in1=pq[:, :, 1], op=ALU.max), reads=["stat"], writes=["stat"])
                        tk.op("dve", lambda v: v.tensor_tensor(out=m2_, in0=pq[:, :, 0], in1=pq[:, :, 1], op=ALU.min), reads=["stat"], writes=["stat"])
                        tk.op("dve", lambda v: v.tensor_tensor(out=pq[:, :, 0], in0=rs_[:, :, 0], in1=rs_[:, :, 1], op=ALU.max), reads=["stat"], writes=["stat"])
                        tk.op("dve", lambda v: v.tensor_tensor(out=m2_, in0=m2_, in1=pq[:, :, 0], op=ALU.max), reads=["stat"], writes=["stat"])
                        tk.op("dve", lambda v: v.tensor_tensor(out=m1, in0=m1, in1=m2_, op=ALU.add), reads=["stat"], writes=["stat"])
                        gmx = w8[:, 0:1]
                        tk.op("dve", lambda v: v.tensor_reduce(out=gmx, in_=m1, axis=AX.X, op=ALU.max), reads=["stat"], writes=["stat"])
                        tk.op("dve", lambda v: v.tensor_scalar(out=m2_, in0=m1, scalar1=gmx, scalar2=None, op0=ALU.is_ge), reads=["stat"], writes=["stat"])
                        tk.op("act", lambda a: a.activation(out=m1, in_=m2_, func=AF.Identity, scale=100.0, bias=-100.0),
                              reads=["stat"], writes=["stat"])
                        sel3 = sel.rearrange("p (g c) -> p g c", g=4)
                        tk.op("dve", lambda v: v.tensor_tensor(out=sel3, in0=sel3, in1=m2_.unsqueeze(2).broadcast_to([128, 4, 4]), op=ALU.mult),
                              reads=["stat"], writes=["stat"])
                        tk.op("dve", lambda v: v.tensor_tensor(out=sel3, in0=sel3, in1=m1.unsqueeze(2).broadcast_to([128, 4, 4]), op=ALU.add),
                              reads=["stat"], writes=["stat"])
                        tk.op("dve", lambda v: v.max(out=w8, in_=sel), reads=["stat"], writes=["stat"])
                        tk.op("dve", lambda v: v.tensor_scalar(out=sel, in0=sel, scalar1=w8[:, 1:2], scalar2=None, op0=ALU.is_ge),
                              reads=["stat"], writes=["stat"])
                        tk.op("dve", lambda v: v.tensor_tensor(out=sc, in0=sc, in1=sel, op=ALU.mult), reads=["stat"], writes=["stat"])
                        tk.op("dve", lambda v: v.tensor_reduce(out=gmx, in_=sc, axis=AX.X, op=ALU.add), reads=["stat"], writes=["stat"])
                        tk.op("dve", lambda v: v.reciprocal(out=gmx, in_=gmx), reads=["stat"], writes=["stat"])
                        wtok, wtokk = tmp()
                        tk.op("dve", lambda v, wtok=wtok: v.tensor_scalar(out=wtok[:, 0:16], in0=sc, scalar1=gmx, scalar2=None, op0=ALU.mult),
                              reads=["stat"], writes=[wtokk])
                        tk.op("pe", lambda t, wtok=wtok: t.transpose(out=PS[7][0:16, 128:256], in_=wtok[:, 0:16], identity=ident_f[:]),
                              reads=[wtokk, "ident_f"], writes=[psk(7)])
                        tk.op("act", lambda a, bl=bl: a.copy(out=wrt_hi[:, q0 + bl * 128:q0 + (bl + 1) * 128], in_=PS[7][0:16, 128:256]),
                              reads=[psk(7)], writes=[("wrt", (q0 // 128) + bl)])
                        tk.op("dve", lambda v, bl=bl: v.tensor_tensor(
                            out=wrt_lo[:, q0 + bl * 128:q0 + (bl + 1) * 128], in0=PS[7][0:16, 128:256],
                            in1=wrt_hi[:, q0 + bl * 128:q0 + (bl + 1) * 128], op=ALU.subtract),
                            reads=[psk(7), ("wrt", (q0 // 128) + bl)], writes=[("wrtl", (q0 // 128) + bl)])
                if dbg == "route":
                    tk.barrier()
                    dump(wrt_hi[:, :], 2304, 0, [])
                    dump(hT[:, 0, 0:2304], 2304, 2304, [])
                    tk.barrier()
                    return nc
                tk.barrier()

                tk.mark("b%d l%d experts" % (b, l))
                items = [(e, q0, n) for e in range(NE) for (q0, n) in ranges]

                def ew_views(e):
                    ew = EW[e % 2]
                    return (ew[:, 0:4096].rearrange("p (j f) -> p j f", j=8), ew[:, 4096:8192].rearrange("p (j f) -> p j f", j=8),
                            ew[:, 8192:12288].rearrange("p (k c) -> p k c", k=4), ("EW", e % 2))

                def hm_buf(idx, fc):
                    if idx % 2 == 0:
                        return HM[:, fc, :]
                    return [PB[0], PB[1], PB[2], TK_][fc]

                def load_expert(e):
                    WG, WU, WD, ewk = ew_views(e)
                    tk.dma("pool", lambda q: q.dma_start(out=WG, in_=wg_d[l, e].rearrange("(j p) f -> p j f", p=128)), writes=[ewk])
                    tk.dma("pool", lambda q: q.dma_start(out=WU, in_=wu_d[l, e].rearrange("(j p) f -> p j f", p=128)), writes=[ewk])
                    tk.dma("pool", lambda q: q.dma_start(out=WD, in_=wd_d[l, e].rearrange("(k p) c -> p k c", p=128)), writes=[ewk])

                def emit_bc(idx):
                    e, q0, n = items[idx]
                    bcb = 0 if idx % 2 == 0 else 7
                    wk = [("wrt", q0 // 128 + z) for z in range(n // 128)] + [("wrtl", q0 // 128 + z) for z in range(n // 128)]
                    tk.op("pe", lambda t: t.matmul(PS[bcb][:, 0:n], selb[0:16, e, :], wrt_hi[:, q0:q0 + n], start=True, stop=False),
                          reads=wk + ["selb"], writes=[psk(bcb)])
                    tk.op("pe", lambda t: t.matmul(PS[bcb][:, 0:n], selb[0:16, e, :], wrt_lo[:, q0:q0 + n], start=False, stop=True),
                          reads=wk + ["selb"], writes=[psk(bcb)])

                def emit_gu(idx, fc):
                    e, q0, n = items[idx]
                    bcb = 0 if idx % 2 == 0 else 7
                    WG, WU, WD, ewk = ew_views(e)
                    hk = hkeys(q0, n)
                    gb = 1 + (fc % 2)
                    ub = 3 + (fc % 2)
                    for j in range(8):
                        tk.op("pe", lambda t, j=j: t.matmul(PS[gb][:, 0:n], WG[:, j, fc * 128:(fc + 1) * 128], hT[:, j, q0:q0 + n],
                                                             start=(j == 0), stop=(j == 7)), reads=[ewk] + hk, writes=[psk(gb)])
                    for j in range(8):
                        tk.op("pe", lambda t, j=j: t.matmul(PS[ub][:, 0:n], WU[:, j, fc * 128:(fc + 1) * 128], hT[:, j, q0:q0 + n],
                                                             start=(j == 0), stop=(j == 7)), reads=[ewk] + hk, writes=[psk(ub)])
                    sg, sgk = tmp()
                    tk.op("act", lambda a: a.activation(out=sg[:, 0:n], in_=PS[gb][:, 0:n], func=AF.Silu), reads=[psk(gb)], writes=[sgk])
                    tk.op("dve", lambda v: v.tensor_tensor(out=sg[:, 0:n], in0=sg[:, 0:n], in1=PS[ub][:, 0:n], op=ALU.mult),
                          reads=[sgk, psk(ub)], writes=[sgk])
                    tk.op("dve", lambda v: v.tensor_tensor(out=hm_buf(idx, fc)[:, 0:n], in0=sg[:, 0:n], in1=PS[bcb][:, 0:n], op=ALU.mult),
                          reads=[sgk, psk(bcb)], writes=[("HM", idx % 2, fc)])

                def emit_down(idx):
                    e, q0, n = items[idx]
                    rr = NB if q0 >= S else b
                    WG, WU, WD, ewk = ew_views(e)
                    for jo in range(8):
                        yb = 5 + (jo % 2)
                        for fc in range(4):
                            tk.op("pe", lambda t, fc=fc: t.matmul(PS[yb][:, 0:n], WD[:, fc, jo * 128:(jo + 1) * 128], hm_buf(idx, fc)[:, 0:n],
                                                                   start=(fc == 0), stop=(fc == 3)), reads=[ewk, ("HM", idx % 2, fc)], writes=[psk(yb)])
                        tk.op("dve", lambda v: v.scalar_tensor_tensor(
                            out=xT[:, jo, q0:q0 + n], in0=PS[yb][:, 0:n], scalar=mT[:, l, 40 + jo, rr:rr + 1],
                            in1=xT[:, jo, q0:q0 + n], op0=ALU.mult, op1=ALU.add),
                            reads=[psk(yb), "mT"] + xkeys(q0, n), writes=xkeys(q0, n))

                load_expert(0)
                emit_bc(0)
                emit_gu(0, 0)
                for idx in range(len(items)):
                    e, q0, n = items[idx]
                    if (q0, n) == ranges[0] and e + 1 < NE:
                        load_expert(e + 1)
                    for fc in range(1, 4):
                        emit_gu(idx, fc)
                    if idx + 1 < len(items):
                        emit_bc(idx + 1)
                        emit_gu(idx + 1, 0)
                    emit_down(idx)
                tk.barrier()
                if dbg == "x2":
                    dump(xT[:, 0, :], 2304, 0, [])
                    dump(xT[:, 5, :], 2304, 2304, [])
                    tk.barrier()
                    return nc

            tk.mark("b%d store" % b)
            for tt in range(16):
                for g in range(2):
                    bank = 6 + g
                    for jj in range(4):
                        j = g * 4 + jj
                        tk.op("pe", lambda t, j=j, jj=jj, bank=bank: t.transpose(
                            out=PS[bank][:, jj * 128:(jj + 1) * 128], in_=xT[:, j, tt * 128:(tt + 1) * 128], identity=ident_f[:]),
                            reads=[("xT", tt), "ident_f"], writes=[psk(bank)], inc=(jj == 3))
                    if g == 0:
                        tk.op("act", lambda a, bank=bank: a.copy(out=XS[:, 0:512], in_=PS[bank][:]), reads=[psk(bank)], writes=["xs"])
                    else:
                        tk.op("dve", lambda v, bank=bank: v.tensor_copy(out=XS[:, 512:1024], in_=PS[bank][:]), reads=[psk(bank)], writes=["xs"])
                tk.dma("sp", lambda q: q.dma_start(out=out_d[b, tt * 128:(tt + 1) * 128, :], in_=XS[:]), reads=["xs"], writes=["out"])
            tk.barrier()
          except StopBuild:
            print("STOPPED at", tk.nops)
            tk.pe_open = False
            tk.limit = 0
            tk.barrier()
            if dbg_d is not None:
                dump(small[:, :], 512, 0, [])
                tk.barrier()
            return nc
        tk.barrier()
        tk.mark("end")
        LAST_MARKS[:] = tk.marks
    return nc


LAST_MARKS = []
dbg_mixer = ["D"]

_W_NAMES = ["ada_w", "ada_b", "norm_mix_g", "norm_ffn_g", "w_in", "qk_g_win", "qk_g_na", "qk_g_diff", "qk_g_gqa",
            "sink_win", "lambda_diff", "out_gain", "w_out", "w_gate", "w_up", "w_down"]


def make_in_maps(inputs, n_cores, NB, DEPTH):
    f = lambda a: np.ascontiguousarray(np.asarray(a, dtype=np.float32))
    shared = {k: f(inputs[k])[:DEPTH] for k in _W_NAMES}
    shared["router_w"] = f(inputs["router_w"])
    shared["router_b"] = f(inputs["router_b"]).reshape(1, NE)
    shared["na_bias"] = host_na_bias(f(inputs["rpb_na"])[:DEPTH])
    shared.update(host_consts())
    x = f(inputs["x"])
    c = f(inputs["c"])
    ctx = f(inputs["ctx"])
    cctx = f(inputs["c_ctx"]).reshape(1, D)
    maps = []
    for i in range(n_cores):
        m = dict(shared)
        m["x"] = x[i * NB:(i + 1) * NB]
        m["ctx"] = ctx[i * NB:(i + 1) * NB]
        m["c"] = np.ascontiguousarray(np.concatenate([c[i * NB:(i + 1) * NB], cctx], 0))
        maps.append(m)
    return maps


def kernel(**inputs):
    NB = inputs["x"].shape[0] // N_CORES
    nc = build(NB, DEPTH_FULL)
    maps = make_in_maps(inputs, N_CORES, NB, DEPTH_FULL)
    res = run_bass_kernel_spmd(nc, maps, core_ids=list(range(N_CORES)))
    return np.concatenate([r["out"] for r in res.results], axis=0).astype(np.float32)
```

```python
import math
import os
from contextlib import ExitStack
import numpy as np
import concourse.bass as bass
import concourse.mybir as mybir
from concourse.bass_utils import run_bass_kernel_spmd

F32 = mybir.dt.float32
BF16 = mybir.dt.bfloat16
AF = mybir.ActivationFunctionType
ALU = mybir.AluOpType
AX = mybir.AxisListType

D = 1024
S = 2048
L = 256
T = S + L
NTT = T // 128
DEPTH_FULL = 4
EPS = 1e-6
NE = 16
DE = 512
N_CORES = 8
NDS = 40
MIX = [("A", 0, 256, 128, 128, 2, 64), ("B", 512, 256, 256, 256, 4, 64),
       ("C", 1280, 256, 256, 256, 4, 32), ("D", 2048, 256, 128, 128, 2, 64)]


FUSE_PE_WAIT = True


class StopBuild(Exception):
    pass


class TK:
    def __init__(self, nc, es):
        self.nc = nc
        self.eng = {"pe": nc.tensor, "act": nc.scalar, "dve": nc.vector, "pool": nc.gpsimd, "sp": nc.sync}
        self.sem = {k: es.enter_context(nc.semaphore("s_" + k)) for k in self.eng}
        self.cnt = {k: 0 for k in self.eng}
        self.seen = {k: {} for k in self.eng}
        self.dsem = [es.enter_context(nc.semaphore("d%d" % i)) for i in range(NDS)]
        self.dval = [0] * NDS
        self.dnext = 0
        self.bw = {}
        self.br = {}
        self.pe_open = False
        self.marks = []
        self.nops = 0
        self.limit = int(os.environ.get("TK_LIMIT", "0"))

    def _tick(self):
        self.nops += 1
        if self.limit and self.nops > self.limit:
            raise StopBuild()

    def _semh(self, sk):
        return self.sem[sk] if isinstance(sk, str) else self.dsem[sk[1]]

    def _need(self, e, reads, writes):
        need = {}

        def add(ev, raw):
            if ev is None:
                return
            sk, v = ev
            if sk == e and (not raw or e == "pe"):
                return
            if need.get(sk, 0) < v:
                need[sk] = v

        for k in reads:
            add(self.bw.get(k), True)
        for k in writes:
            add(self.bw.get(k), False)
            r = self.br.get(k)
            if r:
                for sk, v in r.items():
                    add((sk, v), False)
        return need

    def _emit_waits(self, e, need):
        for sk, v in need.items():
            if self.seen[e].get(sk, 0) >= v:
                continue
            self.eng[e].wait_ge(self._semh(sk), v)
            self.seen[e][sk] = v

    def _record(self, ev, reads, writes):
        sk, v = ev
        for k in reads:
            d = self.br.setdefault(k, {})
            if d.get(sk, 0) < v:
                d[sk] = v
        for k in writes:
            self.bw[k] = ev
            self.br[k] = {}

    def op(self, e, fn, reads=(), writes=(), inc=True):
        inc = True
        self._tick()
        need = self._need(e, reads, writes)
        fuse = None
        if e in ("pe", "act", "dve", "pool") and FUSE_PE_WAIT:
            pend = [(sk, v) for sk, v in need.items() if self.seen[e].get(sk, 0) < v]
            if pend:
                fuse = pend[-1]
                need = dict(pend[:-1])
        self._emit_waits(e, need)
        ins = fn(self.eng[e])
        if fuse is not None:
            ins._wait_ge(self._semh(fuse[0]), fuse[1])
            self.seen[e][fuse[0]] = fuse[1]
        if inc:
            self.cnt[e] += 1
            ins.then_inc(self.sem[e], 1)
            ev = (e, self.cnt[e])
            if e == "pe":
                self.pe_open = False
        else:
            ev = (e, self.cnt[e] + 1)
            if e == "pe":
                self.pe_open = True
        self._record(ev, reads, writes)

    def dma(self, q, fn, reads=(), writes=()):
        self._tick()
        need = self._need(q, reads, writes)
        i = self.dnext
        self.dnext = (i + 1) % NDS
        if self.dval[i] > 0:
            sk = ("d", i)
            if need.get(sk, 0) < self.dval[i]:
                need[sk] = self.dval[i]
        self._emit_waits(q, need)
        ins = fn(self.eng[q])
        self.dval[i] += 16
        ins.then_inc(self.dsem[i], 16)
        self._record((("d", i), self.dval[i]), reads, writes)

    def mark(self, name):
        self.marks.append((name, dict(self.cnt)))

    def barrier(self):
        assert not self.pe_open
        for e in self.eng:
            need = {f: self.cnt[f] for f in self.eng if f != e and self.cnt[f] > 0}
            for i in range(NDS):
                if self.dval[i] > 0:
                    need[("d", i)] = self.dval[i]
            self._emit_waits(e, need)
        self.bw = {}
        self.br = {}


def host_consts():
    pos = np.arange(S)
    row, col = pos // 64, pos % 64

    def tabs(hd):
        e = hd // 4
        inv = 10000.0 ** (-np.arange(e, dtype=np.float32) / e)
        ar = row[:, None].astype(np.float32) * inv
        ac = col[:, None].astype(np.float32) * inv
        cos = np.concatenate([np.cos(ar), np.cos(ar), np.cos(ac), np.cos(ac)], 1)
        sins = np.concatenate([-np.sin(ar), np.sin(ar), -np.sin(ac), np.sin(ac)], 1)
        return cos.astype(np.float32), sins.astype(np.float32)

    c64, s64 = tabs(64)
    c32, s32 = tabs(32)
    rope = np.concatenate([c64, s64, c32, s32], 1)
    b = np.arange(128)[:, None]
    a = np.arange(128)[None, :]
    wmask = np.stack([(a <= b), (b <= a)], 1).astype(np.float32)
    return {"ident": np.eye(128, dtype=np.float32), "rope": np.ascontiguousarray(rope),
            "wmask": np.ascontiguousarray(wmask.reshape(128, 256))}


def na_variant(i):
    return {0: 0, 1: 1, 14: 3, 15: 4}.get(i, 2)


def na_ts(i):
    return min(max(i - 2, 0), 11)


def host_na_bias(rpb):
    dl = rpb.shape[0]
    out = np.empty((dl, 4, 5, 128, 640), np.float32)
    kl = np.arange(128) // 64
    kc = np.arange(128) % 64
    ql = np.arange(128) // 64
    qc = np.arange(128) % 64
    for vi, i in enumerate([0, 1, 5, 14, 15]):
        ts = na_ts(i)
        for j in range(5):
            krow = 2 * (ts + j) + kl[:, None]
            qrow = 2 * i + ql[None, :]
            rs = np.clip(qrow - 4, 0, 24)
            rowok = (krow >= rs) & (krow < rs + 8)
            cs = np.clip(qc[None, :] - 8, 0, 48)
            colok = (kc[:, None] >= cs) & (kc[:, None] < cs + 16)
            dr = np.clip(krow - qrow + 7, 0, 14)
            dc = np.clip(kc[:, None] - qc[None, :] + 15, 0, 30)
            ok = rowok & colok
            g = rpb[:, :, dr, dc]
            out[:, :, vi, :, j * 128:(j + 1) * 128] = np.where(ok[None, None], g, np.float32(-30000.0))
    return out


def build(NB, DEPTH, dbg=None):
    nc = bass.Bass("TRN2", target_bir_lowering=False)

    def dram(name, shape, kind="ExternalInput", dtype=F32):
        return nc.dram_tensor(name, list(shape), dtype, kind=kind).ap()

    x_d = dram("x", [NB, S, D])
    c_d = dram("c", [NB + 1, D])
    ctx_d = dram("ctx", [NB, L, D])
    ada_w = dram("ada_w", [DEPTH, D, 6 * D])
    ada_b = dram("ada_b", [DEPTH, 6 * D])
    ng1 = dram("norm_mix_g", [DEPTH, D])
    ng2 = dram("norm_ffn_g", [DEPTH, D])
    w_in = dram("w_in", [DEPTH, D, 2560])
    qkg = {"A": dram("qk_g_win", [DEPTH, 2, 64]), "B": dram("qk_g_na", [DEPTH, 2, 64]),
           "C": dram("qk_g_diff", [DEPTH, 2, 32]), "D": dram("qk_g_gqa", [DEPTH, 2, 64])}
    sink_d = dram("sink_win", [DEPTH, 4])
    nab_d = dram("na_bias", [DEPTH, 4, 5, 128, 640])
    lam_d = dram("lambda_diff", [DEPTH, 4, 32])
    og_d = dram("out_gain", [DEPTH, D])
    w_out = dram("w_out", [DEPTH, D, D])
    rw_d = dram("router_w", [D, NE])
    rb_d = dram("router_b", [1, NE])
    wg_d = dram("w_gate", [DEPTH, NE, D, DE])
    wu_d = dram("w_up", [DEPTH, NE, D, DE])
    wd_d = dram("w_down", [DEPTH, NE, DE, D])
    ident_d = dram("ident", [128, 128])
    rope_d = dram("rope", [S, 192])
    wmask_d = dram("wmask", [128, 256])
    out_d = dram("out", [NB, S, D], kind="ExternalOutput")
    dbg_d = dram("dbg", [128, 8192], kind="ExternalOutput") if dbg else None

    es = ExitStack()
    with es:
        nc_ctx = es.enter_context(nc.allow_non_contiguous_dma(reason="small strided parameter loads"))
        tk = TK(nc, es)
        build_info = {}

        def sb(name, shape, dtype=F32):
            return es.enter_context(nc.sbuf_tensor(name, list(shape), dtype))

        xT = sb("xT", [128, 8, T])
        hT = sb("hT", [128, 8, T], BF16)
        arena = sb("arena", [128, 32768], BF16)
        ident_f = sb("ident_fs", [128, 128])
        ident_b = sb("ident_bs", [128, 128], BF16)
        ones_b = sb("ones_b", [128, 128], BF16)
        selb = sb("selb", [16, NE, 128], BF16)
        rw_hi = sb("rw_hi", [128, 8, NE], BF16)
        rw_lo = sb("rw_lo", [128, 8, NE], BF16)
        wmask = sb("wmask_sb", [128, 256], BF16)
        mT = sb("mT", [128, DEPTH, 48, NB + 1])
        sT = sb("sT", [128, 8, NB + 1])
        TMP = [sb("tmp%d" % i, [128, 512]) for i in range(5)]
        RB = sb("rbuf", [128, 512])
        XS = arena[:, 0:2048].bitcast(F32)
        PB = [sb("pb%d" % i, [128, 512], BF16) for i in range(3)]
        ropet = sb("ropet", [128, 192])
        small = sb("small", [128, 512])
        TQ = sb("tq", [128, 256], BF16)
        TK_ = sb("tkp", [128, 512], BF16)
        otok = sb("otok", [128, 4, 256])
        ytok = sb("ytok", [128, 256], BF16)
        nabm = sb("nabm", [128, 640], BF16)
        stat = sb("stat", [128, 64])
        PS = [es.enter_context(nc.psum_tensor("ps%d" % i, [128, 512], F32)) for i in range(8)]

        pass

        def psk(i):
            return ("ps", i)

        tmp_i = [0]

        def tmp():
            i = tmp_i[0]
            tmp_i[0] = (i + 1) % len(TMP)
            return TMP[i], ("tmp", i)

        pb_i = [0]

        def pbuf():
            i = pb_i[0]
            pb_i[0] = (i + 1) % len(PB)
            return PB[i], ("pb", i)

        A1 = small[:, 0:16].rearrange("p (j r) -> p j r", r=2)
        A2 = small[:, 16:32].rearrange("p (j r) -> p j r", r=2)
        g1T = small[:, 32:40]
        g2T = small[:, 40:48]
        ogT = small[:, 48:56]
        abT = small[:, 56:104]
        rb_bc = small[:, 104:120]
        sinkx = small[:, 120:124]
        lamt = small[:, 124:128]
        gq = small[:, 128:192]
        gk = small[:, 192:256]
        rwT = small[:, 256:384].rearrange("p (j e) -> p j e", e=NE)
        lamraw = small[:, 384:512]

        tk.dma("sp", lambda q: q.dma_start(out=ident_f[:], in_=ident_d[:, :]), writes=["ident_f"])
        tk.op("dve", lambda v: v.tensor_copy(out=ident_b[:], in_=ident_f[:]), reads=["ident_f"], writes=["ident_b"])
        tk.op("dve", lambda v: v.memset(ones_b[:], 1.0), writes=["ones_b"])
        tk.dma("sp", lambda q: q.dma_start(out=TMP[0][:, 0:256], in_=wmask_d[:, :]), writes=[("tmp", 0)])
        tk.op("dve", lambda v: v.tensor_copy(out=wmask[:], in_=TMP[0][:, 0:256]), reads=[("tmp", 0)], writes=["wmask"])
        tk.dma("sp", lambda q: q.dma_start(out=rb_bc, in_=rb_d[0:1, :].partition_broadcast(128)), writes=["small"])
        tk.dma("sp", lambda q: q.dma_start(out=rwT, in_=rw_d.rearrange("(j p) e -> p j e", p=128)), writes=["small"])
        tk.op("dve", lambda v: v.tensor_copy(out=rw_hi[:], in_=rwT), reads=["small"], writes=["rw"])
        tk.op("dve", lambda v: v.tensor_tensor(out=rw_lo[:], in0=rwT, in1=rw_hi[:], op=ALU.subtract), reads=["small", "rw"], writes=["rw2"])
        tk.op("dve", lambda v: v.tensor_copy(out=selb[:], in_=ident_b[0:16, 0:16].unsqueeze(2).broadcast_to([16, NE, 128])),
              reads=["ident_b"], writes=["selb"])
        tk.op("dve", lambda v: v.memset(TK_[:], 0.0), writes=["tkp0"])
        tk.op("dve", lambda v: v.memset(arena[:], 0.0), writes=["arena"])

        for r in range(NB + 1):
            tk.dma("sp", lambda q, r=r: q.dma_start(out=sT[:, :, r], in_=c_d[r].rearrange("(j p) -> p j", p=128)),
                   writes=["sT"])
        tk.op("act", lambda a: a.activation(out=sT[:], in_=sT[:], func=AF.Silu), reads=["sT"], writes=["sT"])
        for l in range(DEPTH):
            for j6 in range(6):
                tk.dma("sp", lambda q, l=l, j6=j6: q.dma_start(
                    out=abT[:, j6 * 8:(j6 + 1) * 8],
                    in_=ada_b[l, j6 * 1024:(j6 + 1) * 1024].rearrange("(j p) -> p j", p=128)), writes=["abT"])
            psm = PS[0][:, 0:48 * (NB + 1)].rearrange("p (j r) -> p j r", r=NB + 1)
            for j in range(48):
                wt, wk = tmp()
                wt2, wk2 = tmp()
                tk.dma("sp", lambda q, l=l, j=j, wt=wt: q.dma_start(
                    out=wt[:].rearrange("p (k c) -> p k c", c=128),
                    in_=ada_w[l, 0:512, j * 128:(j + 1) * 128].rearrange("(k p) c -> p k c", p=128)), writes=[wk])
                tk.dma("sp", lambda q, l=l, j=j, wt2=wt2: q.dma_start(
                    out=wt2[:].rearrange("p (k c) -> p k c", c=128),
                    in_=ada_w[l, 512:1024, j * 128:(j + 1) * 128].rearrange("(k p) c -> p k c", p=128)), writes=[wk2])
                for kc in range(8):
                    src, sk_ = (wt, wk) if kc < 4 else (wt2, wk2)
                    tk.op("pe", lambda t, j=j, kc=kc, src=src: t.matmul(
                        psm[:, j, :], src[:, (kc % 4) * 128:(kc % 4 + 1) * 128], sT[:, kc, :],
                        start=(kc == 0), stop=(kc == 7)),
                        reads=[sk_, "sT"], writes=[psk(0)], inc=(kc == 7))
            tk.op("dve", lambda v, l=l: v.tensor_tensor(
                out=mT[:, l], in0=psm, in1=abT.unsqueeze(2).broadcast_to([128, 48, NB + 1]), op=ALU.add),
                reads=[psk(0), "abT"], writes=["mT"])

        def load_tokens_T(src_ap, tok0):
            tk.dma("sp", lambda q: q.dma_start(out=XS[:], in_=src_ap), writes=["xs"])
            for g in range(2):
                bank = 6 + g
                for jj in range(4):
                    j = g * 4 + jj
                    tk.op("pe", lambda t, j=j, jj=jj, bank=bank: t.transpose(
                        out=PS[bank][:, jj * 128:(jj + 1) * 128], in_=XS[:, j * 128:(j + 1) * 128],
                        identity=ident_f[:]), reads=["xs", "ident_f"], writes=[psk(bank)], inc=(jj == 3))
                eng = "act" if g == 0 else "dve"
                if eng == "act":
                    tk.op("act", lambda a, g=g, bank=bank: a.copy(
                        out=xT[:, g * 4:(g + 1) * 4, tok0:tok0 + 128],
                        in_=PS[bank][:].rearrange("p (j t) -> p j t", t=128)),
                        reads=[psk(bank)], writes=[("xT", tok0 // 128)])
                else:
                    tk.op("dve", lambda v, g=g, bank=bank: v.tensor_copy(
                        out=xT[:, g * 4:(g + 1) * 4, tok0:tok0 + 128],
                        in_=PS[bank][:].rearrange("p (j t) -> p j t", t=128)),
                        reads=[psk(bank)], writes=[("xT", tok0 // 128)])

        def xkeys(t0, n):
            return [("xT", i) for i in range(t0 // 128, (t0 + n) // 128)]

        def hkeys(t0, n):
            return [("hT", i) for i in range(t0 // 128, (t0 + n) // 128)]

        def norm_to_h(t0, n, Aap, Bap, router_bank=None):
            xk = xkeys(t0, n)
            hk = hkeys(t0, n)
            for j in range(8):
                sq, sqk = pbuf()
                tk.op("act", lambda a, j=j, sq=sq: a.activation(out=sq[:, :n], in_=xT[:, j, t0:t0 + n], func=AF.Square),
                      reads=xk, writes=[sqk])
                tk.op("pe", lambda t, j=j, sq=sq: t.matmul(PS[5][:, :n], ones_b[:], sq[:, :n], start=(j == 0), stop=(j == 7)),
                      reads=[sqk, "ones_b"], writes=[psk(5)], inc=(j == 7))
            rb, rbk = RB, "rbuf"
            tk.op("act", lambda a: a.activation(out=rb[:, :n], in_=PS[5][:, :n], func=AF.Sqrt, bias=EPS, scale=1.0 / D),
                  reads=[psk(5)], writes=[rbk])
            tk.op("dve", lambda v: v.reciprocal(out=rb[:, :n], in_=rb[:, :n]), reads=[rbk], writes=[rbk])
            for j in range(8):
                t1, t1k = tmp()
                tk.op("dve", lambda v, j=j, t1=t1: v.tensor_tensor(out=t1[:, :n], in0=xT[:, j, t0:t0 + n], in1=rb[:, :n],
                                                                    op=ALU.mult), reads=xk + [rbk], writes=[t1k])
                if router_bank is None:
                    tk.op("act", lambda a, j=j, t1=t1: a.activation(
                        out=hT[:, j, t0:t0 + n], in_=t1[:, :n], func=AF.Identity, scale=Aap[:, j:j + 1], bias=Bap[:, j:j + 1]),
                        reads=[t1k, "small", "mT"], writes=hk)
                else:
                    tk.op("act", lambda a, j=j, t1=t1: a.activation(
                        out=t1[:, :n], in_=t1[:, :n], func=AF.Identity, scale=Aap[:, j:j + 1], bias=Bap[:, j:j + 1]),
                        reads=[t1k, "small", "mT"], writes=[t1k])
                    tk.op("pool", lambda g, j=j, t1=t1: g.tensor_copy(out=hT[:, j, t0:t0 + n], in_=t1[:, :n]),
                          reads=[t1k], writes=hk)
                    lo, lok = pbuf()
                    tk.op("dve", lambda v, j=j, t1=t1, lo=lo: v.tensor_tensor(out=lo[:, :n], in0=t1[:, :n], in1=hT[:, j, t0:t0 + n],
                                                                              op=ALU.subtract), reads=[t1k] + hk, writes=[lok])
                    tk.op("pe", lambda t, j=j: t.matmul(PS[router_bank][0:16, :n], rw_hi[:, j, :], hT[:, j, t0:t0 + n],
                                                        start=(j == 0), stop=False),
                          reads=hk + ["rw"], writes=[psk(router_bank)], inc=False)
                    tk.op("pe", lambda t, j=j: t.matmul(PS[router_bank][0:16, :n], rw_lo[:, j, :], hT[:, j, t0:t0 + n],
                                                        start=False, stop=False),
                          reads=hk + ["rw2"], writes=[psk(router_bank)], inc=False)
                    tk.op("pe", lambda t, j=j, lo=lo: t.matmul(PS[router_bank][0:16, :n], rw_hi[:, j, :], lo[:, :n],
                                                               start=False, stop=(j == 7)),
                          reads=[lok, "rw"], writes=[psk(router_bank)], inc=(j == 7))

        def dump(ap, width, col0, keys):
            if dbg_d is None:
                return
            tk.dma("pool", lambda q: q.dma_start(out=dbg_d[0:ap.shape[0], col0:col0 + width], in_=ap), reads=keys)

        QT = arena[:, 0:4608].rearrange("p (c t) -> p c t", c=2)
        KT = arena[:, 4608:13824].rearrange("p (c t) -> p c t", c=4)
        VA = arena[:, 13824:18576].rearrange("p (t h d) -> p t h d", t=NTT, h=4)
        YT = arena[:, 18576:23184].rearrange("p (c t) -> p c t", c=2)
        WI = arena[:, 23184:29328].rearrange("p (j c) -> p j c", j=8)
        WO = arena[:, 29328:31376].rearrange("p (k c) -> p k c", k=2)
        EW = [arena[:, i * 12288:(i + 1) * 12288] for i in range(2)]
        HM = arena[:, 24576:26624].rearrange("p (f t) -> p f t", f=4)
        WT = arena[0:16, 26624:31232].bitcast(F32) if False else None

        wrt_hi = arena[0:16, 26624:26624 + T]
        wrt_lo = arena[0:16, 26624 + T:26624 + 2 * T]

        def qk_post(ps_ap, nh, hd, gain_ap, rope_tile, out_views, is_lat, bank, okey, slot, cs):
            width = nh * hd
            Wb, Wk = TMP[2 * cs], ("tmp", 2 * cs)
            Yb, Yk = TMP[2 * cs + 1], ("tmp", 2 * cs + 1)
            sq = Wb[:, 0:width]
            X = Wb[:, 256:256 + width]
            Y = Yb[:, 0:width]
            tk.op("act", lambda a: a.activation(out=sq, in_=ps_ap, func=AF.Square), reads=[psk(bank)], writes=[Wk])
            yield
            sc0 = [0, 40, 48, 56][slot]
            stk = "statq%d" % slot
            ss = stat[:, sc0:sc0 + nh]
            tk.op("dve", lambda v: v.tensor_reduce(out=ss, in_=sq.rearrange("p (h d) -> p h d", h=nh),
                                                   axis=AX.X, op=ALU.add), reads=[Wk], writes=[stk])
            yield
            tk.op("act", lambda a: a.activation(out=ss, in_=ss, func=AF.Sqrt, bias=EPS, scale=1.0 / hd),
                  reads=[stk], writes=[stk])
            yield
            tk.op("dve", lambda v: v.reciprocal(out=ss, in_=ss), reads=[stk], writes=[stk])
            yield
            tk.op("dve", lambda v: v.tensor_tensor(
                out=X.rearrange("p (h d) -> p h d", h=nh), in0=ps_ap.rearrange("p (h d) -> p h d", h=nh),
                in1=ss.unsqueeze(2).broadcast_to([128, nh, hd]), op=ALU.mult),
                reads=[psk(bank), stk], writes=[Wk])
            yield
            tk.op("pool", lambda g: g.tensor_tensor(
                out=X.rearrange("p (h d) -> p h d", h=nh), in0=X.rearrange("p (h d) -> p h d", h=nh),
                in1=gain_ap.unsqueeze(1).broadcast_to([128, nh, hd]), op=ALU.mult),
                reads=[Wk, "small"], writes=[Wk])
            yield
            if not is_lat:
                for (oap, sel) in out_views:
                    src = X if sel is None else sel(X)
                    tk.op("dve", lambda v, oap=oap, src=src: v.tensor_copy(out=oap, in_=src), reads=[Wk], writes=[okey])
                    yield
                return
            cos_ap, sin_ap = rope_tile
            e4 = hd // 4
            tk.op("pool", lambda g: g.tensor_tensor(
                out=Y.rearrange("p (h d) -> p h d", h=nh), in0=X.rearrange("p (h d) -> p h d", h=nh),
                in1=cos_ap.unsqueeze(1).broadcast_to([128, nh, hd]), op=ALU.mult), reads=[Wk, "ropet"], writes=[Yk])
            yield
            qv = X.rearrange("p (h b s e) -> p h b s e", h=nh, b=2, s=2)
            tv = sq.rearrange("p (h b s e) -> p h b s e", h=nh, b=2, s=2)
            sv = sin_ap.rearrange("p (b s e) -> p b s e", b=2, s=2)
            for s_ in range(2):
                tk.op("dve", lambda v, s_=s_: v.tensor_tensor(
                    out=tv[:, :, :, s_, :], in0=qv[:, :, :, 1 - s_, :],
                    in1=sv[:, :, s_, :].unsqueeze(1).broadcast_to([128, nh, 2, e4]), op=ALU.mult),
                    reads=[Wk, "ropet"], writes=[Wk])
                yield
            for (oap, sel) in out_views:
                a_ = Y if sel is None else sel(Y)
                b_ = sq if sel is None else sel(sq)
                tk.op("dve", lambda v, oap=oap, a_=a_, b_=b_: v.tensor_tensor(out=oap, in0=a_, in1=b_, op=ALU.add),
                      reads=[Wk, Yk], writes=[okey])
                yield

        def run_zipped(gens):
            gens = list(gens)
            while gens:
                for g_ in list(gens):
                    try:
                        next(g_)
                    except StopIteration:
                        gens.remove(g_)

        ps_bank = [0]
        out_key = ["tq"]
        stat_slot = [0]
        unit_ctr = [0]
        nabm_state = [None]
        TQb = [TQ[:, :], arena[:, 31376:31632]]
        TKb = [TK_[:, :], arena[:, 31632:32144]]

        if dbg == "p0":
            tk.barrier()
            dump(mT[:, 0].rearrange("p j r -> p (j r)"), 48 * (NB + 1), 0, [])
            tk.barrier()
            return nc
        for b in range(NB):
          try:
            for tt in range(16):
                load_tokens_T(x_d[b, tt * 128:(tt + 1) * 128, :], tt * 128)
            for tt in range(2):
                load_tokens_T(ctx_d[b, tt * 128:(tt + 1) * 128, :], S + tt * 128)
            tk.barrier()
            if dbg == "ld":
                dump(xT[:, 0, :], 2304, 0, [])
                dump(xT[:, 7, :], 2304, 2304, [])
                tk.barrier()
                return nc

            for l in range(DEPTH):
                pass
                need_ctx = l < DEPTH - 1
                lam_init = 0.8 - 0.6 * math.exp(-0.3 * l)
                mrow = lambda k, r: mT[:, l, k * 8:(k + 1) * 8, r]
                tk.dma("sp", lambda q: q.dma_start(out=g1T, in_=ng1[l].rearrange("(j p) -> p j", p=128)), writes=["small"])
                tk.dma("sp", lambda q: q.dma_start(out=g2T, in_=ng2[l].rearrange("(j p) -> p j", p=128)), writes=["small"])
                tk.dma("sp", lambda q: q.dma_start(out=ogT, in_=og_d[l].rearrange("(j p) -> p j", p=128)), writes=["small"])
                tk.dma("sp", lambda q: q.dma_start(out=sinkx, in_=sink_d[l:l + 1, :].partition_broadcast(128)), writes=["small"])
                tk.dma("sp", lambda q: q.dma_start(
                    out=lamraw, in_=lam_d[l:l + 1].rearrange("o a d -> o (a d)").partition_broadcast(128)), writes=["small"])
                for r in range(2):
                    rr = b if r == 0 else NB
                    tk.op("dve", lambda v, r=r, rr=rr: v.scalar_tensor_tensor(
                        out=A1[:, :, r], in0=mrow(1, rr), scalar=1.0, in1=g1T, op0=ALU.add, op1=ALU.mult),
                        reads=["small", "mT"], writes=["small"])
                    tk.op("dve", lambda v, r=r, rr=rr: v.scalar_tensor_tensor(
                        out=A2[:, :, r], in0=mrow(4, rr), scalar=1.0, in1=g2T, op0=ALU.add, op1=ALU.mult),
                        reads=["small", "mT"], writes=["small"])
                tk.op("act", lambda a: a.activation(out=sinkx, in_=sinkx, func=AF.Exp), reads=["small"], writes=["small"])
                lr = lamraw.rearrange("p (a d) -> p a d", a=4)
                lw = stat[:, 32:36]
                tk.op("dve", lambda v: v.tensor_tensor(out=lamraw[:, 0:32], in0=lr[:, 0, :], in1=lr[:, 1, :], op=ALU.mult),
                      reads=["small"], writes=["small"])
                tk.op("dve", lambda v: v.tensor_tensor(out=lamraw[:, 64:96], in0=lr[:, 2, :], in1=lr[:, 3, :], op=ALU.mult),
                      reads=["small"], writes=["small"])
                tk.op("dve", lambda v: v.tensor_reduce(out=lw[:, 0:2], in_=lamraw.rearrange("p (a d) -> p a d", a=2)[:, :, 0:32],
                                                       axis=AX.X, op=ALU.add), reads=["small"], writes=["stat2"])
                tk.op("act", lambda a: a.activation(out=lw[:, 0:2], in_=lw[:, 0:2], func=AF.Exp), reads=["stat2"], writes=["stat2"])
                tk.op("dve", lambda v: v.tensor_tensor(out=lw[:, 2:3], in0=lw[:, 0:1], in1=lw[:, 1:2], op=ALU.subtract),
                      reads=["stat2"], writes=["stat2"])
                tk.op("act", lambda a: a.activation(out=lamt[:, 0:1], in_=lw[:, 2:3], func=AF.Identity, scale=-1.0, bias=-lam_init),
                      reads=["stat2"], writes=["small"])

                if dbg == "sm":
                    print("nops at sm", tk.nops)
                    tk.barrier()
                    dump(small[:, :], 512, 0, [])
                    tk.barrier()
                    return nc
                tk.mark("b%d l%d norm1" % (b, l))
                for cch in range(4):
                    norm_to_h(cch * 512, 512, A1[:, :, 0], mrow(0, b))
                norm_to_h(S, L, A1[:, :, 1], mrow(0, NB))
                if dbg == "h":
                    tk.barrier()
                    dump(hT[:, 0, 0:2304], 2304, 0, hkeys(0, T))
                    dump(hT[:, 7, 0:2304], 2304, 2304, hkeys(0, T))
                    tk.barrier()
                    return nc

                for mi in (0, 1, 3, 2):
                    (mname, col0, nq, nk, nv, nkv, hd) = MIX[mi]
                    ncols = nq + nk + nv
                    nh_q = nq // hd
                    nh_k = nk // hd
                    scale = hd ** -0.5
                    tk.dma("pool", lambda q: q.dma_start(
                        out=WI[:, :, 0:ncols], in_=w_in[l, :, col0:col0 + ncols].rearrange("(j p) c -> p j c", p=128)),
                        writes=["WI"])
                    tk.dma("pool", lambda q: q.dma_start(
                        out=WO[:, :, :], in_=w_out[l, mi * 256:(mi + 1) * 256, :].rearrange("(k p) c -> p k c", p=128)),
                        writes=["WO"])
                    gd = qkg[mname]
                    tk.dma("sp", lambda q: q.dma_start(out=gq[:, 0:hd], in_=gd[l, 0:1, :].partition_broadcast(128)), writes=["small"])
                    tk.dma("sp", lambda q: q.dma_start(out=gk[:, 0:hd], in_=gd[l, 1:2, :].partition_broadcast(128)), writes=["small"])
                    tk.op("dve", lambda v: v.tensor_scalar(out=gq[:, 0:hd], in0=gq[:, 0:hd], scalar1=scale, scalar2=None,
                                                           op0=ALU.mult), reads=["small"], writes=["small"])
                    tk.op("dve", lambda v: v.memset(VA[:, :, :, 64:65], 1.0), writes=["VA"])

                    tk.mark("b%d l%d %s proj" % (b, l, mname))
                    tk.op("dve", lambda v: v.memset(TKb[0], 0.0), writes=["tkp0"])
                    tk.op("dve", lambda v: v.memset(TKb[1], 0.0), writes=["tkp1"])
                    nkc = 4 if mname == "C" else nkv
                    if mname == "C":
                        tk.op("dve", lambda v: v.memset(PB[0][:], 0.0), writes=[("pb", 0)])
                        tk.op("dve", lambda v: v.memset(PB[1][:], 0.0), writes=[("pb", 1)])

                    def tile_banks(tt):
                        return [0, 1] if tt % 2 == 0 else [2, 3]

                    def emit_mm(tt):
                        banks = tile_banks(tt)
                        pieces = [(0, min(512, ncols), banks[0])]
                        if ncols > 512:
                            pieces.append((512, ncols, banks[1]))
                        for (c0, c1, bank) in pieces:
                            for j in range(8):
                                tk.op("pe", lambda t, j=j: t.matmul(
                                    PS[bank][:, 0:c1 - c0], hT[:, j, tt * 128:(tt + 1) * 128], WI[:, j, c0:c1],
                                    start=(j == 0), stop=(j == 7)), reads=[("hT", tt), "WI"], writes=[psk(bank)])

                    def emit_chains(tt):
                        is_lat = tt < 16 and mname != "B"
                        banks = tile_banks(tt)
                        par = tt % 2
                        TQc = TQb[par]
                        TKc = TKb[par]
                        if tt < 16 and mname != "B":
                            tk.dma("sp", lambda q: q.dma_start(out=ropet[:], in_=rope_d[tt * 128:(tt + 1) * 128, :]), writes=["ropet"])
                        rt = (ropet[:, 0:64], ropet[:, 64:128]) if hd == 64 else (ropet[:, 128:160], ropet[:, 160:192])
                        if mname in ("A", "D"):
                            qviews = [(TQc.rearrange("p (c s d) -> p s c d", c=2, s=2),
                                       (lambda ap: ap.rearrange("p (s c d) -> p s c d", s=2, c=2)))]
                        elif mname == "C":
                            tqv = PB[par][:, 0:512].rearrange("p (c s d) -> p c s d", c=4, s=2)
                            qviews = [(tqv[:, h_, h_ % 2, :], (lambda ap, h_=h_: ap[:, h_ * 64:(h_ + 1) * 64])) for h_ in range(4)]
                        else:
                            qviews = [(TQc, None)]
                        gq_ = qk_post(PS[banks[0]][:, 0:256], nh_q if hd == 64 else 8, hd, gq[:, 0:hd], rt, qviews, is_lat,
                                      banks[0], ("pb", par) if mname == "C" else "tq%d" % par, par * 2, 0)
                        if mname == "C":
                            tkv = TKc.rearrange("p (hp i hh i2 d) -> p hp i hh i2 d", hp=2, i=2, hh=2, i2=2)
                            views = []
                            for i_ in range(2):
                                views.append((tkv[:, :, i_, :, i_, :],
                                              (lambda ap, i_=i_: ap.rearrange("p (hp hh i d) -> p hp hh i d", hp=2, hh=2, i=2)[:, :, :, i_, :])))
                            gk_ = qk_post(PS[banks[0]][:, 256:512], 8, 32, gk[:, 0:32], rt, views, is_lat, banks[0], "tkp%d" % par, par * 2 + 1, 1)
                        else:
                            tkv = TKc[:, 0:nkv * 128].rearrange("p (c s d) -> p c s d", c=nkv, s=2)
                            views = [(tkv[:, kvh, kvh % 2, :], (lambda ap, kvh=kvh: ap[:, kvh * 64:(kvh + 1) * 64])) for kvh in range(nkv)]
                            gk_ = qk_post(PS[banks[0]][:, 256:256 + nk], nh_k, hd, gk[:, 0:hd], rt, views, is_lat,
                                          banks[0], "tkp%d" % par, par * 2 + 1, 1)
                        run_zipped([gq_, gk_])
                        if nk == 128:
                            vsrc = PS[banks[0]][:, 384:512]
                            vb = banks[0]
                        else:
                            vsrc = PS[banks[1]][:, 0:256]
                            vb = banks[1]
                        tk.op("act", lambda a: a.copy(out=VA[:, tt, 0:nkv, 0:64], in_=vsrc.rearrange("p (h d) -> p h d", h=nkv)),
                              reads=[psk(vb)], writes=[("VA", tt)])

                    def emit_tr(tt):
                        par = tt % 2
                        TQc = TQb[par]
                        TKc = TKb[par]
                        if mname == "C":
                            for cc in range(4):
                                tk.op("pe", lambda t, cc=cc: t.transpose(
                                    out=PS[4][:].bitcast(BF16)[:, cc * 128:(cc + 1) * 128], in_=PB[par][:, cc * 128:(cc + 1) * 128],
                                    identity=ident_b[:]), reads=[("pb", par), "ident_b"], writes=[psk(4)])
                            tk.op("act", lambda a: a.copy(out=hT[:, 0:4, tt * 128:(tt + 1) * 128],
                                                          in_=PS[4][:].bitcast(BF16)[:, 0:512].rearrange("p (c t) -> p c t", c=4)),
                                  reads=[psk(4)], writes=[("QT", tt), ("hT", tt)])
                        else:
                            for cc in range(2):
                                tk.op("pe", lambda t, cc=cc: t.transpose(
                                    out=PS[4][:].bitcast(BF16)[:, cc * 128:(cc + 1) * 128], in_=TQc[:, cc * 128:(cc + 1) * 128],
                                    identity=ident_b[:]), reads=["tq%d" % par, "ident_b"], writes=[psk(4)])
                            tk.op("act", lambda a: a.copy(out=QT[:, :, tt * 128:(tt + 1) * 128],
                                                          in_=PS[4][:].bitcast(BF16)[:, 0:256].rearrange("p (c t) -> p c t", c=2)),
                                  reads=[psk(4)], writes=[("QT", tt)])
                        for cc in range(nkc):
                            tk.op("pe", lambda t, cc=cc: t.transpose(
                                out=PS[5][:].bitcast(BF16)[:, cc * 128:(cc + 1) * 128],
                                in_=TKc[:, cc * 128:(cc + 1) * 128], identity=ident_b[:]),
                                reads=["tkp%d" % par, "ident_b"], writes=[psk(5)])
                        tk.op("act", lambda a: a.copy(
                            out=KT[:, 0:nkc, tt * 128:(tt + 1) * 128],
                            in_=PS[5][:].bitcast(BF16)[:, 0:nkc * 128].rearrange("p (c t) -> p c t", c=nkc)),
                            reads=[psk(5)], writes=[("KT", tt)])

                    emit_mm(0)
                    for tt in range(NTT):
                        emit_chains(tt)
                        if tt + 1 < NTT:
                            emit_mm(tt + 1)
                        emit_tr(tt)
                    if dbg == "qkv" and mname == dbg_mixer[0]:
                        tk.barrier()
                        dump(QT[:, 0, :], 2304, 0, [])
                        dump(KT[:, 0, :], 2304, 2304, [])
                        dump(VA[:, 3, :, :].rearrange("p h d -> p (h d)"), 264, 4608, [])
                        dump(KT[:, 3, :], 2304, 4900, [])
                        tk.barrier()
                        return nc

                    tk.mark("b%d l%d %s attn" % (b, l, mname))
                    ranges = [(r0 * 512, 512) for r0 in range(4)] + ([(S, L)] if need_ctx else [])
                    pending = []

                    def flush():
                        for f_ in pending:
                            f_()
                        del pending[:]

                    for (q0, n) in ranges:
                        nblk = n // 128
                        is_ctxq = q0 >= S
                        units = []
                        if mname in ("A", "D"):
                            for h in range(4):
                                units.append((h, 0, h % 2, (h // 2) * 64, h // 2, (h // 2) * 64, h // 2))
                        elif mname == "B":
                            for h in range(4):
                                units.append((h, 0, h // 2, (h % 2) * 64, h, (h % 2) * 64, h))
                        else:
                            for h in range(4):
                                for i_ in range(2):
                                    units.append((h, i_, h // 2, (h % 2) * 64, (h // 2) * 2 + i_, (h % 2) * 64, h))
                        for (h, br, qc, qp, kc_, kp, vh) in units:
                            ob = 6 if unit_ctr[0] % 2 == 0 else 3
                            unit_ctr[0] += 1
                            steps = []
                            if is_ctxq or mname in ("C", "D"):
                                kts = [16, 17] if is_ctxq else list(range(18))
                                for ki, kt in enumerate(kts):
                                    steps.append((0, n, kt, None, None, ki == 0, ki == len(kts) - 1, None))
                            elif mname == "A":
                                for bl in range(nblk):
                                    i = q0 // 128 + bl
                                    lst = []
                                    if i - 1 >= 0:
                                        lst.append((i - 1, wmask[:, 0:128], "wmask"))
                                    lst.append((i, None, None))
                                    if i + 1 < 16:
                                        lst.append((i + 1, wmask[:, 128:256], "wmask"))
                                    lst += [(16, None, None), (17, None, None)]
                                    for ki, (kt, ma, mk_) in enumerate(lst):
                                        steps.append((bl * 128, 128, kt, ma, mk_, ki == 0, ki == len(lst) - 1, None))
                            else:
                                for bl in range(nblk):
                                    i = q0 // 128 + bl
                                    ts = na_ts(i)
                                    vi = na_variant(i)

                                    def load_mask(vi=vi, h=h):
                                        if nabm_state[0] == (b, l, h, vi):
                                            return
                                        nabm_state[0] = (b, l, h, vi)
                                        stg, stgk = tmp()
                                        stg2, stg2k = tmp()
                                        tk.dma("sp", lambda q: q.dma_start(out=stg[:, 0:512], in_=nab_d[l, h, vi, :, 0:512]), writes=[stgk])
                                        tk.dma("sp", lambda q: q.dma_start(out=stg2[:, 0:128], in_=nab_d[l, h, vi, :, 512:640]), writes=[stg2k])
                                        tk.op("act", lambda a: a.activation(out=nabm[:, 0:512], in_=stg[:, 0:512], func=AF.Exp),
                                              reads=[stgk], writes=["nabm"])
                                        tk.op("act", lambda a: a.activation(out=nabm[:, 512:640], in_=stg2[:, 0:128], func=AF.Exp),
                                              reads=[stg2k], writes=["nabm"])
                                    lst = [(ts + j, nabm[:, j * 128:(j + 1) * 128], "nabm") for j in range(5)]
                                    lst += [(16, None, None), (17, None, None)]
                                    for ki, (kt, ma, mk_) in enumerate(lst):
                                        steps.append((bl * 128, 128, kt, ma, mk_, ki == 0, ki == len(lst) - 1, load_mask if ki == 0 else None))

                            def emit_qk(si, st):
                                (qa, qn, kt, mask_ap, mkey, first, last, pre) = st
                                if pre is not None:
                                    pre()
                                sbank = 4 + (si % 2)
                                if mname == "C":
                                    k_ap = KT[:, kc_, kt * 128:(kt + 1) * 128]
                                    q_ap = hT[:, h, q0 + qa:q0 + qa + qn]
                                else:
                                    k_ap = KT[:, kc_, kt * 128:(kt + 1) * 128]
                                    q_ap = QT[:, qc, q0 + qa:q0 + qa + qn]
                                tk.op("pe", lambda t: t.matmul(PS[sbank][:, 0:qn], k_ap, q_ap, start=True, stop=True),
                                    reads=[("KT", kt)] + [("QT", (q0 + qa) // 128 + z) for z in range(qn // 128)], writes=[psk(sbank)])
                                pbt, pbk = pbuf()
                                tk.op("act", lambda a: a.activation(out=pbt[:, 0:qn], in_=PS[sbank][:, 0:qn], func=AF.Exp),
                                      reads=[psk(sbank)], writes=[pbk])
                                if mask_ap is not None:
                                    tk.op("dve", lambda v: v.tensor_tensor(out=pbt[:, 0:qn], in0=pbt[:, 0:qn], in1=mask_ap, op=ALU.mult),
                                          reads=[pbk, mkey], writes=[pbk])
                                return pbt, pbk

                            def emit_pv(st, pbt, pbk):
                                (qa, qn, kt, mask_ap, mkey, first, last, pre) = st
                                tk.op("pe", lambda t: t.matmul(PS[ob][0:65, qa:qa + qn], VA[:, kt, vh, 0:65], pbt[:, 0:qn], start=first, stop=last),
                                      reads=[("VA", kt), "VA", pbk], writes=[psk(ob)])

                            prev = None
                            for si, st in enumerate(steps):
                                cur = emit_qk(si, st)
                                if prev is not None:
                                    emit_pv(*prev)
                                prev = (st,) + cur
                                if si == min(2, len(steps) - 1):
                                    flush()
                            emit_pv(*prev)

                            def post(h=h, br=br, ob=ob, nblk=nblk, n=n):
                                ot, otk = tmp()
                                tk.op("dve", lambda v: v.tensor_copy(out=ot[0:65, 0:n], in_=PS[ob][0:65, 0:n]), reads=[psk(ob)], writes=[otk])
                                for bl in range(nblk):
                                    tk.op("pe", lambda t, bl=bl: t.transpose(
                                        out=PS[7][:, bl * 128:bl * 128 + 65], in_=ot[0:65, bl * 128:(bl + 1) * 128],
                                        identity=ident_f[0:65, 0:65]), reads=[otk, "ident_f"], writes=[psk(7)])
                                p7 = PS[7][:].rearrange("p (b d) -> p b d", d=128)
                                rd = stat[:, 8:8 + nblk]
                                if mname == "A":
                                    tk.op("dve", lambda v: v.tensor_scalar(out=rd, in0=p7[:, 0:nblk, 64], scalar1=sinkx[:, h:h + 1],
                                                                           scalar2=None, op0=ALU.add), reads=[psk(7), "small"], writes=["stat3"])
                                    tk.op("dve", lambda v: v.reciprocal(out=rd, in_=rd), reads=["stat3"], writes=["stat3"])
                                else:
                                    tk.op("dve", lambda v: v.reciprocal(out=rd, in_=p7[:, 0:nblk, 64]), reads=[psk(7)], writes=["stat3"])
                                if br == 1:
                                    tk.op("dve", lambda v: v.tensor_scalar(out=rd, in0=rd, scalar1=lamt[:, 0:1], scalar2=None, op0=ALU.mult),
                                          reads=["stat3", "small"], writes=["stat3"])
                                for bl in range(nblk):
                                    if br == 0:
                                        tk.op("dve", lambda v, bl=bl: v.tensor_scalar(
                                            out=otok[:, bl, h * 64:(h + 1) * 64], in0=p7[:, bl, 0:64], scalar1=rd[:, bl:bl + 1],
                                            scalar2=None, op0=ALU.mult), reads=[psk(7), "stat3"], writes=[("otok", bl)])
                                    else:
                                        tk.op("dve", lambda v, bl=bl: v.scalar_tensor_tensor(
                                            out=otok[:, bl, h * 64:(h + 1) * 64], in0=p7[:, bl, 0:64], scalar=rd[:, bl:bl + 1],
                                            in1=otok[:, bl, h * 64:(h + 1) * 64], op0=ALU.mult, op1=ALU.add),
                                            reads=[psk(7), "stat3", ("otok", bl)], writes=[("otok", bl)])
                            pending.append(post)

                        def merge(q0=q0, nblk=nblk):
                            for bl in range(nblk):
                                tok = q0 + bl * 128
                                ng = 1 if mname != "C" else 4
                                gsz = 256 // ng
                                sq, sqk = tmp()
                                tk.op("act", lambda a: a.activation(out=sq[:, 0:256], in_=otok[:, bl, :], func=AF.Square),
                                      reads=[("otok", bl)], writes=[sqk])
                                ssg = stat[:, 16:16 + ng]
                                tk.op("dve", lambda v: v.tensor_reduce(out=ssg, in_=sq[:, 0:256].rearrange("p (g d) -> p g d", g=ng),
                                                                       axis=AX.X, op=ALU.add), reads=[sqk], writes=["stat4"])
                                tk.op("act", lambda a: a.activation(out=ssg, in_=ssg, func=AF.Sqrt, bias=EPS, scale=1.0 / gsz),
                                      reads=["stat4"], writes=["stat4"])
                                tk.op("dve", lambda v: v.reciprocal(out=ssg, in_=ssg), reads=["stat4"], writes=["stat4"])
                                if mname == "C":
                                    tk.op("dve", lambda v: v.tensor_scalar(out=ssg, in0=ssg, scalar1=(1.0 - lam_init), scalar2=None,
                                                                           op0=ALU.mult), reads=["stat4"], writes=["stat4"])
                                tk.op("dve", lambda v: v.tensor_tensor(
                                    out=ytok[:].rearrange("p (g d) -> p g d", g=ng), in0=otok[:, bl, :].rearrange("p (g d) -> p g d", g=ng),
                                    in1=ssg.unsqueeze(2).broadcast_to([128, ng, gsz]), op=ALU.mult),
                                    reads=[("otok", bl), "stat4"], writes=["ytok"])
                                for cc in range(2):
                                    tk.op("pe", lambda t, cc=cc: t.transpose(
                                        out=PS[2][:].bitcast(BF16)[:, cc * 128:(cc + 1) * 128],
                                        in_=ytok[:, cc * 128:(cc + 1) * 128], identity=ident_b[:]),
                                        reads=["ytok", "ident_b"], writes=[psk(2)])
                                for cc in range(2):
                                    tk.op("act", lambda a, cc=cc: a.activation(
                                        out=YT[:, cc, tok:tok + 128], in_=PS[2][:].bitcast(BF16)[:, cc * 128:(cc + 1) * 128],
                                        func=AF.Identity, scale=ogT[:, mi * 2 + cc:mi * 2 + cc + 1]),
                                        reads=[psk(2), "small"], writes=[("YT", tok // 128)])
                        pending.append(merge)
                    flush()
                    if dbg == "y" and mname == dbg_mixer[0]:
                        tk.barrier()
                        dump(YT[:, 0, :], 2304, 0, [])
                        dump(YT[:, 1, :], 2304, 2304, [])
                        tk.barrier()
                        return nc

                    tk.mark("b%d l%d %s wout" % (b, l, mname))
                    for (q0, n) in ranges:
                        rr = NB if q0 >= S else b
                        for jo in range(8):
                            bank = jo % 4
                            for kc2 in range(2):
                                tk.op("pe", lambda t, jo=jo, kc2=kc2, bank=bank: t.matmul(
                                    PS[bank][:, 0:n], WO[:, kc2, jo * 128:(jo + 1) * 128], YT[:, kc2, q0:q0 + n],
                                    start=(kc2 == 0), stop=(kc2 == 1)),
                                    reads=["WO"] + [("YT", q0 // 128 + z) for z in range(n // 128)], writes=[psk(bank)],
                                    inc=(kc2 == 1))
                            tk.op("dve", lambda v, jo=jo, bank=bank, rr=rr: v.scalar_tensor_tensor(
                                out=xT[:, jo, q0:q0 + n], in0=PS[bank][:, 0:n], scalar=mT[:, l, 16 + jo, rr:rr + 1],
                                in1=xT[:, jo, q0:q0 + n], op0=ALU.mult, op1=ALU.add),
                                reads=[psk(bank), "mT"] + xkeys(q0, n), writes=xkeys(q0, n))
                    tk.barrier()
                if dbg == "xattn":
                    dump(xT[:, 0, :], 2304, 0, [])
                    dump(xT[:, 5, :], 2304, 2304, [])
                    tk.barrier()
                    return nc

                if dbg == "xattn2":
                    dump(xT[:, 0, :], 2304, 0, [])
                    dump(xT[:, 5, :], 2304, 2304, [])
                    tk.barrier()
                    return nc
                tk.mark("b%d l%d norm2+route" % (b, l))
                ranges = [(r0 * 512, 512) for r0 in range(4)] + ([(S, L)] if need_ctx else [])
                for (q0, n) in ranges:
                    r = 1 if q0 >= S else 0
                    rr = NB if q0 >= S else b
                    norm_to_h(q0, n, A2[:, :, r], mrow(3, rr), router_bank=6)
                    lg, lgk = RB, "rbuf"
                    tk.op("dve", lambda v, lg=lg: v.tensor_copy(out=lg[0:16, 0:n], in_=PS[6][0:16, 0:n]), reads=[psk(6)], writes=[lgk])
                    for bl in range(n // 128):
                        tk.op("pe", lambda t, bl=bl, lg=lg: t.transpose(out=PS[7][:, 0:16], in_=lg[0:16, bl * 128:(bl + 1) * 128],
                                                                       identity=ident_f[0:16, 0:16]),
                              reads=[lgk, "ident_f"], writes=[psk(7)])
                        sc = stat[:, 0:16]
                        sel = stat[:, 16:32]
                        w8 = stat[:, 32:40]
                        tk.op("act", lambda a: a.activation(out=sc, in_=PS[7][:, 0:16], func=AF.Sigmoid), reads=[psk(7)], writes=["stat"])
                        tk.op("dve", lambda v: v.tensor_tensor(out=sel, in0=sc, in1=rb_bc, op=ALU.add), reads=["stat", "small"], writes=["stat"])
                        s4 = sel.rearrange("p (g a c) -> p g a c", g=4, a=2)
                        pq = stat[:, 40:48].rearrange("p (g a) -> p g a", g=4)
                        rs_ = stat[:, 48:56].rearrange("p (g a) -> p g a", g=4)
                        tk.op("dve", lambda v: v.tensor_tensor(out=pq, in0=s4[:, :, :, 0], in1=s4[:, :, :, 1], op=ALU.max), reads=["stat"], writes=["stat"])
                        tk.op("dve", lambda v: v.tensor_tensor(out=rs_, in0=s4[:, :, :, 0], in1=s4[:, :, :, 1], op=ALU.min), reads=["stat"], writes=["stat"])
                        m1 = stat[:, 56:60]
                        m2_ = stat[:, 60:64]
                        tk.op("dve", lambda v: v.tensor_tensor(out=m1, in0=pq[:, :, 0], in1=pq[:, :, 1], op=ALU.max), reads=["stat"], writes=["stat"])
                        tk.op("dve", lambda v: v.tensor_tensor(out=m2_, in0=pq[:, :, 0], in1=pq[:, :, 1], op=ALU.min), reads=["stat"], writes=["stat"])
                        tk.op("dve", lambda v: v.tensor_tensor(out=pq[:, :, 0], in0=rs_[:, :, 0], in1=rs_[:, :, 1], op=ALU.max), reads=["stat"], writes=["stat"])
                        tk.op("dve", lambda v: v.tensor_tensor(out=m2_, in0=m2_, in1=pq[:, :, 0], op=ALU.max), reads=["stat"], writes=["stat"])
                        tk.op("dve", lambda v: v.tensor_tensor(out=m1, in0=m1, in1=m2_, op=ALU.add), reads=["stat"], writes=["stat"])
                        gmx = w8[:, 0:1]
                        tk.op("dve", lambda v: v.tensor_reduce(out=gmx, in_=m1, axis=AX.X, op=ALU.max), reads=["stat"], writes=["stat"])
                        tk.op("dve", lambda v: v.tensor_scalar(out=m2_, in0=m1, scalar1=gmx, scalar2=None, op0=ALU.is_ge), reads=["stat"], writes=["stat"])
                        tk.op("act", lambda a: a.activation(out=m1, in_=m2_, func=AF.Identity, scale=100.0, bias=-100.0),
                              reads=["stat"], writes=["stat"])
                        sel3 = sel.rearrange("p (g c) -> p g c", g=4)
                        tk.op("dve", lambda v: v.tensor_tensor(out=sel3, in0=sel3, in1=m2_.unsqueeze(2).broadcast_to([128, 4, 4]), op=ALU.mult),
                              reads=["stat"], writes=["stat"])
                        tk.op("dve", lambda v: v.tensor_tensor(out=sel3, in0=sel3, in1=m1.unsqueeze(2).broadcast_to([128, 4, 4]), op=ALU.add),
                              reads=["stat"], writes=["stat"])
                        tk.op("dve", lambda v: v.max(out=w8, in_=sel), reads=["stat"], writes=["stat"])
                        tk.op("dve", lambda v: v.tensor_scalar(out=sel, in0=sel, scalar1=w8[:, 1:2], scalar2=None, op0=ALU.is_ge),
                              reads=["stat"], writes=["stat"])
                        tk.op("dve", lambda v: v.tensor_tensor(out=sc, in0=sc, in1=sel, op=ALU.mult), reads=["stat"], writes=["stat"])
                        tk.op("dve", lambda v: v.tensor_reduce(out=gmx, in_=sc, axis=AX.X, op=ALU.add), reads=["stat"], writes=["stat"])
                        tk.op("dve", lambda v: v.reciprocal(out=gmx, in_=gmx), reads=["stat"], writes=["stat"])
                        wtok, wtokk = tmp()
                        tk.op("dve", lambda v, wtok=wtok: v.tensor_scalar(out=wtok[:, 0:16], in0=sc, scalar1=gmx, scalar2=None, op0=ALU.mult),
                              reads=["stat"], writes=[wtokk])
                        tk.op("pe", lambda t, wtok=wtok: t.transpose(out=PS[7][0:16, 128:256], in_=wtok[:, 0:16], identity=ident_f[:]),
                              reads=[wtokk, "ident_f"], writes=[psk(7)])
                        tk.op("act", lambda a, bl=bl: a.copy(out=wrt_hi[:, q0 + bl * 128:q0 + (bl + 1) * 128], in_=PS[7][0:16, 128:256]),
                              reads=[psk(7)], writes=[("wrt", (q0 // 128) + bl)])
                        tk.op("dve", lambda v, bl=bl: v.tensor_tensor(
                            out=wrt_lo[:, q0 + bl * 128:q0 + (bl + 1) * 128], in0=PS[7][0:16, 128:256],
                            in1=wrt_hi[:, q0 + bl * 128:q0 + (bl + 1) * 128], op=ALU.subtract),
                            reads=[psk(7), ("wrt", (q0 // 128) + bl)], writes=[("wrtl", (q0 // 128) + bl)])
                if dbg == "route":
                    tk.barrier()
                    dump(wrt_hi[:, :], 2304, 0, [])
                    dump(hT[:, 0, 0:2304], 2304, 2304, [])
                    tk.barrier()
                    return nc
                tk.barrier()

                tk.mark("b%d l%d experts" % (b, l))
                items = [(e, q0, n) for e in range(NE) for (q0, n) in ranges]

                def ew_views(e):
                    ew = EW[e % 2]
                    return (ew[:, 0:4096].rearrange("p (j f) -> p j f", j=8), ew[:, 4096:8192].rearrange("p (j f) -> p j f", j=8),
                            ew[:, 8192:12288].rearrange("p (k c) -> p k c", k=4), ("EW", e % 2))

                def hm_buf(idx, fc):
                    if idx % 2 == 0:
                        return HM[:, fc, :]
                    return [PB[0], PB[1], PB[2], TK_][fc]

                def load_expert(e):
                    WG, WU, WD, ewk = ew_views(e)
                    tk.dma("pool", lambda q: q.dma_start(out=WG, in_=wg_d[l, e].rearrange("(j p) f -> p j f", p=128)), writes=[ewk])
                    tk.dma("pool", lambda q: q.dma_start(out=WU, in_=wu_d[l, e].rearrange("(j p) f -> p j f", p=128)), writes=[ewk])
                    tk.dma("pool", lambda q: q.dma_start(out=WD, in_=wd_d[l, e].rearrange("(k p) c -> p k c", p=128)), writes=[ewk])

                def emit_bc(idx):
                    e, q0, n = items[idx]
                    bcb = 0 if idx % 2 == 0 else 7
                    wk = [("wrt", q0 // 128 + z) for z in range(n // 128)] + [("wrtl", q0 // 128 + z) for z in range(n // 128)]
                    tk.op("pe", lambda t: t.matmul(PS[bcb][:, 0:n], selb[0:16, e, :], wrt_hi[:, q0:q0 + n], start=True, stop=False),
                          reads=wk + ["selb"], writes=[psk(bcb)])
                    tk.op("pe", lambda t: t.matmul(PS[bcb][:, 0:n], selb[0:16, e, :], wrt_lo[:, q0:q0 + n], start=False, stop=True),
                          reads=wk + ["selb"], writes=[psk(bcb)])

                def emit_gu(idx, fc):
                    e, q0, n = items[idx]
                    bcb = 0 if idx % 2 == 0 else 7
                    WG, WU, WD, ewk = ew_views(e)
                    hk = hkeys(q0, n)
                    gb = 1 + (fc % 2)
                    ub = 3 + (fc % 2)
                    for j in range(8):
                        tk.op("pe", lambda t, j=j: t.matmul(PS[gb][:, 0:n], WG[:, j, fc * 128:(fc + 1) * 128], hT[:, j, q0:q0 + n],
                                                             start=(j == 0), stop=(j == 7)), reads=[ewk] + hk, writes=[psk(gb)])
                    for j in range(8):
                        tk.op("pe", lambda t, j=j: t.matmul(PS[ub][:, 0:n], WU[:, j, fc * 128:(fc + 1) * 128], hT[:, j, q0:q0 + n],
                                                             start=(j == 0), stop=(j == 7)), reads=[ewk] + hk, writes=[psk(ub)])
                    sg, sgk = tmp()
                    tk.op("act", lambda a: a.activation(out=sg[:, 0:n], in_=PS[gb][:, 0:n], func=AF.Silu), reads=[psk(gb)], writes=[sgk])
                    tk.op("dve", lambda v: v.tensor_tensor(out=sg[:, 0:n], in0=sg[:, 0:n], in1=PS[ub][:, 0:n], op=ALU.mult),
                          reads=[sgk, psk(ub)], writes=[sgk])
                    tk.op("dve", lambda v: v.tensor_tensor(out=hm_buf(idx, fc)[:, 0:n], in0=sg[:, 0:n], in1=PS[bcb][:, 0:n], op=ALU.mult),
                          reads=[sgk, psk(bcb)], writes=[("HM", idx % 2, fc)])

                def emit_down(idx):
                    e, q0, n = items[idx]
                    rr = NB if q0 >= S else b
                    WG, WU, WD, ewk = ew_views(e)
                    for jo in range(8):
                        yb = 5 + (jo % 2)
                        for fc in range(4):
                            tk.op("pe", lambda t, fc=fc: t.matmul(PS[yb][:, 0:n], WD[:, fc, jo * 128:(jo + 1) * 128], hm_buf(idx, fc)[:, 0:n],
                                                                   start=(fc == 0), stop=(fc == 3)), reads=[ewk, ("HM", idx % 2, fc)], writes=[psk(yb)])
                        tk.op("dve", lambda v: v.scalar_tensor_tensor(
                            out=xT[:, jo, q0:q0 + n], in0=PS[yb][:, 0:n], scalar=mT[:, l, 40 + jo, rr:rr + 1],
                            in1=xT[:, jo, q0:q0 + n], op0=ALU.mult, op1=ALU.add),
                            reads=[psk(yb), "mT"] + xkeys(q0, n), writes=xkeys(q0, n))

                load_expert(0)
                emit_bc(0)
                emit_gu(0, 0)
                for idx in range(len(items)):
                    e, q0, n = items[idx]
                    if (q0, n) == ranges[0] and e + 1 < NE:
                        load_expert(e + 1)
                    for fc in range(1, 4):
                        emit_gu(idx, fc)
                    if idx + 1 < len(items):
                        emit_bc(idx + 1)
                        emit_gu(idx + 1, 0)
                    emit_down(idx)
                tk.barrier()
                if dbg == "x2":
                    dump(xT[:, 0, :], 2304, 0, [])
                    dump(xT[:, 5, :], 2304, 2304, [])
                    tk.barrier()
                    return nc

            tk.mark("b%d store" % b)
            for tt in range(16):
                for g in range(2):
                    bank = 6 + g
                    for jj in range(4):
                        j = g * 4 + jj
                        tk.op("pe", lambda t, j=j, jj=jj, bank=bank: t.transpose(
                            out=PS[bank][:, jj * 128:(jj + 1) * 128], in_=xT[:, j, tt * 128:(tt + 1) * 128], identity=ident_f[:]),
                            reads=[("xT", tt), "ident_f"], writes=[psk(bank)], inc=(jj == 3))
                    if g == 0:
                        tk.op("act", lambda a, bank=bank: a.copy(out=XS[:, 0:512], in_=PS[bank][:]), reads=[psk(bank)], writes=["xs"])
                    else:
                        tk.op("dve", lambda v, bank=bank: v.tensor_copy(out=XS[:, 512:1024], in_=PS[bank][:]), reads=[psk(bank)], writes=["xs"])
                tk.dma("sp", lambda q: q.dma_start(out=out_d[b, tt * 128:(tt + 1) * 128, :], in_=XS[:]), reads=["xs"], writes=["out"])
            tk.barrier()
          except StopBuild:
            print("STOPPED at", tk.nops)
            tk.pe_open = False
            tk.limit = 0
            tk.barrier()
            if dbg_d is not None:
                dump(small[:, :], 512, 0, [])
                tk.barrier()
            return nc
        tk.barrier()
        tk.mark("end")
        LAST_MARKS[:] = tk.marks
    return nc


LAST_MARKS = []
dbg_mixer = ["D"]

_W_NAMES = ["ada_w", "ada_b", "norm_mix_g", "norm_ffn_g", "w_in", "qk_g_win", "qk_g_na", "qk_g_diff", "qk_g_gqa",
            "sink_win", "lambda_diff", "out_gain", "w_out", "w_gate", "w_up", "w_down"]


def make_in_maps(inputs, n_cores, NB, DEPTH):
    f = lambda a: np.ascontiguousarray(np.asarray(a, dtype=np.float32))
    shared = {k: f(inputs[k])[:DEPTH] for k in _W_NAMES}
    shared["router_w"] = f(inputs["router_w"])
    shared["router_b"] = f(inputs["router_b"]).reshape(1, NE)
    shared["na_bias"] = host_na_bias(f(inputs["rpb_na"])[:DEPTH])
    shared.update(host_consts())
    x = f(inputs["x"])
    c = f(inputs["c"])
    ctx = f(inputs["ctx"])
    cctx = f(inputs["c_ctx"]).reshape(1, D)
    maps = []
    for i in range(n_cores):
        m = dict(shared)
        m["x"] = x[i * NB:(i + 1) * NB]
        m["ctx"] = ctx[i * NB:(i + 1) * NB]
        m["c"] = np.ascontiguousarray(np.concatenate([c[i * NB:(i + 1) * NB], cctx], 0))
        maps.append(m)
    return maps


def kernel(**inputs):
    NB = inputs["x"].shape[0] // N_CORES
    nc = build(NB, DEPTH_FULL)
    maps = make_in_maps(inputs, N_CORES, NB, DEPTH_FULL)
    res = run_bass_kernel_spmd(nc, maps, core_ids=list(range(N_CORES)))
    return np.concatenate([r["out"] for r in res.results], axis=0).astype(np.float32)
```

```python
import math
import os
from contextlib import ExitStack
import numpy as np
import concourse.bass as bass
import concourse.mybir as mybir
from concourse.bass_utils import run_bass_kernel_spmd

F32 = mybir.dt.float32
BF16 = mybir.dt.bfloat16
AF = mybir.ActivationFunctionType
ALU = mybir.AluOpType
AX = mybir.AxisListType

D = 1024
S = 2048
L = 256
T = S + L
NTT = T // 128
DEPTH_FULL = 4
EPS = 1e-6
NE = 16
DE = 512
N_CORES = 8
NDS = 40
MIX = [("A", 0, 256, 128, 128, 2, 64), ("B", 512, 256, 256, 256, 4, 64),
       ("C", 1280, 256, 256, 256, 4, 32), ("D", 2048, 256, 128, 128, 2, 64)]


FUSE_PE_WAIT = True


class StopBuild(Exception):
    pass


class TK:
    def __init__(self, nc, es):
        self.nc = nc
        self.eng = {"pe": nc.tensor, "act": nc.scalar, "dve": nc.vector, "pool": nc.gpsimd, "sp": nc.sync}
        self.sem = {k: es.enter_context(nc.semaphore("s_" + k)) for k in self.eng}
        self.cnt = {k: 0 for k in self.eng}
        self.seen = {k: {} for k in self.eng}
        self.dsem = [es.enter_context(nc.semaphore("d%d" % i)) for i in range(NDS)]
        self.dval = [0] * NDS
        self.dnext = 0
        self.bw = {}
        self.br = {}
        self.pe_open = False
        self.marks = []
        self.nops = 0
        self.limit = int(os.environ.get("TK_LIMIT", "0"))

    def _tick(self):
        self.nops += 1
        if self.limit and self.nops > self.limit:
            raise StopBuild()

    def _semh(self, sk):
        return self.sem[sk] if isinstance(sk, str) else self.dsem[sk[1]]

    def _need(self, e, reads, writes):
        need = {}

        def add(ev, raw):
            if ev is None:
                return
            sk, v = ev
            if sk == e and (not raw or e == "pe"):
                return
            if need.get(sk, 0) < v:
                need[sk] = v

        for k in reads:
            add(self.bw.get(k), True)
        for k in writes:
            add(self.bw.get(k), False)
            r = self.br.get(k)
            if r:
                for sk, v in r.items():
                    add((sk, v), False)
        return need

    def _emit_waits(self, e, need):
        for sk, v in need.items():
            if self.seen[e].get(sk, 0) >= v:
                continue
            self.eng[e].wait_ge(self._semh(sk), v)
            self.seen[e][sk] = v

    def _record(self, ev, reads, writes):
        sk, v = ev
        for k in reads:
            d = self.br.setdefault(k, {})
            if d.get(sk, 0) < v:
                d[sk] = v
        for k in writes:
            self.bw[k] = ev
            self.br[k] = {}

    def op(self, e, fn, reads=(), writes=(), inc=True):
        inc = True
        self._tick()
        need = self._need(e, reads, writes)
        fuse = None
        if e in ("pe", "act", "dve", "pool") and FUSE_PE_WAIT:
            pend = [(sk, v) for sk, v in need.items() if self.seen[e].get(sk, 0) < v]
            if pend:
                fuse = pend[-1]
                need = dict(pend[:-1])
        self._emit_waits(e, need)
        ins = fn(self.eng[e])
        if fuse is not None:
            ins._wait_ge(self._semh(fuse[0]), fuse[1])
            self.seen[e][fuse[0]] = fuse[1]
        if inc:
            self.cnt[e] += 1
            ins.then_inc(self.sem[e], 1)
            ev = (e, self.cnt[e])
            if e == "pe":
                self.pe_open = False
        else:
            ev = (e, self.cnt[e] + 1)
            if e == "pe":
                self.pe_open = True
        self._record(ev, reads, writes)

    def dma(self, q, fn, reads=(), writes=()):
        self._tick()
        need = self._need(q, reads, writes)
        i = self.dnext
        self.dnext = (i + 1) % NDS
        if self.dval[i] > 0:
            sk = ("d", i)
            if need.get(sk, 0) < self.dval[i]:
                need[sk] = self.dval[i]
        self._emit_waits(q, need)
        ins = fn(self.eng[q])
        self.dval[i] += 16
        ins.then_inc(self.dsem[i], 16)
        self._record((("d", i), self.dval[i]), reads, writes)

    def mark(self, name):
        self.marks.append((name, dict(self.cnt)))

    def barrier(self):
        assert not self.pe_open
        for e in self.eng:
            need = {f: self.cnt[f] for f in self.eng if f != e and self.cnt[f] > 0}
            for i in range(NDS):
                if self.dval[i] > 0:
                    need[("d", i)] = self.dval[i]
            self._emit_waits(e, need)
        self.bw = {}
        self.br = {}


def host_consts():
    pos = np.arange(S)
    row, col = pos // 64, pos % 64

    def tabs(hd):
        e = hd // 4
        inv = 10000.0 ** (-np.arange(e, dtype=np.float32) / e)
        ar = row[:, None].astype(np.float32) * inv
        ac = col[:, None].astype(np.float32) * inv
        cos = np.concatenate([np.cos(ar), np.cos(ar), np.cos(ac), np.cos(ac)], 1)
        sins = np.concatenate([-np.sin(ar), np.sin(ar), -np.sin(ac), np.sin(ac)], 1)
        return cos.astype(np.float32), sins.astype(np.float32)

    c64, s64 = tabs(64)
    c32, s32 = tabs(32)
    rope = np.concatenate([c64, s64, c32, s32], 1)
    b = np.arange(128)[:, None]
    a = np.arange(128)[None, :]
    wmask = np.stack([(a <= b), (b <= a)], 1).astype(np.float32)
    return {"ident": np.eye(128, dtype=np.float32), "rope": np.ascontiguousarray(rope),
            "wmask": np.ascontiguousarray(wmask.reshape(128, 256))}


def na_variant(i):
    return {0: 0, 1: 1, 14: 3, 15: 4}.get(i, 2)


def na_ts(i):
    return min(max(i - 2, 0), 11)


def host_na_bias(rpb):
    dl = rpb.shape[0]
    out = np.empty((dl, 4, 5, 128, 640), np.float32)
    kl = np.arange(128) // 64
    kc = np.arange(128) % 64
    ql = np.arange(128) // 64
    qc = np.arange(128) % 64
    for vi, i in enumerate([0, 1, 5, 14, 15]):
        ts = na_ts(i)
        for j in range(5):
            krow = 2 * (ts + j) + kl[:, None]
            qrow = 2 * i + ql[None, :]
            rs = np.clip(qrow - 4, 0, 24)
            rowok = (krow >= rs) & (krow < rs + 8)
            cs = np.clip(qc[None, :] - 8, 0, 48)
            colok = (kc[:, None] >= cs) & (kc[:, None] < cs + 16)
            dr = np.clip(krow - qrow + 7, 0, 14)
            dc = np.clip(kc[:, None] - qc[None, :] + 15, 0, 30)
            ok = rowok & colok
            g = rpb[:, :, dr, dc]
            out[:, :, vi, :, j * 128:(j + 1) * 128] = np.where(ok[None, None], g, np.float32(-30000.0))
    return out


def build(NB, DEPTH, dbg=None):
    nc = bass.Bass("TRN2", target_bir_lowering=False)

    def dram(name, shape, kind="ExternalInput", dtype=F32):
        return nc.dram_tensor(name, list(shape), dtype, kind=kind).ap()

    x_d = dram("x", [NB, S, D])
    c_d = dram("c", [NB + 1, D])
    ctx_d = dram("ctx", [NB, L, D])
    ada_w = dram("ada_w", [DEPTH, D, 6 * D])
    ada_b = dram("ada_b", [DEPTH, 6 * D])
    ng1 = dram("norm_mix_g", [DEPTH, D])
    ng2 = dram("norm_ffn_g", [DEPTH, D])
    w_in = dram("w_in", [DEPTH, D, 2560])
    qkg = {"A": dram("qk_g_win", [DEPTH, 2, 64]), "B": dram("qk_g_na", [DEPTH, 2, 64]),
           "C": dram("qk_g_diff", [DEPTH, 2, 32]), "D": dram("qk_g_gqa", [DEPTH, 2, 64])}
    sink_d = dram("sink_win", [DEPTH, 4])
    nab_d = dram("na_bias", [DEPTH, 4, 5, 128, 640])
    lam_d = dram("lambda_diff", [DEPTH, 4, 32])
    og_d = dram("out_gain", [DEPTH, D])
    w_out = dram("w_out", [DEPTH, D, D])
    rw_d = dram("router_w", [D, NE])
    rb_d = dram("router_b", [1, NE])
    wg_d = dram("w_gate", [DEPTH, NE, D, DE])
    wu_d = dram("w_up", [DEPTH, NE, D, DE])
    wd_d = dram("w_down", [DEPTH, NE, DE, D])
    ident_d = dram("ident", [128, 128])
    rope_d = dram("rope", [S, 192])
    wmask_d = dram("wmask", [128, 256])
    out_d = dram("out", [NB, S, D], kind="ExternalOutput")
    dbg_d = dram("dbg", [128, 8192], kind="ExternalOutput") if dbg else None

    es = ExitStack()
    with es:
        nc_ctx = es.enter_context(nc.allow_non_contiguous_dma(reason="small strided parameter loads"))
        tk = TK(nc, es)
        build_info = {}

        def sb(name, shape, dtype=F32):
            return es.enter_context(nc.sbuf_tensor(name, list(shape), dtype))

        xT = sb("xT", [128, 8, T])
        hT = sb("hT", [128, 8, T], BF16)
        arena = sb("arena", [128, 32768], BF16)
        ident_f = sb("ident_fs", [128, 128])
        ident_b = sb("ident_bs", [128, 128], BF16)
        ones_b = sb("ones_b", [128, 128], BF16)
        selb = sb("selb", [16, NE, 128], BF16)
        rw_hi = sb("rw_hi", [128, 8, NE], BF16)
        rw_lo = sb("rw_lo", [128, 8, NE], BF16)
        wmask = sb("wmask_sb", [128, 256], BF16)
        mT = sb("mT", [128, DEPTH, 48, NB + 1])
        sT = sb("sT", [128, 8, NB + 1])
        TMP = [sb("tmp%d" % i, [128, 512]) for i in range(5)]
        RB = sb("rbuf", [128, 512])
        XS = arena[:, 0:2048].bitcast(F32)
        PB = [sb("pb%d" % i, [128, 512], BF16) for i in range(3)]
        ropet = sb("ropet", [128, 192])
        small = sb("small", [128, 512])
        TQ = sb("tq", [128, 256], BF16)
        TK_ = sb("tkp", [128, 512], BF16)
        otok = sb("otok", [128, 4, 256])
        ytok = sb("ytok", [128, 256], BF16)
        nabm = sb("nabm", [128, 640], BF16)
        stat = sb("stat", [128, 64])
        PS = [es.enter_context(nc.psum_tensor("ps%d" % i, [128, 512], F32)) for i in range(8)]

        pass

        def psk(i):
            return ("ps", i)

        tmp_i = [0]

        def tmp():
            i = tmp_i[0]
            tmp_i[0] = (i + 1) % len(TMP)
            return TMP[i], ("tmp", i)

        pb_i = [0]

        def pbuf():
            i = pb_i[0]
            pb_i[0] = (i + 1) % len(PB)
            return PB[i], ("pb", i)

        A1 = small[:, 0:16].rearrange("p (j r) -> p j r", r=2)
        A2 = small[:, 16:32].rearrange("p (j r) -> p j r", r=2)
        g1T = small[:, 32:40]
        g2T = small[:, 40:48]
        ogT = small[:, 48:56]
        abT = small[:, 56:104]
        rb_bc = small[:, 104:120]
        sinkx = small[:, 120:124]
        lamt = small[:, 124:128]
        gq = small[:, 128:192]
        gk = small[:, 192:256]
        rwT = small[:, 256:384].rearrange("p (j e) -> p j e", e=NE)
        lamraw = small[:, 384:512]

        tk.dma("sp", lambda q: q.dma_start(out=ident_f[:], in_=ident_d[:, :]), writes=["ident_f"])
        tk.op("dve", lambda v: v.tensor_copy(out=ident_b[:], in_=ident_f[:]), reads=["ident_f"], writes=["ident_b"])
        tk.op("dve", lambda v: v.memset(ones_b[:], 1.0), writes=["ones_b"])
        tk.dma("sp", lambda q: q.dma_start(out=TMP[0][:, 0:256], in_=wmask_d[:, :]), writes=[("tmp", 0)])
        tk.op("dve", lambda v: v.tensor_copy(out=wmask[:], in_=TMP[0][:, 0:256]), reads=[("tmp", 0)], writes=["wmask"])
        tk.dma("sp", lambda q: q.dma_start(out=rb_bc, in_=rb_d[0:1, :].partition_broadcast(128)), writes=["small"])
        tk.dma("sp", lambda q: q.dma_start(out=rwT, in_=rw_d.rearrange("(j p) e -> p j e", p=128)), writes=["small"])
        tk.op("dve", lambda v: v.tensor_copy(out=rw_hi[:], in_=rwT), reads=["small"], writes=["rw"])
        tk.op("dve", lambda v: v.tensor_tensor(out=rw_lo[:], in0=rwT, in1=rw_hi[:], op=ALU.subtract), reads=["small", "rw"], writes=["rw2"])
        tk.op("dve", lambda v: v.tensor_copy(out=selb[:], in_=ident_b[0:16, 0:16].unsqueeze(2).broadcast_to([16, NE, 128])),
              reads=["ident_b"], writes=["selb"])
        tk.op("dve", lambda v: v.memset(TK_[:], 0.0), writes=["tkp0"])
        tk.op("dve", lambda v: v.memset(arena[:], 0.0), writes=["arena"])

        for r in range(NB + 1):
            tk.dma("sp", lambda q, r=r: q.dma_start(out=sT[:, :, r], in_=c_d[r].rearrange("(j p) -> p j", p=128)),
                   writes=["sT"])
        tk.op("act", lambda a: a.activation(out=sT[:], in_=sT[:], func=AF.Silu), reads=["sT"], writes=["sT"])
        for l in range(DEPTH):
            for j6 in range(6):
                tk.dma("sp", lambda q, l=l, j6=j6: q.dma_start(
                    out=abT[:, j6 * 8:(j6 + 1) * 8],
                    in_=ada_b[l, j6 * 1024:(j6 + 1) * 1024].rearrange("(j p) -> p j", p=128)), writes=["abT"])
            psm = PS[0][:, 0:48 * (NB + 1)].rearrange("p (j r) -> p j r", r=NB + 1)
            for j in range(48):
                wt, wk = tmp()
                wt2, wk2 = tmp()
                tk.dma("sp", lambda q, l=l, j=j, wt=wt: q.dma_start(
                    out=wt[:].rearrange("p (k c) -> p k c", c=128),
                    in_=ada_w[l, 0:512, j * 128:(j + 1) * 128].rearrange("(k p) c -> p k c", p=128)), writes=[wk])
                tk.dma("sp", lambda q, l=l, j=j, wt2=wt2: q.dma_start(
                    out=wt2[:].rearrange("p (k c) -> p k c", c=128),
                    in_=ada_w[l, 512:1024, j * 128:(j + 1) * 128].rearrange("(k p) c -> p k c", p=128)), writes=[wk2])
                for kc in range(8):
                    src, sk_ = (wt, wk) if kc < 4 else (wt2, wk2)
                    tk.op("pe", lambda t, j=j, kc=kc, src=src: t.matmul(
                        psm[:, j, :], src[:, (kc % 4) * 128:(kc % 4 + 1) * 128], sT[:, kc, :],
                        start=(kc == 0), stop=(kc == 7)),
                        reads=[sk_, "sT"], writes=[psk(0)], inc=(kc == 7))
            tk.op("dve", lambda v, l=l: v.tensor_tensor(
                out=mT[:, l], in0=psm, in1=abT.unsqueeze(2).broadcast_to([128, 48, NB + 1]), op=ALU.add),
                reads=[psk(0), "abT"], writes=["mT"])

        def load_tokens_T(src_ap, tok0):
            tk.dma("sp", lambda q: q.dma_start(out=XS[:], in_=src_ap), writes=["xs"])
            for g in range(2):
                bank = 6 + g
                for jj in range(4):
                    j = g * 4 + jj
                    tk.op("pe", lambda t, j=j, jj=jj, bank=bank: t.transpose(
                        out=PS[bank][:, jj * 128:(jj + 1) * 128], in_=XS[:, j * 128:(j + 1) * 128],
                        identity=ident_f[:]), reads=["xs", "ident_f"], writes=[psk(bank)], inc=(jj == 3))
                eng = "act" if g == 0 else "dve"
                if eng == "act":
                    tk.op("act", lambda a, g=g, bank=bank: a.copy(
                        out=xT[:, g * 4:(g + 1) * 4, tok0:tok0 + 128],
                        in_=PS[bank][:].rearrange("p (j t) -> p j t", t=128)),
                        reads=[psk(bank)], writes=[("xT", tok0 // 128)])
                else:
                    tk.op("dve", lambda v, g=g, bank=bank: v.tensor_copy(
                        out=xT[:, g * 4:(g + 1) * 4, tok0:tok0 + 128],
                        in_=PS[bank][:].rearrange("p (j t) -> p j t", t=128)),
                        reads=[psk(bank)], writes=[("xT", tok0 // 128)])

        def xkeys(t0, n):
            return [("xT", i) for i in range(t0 // 128, (t0 + n) // 128)]

        def hkeys(t0, n):
            return [("hT", i) for i in range(t0 // 128, (t0 + n) // 128)]

        def norm_to_h(t0, n, Aap, Bap, router_bank=None):
            xk = xkeys(t0, n)
            hk = hkeys(t0, n)
            for j in range(8):
                sq, sqk = pbuf()
                tk.op("act", lambda a, j=j, sq=sq: a.activation(out=sq[:, :n], in_=xT[:, j, t0:t0 + n], func=AF.Square),
                      reads=xk, writes=[sqk])
                tk.op("pe", lambda t, j=j, sq=sq: t.matmul(PS[5][:, :n], ones_b[:], sq[:, :n], start=(j == 0), stop=(j == 7)),
                      reads=[sqk, "ones_b"], writes=[psk(5)], inc=(j == 7))
            rb, rbk = RB, "rbuf"
            tk.op("act", lambda a: a.activation(out=rb[:, :n], in_=PS[5][:, :n], func=AF.Sqrt, bias=EPS, scale=1.0 / D),
                  reads=[psk(5)], writes=[rbk])
            tk.op("dve", lambda v: v.reciprocal(out=rb[:, :n], in_=rb[:, :n]), reads=[rbk], writes=[rbk])
            for j in range(8):
                t1, t1k = tmp()
                tk.op("dve", lambda v, j=j, t1=t1: v.tensor_tensor(out=t1[:, :n], in0=xT[:, j, t0:t0 + n], in1=rb[:, :n],
                                                                    op=ALU.mult), reads=xk + [rbk], writes=[t1k])
                if router_bank is None:
                    tk.op("act", lambda a, j=j, t1=t1: a.activation(
                        out=hT[:, j, t0:t0 + n], in_=t1[:, :n], func=AF.Identity, scale=Aap[:, j:j + 1], bias=Bap[:, j:j + 1]),
                        reads=[t1k, "small", "mT"], writes=hk)
                else:
                    tk.op("act", lambda a, j=j, t1=t1: a.activation(
                        out=t1[:, :n], in_=t1[:, :n], func=AF.Identity, scale=Aap[:, j:j + 1], bias=Bap[:, j:j + 1]),
                        reads=[t1k, "small", "mT"], writes=[t1k])
                    tk.op("pool", lambda g, j=j, t1=t1: g.tensor_copy(out=hT[:, j, t0:t0 + n], in_=t1[:, :n]),
                          reads=[t1k], writes=hk)
                    lo, lok = pbuf()
                    tk.op("dve", lambda v, j=j, t1=t1, lo=lo: v.tensor_tensor(out=lo[:, :n], in0=t1[:, :n], in1=hT[:, j, t0:t0 + n],
                                                                              op=ALU.subtract), reads=[t1k] + hk, writes=[lok])
                    tk.op("pe", lambda t, j=j: t.matmul(PS[router_bank][0:16, :n], rw_hi[:, j, :], hT[:, j, t0:t0 + n],
                                                        start=(j == 0), stop=False),
                          reads=hk + ["rw"], writes=[psk(router_bank)], inc=False)
                    tk.op("pe", lambda t, j=j: t.matmul(PS[router_bank][0:16, :n], rw_lo[:, j, :], hT[:, j, t0:t0 + n],
                                                        start=False, stop=False),
                          reads=hk + ["rw2"], writes=[psk(router_bank)], inc=False)
                    tk.op("pe", lambda t, j=j, lo=lo: t.matmul(PS[router_bank][0:16, :n], rw_hi[:, j, :], lo[:, :n],
                                                               start=False, stop=(j == 7)),
                          reads=[lok, "rw"], writes=[psk(router_bank)], inc=(j == 7))

        def dump(ap, width, col0, keys):
            if dbg_d is None:
                return
            tk.dma("pool", lambda q: q.dma_start(out=dbg_d[0:ap.shape[0], col0:col0 + width], in_=ap), reads=keys)

        QT = arena[:, 0:4608].rearrange("p (c t) -> p c t", c=2)
        KT = arena[:, 4608:13824].rearrange("p (c t) -> p c t", c=4)
        VA = arena[:, 13824:18576].rearrange("p (t h d) -> p t h d", t=NTT, h=4)
        YT = arena[:, 18576:23184].rearrange("p (c t) -> p c t", c=2)
        WI = arena[:, 23184:29328].rearrange("p (j c) -> p j c", j=8)
        WO = arena[:, 29328:31376].rearrange("p (k c) -> p k c", k=2)
        EW = [arena[:, i * 12288:(i + 1) * 12288] for i in range(2)]
        HM = arena[:, 24576:26624].rearrange("p (f t) -> p f t", f=4)
        WT = arena[0:16, 26624:31232].bitcast(F32) if False else None

        wrt_hi = arena[0:16, 26624:26624 + T]
        wrt_lo = arena[0:16, 26624 + T:26624 + 2 * T]

        def qk_post(ps_ap, nh, hd, gain_ap, rope_tile, out_views, is_lat, bank, okey, slot, cs):
            width = nh * hd
            Wb, Wk = TMP[2 * cs], ("tmp", 2 * cs)
            Yb, Yk = TMP[2 * cs + 1], ("tmp", 2 * cs + 1)
            sq = Wb[:, 0:width]
            X = Wb[:, 256:256 + width]
            Y = Yb[:, 0:width]
            tk.op("act", lambda a: a.activation(out=sq, in_=ps_ap, func=AF.Square), reads=[psk(bank)], writes=[Wk])
            yield
            sc0 = [0, 40, 48, 56][slot]
            stk = "statq%d" % slot
            ss = stat[:, sc0:sc0 + nh]
            tk.op("dve", lambda v: v.tensor_reduce(out=ss, in_=sq.rearrange("p (h d) -> p h d", h=nh),
                                                   axis=AX.X, op=ALU.add), reads=[Wk], writes=[stk])
            yield
            tk.op("act", lambda a: a.activation(out=ss, in_=ss, func=AF.Sqrt, bias=EPS, scale=1.0 / hd),
                  reads=[stk], writes=[stk])
            yield
            tk.op("dve", lambda v: v.reciprocal(out=ss, in_=ss), reads=[stk], writes=[stk])
            yield
            tk.op("dve", lambda v: v.tensor_tensor(
                out=X.rearrange("p (h d) -> p h d", h=nh), in0=ps_ap.rearrange("p (h d) -> p h d", h=nh),
                in1=ss.unsqueeze(2).broadcast_to([128, nh, hd]), op=ALU.mult),
                reads=[psk(bank), stk], writes=[Wk])
            yield
            tk.op("pool", lambda g: g.tensor_tensor(
                out=X.rearrange("p (h d) -> p h d", h=nh), in0=X.rearrange("p (h d) -> p h d", h=nh),
                in1=gain_ap.unsqueeze(1).broadcast_to([128, nh, hd]), op=ALU.mult),
                reads=[Wk, "small"], writes=[Wk])
            yield
            if not is_lat:
                for (oap, sel) in out_views:
                    src = X if sel is None else sel(X)
                    tk.op("dve", lambda v, oap=oap, src=src: v.tensor_copy(out=oap, in_=src), reads=[Wk], writes=[okey])
                    yield
                return
            cos_ap, sin_ap = rope_tile
            e4 = hd // 4
            tk.op("pool", lambda g: g.tensor_tensor(
                out=Y.rearrange("p (h d) -> p h d", h=nh), in0=X.rearrange("p (h d) -> p h d", h=nh),
                in1=cos_ap.unsqueeze(1).broadcast_to([128, nh, hd]), op=ALU.mult), reads=[Wk, "ropet"], writes=[Yk])
            yield
            qv = X.rearrange("p (h b s e) -> p h b s e", h=nh, b=2, s=2)
            tv = sq.rearrange("p (h b s e) -> p h b s e", h=nh, b=2, s=2)
            sv = sin_ap.rearrange("p (b s e) -> p b s e", b=2, s=2)
            for s_ in range(2):
                tk.op("dve", lambda v, s_=s_: v.tensor_tensor(
                    out=tv[:, :, :, s_, :], in0=qv[:, :, :, 1 - s_, :],
                    in1=sv[:, :, s_, :].unsqueeze(1).broadcast_to([128, nh, 2, e4]), op=ALU.mult),
                    reads=[Wk, "ropet"], writes=[Wk])
                yield
            for (oap, sel) in out_views:
                a_ = Y if sel is None else sel(Y)
                b_ = sq if sel is None else sel(sq)
                tk.op("dve", lambda v, oap=oap, a_=a_, b_=b_: v.tensor_tensor(out=oap, in0=a_, in1=b_, op=ALU.add),
                      reads=[Wk, Yk], writes=[okey])
                yield

        def run_zipped(gens):
            gens = list(gens)
            while gens:
                for g_ in list(gens):
                    try:
                        next(g_)
                    except StopIteration:
                        gens.remove(g_)

        ps_bank = [0]
        out_key = ["tq"]
        stat_slot = [0]
        unit_ctr = [0]
        nabm_state = [None]
        TQb = [TQ[:, :], arena[:, 31376:31632]]
        TKb = [TK_[:, :], arena[:, 31632:32144]]

        if dbg == "p0":
            tk.barrier()
            dump(mT[:, 0].rearrange("p j r -> p (j r)"), 48 * (NB + 1), 0, [])
            tk.barrier()
            return nc
        for b in range(NB):
          try:
            for tt in range(16):
                load_tokens_T(x_d[b, tt * 128:(tt + 1) * 128, :], tt * 128)
            for tt in range(2):
                load_tokens_T(ctx_d[b, tt * 128:(tt + 1) * 128, :], S + tt * 128)
            tk.barrier()
            if dbg == "ld":
                dump(xT[:, 0, :], 2304, 0, [])
                dump(xT[:, 7, :], 2304, 2304, [])
                tk.barrier()
                return nc

            for l in range(DEPTH):
                pass
                need_ctx = l < DEPTH - 1
                lam_init = 0.8 - 0.6 * math.exp(-0.3 * l)
                mrow = lambda k, r: mT[:, l, k * 8:(k + 1) * 8, r]
                tk.dma("sp", lambda q: q.dma_start(out=g1T, in_=ng1[l].rearrange("(j p) -> p j", p=128)), writes=["small"])
                tk.dma("sp", lambda q: q.dma_start(out=g2T, in_=ng2[l].rearrange("(j p) -> p j", p=128)), writes=["small"])
                tk.dma("sp", lambda q: q.dma_start(out=ogT, in_=og_d[l].rearrange("(j p) -> p j", p=128)), writes=["small"])
                tk.dma("sp", lambda q: q.dma_start(out=sinkx, in_=sink_d[l:l + 1, :].partition_broadcast(128)), writes=["small"])
                tk.dma("sp", lambda q: q.dma_start(
                    out=lamraw, in_=lam_d[l:l + 1].rearrange("o a d -> o (a d)").partition_broadcast(128)), writes=["small"])
                for r in range(2):
                    rr = b if r == 0 else NB
                    tk.op("dve", lambda v, r=r, rr=rr: v.scalar_tensor_tensor(
                        out=A1[:, :, r], in0=mrow(1, rr), scalar=1.0, in1=g1T, op0=ALU.add, op1=ALU.mult),
                        reads=["small", "mT"], writes=["small"])
                    tk.op("dve", lambda v, r=r, rr=rr: v.scalar_tensor_tensor(
                        out=A2[:, :, r], in0=mrow(4, rr), scalar=1.0, in1=g2T, op0=ALU.add, op1=ALU.mult),
                        reads=["small", "mT"], writes=["small"])
                tk.op("act", lambda a: a.activation(out=sinkx, in_=sinkx, func=AF.Exp), reads=["small"], writes=["small"])
                lr = lamraw.rearrange("p (a d) -> p a d", a=4)
                lw = stat[:, 32:36]
                tk.op("dve", lambda v: v.tensor_tensor(out=lamraw[:, 0:32], in0=lr[:, 0, :], in1=lr[:, 1, :], op=ALU.mult),
                      reads=["small"], writes=["small"])
                tk.op("dve", lambda v: v.tensor_tensor(out=lamraw[:, 64:96], in0=lr[:, 2, :], in1=lr[:, 3, :], op=ALU.mult),
                      reads=["small"], writes=["small"])
                tk.op("dve", lambda v: v.tensor_reduce(out=lw[:, 0:2], in_=lamraw.rearrange("p (a d) -> p a d", a=2)[:, :, 0:32],
                                                       axis=AX.X, op=ALU.add), reads=["small"], writes=["stat2"])
                tk.op("act", lambda a: a.activation(out=lw[:, 0:2], in_=lw[:, 0:2], func=AF.Exp), reads=["stat2"], writes=["stat2"])
                tk.op("dve", lambda v: v.tensor_tensor(out=lw[:, 2:3], in0=lw[:, 0:1], in1=lw[:, 1:2], op=ALU.subtract),
                      reads=["stat2"], writes=["stat2"])
                tk.op("act", lambda a: a.activation(out=lamt[:, 0:1], in_=lw[:, 2:3], func=AF.Identity, scale=-1.0, bias=-lam_init),
                      reads=["stat2"], writes=["small"])

                if dbg == "sm":
                    print("nops at sm", tk.nops)
                    tk.barrier()
                    dump(small[:, :], 512, 0, [])
                    tk.barrier()
                    return nc
                tk.mark("b%d l%d norm1" % (b, l))
                for cch in range(4):
                    norm_to_h(cch * 512, 512, A1[:, :, 0], mrow(0, b))
                norm_to_h(S, L, A1[:, :, 1], mrow(0, NB))
                if dbg == "h":
                    tk.barrier()
                    dump(hT[:, 0, 0:2304], 2304, 0, hkeys(0, T))
                    dump(hT[:, 7, 0:2304], 2304, 2304, hkeys(0, T))
                    tk.barrier()
                    return nc

                for mi in (0, 1, 3, 2):
                    (mname, col0, nq, nk, nv, nkv, hd) = MIX[mi]
                    ncols = nq + nk + nv
                    nh_q = nq // hd
                    nh_k = nk // hd
                    scale = hd ** -0.5
                    def load_WI(mj):
                        (_n, c0_, nq_, nk_, nv_, _kv, _hd) = MIX[mj]
                        nc_ = nq_ + nk_ + nv_
                        tk.dma("pool", lambda q: q.dma_start(
                            out=WI[:, :, 0:nc_], in_=w_in[l, :, c0_:c0_ + nc_].rearrange("(j p) c -> p j c", p=128)),
                            writes=["WI"])
                    if mi == 0:
                        load_WI(0)
                    tk.dma("pool", lambda q: q.dma_start(
                        out=WO[:, :, :], in_=w_out[l, mi * 256:(mi + 1) * 256, :].rearrange("(k p) c -> p k c", p=128)),
                        writes=["WO"])
                    gd = qkg[mname]
                    tk.dma("sp", lambda q: q.dma_start(out=gq[:, 0:hd], in_=gd[l, 0:1, :].partition_broadcast(128)), writes=["small"])
                    tk.dma("sp", lambda q: q.dma_start(out=gk[:, 0:hd], in_=gd[l, 1:2, :].partition_broadcast(128)), writes=["small"])
                    tk.op("dve", lambda v: v.tensor_scalar(out=gq[:, 0:hd], in0=gq[:, 0:hd], scalar1=scale, scalar2=None,
                                                           op0=ALU.mult), reads=["small"], writes=["small"])
                    tk.op("dve", lambda v: v.memset(VA[:, :, :, 64:65], 1.0), writes=["VA"])

                    tk.mark("b%d l%d %s proj" % (b, l, mname))
                    tk.op("dve", lambda v: v.memset(TKb[0], 0.0), writes=["tkp0"])
                    tk.op("dve", lambda v: v.memset(TKb[1], 0.0), writes=["tkp1"])
                    nkc = 4 if mname == "C" else nkv
                    if mname == "C":
                        tk.op("dve", lambda v: v.memset(PB[0][:], 0.0), writes=[("pb", 0)])
                        tk.op("dve", lambda v: v.memset(PB[1][:], 0.0), writes=[("pb", 1)])

                    def tile_banks(tt):
                        return [0, 1] if tt % 2 == 0 else [2, 3]

                    def emit_mm(tt):
                        banks = tile_banks(tt)
                        pieces = [(0, min(512, ncols), banks[0])]
                        if ncols > 512:
                            pieces.append((512, ncols, banks[1]))
                        for (c0, c1, bank) in pieces:
                            for j in range(8):
                                tk.op("pe", lambda t, j=j: t.matmul(
                                    PS[bank][:, 0:c1 - c0], hT[:, j, tt * 128:(tt + 1) * 128], WI[:, j, c0:c1],
                                    start=(j == 0), stop=(j == 7)), reads=[("hT", tt), "WI"], writes=[psk(bank)])

                    def emit_chains(tt):
                        is_lat = tt < 16 and mname != "B"
                        banks = tile_banks(tt)
                        par = tt % 2
                        TQc = TQb[par]
                        TKc = TKb[par]
                        if tt < 16 and mname != "B":
                            tk.dma("sp", lambda q: q.dma_start(out=ropet[:], in_=rope_d[tt * 128:(tt + 1) * 128, :]), writes=["ropet"])
                        rt = (ropet[:, 0:64], ropet[:, 64:128]) if hd == 64 else (ropet[:, 128:160], ropet[:, 160:192])
                        if mname in ("A", "D"):
                            qviews = [(TQc.rearrange("p (c s d) -> p s c d", c=2, s=2),
                                       (lambda ap: ap.rearrange("p (s c d) -> p s c d", s=2, c=2)))]
                        elif mname == "C":
                            tqv = PB[par][:, 0:512].rearrange("p (c s d) -> p c s d", c=4, s=2)
                            qviews = [(tqv[:, h_, h_ % 2, :], (lambda ap, h_=h_: ap[:, h_ * 64:(h_ + 1) * 64])) for h_ in range(4)]
                        else:
                            qviews = [(TQc, None)]
                        gq_ = qk_post(PS[banks[0]][:, 0:256], nh_q if hd == 64 else 8, hd, gq[:, 0:hd], rt, qviews, is_lat,
                                      banks[0], ("pb", par) if mname == "C" else "tq%d" % par, par * 2, 0)
                        if mname == "C":
                            tkv = TKc.rearrange("p (hp i hh i2 d) -> p hp i hh i2 d", hp=2, i=2, hh=2, i2=2)
                            views = []
                            for i_ in range(2):
                                views.append((tkv[:, :, i_, :, i_, :],
                                              (lambda ap, i_=i_: ap.rearrange("p (hp hh i d) -> p hp hh i d", hp=2, hh=2, i=2)[:, :, :, i_, :])))
                            gk_ = qk_post(PS[banks[0]][:, 256:512], 8, 32, gk[:, 0:32], rt, views, is_lat, banks[0], "tkp%d" % par, par * 2 + 1, 1)
                        else:
                            tkv = TKc[:, 0:nkv * 128].rearrange("p (c s d) -> p c s d", c=nkv, s=2)
                            views = [(tkv[:, kvh, kvh % 2, :], (lambda ap, kvh=kvh: ap[:, kvh * 64:(kvh + 1) * 64])) for kvh in range(nkv)]
                            gk_ = qk_post(PS[banks[0]][:, 256:256 + nk], nh_k, hd, gk[:, 0:hd], rt, views, is_lat,
                                          banks[0], "tkp%d" % par, par * 2 + 1, 1)
                        run_zipped([gq_, gk_])
                        if nk == 128:
                            vsrc = PS[banks[0]][:, 384:512]
                            vb = banks[0]
                        else:
                            vsrc = PS[banks[1]][:, 0:256]
                            vb = banks[1]
                        tk.op("act", lambda a: a.copy(out=VA[:, tt, 0:nkv, 0:64], in_=vsrc.rearrange("p (h d) -> p h d", h=nkv)),
                              reads=[psk(vb)], writes=[("VA", tt)])

                    def emit_tr(tt):
                        par = tt % 2
                        TQc = TQb[par]
                        TKc = TKb[par]
                        if mname == "C":
                            for cc in range(4):
                                tk.op("pe", lambda t, cc=cc: t.transpose(
                                    out=PS[4][:].bitcast(BF16)[:, cc * 128:(cc + 1) * 128], in_=PB[par][:, cc * 128:(cc + 1) * 128],
                                    identity=ident_b[:]), reads=[("pb", par), "ident_b"], writes=[psk(4)])
                            tk.op("act", lambda a: a.copy(out=hT[:, 0:4, tt * 128:(tt + 1) * 128],
                                                          in_=PS[4][:].bitcast(BF16)[:, 0:512].rearrange("p (c t) -> p c t", c=4)),
                                  reads=[psk(4)], writes=[("QT", tt), ("hT", tt)])
                        else:
                            for cc in range(2):
                                tk.op("pe", lambda t, cc=cc: t.transpose(
                                    out=PS[4][:].bitcast(BF16)[:, cc * 128:(cc + 1) * 128], in_=TQc[:, cc * 128:(cc + 1) * 128],
                                    identity=ident_b[:]), reads=["tq%d" % par, "ident_b"], writes=[psk(4)])
                            tk.op("act", lambda a: a.copy(out=QT[:, :, tt * 128:(tt + 1) * 128],
                                                          in_=PS[4][:].bitcast(BF16)[:, 0:256].rearrange("p (c t) -> p c t", c=2)),
                                  reads=[psk(4)], writes=[("QT", tt)])
                        for cc in range(nkc):
                            tk.op("pe", lambda t, cc=cc: t.transpose(
                                out=PS[5][:].bitcast(BF16)[:, cc * 128:(cc + 1) * 128],
                                in_=TKc[:, cc * 128:(cc + 1) * 128], identity=ident_b[:]),
                                reads=["tkp%d" % par, "ident_b"], writes=[psk(5)])
                        tk.op("act", lambda a: a.copy(
                            out=KT[:, 0:nkc, tt * 128:(tt + 1) * 128],
                            in_=PS[5][:].bitcast(BF16)[:, 0:nkc * 128].rearrange("p (c t) -> p c t", c=nkc)),
                            reads=[psk(5)], writes=[("KT", tt)])

                    emit_mm(0)
                    for tt in range(NTT):
                        emit_chains(tt)
                        if tt + 1 < NTT:
                            emit_mm(tt + 1)
                        emit_tr(tt)
                    nxt = {0: 1, 1: 3, 3: 2}.get(mi)
                    if nxt is not None:
                        load_WI(nxt)
                    if dbg == "qkv" and mname == dbg_mixer[0]:
                        tk.barrier()
                        dump(QT[:, 0, :], 2304, 0, [])
                        dump(KT[:, 0, :], 2304, 2304, [])
                        dump(VA[:, 3, :, :].rearrange("p h d -> p (h d)"), 264, 4608, [])
                        dump(KT[:, 3, :], 2304, 4900, [])
                        tk.barrier()
                        return nc

                    tk.mark("b%d l%d %s attn" % (b, l, mname))
                    ranges = [(r0 * 512, 512) for r0 in range(4)] + ([(S, L)] if need_ctx else [])
                    pending = []

                    def flush():
                        for f_ in pending:
                            f_()
                        del pending[:]

                    for (q0, n) in ranges:
                        nblk = n // 128
                        is_ctxq = q0 >= S
                        units = []
                        if mname in ("A", "D"):
                            for h in range(4):
                                units.append((h, 0, h % 2, (h // 2) * 64, h // 2, (h // 2) * 64, h // 2))
                        elif mname == "B":
                            for h in range(4):
                                units.append((h, 0, h // 2, (h % 2) * 64, h, (h % 2) * 64, h))
                        else:
                            for h in range(4):
                                for i_ in range(2):
                                    units.append((h, i_, h // 2, (h % 2) * 64, (h // 2) * 2 + i_, (h % 2) * 64, h))
                        for (h, br, qc, qp, kc_, kp, vh) in units:
                            ob = 6 if unit_ctr[0] % 2 == 0 else 3
                            unit_ctr[0] += 1
                            steps = []
                            if is_ctxq or mname in ("C", "D"):
                                kts = [16, 17] if is_ctxq else list(range(18))
                                for ki, kt in enumerate(kts):
                                    steps.append((0, n, kt, None, None, ki == 0, ki == len(kts) - 1, None))
                            elif mname == "A":
                                for bl in range(nblk):
                                    i = q0 // 128 + bl
                                    lst = []
                                    if i - 1 >= 0:
                                        lst.append((i - 1, wmask[:, 0:128], "wmask"))
                                    lst.append((i, None, None))
                                    if i + 1 < 16:
                                        lst.append((i + 1, wmask[:, 128:256], "wmask"))
                                    lst += [(16, None, None), (17, None, None)]
                                    for ki, (kt, ma, mk_) in enumerate(lst):
                                        steps.append((bl * 128, 128, kt, ma, mk_, ki == 0, ki == len(lst) - 1, None))
                            else:
                                for bl in range(nblk):
                                    i = q0 // 128 + bl
                                    ts = na_ts(i)
                                    vi = na_variant(i)

                                    def load_mask(vi=vi, h=h):
                                        if nabm_state[0] == (b, l, h, vi):
                                            return
                                        nabm_state[0] = (b, l, h, vi)
                                        stg, stgk = tmp()
                                        stg2, stg2k = tmp()
                                        tk.dma("sp", lambda q: q.dma_start(out=stg[:, 0:512], in_=nab_d[l, h, vi, :, 0:512]), writes=[stgk])
                                        tk.dma("sp", lambda q: q.dma_start(out=stg2[:, 0:128], in_=nab_d[l, h, vi, :, 512:640]), writes=[stg2k])
                                        tk.op("act", lambda a: a.activation(out=nabm[:, 0:512], in_=stg[:, 0:512], func=AF.Exp),
                                              reads=[stgk], writes=["nabm"])
                                        tk.op("act", lambda a: a.activation(out=nabm[:, 512:640], in_=stg2[:, 0:128], func=AF.Exp),
                                              reads=[stg2k], writes=["nabm"])
                                    lst = [(ts + j, nabm[:, j * 128:(j + 1) * 128], "nabm") for j in range(5)]
                                    lst += [(16, None, None), (17, None, None)]
                                    for ki, (kt, ma, mk_) in enumerate(lst):
                                        steps.append((bl * 128, 128, kt, ma, mk_, ki == 0, ki == len(lst) - 1, load_mask if ki == 0 else None))

                            def emit_qk(si, st):
                                (qa, qn, kt, mask_ap, mkey, first, last, pre) = st
                                if pre is not None:
                                    pre()
                                sbank = 4 + (si % 2)
                                if mname == "C":
                                    k_ap = KT[:, kc_, kt * 128:(kt + 1) * 128]
                                    q_ap = hT[:, h, q0 + qa:q0 + qa + qn]
                                else:
                                    k_ap = KT[:, kc_, kt * 128:(kt + 1) * 128]
                                    q_ap = QT[:, qc, q0 + qa:q0 + qa + qn]
                                tk.op("pe", lambda t: t.matmul(PS[sbank][:, 0:qn], k_ap, q_ap, start=True, stop=True),
                                    reads=[("KT", kt)] + [("QT", (q0 + qa) // 128 + z) for z in range(qn // 128)], writes=[psk(sbank)])
                                pbt, pbk = pbuf()
                                tk.op("act", lambda a: a.activation(out=pbt[:, 0:qn], in_=PS[sbank][:, 0:qn], func=AF.Exp),
                                      reads=[psk(sbank)], writes=[pbk])
                                if mask_ap is not None:
                                    tk.op("dve", lambda v: v.tensor_tensor(out=pbt[:, 0:qn], in0=pbt[:, 0:qn], in1=mask_ap, op=ALU.mult),
                                          reads=[pbk, mkey], writes=[pbk])
                                return pbt, pbk

                            def emit_pv(st, pbt, pbk):
                                (qa, qn, kt, mask_ap, mkey, first, last, pre) = st
                                tk.op("pe", lambda t: t.matmul(PS[ob][0:65, qa:qa + qn], VA[:, kt, vh, 0:65], pbt[:, 0:qn], start=first, stop=last),
                                      reads=[("VA", kt), "VA", pbk], writes=[psk(ob)])

                            prev = None
                            for si, st in enumerate(steps):
                                cur = emit_qk(si, st)
                                if prev is not None:
                                    emit_pv(*prev)
                                prev = (st,) + cur
                                if si == min(2, len(steps) - 1):
                                    flush()
                            emit_pv(*prev)

                            def post(h=h, br=br, ob=ob, nblk=nblk, n=n):
                                ot, otk = tmp()
                                tk.op("dve", lambda v: v.tensor_copy(out=ot[0:65, 0:n], in_=PS[ob][0:65, 0:n]), reads=[psk(ob)], writes=[otk])
                                for bl in range(nblk):
                                    tk.op("pe", lambda t, bl=bl: t.transpose(
                                        out=PS[7][:, bl * 128:bl * 128 + 65], in_=ot[0:65, bl * 128:(bl + 1) * 128],
                                        identity=ident_f[0:65, 0:65]), reads=[otk, "ident_f"], writes=[psk(7)])
                                p7 = PS[7][:].rearrange("p (b d) -> p b d", d=128)
                                rd = stat[:, 8:8 + nblk]
                                if mname == "A":
                                    tk.op("dve", lambda v: v.tensor_scalar(out=rd, in0=p7[:, 0:nblk, 64], scalar1=sinkx[:, h:h + 1],
                                                                           scalar2=None, op0=ALU.add), reads=[psk(7), "small"], writes=["stat3"])
                                    tk.op("dve", lambda v: v.reciprocal(out=rd, in_=rd), reads=["stat3"], writes=["stat3"])
                                else:
                                    tk.op("dve", lambda v: v.reciprocal(out=rd, in_=p7[:, 0:nblk, 64]), reads=[psk(7)], writes=["stat3"])
                                if br == 1:
                                    tk.op("dve", lambda v: v.tensor_scalar(out=rd, in0=rd, scalar1=lamt[:, 0:1], scalar2=None, op0=ALU.mult),
                                          reads=["stat3", "small"], writes=["stat3"])
                                for bl in range(nblk):
                                    if br == 0:
                                        tk.op("dve", lambda v, bl=bl: v.tensor_scalar(
                                            out=otok[:, bl, h * 64:(h + 1) * 64], in0=p7[:, bl, 0:64], scalar1=rd[:, bl:bl + 1],
                                            scalar2=None, op0=ALU.mult), reads=[psk(7), "stat3"], writes=[("otok", bl)])
                                    else:
                                        tk.op("dve", lambda v, bl=bl: v.scalar_tensor_tensor(
                                            out=otok[:, bl, h * 64:(h + 1) * 64], in0=p7[:, bl, 0:64], scalar=rd[:, bl:bl + 1],
                                            in1=otok[:, bl, h * 64:(h + 1) * 64], op0=ALU.mult, op1=ALU.add),
                                            reads=[psk(7), "stat3", ("otok", bl)], writes=[("otok", bl)])
                            pending.append(post)

                        def merge(q0=q0, nblk=nblk):
                            for bl in range(nblk):
                                tok = q0 + bl * 128
                                ng = 1 if mname != "C" else 4
                                gsz = 256 // ng
                                sq, sqk = tmp()
                                tk.op("dve", lambda v: v.tensor_tensor(out=sq[:, 0:256], in0=otok[:, bl, :], in1=otok[:, bl, :], op=ALU.mult),
                                      reads=[("otok", bl)], writes=[sqk])
                                ssg = stat[:, 16:16 + ng]
                                tk.op("dve", lambda v: v.tensor_reduce(out=ssg, in_=sq[:, 0:256].rearrange("p (g d) -> p g d", g=ng),
                                                                       axis=AX.X, op=ALU.add), reads=[sqk], writes=["stat4"])
                                tk.op("act", lambda a: a.activation(out=ssg, in_=ssg, func=AF.Sqrt, bias=EPS, scale=1.0 / gsz),
                                      reads=["stat4"], writes=["stat4"])
                                tk.op("dve", lambda v: v.reciprocal(out=ssg, in_=ssg), reads=["stat4"], writes=["stat4"])
                                if mname == "C":
                                    tk.op("dve", lambda v: v.tensor_scalar(out=ssg, in0=ssg, scalar1=(1.0 - lam_init), scalar2=None,
                                                                           op0=ALU.mult), reads=["stat4"], writes=["stat4"])
                                tk.op("dve", lambda v: v.tensor_tensor(
                                    out=ytok[:].rearrange("p (g d) -> p g d", g=ng), in0=otok[:, bl, :].rearrange("p (g d) -> p g d", g=ng),
                                    in1=ssg.unsqueeze(2).broadcast_to([128, ng, gsz]), op=ALU.mult),
                                    reads=[("otok", bl), "stat4"], writes=["ytok"])
                                for cc in range(2):
                                    tk.op("pe", lambda t, cc=cc: t.transpose(
                                        out=PS[2][:].bitcast(BF16)[:, cc * 128:(cc + 1) * 128],
                                        in_=ytok[:, cc * 128:(cc + 1) * 128], identity=ident_b[:]),
                                        reads=["ytok", "ident_b"], writes=[psk(2)])
                                for cc in range(2):
                                    tk.op("dve", lambda v, cc=cc: v.tensor_scalar(
                                        out=YT[:, cc, tok:tok + 128], in0=PS[2][:].bitcast(BF16)[:, cc * 128:(cc + 1) * 128],
                                        scalar1=ogT[:, mi * 2 + cc:mi * 2 + cc + 1], scalar2=None, op0=ALU.mult),
                                        reads=[psk(2), "small"], writes=[("YT", tok // 128)])
                        pending.append(merge)
                    flush()
                    if dbg == "y" and mname == dbg_mixer[0]:
                        tk.barrier()
                        dump(YT[:, 0, :], 2304, 0, [])
                        dump(YT[:, 1, :], 2304, 2304, [])
                        tk.barrier()
                        return nc

                    tk.mark("b%d l%d %s wout" % (b, l, mname))
                    for (q0, n) in ranges:
                        rr = NB if q0 >= S else b
                        for jo in range(8):
                            bank = jo % 4
                            for kc2 in range(2):
                                tk.op("pe", lambda t, jo=jo, kc2=kc2, bank=bank: t.matmul(
                                    PS[bank][:, 0:n], WO[:, kc2, jo * 128:(jo + 1) * 128], YT[:, kc2, q0:q0 + n],
                                    start=(kc2 == 0), stop=(kc2 == 1)),
                                    reads=["WO"] + [("YT", q0 // 128 + z) for z in range(n // 128)], writes=[psk(bank)],
                                    inc=(kc2 == 1))
                            tk.op("dve", lambda v, jo=jo, bank=bank, rr=rr: v.scalar_tensor_tensor(
                                out=xT[:, jo, q0:q0 + n], in0=PS[bank][:, 0:n], scalar=mT[:, l, 16 + jo, rr:rr + 1],
                                in1=xT[:, jo, q0:q0 + n], op0=ALU.mult, op1=ALU.add),
                                reads=[psk(bank), "mT"] + xkeys(q0, n), writes=xkeys(q0, n))
                    tk.barrier()
                if dbg == "xattn":
                    dump(xT[:, 0, :], 2304, 0, [])
                    dump(xT[:, 5, :], 2304, 2304, [])
                    tk.barrier()
                    return nc

                if dbg == "xattn2":
                    dump(xT[:, 0, :], 2304, 0, [])
                    dump(xT[:, 5, :], 2304, 2304, [])
                    tk.barrier()
                    return nc
                tk.mark("b%d l%d norm2+route" % (b, l))
                ranges = [(r0 * 512, 512) for r0 in range(4)] + ([(S, L)] if need_ctx else [])
                for (q0, n) in ranges:
                    r = 1 if q0 >= S else 0
                    rr = NB if q0 >= S else b
                    norm_to_h(q0, n, A2[:, :, r], mrow(3, rr), router_bank=6)
                    lg, lgk = RB, "rbuf"
                    tk.op("dve", lambda v, lg=lg: v.tensor_copy(out=lg[0:16, 0:n], in_=PS[6][0:16, 0:n]), reads=[psk(6)], writes=[lgk])
                    for bl in range(n // 128):
                        tk.op("pe", lambda t, bl=bl, lg=lg: t.transpose(out=PS[7][:, 0:16], in_=lg[0:16, bl * 128:(bl + 1) * 128],
                                                                       identity=ident_f[0:16, 0:16]),
                              reads=[lgk, "ident_f"], writes=[psk(7)])
                        sc = stat[:, 0:16]
                        sel = stat[:, 16:32]
                        w8 = stat[:, 32:40]
                        tk.op("act", lambda a: a.activation(out=sc, in_=PS[7][:, 0:16], func=AF.Sigmoid), reads=[psk(7)], writes=["stat"])
                        tk.op("dve", lambda v: v.tensor_tensor(out=sel, in0=sc, in1=rb_bc, op=ALU.add), reads=["stat", "small"], writes=["stat"])
                        s4 = sel.rearrange("p (g a c) -> p g a c", g=4, a=2)
                        pq = stat[:, 40:48].rearrange("p (g a) -> p g a", g=4)
                        rs_ = stat[:, 48:56].rearrange("p (g a) -> p g a", g=4)
                        tk.op("dve", lambda v: v.tensor_tensor(out=pq, in0=s4[:, :, :, 0], in1=s4[:, :, :, 1], op=ALU.max), reads=["stat"], writes=["stat"])
                        tk.op("dve", lambda v: v.tensor_tensor(out=rs_, in0=s4[:, :, :, 0], in1=s4[:, :, :, 1], op=ALU.min), reads=["stat"], writes=["stat"])
                        m1 = stat[:, 56:60]
                        m2_ = stat[:, 60:64]
                        tk.op("dve", lambda v: v.tensor_tensor(out=m1, in0=pq[:, :, 0], in1=pq[:, :, 1], op=ALU.max), reads=["stat"], writes=["stat"])
                        tk.op("dve", lambda v: v.tensor_tensor(out=m2_, in0=pq[:, :, 0], in1=pq[:, :, 1], op=ALU.min), reads=["stat"], writes=["stat"])
                        tk.op("dve", lambda v: v.tensor_tensor(out=pq[:, :, 0], in0=rs_[:, :, 0], in1=rs_[:, :, 1], op=ALU.max), reads=["stat"], writes=["stat"])
                        tk.op("dve", lambda v: v.tensor_tensor(out=m2_, in0=m2_, in1=pq[:, :, 0], op=ALU.max), reads=["stat"], writes=["stat"])
                        tk.op("dve", lambda v: v.tensor_tensor(out=m1, in0=m1, in1=m2_, op=ALU.add), reads=["stat"], writes=["stat"])
                        gmx = w8[:, 0:1]
                        tk.op("dve", lambda v: v.tensor_reduce(out=gmx, in_=m1, axis=AX.X, op=ALU.max), reads=["stat"], writes=["stat"])
                        tk.op("dve", lambda v: v.tensor_scalar(out=m2_, in0=m1, scalar1=gmx, scalar2=None, op0=ALU.is_ge), reads=["stat"], writes=["stat"])
                        tk.op("act", lambda a: a.activation(out=m1, in_=m2_, func=AF.Identity, scale=100.0, bias=-100.0),
                              reads=["stat"], writes=["stat"])
                        sel3 = sel.rearrange("p (g c) -> p g c", g=4)
                        tk.op("dve", lambda v: v.tensor_tensor(out=sel3, in0=sel3, in1=m2_.unsqueeze(2).broadcast_to([128, 4, 4]), op=ALU.mult),
                              reads=["stat"], writes=["stat"])
                        tk.op("dve", lambda v: v.tensor_tensor(out=sel3, in0=sel3, in1=m1.unsqueeze(2).broadcast_to([128, 4, 4]), op=ALU.add),
                              reads=["stat"], writes=["stat"])
                        tk.op("dve", lambda v: v.max(out=w8, in_=sel), reads=["stat"], writes=["stat"])
                        tk.op("dve", lambda v: v.tensor_scalar(out=sel, in0=sel, scalar1=w8[:, 1:2], scalar2=None, op0=ALU.is_ge),
                              reads=["stat"], writes=["stat"])
                        tk.op("dve", lambda v: v.tensor_tensor(out=sc, in0=sc, in1=sel, op=ALU.mult), reads=["stat"], writes=["stat"])
                        tk.op("dve", lambda v: v.tensor_reduce(out=gmx, in_=sc, axis=AX.X, op=ALU.add), reads=["stat"], writes=["stat"])
                        tk.op("dve", lambda v: v.reciprocal(out=gmx, in_=gmx), reads=["stat"], writes=["stat"])
                        wtok, wtokk = tmp()
                        tk.op("dve", lambda v, wtok=wtok: v.tensor_scalar(out=wtok[:, 0:16], in0=sc, scalar1=gmx, scalar2=None, op0=ALU.mult),
                              reads=["stat"], writes=[wtokk])
                        tk.op("pe", lambda t, wtok=wtok: t.transpose(out=PS[7][0:16, 128:256], in_=wtok[:, 0:16], identity=ident_f[:]),
                              reads=[wtokk, "ident_f"], writes=[psk(7)])
                        tk.op("act", lambda a, bl=bl: a.copy(out=wrt_hi[:, q0 + bl * 128:q0 + (bl + 1) * 128], in_=PS[7][0:16, 128:256]),
                              reads=[psk(7)], writes=[("wrt", (q0 // 128) + bl)])
                        tk.op("dve", lambda v, bl=bl: v.tensor_tensor(
                            out=wrt_lo[:, q0 + bl * 128:q0 + (bl + 1) * 128], in0=PS[7][0:16, 128:256],
                            in1=wrt_hi[:, q0 + bl * 128:q0 + (bl + 1) * 128], op=ALU.subtract),
                            reads=[psk(7), ("wrt", (q0 // 128) + bl)], writes=[("wrtl", (q0 // 128) + bl)])
                if dbg == "route":
                    tk.barrier()
                    dump(wrt_hi[:, :], 2304, 0, [])
                    dump(hT[:, 0, 0:2304], 2304, 2304, [])
                    tk.barrier()
                    return nc
                tk.barrier()

                tk.mark("b%d l%d experts" % (b, l))
                items = [(e, q0, n) for e in range(NE) for (q0, n) in ranges]

                def ew_views(e):
                    ew = EW[e % 2]
                    return (ew[:, 0:4096].rearrange("p (j f) -> p j f", j=8), ew[:, 4096:8192].rearrange("p (j f) -> p j f", j=8),
                            ew[:, 8192:12288].rearrange("p (k c) -> p k c", k=4), ("EW", e % 2))

                def hm_buf(idx, fc):
                    if idx % 2 == 0:
                        return HM[:, fc, :]
                    return [PB[0], PB[1], PB[2], TK_][fc]

                def load_expert(e):
                    WG, WU, WD, ewk = ew_views(e)
                    tk.dma("pool", lambda q: q.dma_start(out=WG, in_=wg_d[l, e].rearrange("(j p) f -> p j f", p=128)), writes=[ewk])
                    tk.dma("pool", lambda q: q.dma_start(out=WU, in_=wu_d[l, e].rearrange("(j p) f -> p j f", p=128)), writes=[ewk])
                    tk.dma("pool", lambda q: q.dma_start(out=WD, in_=wd_d[l, e].rearrange("(k p) c -> p k c", p=128)), writes=[ewk])

                def emit_bc(idx):
                    e, q0, n = items[idx]
                    bcb = 0 if idx % 2 == 0 else 7
                    wk = [("wrt", q0 // 128 + z) for z in range(n // 128)] + [("wrtl", q0 // 128 + z) for z in range(n // 128)]
                    tk.op("pe", lambda t: t.matmul(PS[bcb][:, 0:n], selb[0:16, e, :], wrt_hi[:, q0:q0 + n], start=True, stop=False),
                          reads=wk + ["selb"], writes=[psk(bcb)])
                    tk.op("pe", lambda t: t.matmul(PS[bcb][:, 0:n], selb[0:16, e, :], wrt_lo[:, q0:q0 + n], start=False, stop=True),
                          reads=wk + ["selb"], writes=[psk(bcb)])

                def emit_gu(idx, fc):
                    e, q0, n = items[idx]
                    bcb = 0 if idx % 2 == 0 else 7
                    WG, WU, WD, ewk = ew_views(e)
                    hk = hkeys(q0, n)
                    gb = 1 + (fc % 2)
                    ub = 3 + (fc % 2)
                    for j in range(8):
                        tk.op("pe", lambda t, j=j: t.matmul(PS[gb][:, 0:n], WG[:, j, fc * 128:(fc + 1) * 128], hT[:, j, q0:q0 + n],
                                                             start=(j == 0), stop=(j == 7)), reads=[ewk] + hk, writes=[psk(gb)])
                    for j in range(8):
                        tk.op("pe", lambda t, j=j: t.matmul(PS[ub][:, 0:n], WU[:, j, fc * 128:(fc + 1) * 128], hT[:, j, q0:q0 + n],
                                                             start=(j == 0), stop=(j == 7)), reads=[ewk] + hk, writes=[psk(ub)])
                    sg, sgk = tmp()
                    tk.op("act", lambda a: a.activation(out=sg[:, 0:n], in_=PS[gb][:, 0:n], func=AF.Silu), reads=[psk(gb)], writes=[sgk])
                    tk.op("dve", lambda v: v.tensor_tensor(out=sg[:, 0:n], in0=sg[:, 0:n], in1=PS[ub][:, 0:n], op=ALU.mult),
                          reads=[sgk, psk(ub)], writes=[sgk])
                    tk.op("dve", lambda v: v.tensor_tensor(out=hm_buf(idx, fc)[:, 0:n], in0=sg[:, 0:n], in1=PS[bcb][:, 0:n], op=ALU.mult),
                          reads=[sgk, psk(bcb)], writes=[("HM", idx % 2, fc)])

                def emit_down(idx):
                    e, q0, n = items[idx]
                    rr = NB if q0 >= S else b
                    WG, WU, WD, ewk = ew_views(e)
                    for jo in range(8):
                        yb = 5 + (jo % 2)
                        for fc in range(4):
                            tk.op("pe", lambda t, fc=fc: t.matmul(PS[yb][:, 0:n], WD[:, fc, jo * 128:(jo + 1) * 128], hm_buf(idx, fc)[:, 0:n],
                                                                   start=(fc == 0), stop=(fc == 3)), reads=[ewk, ("HM", idx % 2, fc)], writes=[psk(yb)])
                        tk.op("dve", lambda v: v.scalar_tensor_tensor(
                            out=xT[:, jo, q0:q0 + n], in0=PS[yb][:, 0:n], scalar=mT[:, l, 40 + jo, rr:rr + 1],
                            in1=xT[:, jo, q0:q0 + n], op0=ALU.mult, op1=ALU.add),
                            reads=[psk(yb), "mT"] + xkeys(q0, n), writes=xkeys(q0, n))

                load_expert(0)
                emit_bc(0)
                emit_gu(0, 0)
                for idx in range(len(items)):
                    e, q0, n = items[idx]
                    if (q0, n) == ranges[0] and e + 1 < NE:
                        load_expert(e + 1)
                    for fc in range(1, 4):
                        emit_gu(idx, fc)
                    if idx + 1 < len(items):
                        emit_bc(idx + 1)
                        emit_gu(idx + 1, 0)
                    emit_down(idx)
                tk.barrier()
                if dbg == "x2":
                    dump(xT[:, 0, :], 2304, 0, [])
                    dump(xT[:, 5, :], 2304, 2304, [])
                    tk.barrier()
                    return nc

            tk.mark("b%d store" % b)
            for tt in range(16):
                for g in range(2):
                    bank = 6 + g
                    for jj in range(4):
                        j = g * 4 + jj
                        tk.op("pe", lambda t, j=j, jj=jj, bank=bank: t.transpose(
                            out=PS[bank][:, jj * 128:(jj + 1) * 128], in_=xT[:, j, tt * 128:(tt + 1) * 128], identity=ident_f[:]),
                            reads=[("xT", tt), "ident_f"], writes=[psk(bank)], inc=(jj == 3))
                    if g == 0:
                        tk.op("act", lambda a, bank=bank: a.copy(out=XS[:, 0:512], in_=PS[bank][:]), reads=[psk(bank)], writes=["xs"])
                    else:
                        tk.op("dve", lambda v, bank=bank: v.tensor_copy(out=XS[:, 512:1024], in_=PS[bank][:]), reads=[psk(bank)], writes=["xs"])
                tk.dma("sp", lambda q: q.dma_start(out=out_d[b, tt * 128:(tt + 1) * 128, :], in_=XS[:]), reads=["xs"], writes=["out"])
            tk.barrier()
          except StopBuild:
            print("STOPPED at", tk.nops)
            tk.pe_open = False
            tk.limit = 0
            tk.barrier()
            if dbg_d is not None:
                dump(small[:, :], 512, 0, [])
                tk.barrier()
            return nc
        tk.barrier()
        tk.mark("end")
        LAST_MARKS[:] = tk.marks
    return nc


LAST_MARKS = []
dbg_mixer = ["D"]

_W_NAMES = ["ada_w", "ada_b", "norm_mix_g", "norm_ffn_g", "w_in", "qk_g_win", "qk_g_na", "qk_g_diff", "qk_g_gqa",
            "sink_win", "lambda_diff", "out_gain", "w_out", "w_gate", "w_up", "w_down"]


def make_in_maps(inputs, n_cores, NB, DEPTH):
    f = lambda a: np.ascontiguousarray(np.asarray(a, dtype=np.float32))
    shared = {k: f(inputs[k])[:DEPTH] for k in _W_NAMES}
    shared["router_w"] = f(inputs["router_w"])
    shared["router_b"] = f(inputs["router_b"]).reshape(1, NE)
    shared["na_bias"] = host_na_bias(f(inputs["rpb_na"])[:DEPTH])
    shared.update(host_consts())
    x = f(inputs["x"])
    c = f(inputs["c"])
    ctx = f(inputs["ctx"])
    cctx = f(inputs["c_ctx"]).reshape(1, D)
    maps = []
    for i in range(n_cores):
        m = dict(shared)
        m["x"] = x[i * NB:(i + 1) * NB]
        m["ctx"] = ctx[i * NB:(i + 1) * NB]
        m["c"] = np.ascontiguousarray(np.concatenate([c[i * NB:(i + 1) * NB], cctx], 0))
        maps.append(m)
    return maps


def kernel(**inputs):
    NB = inputs["x"].shape[0] // N_CORES
    nc = build(NB, DEPTH_FULL)
    maps = make_in_maps(inputs, N_CORES, NB, DEPTH_FULL)
    res = run_bass_kernel_spmd(nc, maps, core_ids=list(range(N_CORES)))
    return np.concatenate([r["out"] for r in res.results], axis=0).astype(np.float32)
```
